# Optimizing a Trainium2 kernel written in Bass

```python
import math
import jax, jax.numpy as jnp
from jax import lax
import numpy as np

D_MODEL = 1024
BATCH = 16
SEQ = 2048
DEPTH = 2

N_BRANCH = 4
HEAD_DIM = 64
BR_HEADS = 4
BR_WIDTH = BR_HEADS * HEAD_DIM
Q_BLOCK = 128
SB_HEADS = BR_HEADS
RW_HEADS = BR_HEADS
RW_N = HEAD_DIM
RW_W_LORA = 64
RW_A_LORA = 64
RW_G_LORA = 128
RW_GN_EPS = 64e-5
RW_IN_SIZES = (BR_WIDTH, BR_WIDTH, BR_WIDTH, RW_W_LORA, RW_A_LORA, RW_G_LORA)
SSD_HEADS = BR_HEADS
SSD_P = HEAD_DIM
SSD_GROUPS = 2
SSD_N = 128
SSD_CONV = 4
SSD_CHUNK = 128
SSD_XBC = BR_WIDTH + 2 * SSD_GROUPS * SSD_N
SSD_IN_SIZES = (BR_WIDTH, SSD_XBC, SSD_HEADS)
MLA_HEADS = BR_HEADS
MLA_Q_RANK = 256
MLA_KV_RANK = 128
MLA_NOPE = 64
MLA_ROPE = 32
MLA_V = 64
ROPE_BASE = 10000.0
MLA_IN_SIZES = (MLA_Q_RANK, MLA_KV_RANK, MLA_ROPE)
MIX_IN_SIZES = (3 * BR_WIDTH, sum(RW_IN_SIZES), sum(SSD_IN_SIZES), sum(MLA_IN_SIZES))
N_IN = sum(MIX_IN_SIZES)
MEM_TOKENS = 256
XA_HEADS = 4
XA_DH = D_MODEL // XA_HEADS
FFN_DENSE = 2816
N_EXPERTS = 8
TOP_K = 2
FFN_EXPERT = 3584
MOE_BLOCK = 256
N_DENSE_LAYERS = (DEPTH + 1) // 2
N_MOE_LAYERS = DEPTH // 2
DN_ALPHA = (2.0 * DEPTH) ** 0.25
DN_BETA = (8.0 * DEPTH) ** -0.25

kernel_name = "hybrid_sb_rwkv7_ssd_mla_moe_deepnorm"


def _offsets(sizes):
    return np.cumsum(sizes)[:-1].tolist()


def _layer_norm(x, g, b, eps=1e-5):
    xf = x.astype(jnp.float32)
    mu = jnp.mean(xf, axis=-1, keepdims=True)
    var = jnp.mean(jnp.square(xf - mu), axis=-1, keepdims=True)
    return ((xf - mu) * lax.rsqrt(var + eps) * g + b).astype(x.dtype)


def _rms_norm(x, g, eps=1e-6):
    xf = x.astype(jnp.float32)
    return (xf * lax.rsqrt(jnp.mean(xf * xf, axis=-1, keepdims=True) + eps) * g).astype(x.dtype)


def _rope(x, cos, sin):
    x1, x2 = jnp.split(x, 2, axis=-1)
    return jnp.concatenate([x1 * cos - x2 * sin, x1 * sin + x2 * cos], axis=-1)


def _stick_breaking_attention(q, k, v):
    s_len = q.shape[1]
    scale = HEAD_DIM ** -0.5
    outs = []
    for blk in range(s_len // Q_BLOCK):
        lo, hi = blk * Q_BLOCK, (blk + 1) * Q_BLOCK
        z = jnp.einsum("bqhd,bkhd->bhqk", q[:, lo:hi], k[:, :hi]).astype(jnp.float32) * scale
        t_pos = lo + jnp.arange(Q_BLOCK)
        s_pos = jnp.arange(hi)
        before = s_pos[None, :] < t_pos[:, None]
        log_keep = jnp.where(before, jax.nn.log_sigmoid(-z), 0.0)
        tail = lax.cumsum(log_keep, axis=3, reverse=True) - log_keep
        w = jnp.where(before, jnp.exp(jax.nn.log_sigmoid(z) + tail), 0.0)
        outs.append(jnp.einsum("bhqk,bkhd->bqhd", w.astype(v.dtype), v[:, :hi]))
    return jnp.concatenate(outs, axis=1)


def _rwkv7_scan(r, decay, k, v, kk, a):
    def step(state, inp):
        r_t, w_t, k_t, v_t, kk_t, a_t = inp
        sa = jnp.einsum("bhij,bhj->bhi", state, kk_t)
        state = (state * w_t[:, :, None, :]
                 - sa[..., None] * (kk_t * a_t)[:, :, None, :]
                 + v_t[..., None] * k_t[:, :, None, :])
        return state, jnp.einsum("bhij,bhj->bhi", state, r_t)

    xs = tuple(jnp.moveaxis(t, 1, 0) for t in (r, decay, k, v, kk, a))
    state0 = jnp.zeros((r.shape[0], RW_HEADS, RW_N, RW_N), jnp.float32)
    _, ys = lax.scan(step, state0, xs)
    return jnp.moveaxis(ys, 0, 1)


def _rwkv7_time_mix(cols, mu, w0, w_up, a0, a_up, g_up, k_k, k_a, r_k, gn_g, gn_b):
    b, s, _ = cols.shape
    p = cols.astype(jnp.float32)
    prev = jnp.pad(p, ((0, 0), (1, 0), (0, 0)))[:, :-1]
    p = p + (prev - p) * mu
    r, k, v, w_lo, a_lo, g_lo = jnp.split(p, _offsets(RW_IN_SIZES), axis=-1)
    w_log = -jax.nn.softplus(-(w0 + jnp.tanh(w_lo) @ w_up)) - 0.5
    decay = jnp.exp(-jnp.exp(w_log))
    a = jax.nn.sigmoid(a0 + a_lo @ a_up)
    g = jax.nn.sigmoid(g_lo) @ g_up
    kk = k * k_k
    k = k * (1.0 + (a - 1.0) * k_a)

    def heads(t):
        return t.reshape(b, s, RW_HEADS, RW_N)

    r, k, v, decay, a, kk = heads(r), heads(k), heads(v), heads(decay), heads(a), heads(kk)
    kk = kk * lax.rsqrt(jnp.maximum(jnp.sum(kk * kk, axis=-1, keepdims=True), 1e-24))
    y = _rwkv7_scan(r, decay, k, v, kk, a)
    y_mu = jnp.mean(y, axis=-1, keepdims=True)
    y_var = jnp.mean(jnp.square(y - y_mu), axis=-1, keepdims=True)
    y = ((y - y_mu) * lax.rsqrt(y_var + RW_GN_EPS)).reshape(b, s, BR_WIDTH) * gn_g + gn_b
    bonus = jnp.sum(r * k * r_k, axis=-1, keepdims=True) * v
    return ((y + bonus.reshape(b, s, BR_WIDTH)) * g).astype(cols.dtype)


def _segsum(v):
    t = v.shape[-1]
    rep = jnp.broadcast_to(v[..., :, None], v.shape + (t,))
    strict = jnp.tril(jnp.ones((t, t), dtype=bool), -1)
    cs = jnp.cumsum(jnp.where(strict, rep, 0.0), axis=-2)
    return jnp.where(jnp.tril(jnp.ones((t, t), dtype=bool)), cs, -jnp.inf)


def _ssd_chunked(xh, dt, a, bh, ch):
    b, s, h, p = xh.shape
    n = bh.shape[-1]
    c = s // SSD_CHUNK
    xd = (xh * dt[..., None]).reshape(b, c, SSD_CHUNK, h, p)
    bc = bh.reshape(b, c, SSD_CHUNK, h, n)
    cc = ch.reshape(b, c, SSD_CHUNK, h, n)
    a_dt = jnp.transpose((dt * a).reshape(b, c, SSD_CHUNK, h), (0, 3, 1, 2))
    a_cs = jnp.cumsum(a_dt, axis=-1)
    decay_in = jnp.exp(_segsum(a_dt))
    cb = jnp.einsum("bclhn,bcshn->bhcls", cc, bc)
    y_diag = jnp.einsum("bhcls,bcshp->bclhp", cb * decay_in, xd)
    decay_to_end = jnp.exp(a_cs[..., -1:] - a_cs)
    states = jnp.einsum("bclhn,bhcl,bclhp->bchpn", bc, decay_to_end, xd)
    states = jnp.concatenate([jnp.zeros_like(states[:, :1]), states], axis=1)
    chunk_a = jnp.pad(a_cs[..., -1], ((0, 0), (0, 0), (1, 0)))
    decay_chunk = jnp.exp(_segsum(chunk_a))
    states = jnp.einsum("bhzc,bchpn->bzhpn", decay_chunk, states)[:, :-1]
    y_off = jnp.einsum("bclhn,bchpn,bhcl->bclhp", cc, states, jnp.exp(a_cs))
    return (y_diag + y_off).reshape(b, s, h, p)


def _causal_depthwise_conv(x, w, bias):
    ch = x.shape[-1]
    y = lax.conv_general_dilated(x, w[:, None, :].astype(x.dtype), window_strides=(1,),
                                 padding=[(SSD_CONV - 1, 0)],
                                 dimension_numbers=("NWC", "WIO", "NWC"),
                                 feature_group_count=ch)
    return y + bias


def _mamba2_ssd_mix(z, xbc, dt_raw, conv_w, conv_b, dt_bias, a_log, d_skip, norm_g):
    b, s, _ = z.shape
    xbc = jax.nn.silu(_causal_depthwise_conv(xbc, conv_w, conv_b)).astype(jnp.float32)
    xs, bm, cm = jnp.split(xbc, _offsets((BR_WIDTH, SSD_GROUPS * SSD_N, SSD_GROUPS * SSD_N)), axis=-1)
    xh = xs.reshape(b, s, SSD_HEADS, SSD_P)
    rep = SSD_HEADS // SSD_GROUPS
    bh = jnp.repeat(bm.reshape(b, s, SSD_GROUPS, SSD_N), rep, axis=2)
    ch = jnp.repeat(cm.reshape(b, s, SSD_GROUPS, SSD_N), rep, axis=2)
    dt = jax.nn.softplus(dt_raw.astype(jnp.float32) + dt_bias)
    a = -jnp.exp(a_log.astype(jnp.float32))
    y = _ssd_chunked(xh, dt, a, bh, ch) + d_skip[:, None] * xh
    y = y.reshape(b, s, BR_WIDTH) * jax.nn.silu(z.astype(jnp.float32))
    yg = y.reshape(b, s, SSD_GROUPS, BR_WIDTH // SSD_GROUPS)
    yg = yg * lax.rsqrt(jnp.mean(yg * yg, axis=-1, keepdims=True) + 1e-5)
    return (yg.reshape(b, s, BR_WIDTH) * norm_g).astype(z.dtype)


def _mla_attention(q_nope, q_rope, k_nope, k_rope, v):
    s_len = q_nope.shape[1]
    scale = (MLA_NOPE + MLA_ROPE) ** -0.5
    outs = []
    for blk in range(s_len // Q_BLOCK):
        lo, hi = blk * Q_BLOCK, (blk + 1) * Q_BLOCK
        z = (jnp.einsum("bqhd,bkhd->bhqk", q_nope[:, lo:hi], k_nope[:, :hi])
             + jnp.einsum("bqhr,bkr->bhqk", q_rope[:, lo:hi], k_rope[:, :hi])).astype(jnp.float32) * scale
        causal = jnp.arange(hi)[None, :] <= (lo + jnp.arange(Q_BLOCK))[:, None]
        prob = jax.nn.softmax(jnp.where(causal, z, -jnp.inf), axis=-1)
        outs.append(jnp.einsum("bhqk,bkhd->bqhd", prob.astype(v.dtype), v[:, :hi]))
    return jnp.concatenate(outs, axis=1)


def _mla_mix(cq, ckv, kr_raw, cos, sin, q_norm_g, w_uq, kv_norm_g, w_ukv):
    b, s, _ = cq.shape
    q = (_rms_norm(cq, q_norm_g) @ w_uq).reshape(b, s, MLA_HEADS, MLA_NOPE + MLA_ROPE)
    q_nope, q_rope = q[..., :MLA_NOPE], _rope(q[..., MLA_NOPE:], cos[:, :, None, :], sin[:, :, None, :])
    kv = (_rms_norm(ckv, kv_norm_g) @ w_ukv).reshape(b, s, MLA_HEADS, MLA_NOPE + MLA_V)
    k_nope, v = kv[..., :MLA_NOPE], kv[..., MLA_NOPE:]
    k_rope = _rope(kr_raw, cos, sin)
    return _mla_attention(q_nope, q_rope, k_nope, k_rope, v).reshape(b, s, BR_WIDTH)


def _hybrid_mixer(x, cos, sin, w_in, rw_mu, rw_w0, rw_w_up, rw_a0, rw_a_up, rw_g_up, rw_k_k, rw_k_a,
                  rw_r_k, rw_gn_g, rw_gn_b, ssd_conv_w, ssd_conv_b, ssd_dt_bias, ssd_a_log, ssd_d,
                  ssd_norm_g, mla_q_norm_g, mla_w_uq, mla_kv_norm_g, mla_w_ukv, w_br_out, w_gate, w_out):
    b, s, _ = x.shape
    sb_cols, rw_cols, ssd_cols, mla_cols = jnp.split(x @ w_in, _offsets(MIX_IN_SIZES), axis=-1)
    q, k, v = (t.reshape(b, s, SB_HEADS, HEAD_DIM) for t in jnp.split(sb_cols, 3, axis=-1))
    y_sb = _stick_breaking_attention(q, k, v).reshape(b, s, BR_WIDTH)
    y_rw = _rwkv7_time_mix(rw_cols, rw_mu, rw_w0, rw_w_up, rw_a0, rw_a_up, rw_g_up, rw_k_k, rw_k_a,
                           rw_r_k, rw_gn_g, rw_gn_b)
    z, xbc, dt_raw = jnp.split(ssd_cols, _offsets(SSD_IN_SIZES), axis=-1)
    y_ssd = _mamba2_ssd_mix(z, xbc, dt_raw, ssd_conv_w, ssd_conv_b, ssd_dt_bias, ssd_a_log, ssd_d, ssd_norm_g)
    cq, ckv, kr_raw = jnp.split(mla_cols, _offsets(MLA_IN_SIZES), axis=-1)
    y_mla = _mla_mix(cq, ckv, kr_raw, cos, sin, mla_q_norm_g, mla_w_uq, mla_kv_norm_g, mla_w_ukv)
    merged = jnp.zeros_like(x)
    for i, y_br in enumerate((y_sb, y_rw, y_ssd, y_mla)):
        merged = merged + jax.nn.sigmoid(x @ w_gate[i]) * (y_br @ w_br_out[i])
    return merged @ w_out


def _memory_cross_attention(x, mem, w_q, w_kv, w_o):
    b, s, d = x.shape
    m = mem.shape[1]
    q = (x @ w_q).reshape(b, s, XA_HEADS, XA_DH)
    k, v = jnp.split(mem @ w_kv, 2, axis=-1)
    k = k.reshape(b, m, XA_HEADS, XA_DH)
    v = v.reshape(b, m, XA_HEADS, XA_DH)
    z = jnp.einsum("bshd,bmhd->bhsm", q, k).astype(jnp.float32) * (XA_DH ** -0.5)
    prob = jax.nn.softmax(z, axis=-1)
    o = jnp.einsum("bhsm,bmhd->bshd", prob.astype(v.dtype), v).reshape(b, s, d)
    return o @ w_o


def _swiglu(x, w13, w2):
    gate, up = jnp.split(x @ w13, 2, axis=-1)
    return (jax.nn.silu(gate) * up) @ w2


def _moe_swiglu(xf, router, w13, w2):
    t, d = xf.shape
    logits = (xf @ router).astype(jnp.float32)
    top_v, top_i = lax.top_k(logits, TOP_K)
    gates = jax.nn.softmax(top_v, axis=-1)
    n_assign = t * TOP_K
    e_flat = top_i.reshape(-1)
    tok_flat = jnp.repeat(jnp.arange(t, dtype=jnp.int32), TOP_K)
    w_flat = gates.reshape(-1).astype(xf.dtype)
    order = jnp.argsort(e_flat)
    e_s, tok_s, w_s = e_flat[order], tok_flat[order], w_flat[order]
    counts = jnp.bincount(e_flat, length=N_EXPERTS)
    starts = jnp.cumsum(counts) - counts
    padded = (counts + MOE_BLOCK - 1) // MOE_BLOCK * MOE_BLOCK
    p_ends = jnp.cumsum(padded)
    p_starts = p_ends - padded
    dest = p_starts[e_s] + (jnp.arange(n_assign) - starts[e_s])
    n_blocks = -(-n_assign // MOE_BLOCK) + N_EXPERTS
    n_rows = n_blocks * MOE_BLOCK
    row_tok = jnp.zeros((n_rows,), jnp.int32).at[dest].set(tok_s)
    row_w = jnp.zeros((n_rows,), xf.dtype).at[dest].set(w_s)
    block_e = jnp.minimum(jnp.searchsorted(p_ends, jnp.arange(n_blocks) * MOE_BLOCK, side="right"),
                          N_EXPERTS - 1)
    xs = xf[row_tok].reshape(n_blocks, MOE_BLOCK, d)

    def expert_block(args):
        xb, e = args
        gate, up = jnp.split(xb @ w13[e], 2, axis=-1)
        return (jax.nn.silu(gate) * up) @ w2[e]

    ys = lax.map(expert_block, (xs, block_e)).reshape(n_rows, d)
    return jnp.zeros_like(xf).at[row_tok].add(ys * row_w[:, None])


def setup_inputs(seed: int = 0) -> dict:
    key = jax.random.key(seed)
    ks = iter(jax.random.split(key, 64))
    L = DEPTH
    f32 = jnp.float32

    def nrm(shape, scale):
        return jax.random.normal(next(ks), shape, f32) * scale

    def gain(shape):
        return 1.0 + nrm(shape, 0.02)

    def unif(shape, lo, hi):
        return jax.random.uniform(next(ks), shape, f32, lo, hi)

    dt0 = jnp.exp(unif((L, SSD_HEADS), math.log(1e-3), math.log(1e-1)))
    return {
        "x": nrm((BATCH, SEQ, D_MODEL), 1.0),
        "mem": nrm((BATCH, MEM_TOKENS, D_MODEL), 1.0),
        "positions": jnp.arange(SEQ, dtype=jnp.int32)[None, :]
                     + jax.random.randint(next(ks), (BATCH, 1), 0, 4096, dtype=jnp.int32),
        "mix_w_in": nrm((L, D_MODEL, N_IN), D_MODEL ** -0.5),
        "rw_mu": unif((L, sum(RW_IN_SIZES)), 0.0, 1.0),
        "rw_w0": unif((L, BR_WIDTH), -6.0, 1.0),
        "rw_w_up": nrm((L, RW_W_LORA, BR_WIDTH), 0.5 * RW_W_LORA ** -0.5),
        "rw_a0": nrm((L, BR_WIDTH), 0.1),
        "rw_a_up": nrm((L, RW_A_LORA, BR_WIDTH), RW_A_LORA ** -0.5),
        "rw_g_up": nrm((L, RW_G_LORA, BR_WIDTH), RW_G_LORA ** -0.5),
        "rw_k_k": 0.85 + nrm((L, BR_WIDTH), 0.05),
        "rw_k_a": 1.0 + nrm((L, BR_WIDTH), 0.05),
        "rw_r_k": nrm((L, RW_HEADS, RW_N), 0.1),
        "rw_gn_g": gain((L, BR_WIDTH)),
        "rw_gn_b": nrm((L, BR_WIDTH), 0.02),
        "ssd_conv_w": nrm((L, SSD_CONV, SSD_XBC), SSD_CONV ** -0.5),
        "ssd_conv_b": nrm((L, SSD_XBC), 0.02),
        "ssd_dt_bias": dt0 + jnp.log(-jnp.expm1(-dt0)),
        "ssd_a_log": jnp.log(unif((L, SSD_HEADS), 1.0, 16.0)),
        "ssd_d": 1.0 + nrm((L, SSD_HEADS), 0.1),
        "ssd_norm_g": gain((L, BR_WIDTH)),
        "mla_q_norm_g": gain((L, MLA_Q_RANK)),
        "mla_w_uq": nrm((L, MLA_Q_RANK, MLA_HEADS * (MLA_NOPE + MLA_ROPE)), MLA_Q_RANK ** -0.5),
        "mla_kv_norm_g": gain((L, MLA_KV_RANK)),
        "mla_w_ukv": nrm((L, MLA_KV_RANK, MLA_HEADS * (MLA_NOPE + MLA_V)), MLA_KV_RANK ** -0.5),
        "mix_w_br_out": nrm((L, N_BRANCH, BR_WIDTH, D_MODEL), BR_WIDTH ** -0.5),
        "mix_w_gate": nrm((L, N_BRANCH, D_MODEL, D_MODEL), D_MODEL ** -0.5),
        "mix_w_out": nrm((L, D_MODEL, D_MODEL), DN_BETA * D_MODEL ** -0.5),
        "ln1_g": gain((L, D_MODEL)),
        "ln1_b": nrm((L, D_MODEL), 0.02),
        "xa_w_q": nrm((L, D_MODEL, D_MODEL), D_MODEL ** -0.5),
        "xa_w_kv": nrm((L, D_MODEL, 2 * D_MODEL), D_MODEL ** -0.5),
        "xa_w_o": nrm((L, D_MODEL, D_MODEL), DN_BETA * D_MODEL ** -0.5),
        "ln2_g": gain((L, D_MODEL)),
        "ln2_b": nrm((L, D_MODEL), 0.02),
        "ffn_w13": nrm((N_DENSE_LAYERS, D_MODEL, 2 * FFN_DENSE), D_MODEL ** -0.5),
        "ffn_w2": nrm((N_DENSE_LAYERS, FFN_DENSE, D_MODEL), DN_BETA * FFN_DENSE ** -0.5),
        "moe_router": nrm((N_MOE_LAYERS, D_MODEL, N_EXPERTS), D_MODEL ** -0.5),
        "moe_w13": nrm((N_MOE_LAYERS, N_EXPERTS, D_MODEL, 2 * FFN_EXPERT), D_MODEL ** -0.5),
        "moe_w2": nrm((N_MOE_LAYERS, N_EXPERTS, FFN_EXPERT, D_MODEL), DN_BETA * FFN_EXPERT ** -0.5),
        "ln3_g": gain((L, D_MODEL)),
        "ln3_b": nrm((L, D_MODEL), 0.02),
    }


def reference(x, mem, positions, mix_w_in, rw_mu, rw_w0, rw_w_up, rw_a0, rw_a_up, rw_g_up, rw_k_k, rw_k_a,
              rw_r_k, rw_gn_g, rw_gn_b, ssd_conv_w, ssd_conv_b, ssd_dt_bias, ssd_a_log, ssd_d, ssd_norm_g,
              mla_q_norm_g, mla_w_uq, mla_kv_norm_g, mla_w_ukv, mix_w_br_out, mix_w_gate, mix_w_out,
              ln1_g, ln1_b, xa_w_q, xa_w_kv, xa_w_o, ln2_g, ln2_b, ffn_w13, ffn_w2, moe_router, moe_w13,
              moe_w2, ln3_g, ln3_b):
    inv_freq = ROPE_BASE ** (-jnp.arange(0, MLA_ROPE, 2, dtype=jnp.float32) / MLA_ROPE)
    ang = positions.astype(jnp.float32)[..., None] * inv_freq
    cos, sin = jnp.cos(ang).astype(x.dtype), jnp.sin(ang).astype(x.dtype)
    for l in range(DEPTH):
        mix = _hybrid_mixer(x, cos, sin, mix_w_in[l], rw_mu[l], rw_w0[l], rw_w_up[l], rw_a0[l], rw_a_up[l],
                            rw_g_up[l], rw_k_k[l], rw_k_a[l], rw_r_k[l], rw_gn_g[l], rw_gn_b[l],
                            ssd_conv_w[l], ssd_conv_b[l], ssd_dt_bias[l], ssd_a_log[l], ssd_d[l],
                            ssd_norm_g[l], mla_q_norm_g[l], mla_w_uq[l], mla_kv_norm_g[l], mla_w_ukv[l],
                            mix_w_br_out[l], mix_w_gate[l], mix_w_out[l])
        x = _layer_norm(DN_ALPHA * x + mix, ln1_g[l], ln1_b[l])
        xa = _memory_cross_attention(x, mem, xa_w_q[l], xa_w_kv[l], xa_w_o[l])
        x = _layer_norm(DN_ALPHA * x + xa, ln2_g[l], ln2_b[l])
        if l % 2 == 0:
            ffn = _swiglu(x, ffn_w13[l // 2], ffn_w2[l // 2])
        else:
            ffn = _moe_swiglu(x.reshape(-1, D_MODEL), moe_router[l // 2], moe_w13[l // 2],
                              moe_w2[l // 2]).reshape(x.shape)
        x = _layer_norm(DN_ALPHA * x + ffn, ln3_g[l], ln3_b[l])
    return x
```

```python
import math
from contextlib import ExitStack
import numpy as np
import concourse.bass as bass
import concourse.mybir as mybir
from concourse.bass_utils import run_bass_kernel_spmd

F32 = mybir.dt.float32
BF16 = mybir.dt.bfloat16
I32 = mybir.dt.int32
AF = mybir.ActivationFunctionType
ALU = mybir.AluOpType
AX = mybir.AxisListType

D = 1024
NCORES = 8
WRITE_KW = ("out", "accum_out")


class T:
    def __init__(self, ap, name):
        self.ap = ap
        self.name = name
        self.w = None
        self.r = {}

    def __getitem__(self, idx):
        return V(self, self.ap[idx])

    @property
    def v(self):
        return V(self, self.ap)


class V:
    def __init__(self, t, ap):
        self.t = t
        self.ap = ap

    def __getitem__(self, idx):
        return V(self.t, self.ap[idx])

    def bc(self, dt):
        return V(self.t, self.ap.bitcast(dt))

    def re(self, pat, **kw):
        return V(self.t, self.ap.rearrange(pat, **kw))

    def bro(self, shape):
        return V(self.t, self.ap.to_broadcast(shape))

    def pbro(self, n):
        return V(self.t, self.ap.partition_broadcast(n))


class K:
    COMPUTE = ("pe", "act", "dve", "pool")

    def __init__(self, nc):
        self.nc = nc
        self.es = ExitStack()
        self.engs = {"pe": nc.tensor, "act": nc.scalar, "dve": nc.vector, "pool": nc.gpsimd, "sp": nc.sync}
        self.sems = {}
        self.cnt = {}
        for e in self.COMPUTE:
            self.sems[e] = self.es.enter_context(nc.semaphore("s_" + e))
            self.cnt[e] = 0
        self.dq = {}
        for q in ("sp", "pool", "act"):
            lst = []
            for i in range(6):
                key = "d_%s%d" % (q, i)
                self.sems[key] = self.es.enter_context(nc.semaphore(key))
                self.cnt[key] = 0
                lst.append(key)
            self.dq[q] = [lst, 0]
        self.seen = {e: {} for e in self.engs}
        self.n_ins = 0
        self.phase_es = None
        self.uid = 0

    def sb(self, shape, dt, name=None):
        self.uid += 1
        name = "%s_%d" % (name or "t", self.uid)
        h = (self.phase_es or self.es).enter_context(self.nc.sbuf_tensor(name, list(shape), dt))
        return T(h[:], name)

    def sbg(self, shape, dt, name=None):
        self.uid += 1
        name = "%s_%d" % (name or "g", self.uid)
        h = self.es.enter_context(self.nc.sbuf_tensor(name, list(shape), dt))
        return T(h[:], name)

    def psum(self, name):
        h = self.es.enter_context(self.nc.psum_tensor(name, [128, 512], F32))
        return T(h[:], name)

    def dram(self, name, shape, dt, kind="Internal"):
        h = self.nc.dram_tensor(name, list(shape), dt, kind=kind)
        return T(h.ap(), name)

    def phase(self):
        k = self

        class _P:
            def __enter__(s):
                k.barrier()
                k.phase_es = ExitStack()
                return s

            def __exit__(s, *a):
                k.barrier()
                k.phase_es.close()
                k.phase_es = None
                return False

        return _P()

    def _wait(self, eng, key, val):
        if self.seen[eng].get(key, 0) >= val:
            return
        self.engs[eng].wait_ge(self.sems[key], val)
        self.seen[eng][key] = val
        self.n_ins += 1

    def _sync(self, eng, reads, writes):
        deps = {}

        def add(d, war=False):
            if d is None:
                return
            key, val = d
            if key == eng and eng == "pe":
                return
            if deps.get(key, 0) < val:
                deps[key] = val

        for t in reads:
            add(t.w)
        for t in writes:
            add(t.w)
            for key, val in t.r.items():
                add((key, val), war=True)
        for key, val in deps.items():
            self._wait(eng, key, val)

    def _done(self, me, reads, writes):
        key, val = me
        for t in reads:
            if t.r.get(key, 0) < val:
                t.r[key] = val
        for t in writes:
            t.w = me
            t.r = {}

    def barrier(self):
        for eng in self.engs:
            for key in self.sems:
                if key == eng:
                    continue
                self._wait(eng, key, self.cnt[key])

    def _split(self, kw):
        reads, writes, args = [], [], {}
        for name, v in kw.items():
            if isinstance(v, V):
                (writes if name in WRITE_KW else reads).append(v.t)
                args[name] = v.ap
            else:
                args[name] = v
        return reads, writes, args

    def I(self, eng, meth, **kw):
        reads, writes, args = self._split(kw)
        self._sync(eng, reads, writes)
        ins = getattr(self.engs[eng], meth)(**args)
        self.cnt[eng] += 1
        ins.then_inc(self.sems[eng], 1)
        self.n_ins += 1
        self._done((eng, self.cnt[eng]), reads, writes)

    def dma(self, out, in_, q="sp", **kw):
        lst, i = self.dq[q]
        key = lst[i % len(lst)]
        self.dq[q][1] = i + 1
        self._wait(q, key, self.cnt[key])
        self._sync(q, [in_.t], [out.t])
        ins = self.engs[q].dma_start(out=out.ap, in_=in_.ap, **kw)
        self.cnt[key] += 16
        ins.then_inc(self.sems[key], 16)
        self.n_ins += 1
        self._done((key, self.cnt[key]), [in_.t], [out.t])

    def mm(self, out, lhsT, rhs, start=True, stop=True):
        self.I("pe", "matmul", out=out, lhsT=lhsT, rhs=rhs, start=start, stop=stop)

    def tr(self, out, in_, ident):
        self.I("pe", "transpose", out=out, in_=in_, identity=ident)

    def act(self, out, in_, func, bias=0.0, scale=1.0, **kw):
        self.I("act", "activation", out=out, in_=in_, func=func, bias=bias, scale=scale, **kw)

    def tt(self, eng, out, in0, in1, op):
        self.I(eng, "tensor_tensor", out=out, in0=in0, in1=in1, op=op)

    def ts(self, eng, out, in0, s1, op0, s2=None, op1=ALU.bypass, **kw):
        self.I(eng, "tensor_scalar", out=out, in0=in0, scalar1=s1, scalar2=s2, op0=op0, op1=op1, **kw)

    def stt(self, out, in0, scalar, in1, op0, op1):
        self.I("dve", "scalar_tensor_tensor", out=out, in0=in0, scalar=scalar, in1=in1, op0=op0, op1=op1)

    def cp(self, eng, out, in_):
        if eng == "act":
            self.I("act", "copy", out=out, in_=in_)
        else:
            self.I(eng, "tensor_copy", out=out, in_=in_)

    def memset(self, eng, out, val):
        reads, writes = [], [out.t]
        self._sync(eng, reads, writes)
        ins = self.engs[eng].memset(out.ap, val)
        self.cnt[eng] += 1
        ins.then_inc(self.sems[eng], 1)
        self._done((eng, self.cnt[eng]), reads, writes)

    def aselect(self, out, in_, pattern, cmp, fill, base, cm):
        self.I("pool", "affine_select", out=out, in_=in_, pattern=pattern, compare_op=cmp, fill=fill,
               base=base, channel_multiplier=cm)


class Consts:
    def __init__(self, k):
        self.k = k
        ones = k.sbg([128, 512], F32, "onesf")
        k.memset("pool", ones.v, 1.0)
        self.ones_f = ones
        idf = k.sbg([128, 128], F32, "idf")
        k.aselect(idf.v, ones[:, 0:128], [[1, 128]], ALU.is_equal, 0.0, 0, -1)
        self.id_f = idf
        idb = k.sbg([128, 128], BF16, "idb")
        k.cp("pool", idb.v, idf.v)
        self.id_b = idb
        onesb = k.sbg([128, 128], BF16, "onesb")
        k.cp("pool", onesb.v, ones[:, 0:128])
        self.ones_b = onesb
        self.eps5 = k.sbg([128, 1], F32, "eps5")
        k.memset("pool", self.eps5.v, 1e-5)
        self.eps6 = k.sbg([128, 1], F32, "eps6")
        k.memset("pool", self.eps6.v, 1e-6)
        self.ps = [k.psum("ps%d" % i) for i in range(8)]
        self.psi = 0
        self.pool = list(range(8))

    def bank(self):
        b = self.ps[self.pool[self.psi % len(self.pool)]]
        self.psi += 1
        return b

    def mask(self, shape_f, base, cm, cmp=ALU.is_ge, dt=F32, name="mask", local=False):
        k = self.k
        m = (k.sb if local else k.sbg)([128, shape_f], F32, name)
        k.aselect(m.v, self.ones_f[:, 0:shape_f], [[1, shape_f]], cmp, 0.0, base, cm)
        if dt == F32:
            return m
        mb = k.sbg([128, shape_f], dt, name + "b")
        k.cp("pool", mb.v, m.v)
        return mb


class WLoader:
    def __init__(self, k, max_elems, nstage=2, nbuf=3, ceng="pool", name="w"):
        self.k = k
        self.st = [k.sb([128, max_elems], F32, name + "st") for _ in range(nstage)]
        self.bf = [k.sb([128, max_elems], BF16, name + "bf") for _ in range(nbuf)]
        self.i = 0
        self.j = 0
        self.ceng = ceng

    def load(self, wv, kc, n, p=128):
        k = self.k
        st = self.st[self.i % len(self.st)]
        self.i += 1
        bf = self.bf[self.j % len(self.bf)]
        self.j += 1
        sv = st[0:p, 0:kc * n].re("p (c n) -> p c n", c=kc)
        k.dma(sv, wv.re("(c p) n -> p c n", p=p))
        bv = bf[0:p, 0:kc * n].re("p (c n) -> p c n", c=kc)
        k.cp(self.ceng, bv, sv)
        return bv


def load_bcast(k, dv, n, name="bc"):
    t = k.sb([128, n], F32, name)
    k.dma(t.v, dv.pbro(128))
    return t


DN_ALPHA = (2.0 * 2) ** 0.25


def interleave(gens):
    gens = list(gens)
    while gens:
        for g in list(gens):
            try:
                next(g)
            except StopIteration:
                gens.remove(g)


class Ring:
    def __init__(self, k, n, shape, dt, name="r"):
        self.ts = [k.sb(shape, dt, name) for _ in range(n)]
        self.i = 0

    def next(self):
        t = self.ts[self.i % len(self.ts)]
        self.i += 1
        return t


def ln_phase(k, C, S, u_d, xres_d, g_d, b_d, xout_d, xt_d, eps=1e-5):
    NT = S // 128
    with k.phase():
        g_bc = load_bcast(k, g_d, 1024, "lng")
        b_bc = load_bcast(k, b_d, 1024, "lnb")
        r_x = Ring(k, 4, [128, 1024], F32, "lnx")
        r_u = Ring(k, 4, [128, 1024], F32, "lnu")
        r_o = Ring(k, 3, [128, 1024], F32, "lno")
        r_b = Ring(k, 3, [128, 1024], BF16, "lnbf")
        r_s = Ring(k, 4, [128, 16], F32, "lns")
        GT = min(4, NT)
        xtgs = [k.sb([128, 8, GT * 128], BF16, "lnxt") for _ in range(2)]
        done = {}

        def tile(j):
            rows = slice(j * 128, (j + 1) * 128)
            xr = r_x.next()
            k.dma(xr.v, xres_d[rows, :])
            u = r_u.next()
            k.dma(u.v, u_d[rows, :])
            yield
            k.stt(u.v, xr.v, DN_ALPHA, u.v, ALU.mult, ALU.add)
            st = r_s.next()
            for h in range(2):
                k.I("dve", "bn_stats", out=st[:, h * 6:(h + 1) * 6], in_=u[:, h * 512:(h + 1) * 512])
            k.I("dve", "bn_aggr", out=st[:, 12:14], in_=st[:, 0:12])
            k.act(st[:, 14:15], st[:, 13:14], AF.Ln, bias=C.eps5[:, 0:1])
            k.act(st[:, 15:16], st[:, 14:15], AF.Exp, scale=-0.5)
            yield
            k.ts("dve", u.v, u.v, st[:, 12:13], ALU.subtract, st[:, 15:16], ALU.mult)
            xo = r_o.next()
            k.tt("pool", xo.v, u.v, g_bc.v, ALU.mult)
            k.tt("pool", xo.v, xo.v, b_bc.v, ALU.add)
            k.dma(xout_d[rows, :], xo.v, q="pool")
            xb = r_b.next()
            k.cp("act", xb.v, xo.v)
            yield
            g = j // GT
            xtg = xtgs[g % 2]
            pv = C.bank().v.bc(BF16)
            for c in range(8):
                k.tr(pv[:, c * 128:(c + 1) * 128], xb[:, c * 128:(c + 1) * 128], C.id_b.v)
            jj = j % GT
            k.cp("dve", xtg[:, :, jj * 128:(jj + 1) * 128], pv.re("p (c t) -> p c t", c=8))
            done[g] = done.get(g, 0) + 1
            if done[g] == GT:
                k.dma(xt_d[:, :, g * GT * 128:(g + 1) * GT * 128], xtg.v, q="pool")
            yield

        def stream(par):
            for j in range(par, NT, 2):
                yield from tile(j)

        interleave([stream(0), stream(1)])


def xt0_phase(k, C, S, x_d, xt_d):
    NT = S // 128
    with k.phase():
        r_x = Ring(k, 2, [128, 1024], F32, "x0")
        r_b = Ring(k, 2, [128, 1024], BF16, "x0b")
        GT = min(4, NT)
        r_g = Ring(k, 2, [128, 8, GT * 128], BF16, "x0t")
        for j in range(NT):
            rows = slice(j * 128, (j + 1) * 128)
            xr = r_x.next()
            k.dma(xr.v, x_d[rows, :])
            xb = r_b.next()
            k.cp("act", xb.v, xr.v)
            if j % GT == 0:
                xtg = r_g.next()
            pst = C.bank()
            pv = pst.v.bc(BF16)
            for c in range(8):
                k.tr(pv[:, c * 128:(c + 1) * 128], xb[:, c * 128:(c + 1) * 128], C.id_b.v)
            jj = j % GT
            k.cp("dve", xtg[:, :, jj * 128:(jj + 1) * 128], pv.re("p (c t) -> p c t", c=8))
            if jj == GT - 1:
                t0 = (j - jj) * 128
                k.dma(xt_d[:, :, t0:t0 + GT * 128], xtg.v, q="pool")


def ffn_phase(k, C, S, xt_d, x_d, u_d, experts, router_d=None):
    NT = S // 128
    TG = min(512, S)
    NTG = S // TG
    TPG = TG // 128
    with k.phase():
        XT = k.sb([128, 8, S], BF16, "ffxt")
        k.dma(XT.v, xt_d.v)
        acc = k.sb([128, NT, 1024], F32, "ffacc")
        G = None
        if router_d is not None:
            G = k.sb([128, NT, 8], F32, "ffG")
            rt = k.sb([128, 8, 8], F32, "ffrt")
            k.dma(rt.v, router_d.re("(c p) e -> p c e", p=128))
            r_x = Ring(k, 1, [128, 1024], F32, "ffx")
            r_t = Ring(k, 1, [128, 1024], F32, "ffxT")
            r_s = Ring(k, 2, [128, 48], F32, "ffs")
            for j in range(NT):
                xr = r_x.next()
                k.dma(xr.v, x_d[j * 128:(j + 1) * 128, :])
                xT = r_t.next()
                for hh in range(2):
                    pb = C.bank()
                    for c in range(4):
                        cc = hh * 4 + c
                        k.tr(pb[:, c * 128:(c + 1) * 128], xr[:, cc * 128:(cc + 1) * 128], C.id_f.v)
                    k.cp("act", xT[:, hh * 512:(hh + 1) * 512], pb.v)
                pl = C.bank()
                for c in range(8):
                    k.mm(pl[:, 0:8], xT[:, c * 128:(c + 1) * 128], rt[:, c, :], start=(c == 0), stop=(c == 7))
                s = r_s.next()
                lg = s[:, 0:8]
                k.cp("dve", lg, pl[:, 0:8])
                k.I("dve", "reduce_max", out=s[:, 8:9], in_=lg, axis=AX.X)
                k.ts("dve", s[:, 16:24], lg, s[:, 8:9], ALU.is_equal)
                k.stt(s[:, 24:32], s[:, 16:24], -1e30, lg, ALU.mult, ALU.add)
                k.I("dve", "reduce_max", out=s[:, 9:10], in_=s[:, 24:32], axis=AX.X)
                k.ts("dve", s[:, 16:24], lg, s[:, 9:10], ALU.is_ge)
                k.ts("dve", s[:, 10:11], s[:, 8:9], -1.0, ALU.mult)
                k.act(s[:, 32:40], lg, AF.Exp, bias=s[:, 10:11])
                k.tt("dve", s[:, 32:40], s[:, 32:40], s[:, 16:24], ALU.mult)
                k.I("dve", "reduce_sum", out=s[:, 11:12], in_=s[:, 32:40], axis=AX.X)
                k.I("dve", "reciprocal", out=s[:, 12:13], in_=s[:, 11:12])
                k.ts("dve", G[:, j, :], s[:, 32:40], s[:, 12:13], ALU.mult)
        WL = WLoader(k, 8 * 512, nstage=2, nbuf=4, name="ffw")
        r_h = Ring(k, 2, [128, 4, TG], BF16, "ffh")
        r_sg = Ring(k, 2, [128, TG], F32, "ffsg")
        first = True
        for e, (w13, w2, H) in enumerate(experts):
            h0 = 0
            while h0 < H:
                hw = min(512, H - h0)
                nc_ = hw // 128
                w1 = WL.load(w13[:, h0:h0 + hw], 8, hw)
                w3 = WL.load(w13[:, H + h0:H + h0 + hw], 8, hw)
                w2s = WL.load(w2[h0:h0 + hw, :], nc_, 1024)
                for tg in range(NTG):
                    tsl = slice(tg * TG, (tg + 1) * TG)
                    hT = r_h.next()
                    for c in range(nc_):
                        gp = C.bank()
                        for kc in range(8):
                            k.mm(gp[:, 0:TG], w1[:, kc, c * 128:(c + 1) * 128], XT[:, kc, tsl], start=(kc == 0), stop=(kc == 7))
                        up = C.bank()
                        for kc in range(8):
                            k.mm(up[:, 0:TG], w3[:, kc, c * 128:(c + 1) * 128], XT[:, kc, tsl], start=(kc == 0), stop=(kc == 7))
                        sg = r_sg.next()
                        k.act(sg.v, gp[:, 0:TG], AF.Silu)
                        k.tt("dve", hT[:, c, :], up[:, 0:TG], sg.v, ALU.mult)
                    for j in range(TPG):
                        tix = tg * TPG + j
                        for hh in range(2):
                            yp = C.bank()
                            for c in range(nc_):
                                k.mm(yp.v, hT[:, c, j * 128:(j + 1) * 128], w2s[:, c, hh * 512:(hh + 1) * 512],
                                     start=(c == 0), stop=(c == nc_ - 1))
                            av = acc[:, tix, hh * 512:(hh + 1) * 512]
                            if G is None:
                                if first:
                                    k.cp("act", av, yp.v)
                                else:
                                    k.tt("dve", av, yp.v, av, ALU.add)
                            else:
                                gs = G[:, tix, e:e + 1]
                                if first:
                                    k.ts("dve", av, yp.v, gs, ALU.mult)
                                else:
                                    k.stt(av, yp.v, gs, av, ALU.mult, ALU.add)
                first = False
                h0 += hw
        for j in range(NT):
            k.dma(u_d[j * 128:(j + 1) * 128, :], acc[:, j, :], q="pool")


def outproj_phase(k, C, S, ht_d, KC, w_d, u_d, p=128):
    NT = S // 128
    with k.phase():
        HT = k.sb([128, KC, S], BF16, "opH")
        k.dma(HT[0:p], ht_d.v)
        WL = WLoader(k, KC * 512, nstage=2, nbuf=2, name="opw")
        wh = [WL.load(w_d[:, hh * 512:(hh + 1) * 512], KC, 512, p=p) for hh in range(2)]
        r_u = Ring(k, 3, [128, 1024], F32, "opu")
        for j in range(NT):
            u = r_u.next()
            for hh in range(2):
                yp = C.bank()
                for c in range(KC):
                    k.mm(yp.v, HT[0:p, c, j * 128:(j + 1) * 128], wh[hh][:, c, :], start=(c == 0), stop=(c == KC - 1))
                k.cp("act" if hh == 0 else "dve", u[:, hh * 512:(hh + 1) * 512], yp.v)
            k.dma(u_d[j * 128:(j + 1) * 128, :], u.v, q="pool")


def to_fm_bf16(k, C, src_d, n_tok, dstT, r_x, r_b):
    for j in range(n_tok // 128):
        xr = r_x.next()
        k.dma(xr.v, src_d[j * 128:(j + 1) * 128, :])
        xb = r_b.next()
        k.cp("act", xb.v, xr.v)
        pv = C.bank().v.bc(BF16)
        for c in range(8):
            k.tr(pv[:, c * 128:(c + 1) * 128], xb[:, c * 128:(c + 1) * 128], C.id_b.v)
        k.cp("dve", dstT[:, :, j * 128:(j + 1) * 128], pv.re("p (c t) -> p c t", c=8))


def xattn_phase(k, C, S, xt_d, mem_d, wq_d, wkv_d, wo_d, u_d):
    TG = min(512, S)
    NTG = S // TG
    TPG = TG // 128
    M = 256
    with k.phase():
        r_xt = Ring(k, 2, [128, 8, TG], BF16, "xaxt")
        memT = k.sb([128, 8, M], BF16, "xamT")
        r_x = Ring(k, 2, [128, 1024], F32, "xamx")
        r_b = Ring(k, 2, [128, 1024], BF16, "xamb")
        to_fm_bf16(k, C, mem_d, M, memT.v, r_x, r_b)
        WL = WLoader(k, 8 * 512, nstage=2, nbuf=6, name="xaw")
        KT = k.sb([128, 8, M], BF16, "xaKT")
        Vm = k.sb([128, 2, 1024], BF16, "xaV")
        for sl in range(4):
            w = WL.load(wkv_d[:, sl * 512:(sl + 1) * 512], 8, 512)
            if sl < 2:
                for c in range(4):
                    pb = C.bank()
                    for kc in range(8):
                        k.mm(pb[:, 0:M], w[:, kc, c * 128:(c + 1) * 128], memT[:, kc, :], start=(kc == 0), stop=(kc == 7))
                    k.cp("act", KT[:, sl * 4 + c, :], pb[:, 0:M])
            else:
                for mt in range(2):
                    pb = C.bank()
                    for kc in range(8):
                        k.mm(pb.v, memT[:, kc, mt * 128:(mt + 1) * 128], w[:, kc, :], start=(kc == 0), stop=(kc == 7))
                    k.cp("act", Vm[:, mt, (sl - 2) * 512:(sl - 1) * 512], pb.v)
        wq = [WL.load(wq_d[:, hh * 512:(hh + 1) * 512], 8, 512) for hh in range(2)]
        wo = [WL.load(wo_d[:, hh * 512:(hh + 1) * 512], 8, 512) for hh in range(2)]
        r_q = Ring(k, 2, [128, 8, TG], BF16, "xaq")
        r_o = Ring(k, 2, [128, 8, TG], BF16, "xao")
        r_p = Ring(k, 4, [128, TG], BF16, "xap")
        r_d = Ring(k, 2, [128, TG], F32, "xad")
        r_u = Ring(k, 2, [128, 1024], F32, "xau")
        for tg in range(NTG):
            tsl = slice(tg * TG, (tg + 1) * TG)
            XT = r_xt.next()
            k.dma(XT.v, xt_d[:, :, tsl])
            qT = r_q.next()
            for n in range(8):
                pb = C.bank()
                for kc in range(8):
                    k.mm(pb[:, 0:TG], wq[n // 4][:, kc, (n % 4) * 128:(n % 4 + 1) * 128], XT[:, kc, :],
                         start=(kc == 0), stop=(kc == 7))
                k.cp("act" if n % 2 == 0 else "dve", qT[:, n, :], pb[:, 0:TG])
            oT = r_o.next()
            for h in range(4):
                P = []
                for mt in range(2):
                    zb = C.bank()
                    for c in range(2):
                        k.mm(zb[:, 0:TG], KT[:, 2 * h + c, mt * 128:(mt + 1) * 128], qT[:, 2 * h + c, :],
                             start=(c == 0), stop=(c == 1))
                    p = r_p.next()
                    k.act(p.v, zb[:, 0:TG], AF.Exp, scale=1.0 / 16.0)
                    P.append(p)
                db = C.bank()
                for mt in range(2):
                    k.mm(db[:, 0:TG], C.ones_b.v, P[mt].v, start=(mt == 0), stop=(mt == 1))
                rd = r_d.next()
                k.I("dve", "reciprocal", out=rd.v, in_=db[:, 0:TG])
                for c in range(2):
                    ob = C.bank()
                    for mt in range(2):
                        k.mm(ob[:, 0:TG], Vm[:, mt, (2 * h + c) * 128:(2 * h + c + 1) * 128], P[mt].v,
                             start=(mt == 0), stop=(mt == 1))
                    k.tt("dve", oT[:, 2 * h + c, :], ob[:, 0:TG], rd.v, ALU.mult)
            for j in range(TPG):
                u = r_u.next()
                for hh in range(2):
                    yp = C.bank()
                    for c in range(8):
                        k.mm(yp.v, oT[:, c, j * 128:(j + 1) * 128], wo[hh][:, c, :], start=(c == 0), stop=(c == 7))
                    k.cp("act" if hh == 0 else "dve", u[:, hh * 512:(hh + 1) * 512], yp.v)
                t0 = tg * TG + j * 128
                k.dma(u_d[t0:t0 + 128, :], u.v, q="pool")


def gate_phase(k, C, S, xt_d, ybr_d, wgate_d, wbr_d, mergedT_d):
    TG = min(512, S)
    NTG = S // TG
    with k.phase():
        XT = k.sb([128, 8, S], BF16, "gxt")
        k.dma(XT.v, xt_d.v)
        Y = []
        for i in range(4):
            y = k.sb([128, 2, S], BF16, "gy")
            k.dma(y.v, ybr_d[i].v)
            Y.append(y)
        WL = WLoader(k, 8 * 512, nstage=2, nbuf=4, name="gw")
        macc = k.sb([128, 4, S], F32, "gacc")
        r_s = Ring(k, 3, [128, TG], F32, "gsg")
        r_m = Ring(k, 2, [128, 4, TG], BF16, "gm")
        for sl in range(2):
            for i in range(4):
                wg = WL.load(wgate_d[i, :, sl * 512:(sl + 1) * 512], 8, 512)
                wb = WL.load(wbr_d[i, :, sl * 512:(sl + 1) * 512], 2, 512)
                for tg in range(NTG):
                    tsl = slice(tg * TG, (tg + 1) * TG)
                    for c in range(4):
                        gp = C.bank()
                        for kc in range(8):
                            k.mm(gp[:, 0:TG], wg[:, kc, c * 128:(c + 1) * 128], XT[:, kc, tsl], start=(kc == 0), stop=(kc == 7))
                        pp = C.bank()
                        for kc in range(2):
                            k.mm(pp[:, 0:TG], wb[:, kc, c * 128:(c + 1) * 128], Y[i][:, kc, tsl], start=(kc == 0), stop=(kc == 1))
                        sg = r_s.next()
                        k.act(sg.v, gp[:, 0:TG], AF.Sigmoid)
                        if i == 0:
                            k.tt("dve", macc[:, c, tsl], pp[:, 0:TG], sg.v, ALU.mult)
                        else:
                            k.tt("dve", sg.v, pp[:, 0:TG], sg.v, ALU.mult)
                            k.tt("pool", macc[:, c, tsl], macc[:, c, tsl], sg.v, ALU.add)
            for tg in range(NTG):
                tsl = slice(tg * TG, (tg + 1) * TG)
                mb = r_m.next()
                k.cp("act", mb.v, macc[:, :, tsl])
                k.dma(mergedT_d[:, sl * 4:(sl + 1) * 4, tsl], mb.v, q="pool")


def sb_consts(k, C, TG):
    if hasattr(C, "sbm"):
        return
    m = k.sbg([128, 128], F32, "triu")
    k.aselect(m.v, C.ones_f[:, 0:128], [[-1, 128]], ALU.is_gt, 0.0, 0, 1)
    C.tri_gt = k.sbg([128, 128], BF16, "triub")
    k.cp("pool", C.tri_gt.v, m.v)
    m2 = k.sbg([128, 128], F32, "tril")
    k.aselect(m2.v, C.ones_f[:, 0:128], [[1, 128]], ALU.is_ge, 0.0, 0, -1)
    C.tri_le = k.sbg([128, 128], BF16, "trilb")
    k.cp("pool", C.tri_le.v, m2.v)
    C.one = k.sbg([128, 1], F32, "one")
    k.memset("pool", C.one.v, 1.0)


def sb_phase(k, C, S, xt_d, win_d, y_d):
    TG = min(512, S)
    NTG = S // TG
    NT = S // 128
    sb_consts(k, C, TG)
    with k.phase():
        C.sbm = [C.mask(TG, -r * 128, -1, ALU.is_gt, F32, "sbm%d" % r, local=True) for r in range(TG // 128)]
        XT = k.sb([128, 8, S], BF16, "sbxt")
        k.dma(XT.v, xt_d.v)
        WL = WLoader(k, 8 * 512, nstage=2, nbuf=2, name="sbw")
        wqk = WL.load(win_d[:, 0:512], 8, 512)
        wv = WL.load(win_d[:, 512:768], 8, 256)
        qk = k.sb([128, 4, S], BF16, "sbqk")
        Vt = k.sb([128, NT, 256], BF16, "sbv")
        for tg in range(NTG):
            tsl = slice(tg * TG, (tg + 1) * TG)
            for n in range(4):
                pb = C.bank()
                for kc in range(8):
                    k.mm(pb[:, 0:TG], wqk[:, kc, n * 128:(n + 1) * 128], XT[:, kc, tsl], start=(kc == 0), stop=(kc == 7))
                k.cp("act", qk[:, n, tsl], pb[:, 0:TG])
        for j in range(NT):
            pb = C.bank()
            for kc in range(8):
                k.mm(pb[:, 0:256], XT[:, kc, j * 128:(j + 1) * 128], wv[:, kc, :], start=(kc == 0), stop=(kc == 7))
            k.cp("act", Vt[:, j, :], pb[:, 0:256])
        YT = k.sb([128, 2, S], BF16, "sby")
        r_e = Ring(k, 6, [128, TG], F32, "sbe")
        r_sp = Ring(k, 2, [128, TG], F32, "sbsp")
        r_sm = Ring(k, 8, [128, TG], BF16, "sbsm")
        r_u = Ring(k, 6, [128, TG], F32, "sbu")
        r_w = Ring(k, 8, [128, TG], BF16, "sbw_")
        r_wf = Ring(k, 2, [128, TG], F32, "sbwf")
        C.pool = [0, 1]

        def unit(h, qg, slot):
            hp, bp = h // 2, (h % 2) * 64
            q0 = qg * TG
            qsl = slice(q0, q0 + TG)
            tailb = C.ps[4 + slot]
            yb = C.ps[2 + slot // 2]
            last = (q0 + TG) // 128 - 1
            for kt in range(last, -1, -1):
                first = kt == last
                zb = C.bank()
                k.mm(zb[:, 0:TG], qk[bp:bp + 64, 2 + hp, kt * 128:(kt + 1) * 128], qk[bp:bp + 64, hp, qsl])
                e = r_e.next()
                k.act(e.v, zb[:, 0:TG], AF.Exp, scale=0.125)
                diag = kt * 128 >= q0
                sm = r_sm.next()
                if diag:
                    r = (kt * 128 - q0) // 128
                    sp = r_sp.next()
                    k.act(sp.v, e.v, AF.Ln, bias=C.one[:, 0:1])
                    k.tt("dve", sm.v, sp.v, C.sbm[r].v, ALU.mult)
                else:
                    k.act(sm.v, e.v, AF.Ln, bias=C.one[:, 0:1])
                k.I("pe", "matmul", out=tailb[:, 0:TG], lhsT=C.tri_gt.v, rhs=sm.v, start=first, stop=True,
                    skip_group_check=True)
                u = r_u.next()
                k.stt(u.v, zb[:, 0:TG], 0.125, sm.v, ALU.mult, ALU.subtract)
                yield
                k.tt("dve", u.v, u.v, tailb[:, 0:TG], ALU.subtract)
                w = r_w.next()
                if diag:
                    wf = r_wf.next()
                    k.act(wf.v, u.v, AF.Exp)
                    k.tt("pool", w.v, wf.v, C.sbm[r].v, ALU.mult)
                else:
                    k.act(w.v, u.v, AF.Exp)
                if kt > 0:
                    k.I("pe", "matmul", out=tailb[:, 0:TG], lhsT=C.tri_le.v, rhs=sm.v, start=False, stop=True,
                        skip_group_check=True)
                k.I("pe", "matmul", out=yb[bp:bp + 64, 0:TG], lhsT=Vt[:, kt, h * 64:(h + 1) * 64], rhs=w.v,
                    start=first, stop=(kt == 0), skip_group_check=True)
                yield
            k.cp("act", YT[bp:bp + 64, hp, qsl], yb[bp:bp + 64, 0:TG])

        units = [(h, qg) for qg in range(NTG) for h in range(4)]

        def stream(slot):
            for (h, qg) in units[slot::4]:
                yield from unit(h, qg, slot)

        interleave([stream(i) for i in range(4)])
        C.pool = list(range(8))
        k.dma(y_d.v, YT.v, q="pool")


def rope_phase(k, C, S, pos_d, rope_d):
    with k.phase():
        pi_ = k.sb([128, S], I32, "rpi")
        k.dma(pi_.v, pos_d.pbro(128))
        pf = k.sb([128, S], F32, "rpf")
        k.cp("dve", pf.v, pi_.v)
        pidx = k.sb([128, 4], I32, "rpx")
        k.I("pool", "iota", out=pidx[:, 0:1], pattern=[[0, 1]], base=0, channel_multiplier=1)
        k.ts("dve", pidx[:, 1:2], pidx[:, 0:1], 15, ALU.bitwise_and)
        k.ts("dve", pidx[:, 2:3], pidx[:, 0:1], 16, ALU.bitwise_and)
        cf = k.sb([128, 8], F32, "rcf")
        k.cp("dve", cf[:, 0:1], pidx[:, 1:2])
        k.cp("dve", cf[:, 1:2], pidx[:, 2:3])
        k.act(cf[:, 2:3], cf[:, 0:1], AF.Exp, scale=-math.log(10000.0) / 16.0)
        k.ts("dve", cf[:, 3:4], cf[:, 1:2], 0.125, ALU.mult, -1.0, ALU.add)
        ang = k.sb([128, S], F32, "rang")
        k.ts("dve", ang.v, pf.v, cf[:, 2:3], ALU.mult)
        C1, C2 = 6.28125, 2 * math.pi - 6.28125
        MAGIC = 12582912.0
        kf = k.sb([128, S], F32, "rkf")
        r = k.sb([128, S], F32, "rr")
        out = k.sb([128, S], F32, "rout")
        for which in range(2):
            off = 0.25 if which == 0 else 0.0
            k.ts("dve", kf.v, ang.v, 1.0 / (2 * math.pi), ALU.mult, off, ALU.add)
            k.ts("dve", kf.v, kf.v, MAGIC, ALU.add)
            k.ts("dve", kf.v, kf.v, MAGIC, ALU.subtract)
            k.stt(r.v, kf.v, -C1, ang.v, ALU.mult, ALU.add)
            k.stt(r.v, kf.v, -C2, r.v, ALU.mult, ALU.add)
            if which == 0:
                k.ts("dve", r.v, r.v, math.pi / 2, ALU.add)
            k.ts("dve", r.v, r.v, 3.1415925, ALU.min, -3.1415925, ALU.max)
            k.act(out.v, r.v, AF.Sin)
            if which == 1:
                k.ts("dve", out.v, out.v, cf[:, 3:4], ALU.mult)
            k.dma(rope_d[which], out.v, q="pool")


MLA_OFF = 768 + 1024 + 1028


def mla_phase(k, C, S, xt_d, win_d, qg_d, wuq_d, kvg_d, wukv_d, rope_d, y_d):
    TG = min(512, S)
    NTG = S // TG
    NT = S // 128
    TPG = TG // 128
    sb_consts(k, C, TG)
    SC = 96.0 ** -0.5
    with k.phase():
        C.cam = [C.mask(TG, -r * 128, -1, ALU.is_ge, F32, "cam%d" % r, local=True) for r in range(TG // 128)]
        WL = WLoader(k, 8 * 512, nstage=2, nbuf=1, name="mlw")
        wm = WL.load(win_d[:, 0:416], 8, 416)
        wkr2 = k.sb([128, 8, 64], BF16, "mlkr")
        k.cp("pool", wkr2[:, :, 0:32], wm[:, :, 384:416])
        k.cp("pool", wkr2[:, :, 32:48], wm[:, :, 400:416])
        k.cp("pool", wkr2[:, :, 48:64], wm[:, :, 384:400])
        gq = k.sb([128, 4], F32, "mlg")
        for c in range(2):
            k.dma(gq[:, c:c + 1], qg_d[c * 128:(c + 1) * 128].re("(p o) -> p o", o=1))
        k.dma(gq[:, 2:3], kvg_d.re("(p o) -> p o", o=1))
        st = k.sb([128, 2 * 384], F32, "mlst")
        k.dma(st.v.re("p (c n) -> p c n", c=2), wuq_d.re("(c p) n -> p c n", p=128))
        wuq = k.sb([128, 2, 384], BF16, "mluq")
        for c in range(2):
            k.ts("pool", wuq[:, c, :], st[:, c * 384:(c + 1) * 384], gq[:, c:c + 1], ALU.mult)
        wuqs = k.sb([128, 2, 128], BF16, "mluqs")
        for h in range(4):
            k.cp("pool", wuqs[:, :, h * 32:h * 32 + 16], wuq[:, :, h * 96 + 80:h * 96 + 96])
            k.cp("pool", wuqs[:, :, h * 32 + 16:h * 32 + 32], wuq[:, :, h * 96 + 64:h * 96 + 80])
        st2 = k.sb([128, 512], F32, "mlst2")
        k.dma(st2.v, wukv_d)
        wukv = k.sb([128, 512], BF16, "mlukv")
        k.ts("pool", wukv.v, st2.v, gq[:, 2:3], ALU.mult)
        wv = k.sb([128, 256], BF16, "mlwv")
        for h in range(4):
            k.cp("pool", wv[:, h * 64:(h + 1) * 64], wukv[:, h * 128 + 64:h * 128 + 128])
        cs = k.sb([128, 2, S], F32, "mlcs")
        for w_ in range(2):
            k.dma(cs[:, w_, :], rope_d[w_])
        cT = k.sb([128, 3, S], BF16, "mlcT")
        qT = k.sb([128, 4, S], BF16, "mlqT")
        kT = k.sb([128, 4, S], BF16, "mlkT")
        Vt = k.sb([128, NT, 256], BF16, "mlV")
        r_xt = Ring(k, 2, [128, 8, TG], BF16, "mlxt")
        r_c = Ring(k, 2, [128, 384], BF16, "mlc")
        r_j = Ring(k, 2, [128, 256], F32, "mlj")
        r_s = Ring(k, 2, [128, 8], F32, "mls")
        r_t = Ring(k, 4, [128, TG], F32, "mlt")
        for tg in range(NTG):
            tsl = slice(tg * TG, (tg + 1) * TG)
            XT = r_xt.next()
            k.dma(XT.v, xt_d[:, :, tsl])
            for jj in range(TPG):
                j = tg * TPG + jj
                pb = C.bank()
                for kc in range(8):
                    k.mm(pb[:, 0:384], XT[:, kc, jj * 128:(jj + 1) * 128], wm[:, kc, 0:384], start=(kc == 0), stop=(kc == 7))
                s = r_s.next()
                jk = r_j.next()
                k.act(jk[:, 0:256], pb[:, 0:256], AF.Square, accum_out=s[:, 0:1])
                k.act(jk[:, 0:128], pb[:, 256:384], AF.Square, accum_out=s[:, 1:2])
                k.act(s[:, 2:3], s[:, 0:1], AF.Ln, scale=1.0 / 256, bias=C.eps6[:, 0:1])
                k.act(s[:, 3:4], s[:, 1:2], AF.Ln, scale=1.0 / 128, bias=C.eps6[:, 0:1])
                k.act(s[:, 4:6], s[:, 2:4], AF.Exp, scale=-0.5)
                cb = r_c.next()
                k.ts("dve", cb[:, 0:256], pb[:, 0:256], s[:, 4:5], ALU.mult)
                k.ts("dve", cb[:, 256:384], pb[:, 256:384], s[:, 5:6], ALU.mult)
                pv = C.bank().v.bc(BF16)
                for c in range(3):
                    k.tr(pv[:, c * 128:(c + 1) * 128], cb[:, c * 128:(c + 1) * 128], C.id_b.v)
                k.cp("act", cT[:, :, j * 128:(j + 1) * 128], pv[:, 0:384].re("p (c t) -> p c t", c=3))
                pvb = C.bank()
                k.mm(pvb[:, 0:256], cT[:, 2, j * 128:(j + 1) * 128], wv.v)
                k.cp("act", Vt[:, j, :], pvb[:, 0:256])
            p1 = C.bank()
            p2 = C.bank()
            for kc in range(8):
                k.mm(p1[64:96, 0:TG], wkr2[:, kc, 0:32], XT[:, kc, :], start=(kc == 0), stop=(kc == 7))
            for kc in range(8):
                k.mm(p2[64:96, 0:TG], wkr2[:, kc, 32:64], XT[:, kc, :], start=(kc == 0), stop=(kc == 7))
            t1 = r_t.next()
            t2 = r_t.next()
            k.tt("dve", t1[64:96, :], p1[64:96, 0:TG], cs[64:96, 0, tsl], ALU.mult)
            k.tt("dve", t2[64:96, :], p2[64:96, 0:TG], cs[64:96, 1, tsl], ALU.mult)
            k.tt("dve", kT[64:96, 0, tsl], t1[64:96, :], t2[64:96, :], ALU.add)
            for h in range(1, 4):
                k.cp("pool", kT[64:96, h, tsl], kT[64:96, 0, tsl])
            for h in range(4):
                pk = C.bank()
                k.mm(pk[0:64, 0:TG], wukv[:, h * 128:h * 128 + 64], cT[:, 2, tsl])
                k.cp("act", kT[0:64, h, tsl], pk[0:64, 0:TG])
                pq = C.bank()
                for c in range(2):
                    k.mm(pq[0:96, 0:TG], wuq[:, c, h * 96:(h + 1) * 96], cT[:, c, tsl], start=(c == 0), stop=(c == 1))
                pq2 = C.bank()
                for c in range(2):
                    k.mm(pq2[64:96, 0:TG], wuqs[:, c, h * 32:(h + 1) * 32], cT[:, c, tsl], start=(c == 0), stop=(c == 1))
                k.cp("act", qT[0:64, h, tsl], pq[0:64, 0:TG])
                t1 = r_t.next()
                t2 = r_t.next()
                k.tt("dve", t1[64:96, :], pq[64:96, 0:TG], cs[64:96, 0, tsl], ALU.mult)
                k.tt("dve", t2[64:96, :], pq2[64:96, 0:TG], cs[64:96, 1, tsl], ALU.mult)
                k.tt("dve", qT[64:96, h, tsl], t1[64:96, :], t2[64:96, :], ALU.add)
        YT = k.sb([128, 2, S], BF16, "mly")
        r_p = Ring(k, 4, [128, TG], BF16, "mlp")
        r_pf = Ring(k, 2, [128, TG], F32, "mlpf")
        r_d = Ring(k, 2, [128, TG], F32, "mld")
        C.pool = [0, 1, 2, 3]

        def unit(h, qg, slot):
            hp, bp = h // 2, (h % 2) * 64
            q0 = qg * TG
            qsl = slice(q0, q0 + TG)
            db = C.ps[4 + slot]
            yb = C.ps[6 + slot]
            last = (q0 + TG) // 128 - 1
            for kt in range(last + 1):
                zb = C.bank()
                k.mm(zb[:, 0:TG], kT[0:96, h, kt * 128:(kt + 1) * 128], qT[0:96, h, qsl])
                p = r_p.next()
                if kt * 128 >= q0:
                    r = (kt * 128 - q0) // 128
                    pf = r_pf.next()
                    k.act(pf.v, zb[:, 0:TG], AF.Exp, scale=SC)
                    k.tt("pool", p.v, pf.v, C.cam[r].v, ALU.mult)
                else:
                    k.act(p.v, zb[:, 0:TG], AF.Exp, scale=SC)
                yield
                k.I("pe", "matmul", out=yb[bp:bp + 64, 0:TG], lhsT=Vt[:, kt, h * 64:(h + 1) * 64], rhs=p.v,
                    start=(kt == 0), stop=(kt == last), skip_group_check=True)
                k.I("pe", "matmul", out=db[bp:bp + 64, 0:TG], lhsT=C.ones_b[:, 0:64], rhs=p.v,
                    start=(kt == 0), stop=(kt == last), skip_group_check=True)
            rd = r_d.next()
            k.I("dve", "reciprocal", out=rd[bp:bp + 64, :], in_=db[bp:bp + 64, 0:TG])
            k.tt("dve", YT[bp:bp + 64, hp, qsl], yb[bp:bp + 64, 0:TG], rd[bp:bp + 64, :], ALU.mult)
            yield

        units = [(h, qg) for qg in range(NTG) for h in range(4)]

        def stream(slot):
            for (h, qg) in units[slot::2]:
                yield from unit(h, qg, slot)

        interleave([stream(0), stream(1)])
        C.pool = list(range(8))
        k.dma(y_d.v, YT.v, q="pool")


def load_cols(k, dv, nchunk, name="pc", p=128):
    t = k.sb([128, nchunk], F32, name)
    for c in range(nchunk):
        k.dma(t[0:p, c:c + 1], dv[c * p:(c + 1) * p].re("(p o) -> p o", o=1))
    return t


RW_OFF = 768
EXPM05 = math.exp(-0.5)


def rwkv_phase(k, C, S, xt_d, win_d, prm, y_d, stop=None):
    RG = min(128, S)
    NRG = S // RG
    CH = 64
    NCH = RG // CH
    NQ = 14
    with k.phase():
        o64 = C.ones_f[0:64, 0:256].re("p (h s) -> p h s", h=4)
        ones64 = C.ones_b[0:64, 0:64]
        idb64 = C.id_b[0:64, 0:64]
        mLs = k.sb([64, 4, 64], F32, "rwLs")
        k.aselect(mLs.v, o64, [[0, 4], [-1, 64]], ALU.is_gt, 0.0, 0, 1)
        mUs = k.sb([64, 4, 64], F32, "rwUs")
        k.aselect(mUs.v, o64, [[0, 4], [1, 64]], ALU.is_gt, 0.0, 0, -1)
        mUi = k.sb([64, 4, 64], F32, "rwUi")
        k.aselect(mUi.v, o64, [[0, 4], [1, 64]], ALU.is_ge, 0.0, 0, -1)
        mI = k.sb([64, 4, 64], F32, "rwI")
        k.aselect(mI.v, o64, [[0, 4], [1, 64]], ALU.is_equal, 0.0, 0, -1)
        rmi = k.sb([64, 4 * RG], I32, "rwrmi")
        k.I("pool", "iota", out=rmi.v, pattern=[[1, 4 * RG]], base=0, channel_multiplier=0)
        k.ts("dve", rmi.v, rmi.v, CH - 1, ALU.bitwise_and)
        rmask = k.sb([64, 4 * RG], F32, "rwrm")
        k.cp("dve", rmask.v, rmi.v)
        k.ts("dve", rmask.v, rmask.v, 1.0, ALU.min)
        mu = load_cols(k, prm["mu"][0:896], NQ, "rwmu", p=64)
        mug = load_cols(k, prm["mu"][896:1024], 1, "rwmug")
        w0 = load_cols(k, prm["w0"], 4, "rww0", p=64)
        a0 = load_cols(k, prm["a0"], 4, "rwa0", p=64)
        kk_ = load_cols(k, prm["k_k"], 4, "rwkk", p=64)
        ka = load_cols(k, prm["k_a"], 4, "rwka", p=64)
        rk = load_cols(k, prm["r_k"], 4, "rwrk", p=64)
        gng = load_cols(k, prm["gn_g"], 4, "rwgg", p=64)
        gnb = load_cols(k, prm["gn_b"], 4, "rwgb", p=64)
        omka = k.sb([128, 4], F32, "rwomka")
        k.ts("dve", omka[0:64, :], ka[0:64, :], -1.0, ALU.mult, 1.0, ALU.add)
        eps_gn = k.sb([128, 1], F32, "rwepsgn")
        k.memset("pool", eps_gn.v, 64e-5)
        WL = WLoader(k, 8 * 512, nstage=1, nbuf=2, name="rww")
        wrw = [WL.load(win_d[:, hh * 512:(hh + 1) * 512], 8, 512) for hh in range(2)]
        lst = k.sb([128, 768], F32, "rwlst")
        k.dma(lst[0:64, 0:256], prm["w_up"])
        k.dma(lst[0:64, 256:512], prm["a_up"])
        k.dma(lst[:, 512:768], prm["g_up"])
        lup = k.sb([128, 768], BF16, "rwlup")
        k.cp("pool", lup[0:64, 0:512], lst[0:64, 0:512])
        k.cp("pool", lup[:, 512:768], lst[:, 512:768])
        ST = k.sb([64, 4, 64], F32, "rwST")
        k.memset("pool", ST.v, 0.0)
        carry = k.sb([64, NQ, 1], F32, "rwcar")
        k.memset("pool", carry.v, 0.0)
        carryg = k.sb([128, 1], F32, "rwcarg")
        k.memset("pool", carryg.v, 0.0)
        SG = min(512, S)
        GPS = SG // RG
        r_xt = Ring(k, 1, [128, 8, SG], BF16, "rwxt")
        pT = k.sb([64, NQ, SG + 1], F32, "rwpT")
        pG = k.sb([128, SG + 1], F32, "rwpG")
        ps = k.sb([64, NQ, RG], F32, "rwps")
        psg = k.sb([128, RG], F32, "rwpsg")
        tmp = Ring(k, 4, [64, 4, RG], F32, "rwtmp")
        tmp2 = Ring(k, 3, [64, 4, RG], F32, "rwtmp2")
        tmpg = k.sb([128, RG], F32, "rwtmpg")
        r_bf = Ring(k, 2, [128, RG], BF16, "rwbf")

        def arr(name):
            return k.sb([64, 4, RG], F32, name)

        A_a, A_kap, A_kp, A_lw, A_cl = arr("rwa"), arr("rwkap"), arr("rwkp"), arr("rwlw"), arr("rwcl")
        def arr16(name):
            return k.sb([64, 4, RG], BF16, name)

        A_kt, A_bt, A_kbar, A_bbar, A_y = arr16("rwkt"), arr16("rwbt"), arr16("rwkbar"), arr16("rwbbar"), arr("rwy")
        A_yb, Vb = arr16("rwyb"), arr16("rwvb")
        tmpb = Ring(k, 3, [64, 4, RG], BF16, "rwtmpb")
        STb = k.sb([64, 4, 64], BF16, "rwSTb")
        k.memset("pool", STb.v, 0.0)
        yout = Ring(k, 2, [64, 4, RG], BF16, "rwyo")
        sg = k.sb([128, RG], BF16, "rwsg")

        def cset(n):
            return [dict(G=[k.sb([64, 4, 64], F32 if i < 2 else BF16, "rwG%d" % i) for i in range(5)],
                         Pb=k.sb([64, 4, 64], BF16, "rwPb"),
                         N=[k.sb([64, 4, 64], F32, "rwN%d" % i) for i in range(2)],
                         M=[k.sb([64, 4, 64], F32, "rwM%d" % i) for i in range(2)],
                         P=[k.sb([64, 4, 64], F32, "rwP%d" % i) for i in range(2)],
                         tok=[k.sb([64, 4, 64], BF16, "rwtok%d" % i) for i in range(3)]) for _ in range(n)]

        def mkset():
            return dict(A_rt=arr16("rwrt"), A_kapt=arr16("rwkapt"), A_g=arr("rwg"), A_bon=arr("rwbon"),
                        gC=k.sb([64, 4, NCH], F32, "rwgC"), CS=cset(NCH), curP=[None] * NCH)

        SETS = [mkset(), mkset()]
        Wt = k.sb([64, 4, 64], BF16, "rwW")
        Ut = k.sb([64, 4, 64], BF16, "rwU")

        def v4(bank):
            return bank[0:64, 0:256].re("p (h s) -> p h s", h=4)

        def fm(Aarr, h, ch):
            return Aarr[:, h, ch * CH:(ch + 1) * CH]

        def gen_AB(rg, B):
            A_rt, A_kapt, A_g, A_bon, gC, CS = B["A_rt"], B["A_kapt"], B["A_g"], B["A_bon"], B["gC"], B["CS"]
            tsl = slice(rg * RG, (rg + 1) * RG)
            o = (rg % GPS) * RG
            if rg % GPS == 0:
                XT = r_xt.next()
                k.dma(XT.v, xt_d[:, :, rg * RG:rg * RG + SG])
                k.cp("pool", pT[:, :, 0:1], carry.v)
                k.cp("pool", pG[:, 0:1], carryg.v)
                for q in range(NQ):
                    c0 = q * 64
                    pb = C.bank()
                    for kc in range(8):
                        k.mm(pb[0:64, 0:SG], wrw[c0 // 512][:, kc, c0 % 512:c0 % 512 + 64], XT[:, kc, :],
                             start=(kc == 0), stop=(kc == 7))
                    k.cp("act", pT[:, q, 1:SG + 1], pb[0:64, 0:SG])
                    if q % 2 == 1:
                        yield
                pb = C.bank()
                for kc in range(8):
                    k.mm(pb[:, 0:SG], wrw[1][:, kc, 384:512], XT[:, kc, :], start=(kc == 0), stop=(kc == 7))
                k.cp("act", pG[:, 1:SG + 1], pb[:, 0:SG])
                k.cp("pool", carry.v, pT[:, :, SG:SG + 1])
                k.cp("pool", carryg.v, pG[:, SG:SG + 1])
                yield
            for q0 in range(0, NQ, 4):
                nq = min(4, NQ - q0)
                t = tmp.next()
                k.tt("pool", t[:, 0:nq, :], pT[:, q0:q0 + nq, o:o + RG], pT[:, q0:q0 + nq, o + 1:o + RG + 1], ALU.subtract)
                for q in range(q0, q0 + nq):
                    k.stt(ps[:, q, :], t[:, q - q0, :], mu[0:64, q:q + 1], pT[:, q, o + 1:o + RG + 1], ALU.mult, ALU.add)
                yield
            k.tt("pool", tmpg.v, pG[:, o:o + RG], pG[:, o + 1:o + RG + 1], ALU.subtract)
            k.stt(psg.v, tmpg.v, mug[:, 0:1], pG[:, o + 1:o + RG + 1], ALU.mult, ALU.add)
            R_, K_, V_ = ps[:, 0:4, :], ps[:, 4:8, :], ps[:, 8:12, :]
            tw = r_bf.next()
            k.act(tw[0:64, :], ps[:, 12, :], AF.Tanh)
            alo = r_bf.next()
            k.cp("pool", alo[0:64, :], ps[:, 13, :])
            k.act(sg.v, psg.v, AF.Sigmoid)
            yield
            for hq in range(2):
                pw = C.bank()
                pa = C.bank()
                pg = C.bank()
                for hh in range(2):
                    h = hq * 2 + hh
                    k.mm(pw[0:64, hh * RG:(hh + 1) * RG], lup[0:64, h * 64:(h + 1) * 64], tw[0:64, :])
                    k.mm(pa[0:64, hh * RG:(hh + 1) * RG], lup[0:64, 256 + h * 64:256 + (h + 1) * 64], alo[0:64, :])
                    k.mm(pg[0:64, hh * RG:(hh + 1) * RG], lup[:, 512 + h * 64:512 + (h + 1) * 64], sg.v)
                for hh in range(2):
                    h = hq * 2 + hh
                    k.act(A_lw[:, h, :], pw[0:64, hh * RG:(hh + 1) * RG], AF.Sigmoid, bias=w0[0:64, h:h + 1])
                    k.act(A_a[:, h, :], pa[0:64, hh * RG:(hh + 1) * RG], AF.Sigmoid, bias=a0[0:64, h:h + 1])
                    k.cp("act", A_g[:, h, :], pg[0:64, hh * RG:(hh + 1) * RG])
                yield
            k.ts("pool", A_lw.v, A_lw.v, -EXPM05, ALU.mult)
            kkv = tmp.next()
            for h in range(4):
                k.ts("dve", kkv[:, h, :], K_[:, h, :], kk_[0:64, h:h + 1], ALU.mult)
            sq = tmpb.next()
            k.tt("pool", sq.v, kkv.v, kkv.v, ALU.mult)
            k.cp("pool", Vb.v, V_)
            yield
            rs = tmp.next()
            for hq in range(2):
                pss = C.bank()
                for hh in range(2):
                    k.mm(pss[0:64, hh * RG:(hh + 1) * RG], ones64, sq[:, hq * 2 + hh, :])
                k.ts("dve", rs[:, hq * 2:hq * 2 + 2, :], pss[0:64, 0:2 * RG].re("p (h t) -> p h t", h=2), 1e-24, ALU.max)
            k.act(rs.v, rs.v, AF.Ln)
            k.act(rs.v, rs.v, AF.Exp, scale=-0.5)
            k.tt("dve", A_kap.v, kkv.v, rs.v, ALU.mult)
            yield
            k.I("dve", "tensor_tensor_scan", out=A_cl.v.re("p h t -> p (h t)"), data0=rmask.v,
                data1=A_lw.v.re("p h t -> p (h t)"), initial=0.0, op0=ALU.mult, op1=ALU.add)
            t = tmp.next()
            for h in range(4):
                k.ts("dve", t[:, h, :], A_a[:, h, :], ka[0:64, h:h + 1], ALU.mult, omka[0:64, h:h + 1], ALU.add)
            k.tt("pool", A_kp.v, K_, t.v, ALU.mult)
            yield
            t2 = tmpb.next()
            for h in range(4):
                k.stt(t2[:, h, :], R_[:, h, :], rk[0:64, h:h + 1], A_kp[:, h, :], ALU.mult, ALU.mult)
            for hq in range(2):
                pbn = C.bank()
                for hh in range(2):
                    k.mm(pbn[0:64, hh * RG:(hh + 1) * RG], ones64, t2[:, hq * 2 + hh, :])
                k.tt("dve", A_bon[:, hq * 2:hq * 2 + 2, :], pbn[0:64, 0:2 * RG].re("p (h t) -> p h t", h=2),
                     V_[:, hq * 2:hq * 2 + 2, :], ALU.mult)
            yield
            ep = tmp.next()
            k.act(ep.v, A_cl.v, AF.Exp)
            k.tt("pool", A_rt.v, R_, ep.v, ALU.mult)
            en = tmp.next()
            k.act(en.v, A_cl.v, AF.Exp, scale=-1.0)
            k.tt("dve", A_kt.v, A_kp.v, en.v, ALU.mult)
            b_ = tmp.next()
            k.tt("pool", b_.v, A_kap.v, A_a.v, ALU.mult)
            k.tt("dve", A_bt.v, b_.v, en.v, ALU.mult)
            yield
            t3 = tmp.next()
            k.tt("pool", t3.v, A_cl.v, A_lw.v, ALU.subtract)
            k.act(t3.v, t3.v, AF.Exp)
            k.tt("dve", A_kapt.v, A_kap.v, t3.v, ALU.mult)
            clv = A_cl.v.re("p h (c t) -> p (h c) t", t=CH)
            t4 = tmp.next()
            t4v = t4.v.re("p h (c t) -> p (h c) t", t=CH)
            k.tt("pool", t4v, clv[:, :, CH - 1:CH].bro([64, 4 * NCH, CH]), clv, ALU.subtract)
            k.act(t4.v, t4.v, AF.Exp)
            k.tt("dve", A_kbar.v, A_kp.v, t4.v, ALU.mult)
            k.tt("pool", A_bbar.v, b_.v, t4.v, ALU.mult)
            k.act(gC.v.re("p h c -> p (h c)"), clv[:, :, CH - 1], AF.Exp)
            yield
            for ch in range(NCH):
                cs = CS[ch]
                specs = [(A_kapt, A_bt, mLs, 1.0), (A_bt, A_kapt, mUs, 1.0), (A_kt, A_kapt, mUs, 1.0),
                         (A_kt, A_rt, mUi, 1.0), (A_bt, A_rt, mUi, -1.0)]
                for gi, (La, Ra, msk, sgn) in enumerate(specs):
                    pb = C.bank()
                    for h in range(4):
                        k.mm(pb[0:64, h * 64:(h + 1) * 64], fm(La, h, ch), fm(Ra, h, ch))
                    if sgn == 1.0:
                        k.tt("dve", cs["G"][gi].v, v4(pb), msk.v, ALU.mult)
                    else:
                        k.stt(cs["G"][gi].v, v4(pb), sgn, msk.v, ALU.mult, ALU.mult)
                    yield
                k.tt("pool", cs["P"][0].v, mI.v, cs["G"][1].v, ALU.subtract)
                for ti, sgn in enumerate([1.0, 1.0, -1.0]):
                    pbb = C.bank().v.bc(BF16)
                    for h in range(4):
                        src = fm(Vb if ti == 0 else (A_kbar if ti == 1 else A_bbar), h, ch)
                        k.tr(pbb[0:64, h * 64:(h + 1) * 64], src, idb64)
                    pv_ = pbb[0:64, 0:256].re("p (h s) -> p h s", h=4)
                    if sgn == 1.0:
                        k.cp("act", cs["tok"][ti].v, pv_)
                    else:
                        k.act(cs["tok"][ti].v, pv_, AF.Copy, scale=-1.0)
                    yield
            curN = [CS[ch]["G"][0] for ch in range(NCH)]
            curM = [CS[ch]["G"][1] for ch in range(NCH)]
            curP = [CS[ch]["P"][0] for ch in range(NCH)]
            for st in range(5):
                lastst = st == 4
                for ch in range(NCH):
                    cs = CS[ch]
                    Nn, Mn, Pn = cs["N"][st % 2], cs["M"][st % 2], cs["P"][(st + 1) % 2]
                    pb = C.bank()
                    for h in range(4):
                        k.mm(pb[0:64, h * 64:(h + 1) * 64], curM[ch][:, h, :], curN[ch][:, h, :])
                    k.cp("act", Nn.v, v4(pb))
                    if not lastst:
                        pb2 = C.bank()
                        for h in range(4):
                            k.mm(pb2[0:64, h * 64:(h + 1) * 64], curN[ch][:, h, :], curM[ch][:, h, :])
                        k.cp("act", Mn.v, v4(pb2))
                    pb3 = C.bank()
                    for h in range(4):
                        k.mm(pb3[0:64, h * 64:(h + 1) * 64], Nn[:, h, :], curP[ch][:, h, :])
                    if lastst:
                        k.tt("dve", cs["Pb"].v, v4(pb3), curP[ch].v, ALU.add)
                        Pn = cs["Pb"]
                    else:
                        k.tt("dve", Pn.v, v4(pb3), curP[ch].v, ALU.add)
                    curN[ch], curM[ch], curP[ch] = Nn, Mn, Pn
                    yield
            B["curP"] = curP

        def gen_CD(rg, B):
            A_rt, A_kapt, A_g, A_bon, gC, CS = B["A_rt"], B["A_kapt"], B["A_g"], B["A_bon"], B["gC"], B["CS"]
            curP = B["curP"]
            tsl = slice(rg * RG, (rg + 1) * RG)
            for ch in range(NCH):
                cs = CS[ch]
                TT = curP[ch]
                Vt_, KB, BBn = cs["tok"]
                AkkT, ArkT, ArbTn = cs["G"][2], cs["G"][3], cs["G"][4]
                pw = C.bank()
                for h in range(4):
                    k.I("pe", "matmul", out=pw[0:64, h * 64:(h + 1) * 64], lhsT=fm(A_kapt, h, ch), rhs=STb[:, h, :],
                        start=True, stop=False, skip_group_check=True)
                    k.I("pe", "matmul", out=pw[0:64, h * 64:(h + 1) * 64], lhsT=AkkT[:, h, :], rhs=Vt_[:, h, :],
                        start=False, stop=True, skip_group_check=True)
                k.cp("act", Wt.v, v4(pw))
                yield
                pu = C.bank()
                for h in range(4):
                    k.mm(pu[0:64, h * 64:(h + 1) * 64], TT[:, h, :], Wt[:, h, :])
                k.cp("act", Ut.v, v4(pu))
                yield
                py = C.bank()
                pst = C.bank()
                for h in range(4):
                    yo = py[0:64, h * 64:(h + 1) * 64]
                    k.I("pe", "matmul", out=yo, lhsT=STb[:, h, :], rhs=fm(A_rt, h, ch), start=True, stop=False,
                        skip_group_check=True)
                    k.I("pe", "matmul", out=yo, lhsT=Vt_[:, h, :], rhs=ArkT[:, h, :], start=False, stop=False,
                        skip_group_check=True)
                    k.I("pe", "matmul", out=yo, lhsT=Ut[:, h, :], rhs=ArbTn[:, h, :], start=False, stop=True,
                        skip_group_check=True)
                    so = pst[0:64, h * 64:(h + 1) * 64]
                    k.I("pe", "matmul", out=so, lhsT=KB[:, h, :], rhs=Vt_[:, h, :], start=True, stop=False,
                        skip_group_check=True)
                    k.I("pe", "matmul", out=so, lhsT=BBn[:, h, :], rhs=Ut[:, h, :], start=False, stop=True,
                        skip_group_check=True)
                k.cp("act", A_y[:, :, ch * CH:(ch + 1) * CH], v4(py))
                for h in range(4):
                    k.stt(ST[:, h, :], ST[:, h, :], gC[:, h, ch:ch + 1], pst[0:64, h * 64:(h + 1) * 64], ALU.mult, ALU.add)
                k.cp("pool", STb.v, ST.v)
                yield
            yo_t = yout.next()
            yc = tmp2.next()
            k.cp("pool", A_yb.v, A_y.v)
            for hq in range(2):
                pm = C.bank()
                for hh in range(2):
                    k.mm(pm[0:64, hh * RG:(hh + 1) * RG], ones64, A_yb[:, hq * 2 + hh, :])
                k.stt(yc[:, hq * 2:hq * 2 + 2, :], pm[0:64, 0:2 * RG].re("p (h t) -> p h t", h=2), -1.0 / 64,
                      A_y[:, hq * 2:hq * 2 + 2, :], ALU.mult, ALU.add)
            sq = tmpb.next()
            k.tt("pool", sq.v, yc.v, yc.v, ALU.mult)
            yield
            rs = tmp2.next()
            for hq in range(2):
                pvv = C.bank()
                for hh in range(2):
                    k.mm(pvv[0:64, hh * RG:(hh + 1) * RG], ones64, sq[:, hq * 2 + hh, :])
                k.act(rs[:, hq * 2:hq * 2 + 2, :], pvv[0:64, 0:2 * RG].re("p (h t) -> p h t", h=2), AF.Ln, scale=1.0 / 64,
                      bias=eps_gn[0:64, 0:1])
            k.act(rs.v, rs.v, AF.Exp, scale=-0.5)
            k.tt("dve", yc.v, yc.v, rs.v, ALU.mult)
            yield
            for h in range(4):
                k.ts("dve", yc[:, h, :], yc[:, h, :], gng[0:64, h:h + 1], ALU.mult, gnb[0:64, h:h + 1], ALU.add)
            k.tt("pool", yc.v, yc.v, A_bon.v, ALU.add)
            k.tt("pool", yo_t.v, yc.v, A_g.v, ALU.mult)
            for h in range(4):
                k.dma(y_d[(h % 2) * 64:(h % 2) * 64 + 64, h // 2, tsl], yo_t[:, h, :], q="pool")
            yield

        for step in range(NRG + 1):
            gens = []
            if step < NRG:
                gens.append(gen_AB(step, SETS[step % 2]))
            if step >= 1:
                gens.append(gen_CD(step - 1, SETS[(step - 1) % 2]))
            interleave(gens)


SSD_OFF = 768 + 1024


def ssd_phase(k, C, S, xt_d, win_d, prm, y_d):
    TG = min(512, S)
    NTG = S // TG
    TPG = TG // 128
    with k.phase():
        Ui = k.sb([128, 128], F32, "sdUi")
        k.aselect(Ui.v, C.ones_f[:, 0:128], [[1, 128]], ALU.is_ge, 0.0, 0, -1)
        WL = WLoader(k, 8 * 512, nstage=2, nbuf=2, name="sdw")
        w_zx = WL.load(win_d[:, 0:512], 8, 512)
        w_bc = WL.load(win_d[:, 512:1024], 8, 512)
        st = k.sb([128, 8, 4], F32, "sdst")
        k.dma(st.v, win_d[:, 1024:1028].re("(c p) n -> p c n", p=128))
        w_dt = k.sb([128, 8, 4], BF16, "sdwdt")
        k.cp("pool", w_dt.v, st.v)
        cw = k.sb([128, 6, 4], F32, "sdcw")
        for n in range(6):
            for j in range(4):
                k.dma(cw[:, n, j:j + 1], prm["conv_w"][j, n * 128:(n + 1) * 128].re("(p o) -> p o", o=1))
        cb = load_cols(k, prm["conv_b"], 6, "sdcb")
        dtb = load_bcast(k, prm["dt_bias"], 4, "sddtb")
        alog = load_bcast(k, prm["a_log"], 4, "sdalog")
        dsk = load_bcast(k, prm["d"], 4, "sddsk")
        ng = load_bcast(k, prm["norm_g"], 256, "sdng")
        aneg = k.sb([128, 4], F32, "sdaneg")
        k.act(aneg.v, alog.v, AF.Exp)
        k.ts("dve", aneg.v, aneg.v, -1.0, ALU.mult)
        eps5g = C.eps5
        Sst = k.sb([128, 4, 64], F32, "sdS")
        k.memset("pool", Sst.v, 0.0)
        Sbf = k.sb([128, 4, 64], BF16, "sdSb")
        k.memset("pool", Sbf.v, 0.0)
        xraw = k.sb([128, 6, TG + 3], F32, "sdxr")
        k.memset("pool", xraw[:, :, 0:3], 0.0)
        xc = k.sb([128, 6, TG], F32, "sdxc")
        bcb = k.sb([128, 4, TG], BF16, "sdbcb")
        r_xt = Ring(k, 2, [128, 8, TG], BF16, "sdxt")
        r_acc = Ring(k, 2, [128, TG], F32, "sdacc")
        r_s = Ring(k, 4, [128, 48], F32, "sds")
        r_m = Ring(k, 8, [128, 128], F32, "sdm")
        r_mb = Ring(k, 20, [128, 128], BF16, "sdmb")
        r_tok = Ring(k, 3, [128, 512], F32, "sdtok")
        r_tb = Ring(k, 3, [128, 768], BF16, "sdtb")
        r_y = Ring(k, 3, [128, 256], F32, "sdy")
        r_z = Ring(k, 3, [128, 256], F32, "sdz")
        r_gt = Ring(k, 3, [128, 256], F32, "sdgt")
        r_yb = Ring(k, 3, [128, 256], BF16, "sdyb")
        r_yo = Ring(k, 2, [128, 2, TG], BF16, "sdyo")
        for tg in range(NTG):
            tsl = slice(tg * TG, (tg + 1) * TG)
            XT = r_xt.next()
            k.dma(XT.v, xt_d[:, :, tsl])
            for n in range(6):
                pb = C.bank()
                wsl = w_zx[:, :, 256 + n * 128:256 + (n + 1) * 128] if n < 2 else w_bc[:, :, (n - 2) * 128:(n - 1) * 128]
                for kc in range(8):
                    k.mm(pb[:, 0:TG], wsl[:, kc, :], XT[:, kc, :], start=(kc == 0), stop=(kc == 7))
                k.cp("act", xraw[:, n, 3:TG + 3], pb[:, 0:TG])
                acc = r_acc.next()
                k.ts("dve", acc.v, xraw[:, n, 3:TG + 3], cw[:, n, 3:4], ALU.mult, cb[:, n:n + 1], ALU.add)
                for j in (2, 1, 0):
                    k.stt(acc.v, xraw[:, n, j:TG + j], cw[:, n, j:j + 1], acc.v, ALU.mult, ALU.add)
                k.act(xc[:, n, :], acc.v, AF.Silu)
                if n >= 2:
                    k.cp("pool", bcb[:, n - 2, :], xc[:, n, :])
            yo = r_yo.next()

            def tile(jj, slot, XT=XT, yo=yo):
                lsl = slice(jj * 128, (jj + 1) * 128)
                s = r_s.next()
                py = C.ps[4 + 2 * slot]
                pst = C.ps[5 + 2 * slot]
                pd = C.bank()
                for kc in range(8):
                    k.mm(pd[:, 0:4], XT[:, kc, lsl], w_dt[:, kc, :], start=(kc == 0), stop=(kc == 7))
                k.tt("dve", s[:, 0:4], pd[:, 0:4], dtb.v, ALU.add)
                k.act(s[:, 0:4], s[:, 0:4], AF.Exp)
                k.act(s[:, 4:8], s[:, 0:4], AF.Ln, bias=C.one[:, 0:1])
                k.tt("dve", s[:, 8:12], s[:, 4:8], aneg.v, ALU.mult)
                pc = C.bank()
                k.mm(pc[:, 0:4], Ui.v, s[:, 8:12])
                k.mm(pc[:, 8:12], C.ones_f[:, 0:128], s[:, 8:12])
                k.cp("dve", s[:, 12:16], pc[:, 0:4])
                k.cp("dve", s[:, 16:20], pc[:, 8:12])
                yield
                k.tt("dve", s[:, 20:24], s[:, 16:20], s[:, 12:16], ALU.subtract)
                k.act(s[:, 20:24], s[:, 20:24], AF.Exp)
                k.act(s[:, 24:28], s[:, 16:20], AF.Exp)
                pz = C.bank()
                for kc in range(8):
                    k.mm(pz[:, 0:256], XT[:, kc, lsl], w_zx[:, kc, 0:256], start=(kc == 0), stop=(kc == 7))
                zs = r_z.next()
                k.act(zs.v, pz[:, 0:256], AF.Silu)
                yield
                pt = C.bank()
                for n in range(4):
                    k.tr(pt[:, n * 128:(n + 1) * 128], xc[:, n, lsl], C.id_f.v)
                tok = r_tok.next()
                k.cp("act", tok.v, pt.v)
                yield
                tb = r_tb.next()
                for h in range(4):
                    k.ts("dve", tb[:, h * 64:(h + 1) * 64], tok[:, h * 64:(h + 1) * 64], s[:, 4 + h:5 + h], ALU.mult)
                    k.stt(tb[:, 512 + h * 64:512 + (h + 1) * 64], tok[:, h * 64:(h + 1) * 64], s[:, 4 + h:5 + h],
                          s[:, 20 + h:21 + h].bro([128, 64]), ALU.mult, ALU.mult)
                k.cp("pool", tb[:, 256:512], tok[:, 256:512])
                pg = C.bank()
                for g in range(2):
                    k.mm(pg[:, g * 128:(g + 1) * 128], bcb[:, g, lsl], bcb[:, 2 + g, lsl])
                gts = r_gt.next()
                k.cp("act", gts.v, pg[:, 0:256])
                yield
                csts = []
                for h in range(4):
                    g = h // 2
                    abc = r_m.next()
                    k.ts("dve", abc.v, C.ones_f[:, 0:128], s[:, 8 + h:9 + h], ALU.mult)
                    pr = C.bank()
                    k.mm(pr[:, 0:128], abc.v, Ui.v)
                    dm = r_m.next()
                    k.ts("dve", dm.v, pr[:, 0:128], s[:, 12 + h:13 + h], ALU.subtract, 0.0, ALU.min)
                    k.act(dm.v, dm.v, AF.Exp)
                    k.tt("dve", dm.v, dm.v, gts[:, g * 128:(g + 1) * 128], ALU.mult)
                    mh = r_mb.next()
                    k.tt("pool", mh.v, dm.v, Ui.v, ALU.mult)
                    er = r_m.next()
                    k.act(er.v, pr[:, 0:128], AF.Exp)
                    cst = r_mb.next()
                    k.tt("pool", cst.v, xc[:, 4 + g, lsl], er.v, ALU.mult)
                    csts.append(cst)
                    k.I("pe", "matmul", out=py[:, h * 64:(h + 1) * 64], lhsT=mh.v, rhs=tb[:, h * 64:(h + 1) * 64],
                        start=(h == 0), stop=False, skip_group_check=True)
                    k.mm(pst[:, h * 64:(h + 1) * 64], tb[:, 256 + g * 128:256 + (g + 1) * 128],
                         tb[:, 512 + h * 64:512 + (h + 1) * 64])
                    yield
                for h in range(4):
                    k.I("pe", "matmul", out=py[:, h * 64:(h + 1) * 64], lhsT=csts[h].v, rhs=Sbf[:, h, :],
                        start=False, stop=True, skip_group_check=True)
                for h in range(4):
                    k.stt(Sst[:, h, :], Sst[:, h, :], s[:, 24 + h:25 + h], pst[:, h * 64:(h + 1) * 64], ALU.mult, ALU.add)
                k.cp("pool", Sbf.v, Sst.v)
                y = r_y.next()
                for h in range(4):
                    k.stt(y[:, h * 64:(h + 1) * 64], tok[:, h * 64:(h + 1) * 64], dsk[:, h:h + 1], py[:, h * 64:(h + 1) * 64],
                          ALU.mult, ALU.add)
                yield
                k.tt("dve", y.v, y.v, zs.v, ALU.mult)
                for g in range(2):
                    k.act(zs[:, g * 128:(g + 1) * 128], y[:, g * 128:(g + 1) * 128], AF.Square, accum_out=s[:, 28 + g:29 + g])
                k.act(s[:, 30:32], s[:, 28:30], AF.Ln, scale=1.0 / 128, bias=eps5g[:, 0:1])
                k.act(s[:, 32:34], s[:, 30:32], AF.Exp, scale=-0.5)
                yb = r_yb.next()
                for g in range(2):
                    k.stt(yb[:, g * 128:(g + 1) * 128], y[:, g * 128:(g + 1) * 128], s[:, 32 + g:33 + g],
                          ng[:, g * 128:(g + 1) * 128], ALU.mult, ALU.mult)
                yield
                pv = C.bank().v.bc(BF16)
                for c in range(2):
                    k.tr(pv[:, c * 128:(c + 1) * 128], yb[:, c * 128:(c + 1) * 128], C.id_b.v)
                k.cp("act", yo[:, :, lsl], pv[:, 0:256].re("p (c t) -> p c t", c=2))
                yield

            def stream(par):
                if par == 1:
                    yield
                for jj in range(par, TPG, 2):
                    yield from tile(jj, par)

            C.pool = [0, 1, 2, 3]
            interleave([stream(0), stream(1)])
            C.pool = list(range(8))
            k.cp("pool", xraw[:, :, 0:3], xraw[:, :, TG:TG + 3])
            k.dma(y_d[:, :, tsl], yo.v, q="pool")


IN_SPECS = [
    ("mix_w_in", [2, 1024, 3236]), ("rw_mu", [2, 1024]), ("rw_w0", [2, 256]), ("rw_w_up", [2, 64, 256]),
    ("rw_a0", [2, 256]), ("rw_a_up", [2, 64, 256]), ("rw_g_up", [2, 128, 256]), ("rw_k_k", [2, 256]),
    ("rw_k_a", [2, 256]), ("rw_r_k", [2, 4, 64]), ("rw_gn_g", [2, 256]), ("rw_gn_b", [2, 256]),
    ("ssd_conv_w", [2, 4, 768]), ("ssd_conv_b", [2, 768]), ("ssd_dt_bias", [2, 4]), ("ssd_a_log", [2, 4]),
    ("ssd_d", [2, 4]), ("ssd_norm_g", [2, 256]), ("mla_q_norm_g", [2, 256]), ("mla_w_uq", [2, 256, 384]),
    ("mla_kv_norm_g", [2, 128]), ("mla_w_ukv", [2, 128, 512]), ("mix_w_br_out", [2, 4, 256, 1024]),
    ("mix_w_gate", [2, 4, 1024, 1024]), ("mix_w_out", [2, 1024, 1024]), ("ln1_g", [2, 1024]), ("ln1_b", [2, 1024]),
    ("xa_w_q", [2, 1024, 1024]), ("xa_w_kv", [2, 1024, 2048]), ("xa_w_o", [2, 1024, 1024]), ("ln2_g", [2, 1024]),
    ("ln2_b", [2, 1024]), ("ffn_w13", [1, 1024, 5632]), ("ffn_w2", [1, 2816, 1024]), ("moe_router", [1, 1024, 8]),
    ("moe_w13", [1, 8, 1024, 7168]), ("moe_w2", [1, 8, 3584, 1024]), ("ln3_g", [2, 1024]), ("ln3_b", [2, 1024]),
]


def build_program(S, NB, L=2):
    nc = bass.Bass("TRN2", target_bir_lowering=False)
    k = K(nc)
    x = k.dram("x", [NB, S, 1024], F32, kind="ExternalInput")
    mem = k.dram("mem", [NB, 256, 1024], F32, kind="ExternalInput")
    pos = k.dram("positions", [NB, S], I32, kind="ExternalInput")
    W = {n: k.dram(n, shp, F32, kind="ExternalInput") for n, shp in IN_SPECS}
    out = k.dram("out", [NB, S, 1024], F32, kind="ExternalOutput")
    xt = k.dram("s_xt", [128, 8, S], BF16)
    Xa = k.dram("s_xa", [S, 1024], F32)
    Xb = k.dram("s_xb", [S, 1024], F32)
    Xc = k.dram("s_xc", [S, 1024], F32)
    U = k.dram("s_u", [S, 1024], F32)
    ybr = [k.dram("s_y%d" % i, [128, 2, S], BF16) for i in range(4)]
    mT = k.dram("s_mt", [128, 8, S], BF16)
    rope = k.dram("s_rope", [2, 128, S], F32)
    C = Consts(k)
    sb_consts(k, C, min(512, S))
    for b in range(NB):
        rope_phase(k, C, S, pos[b], rope)
        xt0_phase(k, C, S, x[b], xt)
        xcur = x[b]
        for l in range(L):
            win = W["mix_w_in"][l]
            sb_phase(k, C, S, xt, win[:, 0:768], ybr[0])
            rprm = dict(mu=W["rw_mu"][l], w0=W["rw_w0"][l], w_up=W["rw_w_up"][l], a0=W["rw_a0"][l], a_up=W["rw_a_up"][l],
                        g_up=W["rw_g_up"][l], k_k=W["rw_k_k"][l], k_a=W["rw_k_a"][l],
                        r_k=W["rw_r_k"][l].re("h n -> (h n)"), gn_g=W["rw_gn_g"][l], gn_b=W["rw_gn_b"][l])
            rwkv_phase(k, C, S, xt, win[:, RW_OFF:RW_OFF + 1024], rprm, ybr[1])
            sprm = dict(conv_w=W["ssd_conv_w"][l], conv_b=W["ssd_conv_b"][l], dt_bias=W["ssd_dt_bias"][l],
                        a_log=W["ssd_a_log"][l], d=W["ssd_d"][l], norm_g=W["ssd_norm_g"][l])
            ssd_phase(k, C, S, xt, win[:, SSD_OFF:SSD_OFF + 1028], sprm, ybr[2])
            mla_phase(k, C, S, xt, win[:, MLA_OFF:MLA_OFF + 416], W["mla_q_norm_g"][l], W["mla_w_uq"][l],
                      W["mla_kv_norm_g"][l], W["mla_w_ukv"][l], rope, ybr[3])
            gate_phase(k, C, S, xt, ybr, W["mix_w_gate"][l], W["mix_w_br_out"][l], mT)
            outproj_phase(k, C, S, mT, 8, W["mix_w_out"][l], U)
            ln_phase(k, C, S, U, xcur, W["ln1_g"][l], W["ln1_b"][l], Xa, xt)
            xattn_phase(k, C, S, xt, mem[b], W["xa_w_q"][l], W["xa_w_kv"][l], W["xa_w_o"][l], U)
            ln_phase(k, C, S, U, Xa.v, W["ln2_g"][l], W["ln2_b"][l], Xb, xt)
            if l % 2 == 0:
                ffn_phase(k, C, S, xt, Xb.v, U, [(W["ffn_w13"][l // 2], W["ffn_w2"][l // 2], 2816)])
            else:
                ex = [(W["moe_w13"][l // 2, e], W["moe_w2"][l // 2, e], 3584) for e in range(8)]
                ffn_phase(k, C, S, xt, Xb.v, U, ex, router_d=W["moe_router"][l // 2])
            last = l == L - 1
            ln_phase(k, C, S, U, Xb.v, W["ln3_g"][l], W["ln3_b"][l], out[b] if last else Xc, xt)
            xcur = Xc.v
    k.barrier()
    return nc, k


def kernel(**inputs):
    B, S = inputs["x"].shape[0], inputs["x"].shape[1]
    NB = B // NCORES
    nc, _ = build_program(S, NB)
    shared = {n: np.ascontiguousarray(inputs[n], dtype=np.float32) for n, _ in IN_SPECS}
    in_maps = []
    for c in range(NCORES):
        sl = slice(c * NB, (c + 1) * NB)
        m = dict(shared)
        m["x"] = np.ascontiguousarray(inputs["x"][sl], dtype=np.float32)
        m["mem"] = np.ascontiguousarray(inputs["mem"][sl], dtype=np.float32)
        m["positions"] = np.ascontiguousarray(inputs["positions"][sl], dtype=np.int32)
        in_maps.append(m)
    res = run_bass_kernel_spmd(nc, in_maps, core_ids=list(range(NCORES)))
    return np.concatenate([np.asarray(r["out"]) for r in res.results], axis=0).astype(np.float32)
```

```python
import math
from contextlib import ExitStack
import numpy as np
import concourse.bass as bass
import concourse.mybir as mybir
from concourse.bass_utils import run_bass_kernel_spmd

F32 = mybir.dt.float32
BF16 = mybir.dt.bfloat16
I32 = mybir.dt.int32
AF = mybir.ActivationFunctionType
ALU = mybir.AluOpType
AX = mybir.AxisListType

D = 1024
NCORES = 8
WRITE_KW = ("out", "accum_out")


class T:
    def __init__(self, ap, name):
        self.ap = ap
        self.name = name
        self.w = None
        self.r = {}

    def __getitem__(self, idx):
        return V(self, self.ap[idx])

    @property
    def v(self):
        return V(self, self.ap)


class V:
    def __init__(self, t, ap):
        self.t = t
        self.ap = ap

    def __getitem__(self, idx):
        return V(self.t, self.ap[idx])

    def bc(self, dt):
        return V(self.t, self.ap.bitcast(dt))

    def re(self, pat, **kw):
        return V(self.t, self.ap.rearrange(pat, **kw))

    def bro(self, shape):
        return V(self.t, self.ap.to_broadcast(shape))

    def pbro(self, n):
        return V(self.t, self.ap.partition_broadcast(n))


class K:
    COMPUTE = ("pe", "act", "dve", "pool")

    def __init__(self, nc):
        self.nc = nc
        self.es = ExitStack()
        self.engs = {"pe": nc.tensor, "act": nc.scalar, "dve": nc.vector, "pool": nc.gpsimd, "sp": nc.sync}
        self.sems = {}
        self.cnt = {}
        for e in self.COMPUTE:
            self.sems[e] = self.es.enter_context(nc.semaphore("s_" + e))
            self.cnt[e] = 0
        self.dq = {}
        for q in ("sp", "pool", "act"):
            lst = []
            for i in range(6):
                key = "d_%s%d" % (q, i)
                self.sems[key] = self.es.enter_context(nc.semaphore(key))
                self.cnt[key] = 0
                lst.append(key)
            self.dq[q] = [lst, 0]
        self.seen = {e: {} for e in self.engs}
        self.n_ins = 0
        self.phase_es = None
        self.uid = 0

    def sb(self, shape, dt, name=None):
        self.uid += 1
        name = "%s_%d" % (name or "t", self.uid)
        h = (self.phase_es or self.es).enter_context(self.nc.sbuf_tensor(name, list(shape), dt))
        return T(h[:], name)

    def sbg(self, shape, dt, name=None):
        self.uid += 1
        name = "%s_%d" % (name or "g", self.uid)
        h = self.es.enter_context(self.nc.sbuf_tensor(name, list(shape), dt))
        return T(h[:], name)

    def psum(self, name):
        h = self.es.enter_context(self.nc.psum_tensor(name, [128, 512], F32))
        return T(h[:], name)

    def dram(self, name, shape, dt, kind="Internal"):
        h = self.nc.dram_tensor(name, list(shape), dt, kind=kind)
        return T(h.ap(), name)

    def phase(self):
        k = self

        class _P:
            def __enter__(s):
                k.barrier()
                k.phase_es = ExitStack()
                return s

            def __exit__(s, *a):
                k.barrier()
                k.phase_es.close()
                k.phase_es = None
                return False

        return _P()

    def _wait(self, eng, key, val):
        if self.seen[eng].get(key, 0) >= val:
            return
        self.engs[eng].wait_ge(self.sems[key], val)
        self.seen[eng][key] = val
        self.n_ins += 1

    def _sync(self, eng, reads, writes):
        deps = {}

        def add(d, war=False):
            if d is None:
                return
            key, val = d
            if key == eng and eng == "pe":
                return
            if deps.get(key, 0) < val:
                deps[key] = val

        for t in reads:
            add(t.w)
        for t in writes:
            add(t.w)
            for key, val in t.r.items():
                add((key, val), war=True)
        for key, val in deps.items():
            self._wait(eng, key, val)

    def _done(self, me, reads, writes):
        key, val = me
        for t in reads:
            if t.r.get(key, 0) < val:
                t.r[key] = val
        for t in writes:
            t.w = me
            t.r = {}

    def barrier(self):
        for eng in self.engs:
            for key in self.sems:
                if key == eng:
                    continue
                self._wait(eng, key, self.cnt[key])

    def _split(self, kw):
        reads, writes, args = [], [], {}
        for name, v in kw.items():
            if isinstance(v, V):
                (writes if name in WRITE_KW else reads).append(v.t)
                args[name] = v.ap
            else:
                args[name] = v
        return reads, writes, args

    def I(self, eng, meth, **kw):
        reads, writes, args = self._split(kw)
        self._sync(eng, reads, writes)
        ins = getattr(self.engs[eng], meth)(**args)
        self.cnt[eng] += 1
        ins.then_inc(self.sems[eng], 1)
        self.n_ins += 1
        self._done((eng, self.cnt[eng]), reads, writes)

    def dma(self, out, in_, q="sp", **kw):
        lst, i = self.dq[q]
        key = lst[i % len(lst)]
        self.dq[q][1] = i + 1
        self._wait(q, key, self.cnt[key])
        self._sync(q, [in_.t], [out.t])
        ins = self.engs[q].dma_start(out=out.ap, in_=in_.ap, **kw)
        self.cnt[key] += 16
        ins.then_inc(self.sems[key], 16)
        self.n_ins += 1
        self._done((key, self.cnt[key]), [in_.t], [out.t])

    def mm(self, out, lhsT, rhs, start=True, stop=True):
        self.I("pe", "matmul", out=out, lhsT=lhsT, rhs=rhs, start=start, stop=stop)

    def tr(self, out, in_, ident):
        self.I("pe", "transpose", out=out, in_=in_, identity=ident)

    def act(self, out, in_, func, bias=0.0, scale=1.0, **kw):
        self.I("act", "activation", out=out, in_=in_, func=func, bias=bias, scale=scale, **kw)

    def tt(self, eng, out, in0, in1, op):
        self.I(eng, "tensor_tensor", out=out, in0=in0, in1=in1, op=op)

    def ts(self, eng, out, in0, s1, op0, s2=None, op1=ALU.bypass, **kw):
        self.I(eng, "tensor_scalar", out=out, in0=in0, scalar1=s1, scalar2=s2, op0=op0, op1=op1, **kw)

    def stt(self, out, in0, scalar, in1, op0, op1):
        self.I("dve", "scalar_tensor_tensor", out=out, in0=in0, scalar=scalar, in1=in1, op0=op0, op1=op1)

    def cp(self, eng, out, in_):
        if eng == "act":
            self.I("act", "copy", out=out, in_=in_)
        else:
            self.I(eng, "tensor_copy", out=out, in_=in_)

    def memset(self, eng, out, val):
        reads, writes = [], [out.t]
        self._sync(eng, reads, writes)
        ins = self.engs[eng].memset(out.ap, val)
        self.cnt[eng] += 1
        ins.then_inc(self.sems[eng], 1)
        self._done((eng, self.cnt[eng]), reads, writes)

    def aselect(self, out, in_, pattern, cmp, fill, base, cm):
        self.I("pool", "affine_select", out=out, in_=in_, pattern=pattern, compare_op=cmp, fill=fill,
               base=base, channel_multiplier=cm)


class Consts:
    def __init__(self, k):
        self.k = k
        ones = k.sbg([128, 512], F32, "onesf")
        k.memset("pool", ones.v, 1.0)
        self.ones_f = ones
        idf = k.sbg([128, 128], F32, "idf")
        k.aselect(idf.v, ones[:, 0:128], [[1, 128]], ALU.is_equal, 0.0, 0, -1)
        self.id_f = idf
        idb = k.sbg([128, 128], BF16, "idb")
        k.cp("pool", idb.v, idf.v)
        self.id_b = idb
        onesb = k.sbg([128, 128], BF16, "onesb")
        k.cp("pool", onesb.v, ones[:, 0:128])
        self.ones_b = onesb
        self.eps5 = k.sbg([128, 1], F32, "eps5")
        k.memset("pool", self.eps5.v, 1e-5)
        self.eps6 = k.sbg([128, 1], F32, "eps6")
        k.memset("pool", self.eps6.v, 1e-6)
        self.ps = [k.psum("ps%d" % i) for i in range(8)]
        self.psi = 0
        self.pool = list(range(8))

    def bank(self):
        b = self.ps[self.pool[self.psi % len(self.pool)]]
        self.psi += 1
        return b

    def mask(self, shape_f, base, cm, cmp=ALU.is_ge, dt=F32, name="mask", local=False):
        k = self.k
        m = (k.sb if local else k.sbg)([128, shape_f], F32, name)
        k.aselect(m.v, self.ones_f[:, 0:shape_f], [[1, shape_f]], cmp, 0.0, base, cm)
        if dt == F32:
            return m
        mb = k.sbg([128, shape_f], dt, name + "b")
        k.cp("pool", mb.v, m.v)
        return mb


class WLoader:
    def __init__(self, k, max_elems, nstage=2, nbuf=3, ceng="pool", name="w"):
        self.k = k
        self.st = [k.sb([128, max_elems], F32, name + "st") for _ in range(nstage)]
        self.bf = [k.sb([128, max_elems], BF16, name + "bf") for _ in range(nbuf)]
        self.i = 0
        self.j = 0
        self.ceng = ceng

    def load(self, wv, kc, n, p=128):
        k = self.k
        st = self.st[self.i % len(self.st)]
        self.i += 1
        bf = self.bf[self.j % len(self.bf)]
        self.j += 1
        sv = st[0:p, 0:kc * n].re("p (c n) -> p c n", c=kc)
        k.dma(sv, wv.re("(c p) n -> p c n", p=p))
        bv = bf[0:p, 0:kc * n].re("p (c n) -> p c n", c=kc)
        k.cp(self.ceng, bv, sv)
        return bv


def load_bcast(k, dv, n, name="bc"):
    t = k.sb([128, n], F32, name)
    k.dma(t.v, dv.pbro(128))
    return t


DN_ALPHA = (2.0 * 2) ** 0.25


def interleave(gens):
    gens = list(gens)
    while gens:
        for g in list(gens):
            try:
                next(g)
            except StopIteration:
                gens.remove(g)


class Ring:
    def __init__(self, k, n, shape, dt, name="r"):
        self.ts = [k.sb(shape, dt, name) for _ in range(n)]
        self.i = 0

    def next(self):
        t = self.ts[self.i % len(self.ts)]
        self.i += 1
        return t


def ln_phase(k, C, S, u_d, xres_d, g_d, b_d, xout_d, xt_d, eps=1e-5):
    NT = S // 128
    with k.phase():
        g_bc = load_bcast(k, g_d, 1024, "lng")
        b_bc = load_bcast(k, b_d, 1024, "lnb")
        r_x = Ring(k, 4, [128, 1024], F32, "lnx")
        r_u = Ring(k, 4, [128, 1024], F32, "lnu")
        r_o = Ring(k, 3, [128, 1024], F32, "lno")
        r_b = Ring(k, 3, [128, 1024], BF16, "lnbf")
        r_s = Ring(k, 4, [128, 16], F32, "lns")
        GT = min(4, NT)
        xtgs = [k.sb([128, 8, GT * 128], BF16, "lnxt") for _ in range(2)]
        done = {}

        def tile(j):
            rows = slice(j * 128, (j + 1) * 128)
            xr = r_x.next()
            k.dma(xr.v, xres_d[rows, :])
            u = r_u.next()
            k.dma(u.v, u_d[rows, :])
            yield
            k.stt(u.v, xr.v, DN_ALPHA, u.v, ALU.mult, ALU.add)
            st = r_s.next()
            for h in range(2):
                k.I("dve", "bn_stats", out=st[:, h * 6:(h + 1) * 6], in_=u[:, h * 512:(h + 1) * 512])
            k.I("dve", "bn_aggr", out=st[:, 12:14], in_=st[:, 0:12])
            k.act(st[:, 14:15], st[:, 13:14], AF.Ln, bias=C.eps5[:, 0:1])
            k.act(st[:, 15:16], st[:, 14:15], AF.Exp, scale=-0.5)
            yield
            k.ts("dve", u.v, u.v, st[:, 12:13], ALU.subtract, st[:, 15:16], ALU.mult)
            xo = r_o.next()
            k.tt("pool", xo.v, u.v, g_bc.v, ALU.mult)
            k.tt("dve", xo.v, xo.v, b_bc.v, ALU.add)
            k.dma(xout_d[rows, :], xo.v, q="sp")
            xb = r_b.next()
            k.cp("act", xb.v, xo.v)
            yield
            g = j // GT
            xtg = xtgs[g % 2]
            pv = C.bank().v.bc(BF16)
            for c in range(8):
                k.tr(pv[:, c * 128:(c + 1) * 128], xb[:, c * 128:(c + 1) * 128], C.id_b.v)
            jj = j % GT
            k.cp("dve", xtg[:, :, jj * 128:(jj + 1) * 128], pv.re("p (c t) -> p c t", c=8))
            done[g] = done.get(g, 0) + 1
            if done[g] == GT:
                k.dma(xt_d[:, :, g * GT * 128:(g + 1) * GT * 128], xtg.v, q="sp")
            yield

        def stream(par):
            for j in range(par, NT, 2):
                yield from tile(j)

        interleave([stream(0), stream(1)])


def xt0_phase(k, C, S, x_d, xt_d):
    NT = S // 128
    with k.phase():
        r_x = Ring(k, 2, [128, 1024], F32, "x0")
        r_b = Ring(k, 2, [128, 1024], BF16, "x0b")
        GT = min(4, NT)
        r_g = Ring(k, 2, [128, 8, GT * 128], BF16, "x0t")
        for j in range(NT):
            rows = slice(j * 128, (j + 1) * 128)
            xr = r_x.next()
            k.dma(xr.v, x_d[rows, :])
            xb = r_b.next()
            k.cp("act", xb.v, xr.v)
            if j % GT == 0:
                xtg = r_g.next()
            pst = C.bank()
            pv = pst.v.bc(BF16)
            for c in range(8):
                k.tr(pv[:, c * 128:(c + 1) * 128], xb[:, c * 128:(c + 1) * 128], C.id_b.v)
            jj = j % GT
            k.cp("dve", xtg[:, :, jj * 128:(jj + 1) * 128], pv.re("p (c t) -> p c t", c=8))
            if jj == GT - 1:
                t0 = (j - jj) * 128
                k.dma(xt_d[:, :, t0:t0 + GT * 128], xtg.v, q="pool")


def ffn_phase(k, C, S, xt_d, x_d, u_d, experts, router_d=None):
    NT = S // 128
    TG = min(512, S)
    NTG = S // TG
    TPG = TG // 128
    with k.phase():
        XT = k.sb([128, 8, S], BF16, "ffxt")
        k.dma(XT.v, xt_d.v)
        acc = k.sb([128, NT, 1024], F32, "ffacc")
        G = None
        if router_d is not None:
            G = k.sb([128, NT, 8], F32, "ffG")
            rt = k.sb([128, 8, 8], F32, "ffrt")
            k.dma(rt.v, router_d.re("(c p) e -> p c e", p=128))
            r_x = Ring(k, 1, [128, 1024], F32, "ffx")
            r_t = Ring(k, 1, [128, 1024], F32, "ffxT")
            r_s = Ring(k, 2, [128, 48], F32, "ffs")
            for j in range(NT):
                xr = r_x.next()
                k.dma(xr.v, x_d[j * 128:(j + 1) * 128, :])
                xT = r_t.next()
                for hh in range(2):
                    pb = C.bank()
                    for c in range(4):
                        cc = hh * 4 + c
                        k.tr(pb[:, c * 128:(c + 1) * 128], xr[:, cc * 128:(cc + 1) * 128], C.id_f.v)
                    k.cp("act", xT[:, hh * 512:(hh + 1) * 512], pb.v)
                pl = C.bank()
                for c in range(8):
                    k.mm(pl[:, 0:8], xT[:, c * 128:(c + 1) * 128], rt[:, c, :], start=(c == 0), stop=(c == 7))
                s = r_s.next()
                lg = s[:, 0:8]
                k.cp("dve", lg, pl[:, 0:8])
                k.I("dve", "reduce_max", out=s[:, 8:9], in_=lg, axis=AX.X)
                k.ts("dve", s[:, 16:24], lg, s[:, 8:9], ALU.is_equal)
                k.stt(s[:, 24:32], s[:, 16:24], -1e30, lg, ALU.mult, ALU.add)
                k.I("dve", "reduce_max", out=s[:, 9:10], in_=s[:, 24:32], axis=AX.X)
                k.ts("dve", s[:, 16:24], lg, s[:, 9:10], ALU.is_ge)
                k.ts("dve", s[:, 10:11], s[:, 8:9], -1.0, ALU.mult)
                k.act(s[:, 32:40], lg, AF.Exp, bias=s[:, 10:11])
                k.tt("dve", s[:, 32:40], s[:, 32:40], s[:, 16:24], ALU.mult)
                k.I("dve", "reduce_sum", out=s[:, 11:12], in_=s[:, 32:40], axis=AX.X)
                k.I("dve", "reciprocal", out=s[:, 12:13], in_=s[:, 11:12])
                k.ts("dve", G[:, j, :], s[:, 32:40], s[:, 12:13], ALU.mult)
        WL = WLoader(k, 8 * 512, nstage=2, nbuf=4, name="ffw")
        r_h = Ring(k, 2, [128, 4, TG], BF16, "ffh")
        r_sg = Ring(k, 2, [128, TG], F32, "ffsg")
        first = True
        for e, (w13, w2, H) in enumerate(experts):
            h0 = 0
            while h0 < H:
                hw = min(512, H - h0)
                nc_ = hw // 128
                w1 = WL.load(w13[:, h0:h0 + hw], 8, hw)
                w3 = WL.load(w13[:, H + h0:H + h0 + hw], 8, hw)
                w2s = WL.load(w2[h0:h0 + hw, :], nc_, 1024)
                for tg in range(NTG):
                    tsl = slice(tg * TG, (tg + 1) * TG)
                    hT = r_h.next()
                    for c in range(nc_):
                        gp = C.bank()
                        for kc in range(8):
                            k.mm(gp[:, 0:TG], w1[:, kc, c * 128:(c + 1) * 128], XT[:, kc, tsl], start=(kc == 0), stop=(kc == 7))
                        up = C.bank()
                        for kc in range(8):
                            k.mm(up[:, 0:TG], w3[:, kc, c * 128:(c + 1) * 128], XT[:, kc, tsl], start=(kc == 0), stop=(kc == 7))
                        sg = r_sg.next()
                        k.act(sg.v, gp[:, 0:TG], AF.Silu)
                        k.tt("dve", hT[:, c, :], up[:, 0:TG], sg.v, ALU.mult)
                    for j in range(TPG):
                        tix = tg * TPG + j
                        for hh in range(2):
                            yp = C.bank()
                            for c in range(nc_):
                                k.mm(yp.v, hT[:, c, j * 128:(j + 1) * 128], w2s[:, c, hh * 512:(hh + 1) * 512],
                                     start=(c == 0), stop=(c == nc_ - 1))
                            av = acc[:, tix, hh * 512:(hh + 1) * 512]
                            if G is None:
                                if first:
                                    k.cp("act", av, yp.v)
                                else:
                                    k.tt("dve", av, yp.v, av, ALU.add)
                            else:
                                gs = G[:, tix, e:e + 1]
                                if first:
                                    k.ts("dve", av, yp.v, gs, ALU.mult)
                                else:
                                    k.stt(av, yp.v, gs, av, ALU.mult, ALU.add)
                first = False
                h0 += hw
        for j in range(NT):
            k.dma(u_d[j * 128:(j + 1) * 128, :], acc[:, j, :], q="pool")


def outproj_phase(k, C, S, ht_d, KC, w_d, u_d, p=128):
    NT = S // 128
    with k.phase():
        HT = k.sb([128, KC, S], BF16, "opH")
        k.dma(HT[0:p], ht_d.v)
        WL = WLoader(k, KC * 512, nstage=2, nbuf=2, name="opw")
        wh = [WL.load(w_d[:, hh * 512:(hh + 1) * 512], KC, 512, p=p) for hh in range(2)]
        r_u = Ring(k, 3, [128, 1024], F32, "opu")
        for j in range(NT):
            u = r_u.next()
            for hh in range(2):
                yp = C.bank()
                for c in range(KC):
                    k.mm(yp.v, HT[0:p, c, j * 128:(j + 1) * 128], wh[hh][:, c, :], start=(c == 0), stop=(c == KC - 1))
                k.cp("act" if hh == 0 else "dve", u[:, hh * 512:(hh + 1) * 512], yp.v)
            k.dma(u_d[j * 128:(j + 1) * 128, :], u.v, q="pool")


def to_fm_bf16(k, C, src_d, n_tok, dstT, r_x, r_b):
    for j in range(n_tok // 128):
        xr = r_x.next()
        k.dma(xr.v, src_d[j * 128:(j + 1) * 128, :])
        xb = r_b.next()
        k.cp("act", xb.v, xr.v)
        pv = C.bank().v.bc(BF16)
        for c in range(8):
            k.tr(pv[:, c * 128:(c + 1) * 128], xb[:, c * 128:(c + 1) * 128], C.id_b.v)
        k.cp("dve", dstT[:, :, j * 128:(j + 1) * 128], pv.re("p (c t) -> p c t", c=8))


def xattn_phase(k, C, S, xt_d, mem_d, wq_d, wkv_d, wo_d, u_d):
    TG = min(512, S)
    NTG = S // TG
    TPG = TG // 128
    M = 256
    with k.phase():
        r_xt = Ring(k, 2, [128, 8, TG], BF16, "xaxt")
        memT = k.sb([128, 8, M], BF16, "xamT")
        r_x = Ring(k, 2, [128, 1024], F32, "xamx")
        r_b = Ring(k, 2, [128, 1024], BF16, "xamb")
        to_fm_bf16(k, C, mem_d, M, memT.v, r_x, r_b)
        WL = WLoader(k, 8 * 512, nstage=2, nbuf=6, name="xaw")
        KT = k.sb([128, 8, M], BF16, "xaKT")
        Vm = k.sb([128, 2, 1024], BF16, "xaV")
        for sl in range(4):
            w = WL.load(wkv_d[:, sl * 512:(sl + 1) * 512], 8, 512)
            if sl < 2:
                for c in range(4):
                    pb = C.bank()
                    for kc in range(8):
                        k.mm(pb[:, 0:M], w[:, kc, c * 128:(c + 1) * 128], memT[:, kc, :], start=(kc == 0), stop=(kc == 7))
                    k.cp("act", KT[:, sl * 4 + c, :], pb[:, 0:M])
            else:
                for mt in range(2):
                    pb = C.bank()
                    for kc in range(8):
                        k.mm(pb.v, memT[:, kc, mt * 128:(mt + 1) * 128], w[:, kc, :], start=(kc == 0), stop=(kc == 7))
                    k.cp("act", Vm[:, mt, (sl - 2) * 512:(sl - 1) * 512], pb.v)
        wq = [WL.load(wq_d[:, hh * 512:(hh + 1) * 512], 8, 512) for hh in range(2)]
        wo = [WL.load(wo_d[:, hh * 512:(hh + 1) * 512], 8, 512) for hh in range(2)]
        r_q = Ring(k, 2, [128, 8, TG], BF16, "xaq")
        r_o = Ring(k, 2, [128, 8, TG], BF16, "xao")
        r_p = Ring(k, 4, [128, TG], BF16, "xap")
        r_d = Ring(k, 2, [128, TG], F32, "xad")
        r_u = Ring(k, 2, [128, 1024], F32, "xau")
        for tg in range(NTG):
            tsl = slice(tg * TG, (tg + 1) * TG)
            XT = r_xt.next()
            k.dma(XT.v, xt_d[:, :, tsl])
            qT = r_q.next()
            for n in range(8):
                pb = C.bank()
                for kc in range(8):
                    k.mm(pb[:, 0:TG], wq[n // 4][:, kc, (n % 4) * 128:(n % 4 + 1) * 128], XT[:, kc, :],
                         start=(kc == 0), stop=(kc == 7))
                k.cp("act" if n % 2 == 0 else "dve", qT[:, n, :], pb[:, 0:TG])
            oT = r_o.next()
            for h in range(4):
                P = []
                for mt in range(2):
                    zb = C.bank()
                    for c in range(2):
                        k.mm(zb[:, 0:TG], KT[:, 2 * h + c, mt * 128:(mt + 1) * 128], qT[:, 2 * h + c, :],
                             start=(c == 0), stop=(c == 1))
                    p = r_p.next()
                    k.act(p.v, zb[:, 0:TG], AF.Exp, scale=1.0 / 16.0)
                    P.append(p)
                db = C.bank()
                for mt in range(2):
                    k.mm(db[:, 0:TG], C.ones_b.v, P[mt].v, start=(mt == 0), stop=(mt == 1))
                rd = r_d.next()
                k.I("dve", "reciprocal", out=rd.v, in_=db[:, 0:TG])
                for c in range(2):
                    ob = C.bank()
                    for mt in range(2):
                        k.mm(ob[:, 0:TG], Vm[:, mt, (2 * h + c) * 128:(2 * h + c + 1) * 128], P[mt].v,
                             start=(mt == 0), stop=(mt == 1))
                    k.tt("dve", oT[:, 2 * h + c, :], ob[:, 0:TG], rd.v, ALU.mult)
            for j in range(TPG):
                u = r_u.next()
                for hh in range(2):
                    yp = C.bank()
                    for c in range(8):
                        k.mm(yp.v, oT[:, c, j * 128:(j + 1) * 128], wo[hh][:, c, :], start=(c == 0), stop=(c == 7))
                    k.cp("act" if hh == 0 else "dve", u[:, hh * 512:(hh + 1) * 512], yp.v)
                t0 = tg * TG + j * 128
                k.dma(u_d[t0:t0 + 128, :], u.v, q="pool")


def gate_phase(k, C, S, xt_d, ybr_d, wgate_d, wbr_d, mergedT_d):
    TG = min(512, S)
    NTG = S // TG
    with k.phase():
        XT = k.sb([128, 8, S], BF16, "gxt")
        k.dma(XT.v, xt_d.v)
        Y = []
        for i in range(4):
            y = k.sb([128, 2, S], BF16, "gy")
            k.dma(y.v, ybr_d[i].v)
            Y.append(y)
        WL = WLoader(k, 8 * 512, nstage=2, nbuf=4, name="gw")
        macc = k.sb([128, 4, S], F32, "gacc")
        r_s = Ring(k, 3, [128, TG], F32, "gsg")
        r_m = Ring(k, 2, [128, 4, TG], BF16, "gm")
        for sl in range(2):
            for i in range(4):
                wg = WL.load(wgate_d[i, :, sl * 512:(sl + 1) * 512], 8, 512)
                wb = WL.load(wbr_d[i, :, sl * 512:(sl + 1) * 512], 2, 512)
                for tg in range(NTG):
                    tsl = slice(tg * TG, (tg + 1) * TG)
                    for c in range(4):
                        gp = C.bank()
                        for kc in range(8):
                            k.mm(gp[:, 0:TG], wg[:, kc, c * 128:(c + 1) * 128], XT[:, kc, tsl], start=(kc == 0), stop=(kc == 7))
                        pp = C.bank()
                        for kc in range(2):
                            k.mm(pp[:, 0:TG], wb[:, kc, c * 128:(c + 1) * 128], Y[i][:, kc, tsl], start=(kc == 0), stop=(kc == 1))
                        sg = r_s.next()
                        k.act(sg.v, gp[:, 0:TG], AF.Sigmoid)
                        if i == 0:
                            k.tt("dve", macc[:, c, tsl], pp[:, 0:TG], sg.v, ALU.mult)
                        else:
                            k.tt("dve", sg.v, pp[:, 0:TG], sg.v, ALU.mult)
                            k.tt("pool", macc[:, c, tsl], macc[:, c, tsl], sg.v, ALU.add)
            for tg in range(NTG):
                tsl = slice(tg * TG, (tg + 1) * TG)
                mb = r_m.next()
                k.cp("act", mb.v, macc[:, :, tsl])
                k.dma(mergedT_d[:, sl * 4:(sl + 1) * 4, tsl], mb.v, q="pool")


def sb_consts(k, C, TG):
    if hasattr(C, "sbm"):
        return
    m = k.sbg([128, 128], F32, "triu")
    k.aselect(m.v, C.ones_f[:, 0:128], [[-1, 128]], ALU.is_gt, 0.0, 0, 1)
    C.tri_gt = k.sbg([128, 128], BF16, "triub")
    k.cp("pool", C.tri_gt.v, m.v)
    m2 = k.sbg([128, 128], F32, "tril")
    k.aselect(m2.v, C.ones_f[:, 0:128], [[1, 128]], ALU.is_ge, 0.0, 0, -1)
    C.tri_le = k.sbg([128, 128], BF16, "trilb")
    k.cp("pool", C.tri_le.v, m2.v)
    C.one = k.sbg([128, 1], F32, "one")
    k.memset("pool", C.one.v, 1.0)


def sb_phase(k, C, S, xt_d, win_d, y_d):
    TG = min(512, S)
    NTG = S // TG
    NT = S // 128
    sb_consts(k, C, TG)
    with k.phase():
        C.sbm = [C.mask(TG, -r * 128, -1, ALU.is_gt, F32, "sbm%d" % r, local=True) for r in range(TG // 128)]
        XT = k.sb([128, 8, S], BF16, "sbxt")
        k.dma(XT.v, xt_d.v)
        WL = WLoader(k, 8 * 512, nstage=2, nbuf=2, name="sbw")
        wqk = WL.load(win_d[:, 0:512], 8, 512)
        wv = WL.load(win_d[:, 512:768], 8, 256)
        qk = k.sb([128, 4, S], BF16, "sbqk")
        Vt = k.sb([128, NT, 256], BF16, "sbv")
        for tg in range(NTG):
            tsl = slice(tg * TG, (tg + 1) * TG)
            for n in range(4):
                pb = C.bank()
                for kc in range(8):
                    k.mm(pb[:, 0:TG], wqk[:, kc, n * 128:(n + 1) * 128], XT[:, kc, tsl], start=(kc == 0), stop=(kc == 7))
                k.cp("act", qk[:, n, tsl], pb[:, 0:TG])
        for j in range(NT):
            pb = C.bank()
            for kc in range(8):
                k.mm(pb[:, 0:256], XT[:, kc, j * 128:(j + 1) * 128], wv[:, kc, :], start=(kc == 0), stop=(kc == 7))
            k.cp("act", Vt[:, j, :], pb[:, 0:256])
        YT = k.sb([128, 2, S], BF16, "sby")
        r_e = Ring(k, 6, [128, TG], F32, "sbe")
        r_sp = Ring(k, 2, [128, TG], F32, "sbsp")
        r_sm = Ring(k, 8, [128, TG], BF16, "sbsm")
        r_u = Ring(k, 6, [128, TG], F32, "sbu")
        r_w = Ring(k, 8, [128, TG], BF16, "sbw_")
        r_wf = Ring(k, 2, [128, TG], F32, "sbwf")
        C.pool = [0, 1]

        def unit(h, qg, slot):
            hp, bp = h // 2, (h % 2) * 64
            q0 = qg * TG
            qsl = slice(q0, q0 + TG)
            tailb = C.ps[4 + slot]
            yb = C.ps[2 + slot // 2]
            last = (q0 + TG) // 128 - 1
            for kt in range(last, -1, -1):
                first = kt == last
                zb = C.bank()
                k.mm(zb[:, 0:TG], qk[bp:bp + 64, 2 + hp, kt * 128:(kt + 1) * 128], qk[bp:bp + 64, hp, qsl])
                e = r_e.next()
                k.act(e.v, zb[:, 0:TG], AF.Exp, scale=0.125)
                diag = kt * 128 >= q0
                sm = r_sm.next()
                if diag:
                    r = (kt * 128 - q0) // 128
                    sp = r_sp.next()
                    k.act(sp.v, e.v, AF.Ln, bias=C.one[:, 0:1])
                    k.tt("dve", sm.v, sp.v, C.sbm[r].v, ALU.mult)
                else:
                    k.act(sm.v, e.v, AF.Ln, bias=C.one[:, 0:1])
                k.I("pe", "matmul", out=tailb[:, 0:TG], lhsT=C.tri_gt.v, rhs=sm.v, start=first, stop=True,
                    skip_group_check=True)
                u = r_u.next()
                k.stt(u.v, zb[:, 0:TG], 0.125, sm.v, ALU.mult, ALU.subtract)
                yield
                k.tt("dve", u.v, u.v, tailb[:, 0:TG], ALU.subtract)
                w = r_w.next()
                if diag:
                    wf = r_wf.next()
                    k.act(wf.v, u.v, AF.Exp)
                    k.tt("pool", w.v, wf.v, C.sbm[r].v, ALU.mult)
                else:
                    k.act(w.v, u.v, AF.Exp)
                if kt > 0:
                    k.I("pe", "matmul", out=tailb[:, 0:TG], lhsT=C.tri_le.v, rhs=sm.v, start=False, stop=True,
                        skip_group_check=True)
                k.I("pe", "matmul", out=yb[bp:bp + 64, 0:TG], lhsT=Vt[:, kt, h * 64:(h + 1) * 64], rhs=w.v,
                    start=first, stop=(kt == 0), skip_group_check=True)
                yield
            k.cp("act", YT[bp:bp + 64, hp, qsl], yb[bp:bp + 64, 0:TG])

        units = [(h, qg) for qg in range(NTG) for h in range(4)]

        def stream(slot):
            for (h, qg) in units[slot::4]:
                yield from unit(h, qg, slot)

        interleave([stream(i) for i in range(4)])
        C.pool = list(range(8))
        k.dma(y_d.v, YT.v, q="pool")


def rope_phase(k, C, S, pos_d, rope_d):
    with k.phase():
        pi_ = k.sb([128, S], I32, "rpi")
        k.dma(pi_.v, pos_d.pbro(128))
        pf = k.sb([128, S], F32, "rpf")
        k.cp("dve", pf.v, pi_.v)
        pidx = k.sb([128, 4], I32, "rpx")
        k.I("pool", "iota", out=pidx[:, 0:1], pattern=[[0, 1]], base=0, channel_multiplier=1)
        k.ts("dve", pidx[:, 1:2], pidx[:, 0:1], 15, ALU.bitwise_and)
        k.ts("dve", pidx[:, 2:3], pidx[:, 0:1], 16, ALU.bitwise_and)
        cf = k.sb([128, 8], F32, "rcf")
        k.cp("dve", cf[:, 0:1], pidx[:, 1:2])
        k.cp("dve", cf[:, 1:2], pidx[:, 2:3])
        k.act(cf[:, 2:3], cf[:, 0:1], AF.Exp, scale=-math.log(10000.0) / 16.0)
        k.ts("dve", cf[:, 3:4], cf[:, 1:2], 0.125, ALU.mult, -1.0, ALU.add)
        ang = k.sb([128, S], F32, "rang")
        k.ts("dve", ang.v, pf.v, cf[:, 2:3], ALU.mult)
        C1, C2 = 6.28125, 2 * math.pi - 6.28125
        MAGIC = 12582912.0
        kf = k.sb([128, S], F32, "rkf")
        r = k.sb([128, S], F32, "rr")
        out = k.sb([128, S], F32, "rout")
        for which in range(2):
            off = 0.25 if which == 0 else 0.0
            k.ts("dve", kf.v, ang.v, 1.0 / (2 * math.pi), ALU.mult, off, ALU.add)
            k.ts("dve", kf.v, kf.v, MAGIC, ALU.add)
            k.ts("dve", kf.v, kf.v, MAGIC, ALU.subtract)
            k.stt(r.v, kf.v, -C1, ang.v, ALU.mult, ALU.add)
            k.stt(r.v, kf.v, -C2, r.v, ALU.mult, ALU.add)
            if which == 0:
                k.ts("dve", r.v, r.v, math.pi / 2, ALU.add)
            k.ts("dve", r.v, r.v, 3.1415925, ALU.min, -3.1415925, ALU.max)
            k.act(out.v, r.v, AF.Sin)
            if which == 1:
                k.ts("dve", out.v, out.v, cf[:, 3:4], ALU.mult)
            k.dma(rope_d[which], out.v, q="pool")


MLA_OFF = 768 + 1024 + 1028


def mla_phase(k, C, S, xt_d, win_d, qg_d, wuq_d, kvg_d, wukv_d, rope_d, y_d):
    TG = min(512, S)
    NTG = S // TG
    NT = S // 128
    TPG = TG // 128
    sb_consts(k, C, TG)
    SC = 96.0 ** -0.5
    with k.phase():
        C.cam = [C.mask(TG, -r * 128, -1, ALU.is_ge, F32, "cam%d" % r, local=True) for r in range(TG // 128)]
        WL = WLoader(k, 8 * 512, nstage=2, nbuf=1, name="mlw")
        wm = WL.load(win_d[:, 0:416], 8, 416)
        wkr2 = k.sb([128, 8, 64], BF16, "mlkr")
        k.cp("pool", wkr2[:, :, 0:32], wm[:, :, 384:416])
        k.cp("pool", wkr2[:, :, 32:48], wm[:, :, 400:416])
        k.cp("pool", wkr2[:, :, 48:64], wm[:, :, 384:400])
        gq = k.sb([128, 4], F32, "mlg")
        for c in range(2):
            k.dma(gq[:, c:c + 1], qg_d[c * 128:(c + 1) * 128].re("(p o) -> p o", o=1))
        k.dma(gq[:, 2:3], kvg_d.re("(p o) -> p o", o=1))
        st = k.sb([128, 2 * 384], F32, "mlst")
        k.dma(st.v.re("p (c n) -> p c n", c=2), wuq_d.re("(c p) n -> p c n", p=128))
        wuq = k.sb([128, 2, 384], BF16, "mluq")
        for c in range(2):
            k.ts("pool", wuq[:, c, :], st[:, c * 384:(c + 1) * 384], gq[:, c:c + 1], ALU.mult)
        wuqs = k.sb([128, 2, 128], BF16, "mluqs")
        for h in range(4):
            k.cp("pool", wuqs[:, :, h * 32:h * 32 + 16], wuq[:, :, h * 96 + 80:h * 96 + 96])
            k.cp("pool", wuqs[:, :, h * 32 + 16:h * 32 + 32], wuq[:, :, h * 96 + 64:h * 96 + 80])
        st2 = k.sb([128, 512], F32, "mlst2")
        k.dma(st2.v, wukv_d)
        wukv = k.sb([128, 512], BF16, "mlukv")
        k.ts("pool", wukv.v, st2.v, gq[:, 2:3], ALU.mult)
        wv = k.sb([128, 256], BF16, "mlwv")
        for h in range(4):
            k.cp("pool", wv[:, h * 64:(h + 1) * 64], wukv[:, h * 128 + 64:h * 128 + 128])
        cs = k.sb([128, 2, S], F32, "mlcs")
        for w_ in range(2):
            k.dma(cs[:, w_, :], rope_d[w_])
        cT = k.sb([128, 3, S], BF16, "mlcT")
        qT = k.sb([128, 4, S], BF16, "mlqT")
        kT = k.sb([128, 4, S], BF16, "mlkT")
        Vt = k.sb([128, NT, 256], BF16, "mlV")
        r_xt = Ring(k, 2, [128, 8, TG], BF16, "mlxt")
        r_c = Ring(k, 2, [128, 384], BF16, "mlc")
        r_j = Ring(k, 2, [128, 256], F32, "mlj")
        r_s = Ring(k, 2, [128, 8], F32, "mls")
        r_t = Ring(k, 4, [128, TG], F32, "mlt")
        for tg in range(NTG):
            tsl = slice(tg * TG, (tg + 1) * TG)
            XT = r_xt.next()
            k.dma(XT.v, xt_d[:, :, tsl])
            for jj in range(TPG):
                j = tg * TPG + jj
                pb = C.bank()
                for kc in range(8):
                    k.mm(pb[:, 0:384], XT[:, kc, jj * 128:(jj + 1) * 128], wm[:, kc, 0:384], start=(kc == 0), stop=(kc == 7))
                s = r_s.next()
                jk = r_j.next()
                k.act(jk[:, 0:256], pb[:, 0:256], AF.Square, accum_out=s[:, 0:1])
                k.act(jk[:, 0:128], pb[:, 256:384], AF.Square, accum_out=s[:, 1:2])
                k.act(s[:, 2:3], s[:, 0:1], AF.Ln, scale=1.0 / 256, bias=C.eps6[:, 0:1])
                k.act(s[:, 3:4], s[:, 1:2], AF.Ln, scale=1.0 / 128, bias=C.eps6[:, 0:1])
                k.act(s[:, 4:6], s[:, 2:4], AF.Exp, scale=-0.5)
                cb = r_c.next()
                k.ts("dve", cb[:, 0:256], pb[:, 0:256], s[:, 4:5], ALU.mult)
                k.ts("dve", cb[:, 256:384], pb[:, 256:384], s[:, 5:6], ALU.mult)
                pv = C.bank().v.bc(BF16)
                for c in range(3):
                    k.tr(pv[:, c * 128:(c + 1) * 128], cb[:, c * 128:(c + 1) * 128], C.id_b.v)
                k.cp("act", cT[:, :, j * 128:(j + 1) * 128], pv[:, 0:384].re("p (c t) -> p c t", c=3))
                pvb = C.bank()
                k.mm(pvb[:, 0:256], cT[:, 2, j * 128:(j + 1) * 128], wv.v)
                k.cp("act", Vt[:, j, :], pvb[:, 0:256])
            p1 = C.bank()
            p2 = C.bank()
            for kc in range(8):
                k.mm(p1[64:96, 0:TG], wkr2[:, kc, 0:32], XT[:, kc, :], start=(kc == 0), stop=(kc == 7))
            for kc in range(8):
                k.mm(p2[64:96, 0:TG], wkr2[:, kc, 32:64], XT[:, kc, :], start=(kc == 0), stop=(kc == 7))
            t1 = r_t.next()
            t2 = r_t.next()
            k.tt("dve", t1[64:96, :], p1[64:96, 0:TG], cs[64:96, 0, tsl], ALU.mult)
            k.tt("dve", t2[64:96, :], p2[64:96, 0:TG], cs[64:96, 1, tsl], ALU.mult)
            k.tt("dve", kT[64:96, 0, tsl], t1[64:96, :], t2[64:96, :], ALU.add)
            for h in range(1, 4):
                k.cp("pool", kT[64:96, h, tsl], kT[64:96, 0, tsl])
            for h in range(4):
                pk = C.bank()
                k.mm(pk[0:64, 0:TG], wukv[:, h * 128:h * 128 + 64], cT[:, 2, tsl])
                k.cp("act", kT[0:64, h, tsl], pk[0:64, 0:TG])
                pq = C.bank()
                for c in range(2):
                    k.mm(pq[0:96, 0:TG], wuq[:, c, h * 96:(h + 1) * 96], cT[:, c, tsl], start=(c == 0), stop=(c == 1))
                pq2 = C.bank()
                for c in range(2):
                    k.mm(pq2[64:96, 0:TG], wuqs[:, c, h * 32:(h + 1) * 32], cT[:, c, tsl], start=(c == 0), stop=(c == 1))
                k.cp("act", qT[0:64, h, tsl], pq[0:64, 0:TG])
                t1 = r_t.next()
                t2 = r_t.next()
                k.tt("dve", t1[64:96, :], pq[64:96, 0:TG], cs[64:96, 0, tsl], ALU.mult)
                k.tt("dve", t2[64:96, :], pq2[64:96, 0:TG], cs[64:96, 1, tsl], ALU.mult)
                k.tt("dve", qT[64:96, h, tsl], t1[64:96, :], t2[64:96, :], ALU.add)
        YT = k.sb([128, 2, S], BF16, "mly")
        r_p = Ring(k, 4, [128, TG], BF16, "mlp")
        r_pf = Ring(k, 2, [128, TG], F32, "mlpf")
        r_d = Ring(k, 2, [128, TG], F32, "mld")
        C.pool = [0, 1, 2, 3]

        def unit(h, qg, slot):
            hp, bp = h // 2, (h % 2) * 64
            q0 = qg * TG
            qsl = slice(q0, q0 + TG)
            db = C.ps[4 + slot]
            yb = C.ps[6 + slot]
            last = (q0 + TG) // 128 - 1
            for kt in range(last + 1):
                zb = C.bank()
                k.mm(zb[:, 0:TG], kT[0:96, h, kt * 128:(kt + 1) * 128], qT[0:96, h, qsl])
                p = r_p.next()
                if kt * 128 >= q0:
                    r = (kt * 128 - q0) // 128
                    pf = r_pf.next()
                    k.act(pf.v, zb[:, 0:TG], AF.Exp, scale=SC)
                    k.tt("pool", p.v, pf.v, C.cam[r].v, ALU.mult)
                else:
                    k.act(p.v, zb[:, 0:TG], AF.Exp, scale=SC)
                yield
                k.I("pe", "matmul", out=yb[bp:bp + 64, 0:TG], lhsT=Vt[:, kt, h * 64:(h + 1) * 64], rhs=p.v,
                    start=(kt == 0), stop=(kt == last), skip_group_check=True)
                k.I("pe", "matmul", out=db[bp:bp + 64, 0:TG], lhsT=C.ones_b[:, 0:64], rhs=p.v,
                    start=(kt == 0), stop=(kt == last), skip_group_check=True)
            rd = r_d.next()
            k.I("dve", "reciprocal", out=rd[bp:bp + 64, :], in_=db[bp:bp + 64, 0:TG])
            k.tt("dve", YT[bp:bp + 64, hp, qsl], yb[bp:bp + 64, 0:TG], rd[bp:bp + 64, :], ALU.mult)
            yield

        units = [(h, qg) for qg in range(NTG) for h in range(4)]

        def stream(slot):
            for (h, qg) in units[slot::2]:
                yield from unit(h, qg, slot)

        interleave([stream(0), stream(1)])
        C.pool = list(range(8))
        k.dma(y_d.v, YT.v, q="pool")


def load_cols(k, dv, nchunk, name="pc", p=128):
    t = k.sb([128, nchunk], F32, name)
    for c in range(nchunk):
        k.dma(t[0:p, c:c + 1], dv[c * p:(c + 1) * p].re("(p o) -> p o", o=1))
    return t


RW_OFF = 768
EXPM05 = math.exp(-0.5)


def rwkv_phase(k, C, S, xt_d, win_d, prm, y_d, stop=None):
    RG = min(128, S)
    NRG = S // RG
    CH = 64
    NCH = RG // CH
    NQ = 14
    with k.phase():
        o64 = C.ones_f[0:64, 0:256].re("p (h s) -> p h s", h=4)
        ones64 = C.ones_b[0:64, 0:64]
        idb64 = C.id_b[0:64, 0:64]
        mLs = k.sb([64, 4, 64], F32, "rwLs")
        k.aselect(mLs.v, o64, [[0, 4], [-1, 64]], ALU.is_gt, 0.0, 0, 1)
        mUs = k.sb([64, 4, 64], F32, "rwUs")
        k.aselect(mUs.v, o64, [[0, 4], [1, 64]], ALU.is_gt, 0.0, 0, -1)
        mUi = k.sb([64, 4, 64], F32, "rwUi")
        k.aselect(mUi.v, o64, [[0, 4], [1, 64]], ALU.is_ge, 0.0, 0, -1)
        mI = k.sb([64, 4, 64], F32, "rwI")
        k.aselect(mI.v, o64, [[0, 4], [1, 64]], ALU.is_equal, 0.0, 0, -1)
        rmi = k.sb([64, 4 * RG], I32, "rwrmi")
        k.I("pool", "iota", out=rmi.v, pattern=[[1, 4 * RG]], base=0, channel_multiplier=0)
        k.ts("dve", rmi.v, rmi.v, CH - 1, ALU.bitwise_and)
        rmask = k.sb([64, 4 * RG], F32, "rwrm")
        k.cp("dve", rmask.v, rmi.v)
        k.ts("dve", rmask.v, rmask.v, 1.0, ALU.min)
        mu = load_cols(k, prm["mu"][0:896], NQ, "rwmu", p=64)
        mug = load_cols(k, prm["mu"][896:1024], 1, "rwmug")
        w0 = load_cols(k, prm["w0"], 4, "rww0", p=64)
        a0 = load_cols(k, prm["a0"], 4, "rwa0", p=64)
        kk_ = load_cols(k, prm["k_k"], 4, "rwkk", p=64)
        ka = load_cols(k, prm["k_a"], 4, "rwka", p=64)
        rk = load_cols(k, prm["r_k"], 4, "rwrk", p=64)
        gng = load_cols(k, prm["gn_g"], 4, "rwgg", p=64)
        gnb = load_cols(k, prm["gn_b"], 4, "rwgb", p=64)
        omka = k.sb([128, 4], F32, "rwomka")
        k.ts("dve", omka[0:64, :], ka[0:64, :], -1.0, ALU.mult, 1.0, ALU.add)
        eps_gn = k.sb([128, 1], F32, "rwepsgn")
        k.memset("pool", eps_gn.v, 64e-5)
        WL = WLoader(k, 8 * 512, nstage=1, nbuf=2, name="rww")
        wrw = [WL.load(win_d[:, hh * 512:(hh + 1) * 512], 8, 512) for hh in range(2)]
        lst = k.sb([128, 768], F32, "rwlst")
        k.dma(lst[0:64, 0:256], prm["w_up"])
        k.dma(lst[0:64, 256:512], prm["a_up"])
        k.dma(lst[:, 512:768], prm["g_up"])
        lup = k.sb([128, 768], BF16, "rwlup")
        k.cp("pool", lup[0:64, 0:512], lst[0:64, 0:512])
        k.cp("pool", lup[:, 512:768], lst[:, 512:768])
        ST = k.sb([64, 4, 64], F32, "rwST")
        k.memset("pool", ST.v, 0.0)
        carry = k.sb([64, NQ, 1], F32, "rwcar")
        k.memset("pool", carry.v, 0.0)
        carryg = k.sb([128, 1], F32, "rwcarg")
        k.memset("pool", carryg.v, 0.0)
        SG = min(512, S)
        GPS = SG // RG
        r_xt = Ring(k, 1, [128, 8, SG], BF16, "rwxt")
        pT = k.sb([64, NQ, SG + 1], F32, "rwpT")
        pG = k.sb([128, SG + 1], F32, "rwpG")
        ps = k.sb([64, NQ, RG], F32, "rwps")
        psg = k.sb([128, RG], F32, "rwpsg")
        tmp = Ring(k, 4, [64, 4, RG], F32, "rwtmp")
        tmp2 = Ring(k, 3, [64, 4, RG], F32, "rwtmp2")
        tmpg = k.sb([128, RG], F32, "rwtmpg")
        r_bf = Ring(k, 2, [128, RG], BF16, "rwbf")

        def arr(name):
            return k.sb([64, 4, RG], F32, name)

        A_a, A_kap, A_kp, A_lw, A_cl = arr("rwa"), arr("rwkap"), arr("rwkp"), arr("rwlw"), arr("rwcl")
        def arr16(name):
            return k.sb([64, 4, RG], BF16, name)

        A_kt, A_bt, A_kbar, A_bbar, A_y = arr16("rwkt"), arr16("rwbt"), arr16("rwkbar"), arr16("rwbbar"), arr("rwy")
        A_yb, Vb = arr16("rwyb"), arr16("rwvb")
        tmpb = Ring(k, 3, [64, 4, RG], BF16, "rwtmpb")
        STb = k.sb([64, 4, 64], BF16, "rwSTb")
        k.memset("pool", STb.v, 0.0)
        yout = Ring(k, 2, [64, 4, RG], BF16, "rwyo")
        sg = k.sb([128, RG], BF16, "rwsg")

        def cset(n):
            return [dict(G=[k.sb([64, 4, 64], BF16, "rwG%d" % i) for i in range(5)],
                         Pb=k.sb([64, 4, 64], BF16, "rwPb"),
                         N=[k.sb([64, 4, 64], BF16, "rwN%d" % i) for i in range(2)],
                         M=[k.sb([64, 4, 64], BF16, "rwM%d" % i) for i in range(2)],
                         P=[k.sb([64, 4, 64], BF16, "rwP%d" % i) for i in range(2)],
                         tok=[k.sb([64, 4, 64], BF16, "rwtok%d" % i) for i in range(3)]) for _ in range(n)]

        def mkset():
            return dict(A_rt=arr16("rwrt"), A_kapt=arr16("rwkapt"), A_g=arr("rwg"), A_bon=arr("rwbon"),
                        gC=k.sb([64, 4, NCH], F32, "rwgC"), CS=cset(NCH), curP=[None] * NCH)

        SETS = [mkset(), mkset()]
        Wt = k.sb([64, 4, 64], BF16, "rwW")
        Ut = k.sb([64, 4, 64], BF16, "rwU")

        def v4(bank):
            return bank[0:64, 0:256].re("p (h s) -> p h s", h=4)

        def fm(Aarr, h, ch):
            return Aarr[:, h, ch * CH:(ch + 1) * CH]

        def gen_AB(rg, B):
            A_rt, A_kapt, A_g, A_bon, gC, CS = B["A_rt"], B["A_kapt"], B["A_g"], B["A_bon"], B["gC"], B["CS"]
            tsl = slice(rg * RG, (rg + 1) * RG)
            o = (rg % GPS) * RG
            if rg % GPS == 0:
                XT = r_xt.next()
                k.dma(XT.v, xt_d[:, :, rg * RG:rg * RG + SG])
                k.cp("pool", pT[:, :, 0:1], carry.v)
                k.cp("pool", pG[:, 0:1], carryg.v)
                for q in range(NQ):
                    c0 = q * 64
                    pb = C.bank()
                    for kc in range(8):
                        k.mm(pb[0:64, 0:SG], wrw[c0 // 512][:, kc, c0 % 512:c0 % 512 + 64], XT[:, kc, :],
                             start=(kc == 0), stop=(kc == 7))
                    k.cp("act", pT[:, q, 1:SG + 1], pb[0:64, 0:SG])
                    if q % 2 == 1:
                        yield
                pb = C.bank()
                for kc in range(8):
                    k.mm(pb[:, 0:SG], wrw[1][:, kc, 384:512], XT[:, kc, :], start=(kc == 0), stop=(kc == 7))
                k.cp("act", pG[:, 1:SG + 1], pb[:, 0:SG])
                k.cp("pool", carry.v, pT[:, :, SG:SG + 1])
                k.cp("pool", carryg.v, pG[:, SG:SG + 1])
                yield
            for q0 in range(0, NQ, 4):
                nq = min(4, NQ - q0)
                t = tmp.next()
                k.tt("pool", t[:, 0:nq, :], pT[:, q0:q0 + nq, o:o + RG], pT[:, q0:q0 + nq, o + 1:o + RG + 1], ALU.subtract)
                for q in range(q0, q0 + nq):
                    k.stt(ps[:, q, :], t[:, q - q0, :], mu[0:64, q:q + 1], pT[:, q, o + 1:o + RG + 1], ALU.mult, ALU.add)
                yield
            k.tt("pool", tmpg.v, pG[:, o:o + RG], pG[:, o + 1:o + RG + 1], ALU.subtract)
            k.stt(psg.v, tmpg.v, mug[:, 0:1], pG[:, o + 1:o + RG + 1], ALU.mult, ALU.add)
            R_, K_, V_ = ps[:, 0:4, :], ps[:, 4:8, :], ps[:, 8:12, :]
            tw = r_bf.next()
            k.act(tw[0:64, :], ps[:, 12, :], AF.Tanh)
            alo = r_bf.next()
            k.cp("pool", alo[0:64, :], ps[:, 13, :])
            k.act(sg.v, psg.v, AF.Sigmoid)
            yield
            for hq in range(2):
                pw = C.bank()
                pa = C.bank()
                pg = C.bank()
                for hh in range(2):
                    h = hq * 2 + hh
                    k.mm(pw[0:64, hh * RG:(hh + 1) * RG], lup[0:64, h * 64:(h + 1) * 64], tw[0:64, :])
                    k.mm(pa[0:64, hh * RG:(hh + 1) * RG], lup[0:64, 256 + h * 64:256 + (h + 1) * 64], alo[0:64, :])
                    k.mm(pg[0:64, hh * RG:(hh + 1) * RG], lup[:, 512 + h * 64:512 + (h + 1) * 64], sg.v)
                for hh in range(2):
                    h = hq * 2 + hh
                    k.act(A_lw[:, h, :], pw[0:64, hh * RG:(hh + 1) * RG], AF.Sigmoid, bias=w0[0:64, h:h + 1])
                    k.act(A_a[:, h, :], pa[0:64, hh * RG:(hh + 1) * RG], AF.Sigmoid, bias=a0[0:64, h:h + 1])
                    k.cp("act", A_g[:, h, :], pg[0:64, hh * RG:(hh + 1) * RG])
                yield
            k.ts("pool", A_lw.v, A_lw.v, -EXPM05, ALU.mult)
            kkv = tmp.next()
            for h in range(4):
                k.ts("dve", kkv[:, h, :], K_[:, h, :], kk_[0:64, h:h + 1], ALU.mult)
            sq = tmpb.next()
            k.tt("pool", sq.v, kkv.v, kkv.v, ALU.mult)
            k.cp("pool", Vb.v, V_)
            yield
            rs = tmp.next()
            for hq in range(2):
                pss = C.bank()
                for hh in range(2):
                    k.mm(pss[0:64, hh * RG:(hh + 1) * RG], ones64, sq[:, hq * 2 + hh, :])
                k.ts("dve", rs[:, hq * 2:hq * 2 + 2, :], pss[0:64, 0:2 * RG].re("p (h t) -> p h t", h=2), 1e-24, ALU.max)
            k.act(rs.v, rs.v, AF.Ln)
            k.act(rs.v, rs.v, AF.Exp, scale=-0.5)
            k.tt("dve", A_kap.v, kkv.v, rs.v, ALU.mult)
            yield
            k.I("dve", "tensor_tensor_scan", out=A_cl.v.re("p h t -> p (h t)"), data0=rmask.v,
                data1=A_lw.v.re("p h t -> p (h t)"), initial=0.0, op0=ALU.mult, op1=ALU.add)
            t = tmp.next()
            for h in range(4):
                k.ts("dve", t[:, h, :], A_a[:, h, :], ka[0:64, h:h + 1], ALU.mult, omka[0:64, h:h + 1], ALU.add)
            k.tt("pool", A_kp.v, K_, t.v, ALU.mult)
            yield
            t2 = tmpb.next()
            for h in range(4):
                k.stt(t2[:, h, :], R_[:, h, :], rk[0:64, h:h + 1], A_kp[:, h, :], ALU.mult, ALU.mult)
            for hq in range(2):
                pbn = C.bank()
                for hh in range(2):
                    k.mm(pbn[0:64, hh * RG:(hh + 1) * RG], ones64, t2[:, hq * 2 + hh, :])
                k.tt("dve", A_bon[:, hq * 2:hq * 2 + 2, :], pbn[0:64, 0:2 * RG].re("p (h t) -> p h t", h=2),
                     V_[:, hq * 2:hq * 2 + 2, :], ALU.mult)
            yield
            ep = tmp.next()
            k.act(ep.v, A_cl.v, AF.Exp)
            k.tt("pool", A_rt.v, R_, ep.v, ALU.mult)
            en = tmp.next()
            k.act(en.v, A_cl.v, AF.Exp, scale=-1.0)
            k.tt("dve", A_kt.v, A_kp.v, en.v, ALU.mult)
            b_ = tmp.next()
            k.tt("pool", b_.v, A_kap.v, A_a.v, ALU.mult)
            k.tt("dve", A_bt.v, b_.v, en.v, ALU.mult)
            yield
            t3 = tmp.next()
            k.tt("pool", t3.v, A_cl.v, A_lw.v, ALU.subtract)
            k.act(t3.v, t3.v, AF.Exp)
            k.tt("dve", A_kapt.v, A_kap.v, t3.v, ALU.mult)
            clv = A_cl.v.re("p h (c t) -> p (h c) t", t=CH)
            t4 = tmp.next()
            t4v = t4.v.re("p h (c t) -> p (h c) t", t=CH)
            k.tt("pool", t4v, clv[:, :, CH - 1:CH].bro([64, 4 * NCH, CH]), clv, ALU.subtract)
            k.act(t4.v, t4.v, AF.Exp)
            k.tt("dve", A_kbar.v, A_kp.v, t4.v, ALU.mult)
            k.tt("pool", A_bbar.v, b_.v, t4.v, ALU.mult)
            k.act(gC.v.re("p h c -> p (h c)"), clv[:, :, CH - 1], AF.Exp)
            yield
            for ch in range(NCH):
                cs = CS[ch]
                specs = [(A_kapt, A_bt, mLs, 1.0), (A_bt, A_kapt, mUs, 1.0), (A_kt, A_kapt, mUs, 1.0),
                         (A_kt, A_rt, mUi, 1.0), (A_bt, A_rt, mUi, -1.0)]
                for gi, (La, Ra, msk, sgn) in enumerate(specs):
                    pb = C.bank()
                    for h in range(4):
                        k.mm(pb[0:64, h * 64:(h + 1) * 64], fm(La, h, ch), fm(Ra, h, ch))
                    if sgn == 1.0:
                        k.tt("dve", cs["G"][gi].v, v4(pb), msk.v, ALU.mult)
                    else:
                        k.stt(cs["G"][gi].v, v4(pb), sgn, msk.v, ALU.mult, ALU.mult)
                    yield
                k.tt("pool", cs["P"][0].v, mI.v, cs["G"][1].v, ALU.subtract)
                for ti, sgn in enumerate([1.0, 1.0, -1.0]):
                    pbb = C.bank().v.bc(BF16)
                    for h in range(4):
                        src = fm(Vb if ti == 0 else (A_kbar if ti == 1 else A_bbar), h, ch)
                        k.tr(pbb[0:64, h * 64:(h + 1) * 64], src, idb64)
                    pv_ = pbb[0:64, 0:256].re("p (h s) -> p h s", h=4)
                    if sgn == 1.0:
                        k.cp("act", cs["tok"][ti].v, pv_)
                    else:
                        k.act(cs["tok"][ti].v, pv_, AF.Copy, scale=-1.0)
                    yield
            curN = [CS[ch]["G"][0] for ch in range(NCH)]
            curM = [CS[ch]["G"][1] for ch in range(NCH)]
            curP = [CS[ch]["P"][0] for ch in range(NCH)]
            for st in range(5):
                lastst = st == 4
                for ch in range(NCH):
                    cs = CS[ch]
                    Nn, Mn, Pn = cs["N"][st % 2], cs["M"][st % 2], cs["P"][(st + 1) % 2]
                    pb = C.bank()
                    for h in range(4):
                        k.mm(pb[0:64, h * 64:(h + 1) * 64], curM[ch][:, h, :], curN[ch][:, h, :])
                    k.cp("act", Nn.v, v4(pb))
                    if not lastst:
                        pb2 = C.bank()
                        for h in range(4):
                            k.mm(pb2[0:64, h * 64:(h + 1) * 64], curN[ch][:, h, :], curM[ch][:, h, :])
                        k.cp("act", Mn.v, v4(pb2))
                    pb3 = C.bank()
                    for h in range(4):
                        k.mm(pb3[0:64, h * 64:(h + 1) * 64], Nn[:, h, :], curP[ch][:, h, :])
                    if lastst:
                        k.tt("dve", cs["Pb"].v, v4(pb3), curP[ch].v, ALU.add)
                        Pn = cs["Pb"]
                    else:
                        k.tt("dve", Pn.v, v4(pb3), curP[ch].v, ALU.add)
                    curN[ch], curM[ch], curP[ch] = Nn, Mn, Pn
                    yield
            B["curP"] = curP

        def gen_CD(rg, B):
            A_rt, A_kapt, A_g, A_bon, gC, CS = B["A_rt"], B["A_kapt"], B["A_g"], B["A_bon"], B["gC"], B["CS"]
            curP = B["curP"]
            tsl = slice(rg * RG, (rg + 1) * RG)
            for ch in range(NCH):
                cs = CS[ch]
                TT = curP[ch]
                Vt_, KB, BBn = cs["tok"]
                AkkT, ArkT, ArbTn = cs["G"][2], cs["G"][3], cs["G"][4]
                pw = C.bank()
                for h in range(4):
                    k.I("pe", "matmul", out=pw[0:64, h * 64:(h + 1) * 64], lhsT=fm(A_kapt, h, ch), rhs=STb[:, h, :],
                        start=True, stop=False, skip_group_check=True)
                    k.I("pe", "matmul", out=pw[0:64, h * 64:(h + 1) * 64], lhsT=AkkT[:, h, :], rhs=Vt_[:, h, :],
                        start=False, stop=True, skip_group_check=True)
                k.cp("act", Wt.v, v4(pw))
                yield
                pu = C.bank()
                for h in range(4):
                    k.mm(pu[0:64, h * 64:(h + 1) * 64], TT[:, h, :], Wt[:, h, :])
                k.cp("act", Ut.v, v4(pu))
                yield
                py = C.bank()
                pst = C.bank()
                for h in range(4):
                    yo = py[0:64, h * 64:(h + 1) * 64]
                    k.I("pe", "matmul", out=yo, lhsT=STb[:, h, :], rhs=fm(A_rt, h, ch), start=True, stop=False,
                        skip_group_check=True)
                    k.I("pe", "matmul", out=yo, lhsT=Vt_[:, h, :], rhs=ArkT[:, h, :], start=False, stop=False,
                        skip_group_check=True)
                    k.I("pe", "matmul", out=yo, lhsT=Ut[:, h, :], rhs=ArbTn[:, h, :], start=False, stop=True,
                        skip_group_check=True)
                    so = pst[0:64, h * 64:(h + 1) * 64]
                    k.I("pe", "matmul", out=so, lhsT=KB[:, h, :], rhs=Vt_[:, h, :], start=True, stop=False,
                        skip_group_check=True)
                    k.I("pe", "matmul", out=so, lhsT=BBn[:, h, :], rhs=Ut[:, h, :], start=False, stop=True,
                        skip_group_check=True)
                k.cp("act", A_y[:, :, ch * CH:(ch + 1) * CH], v4(py))
                for h in range(4):
                    k.stt(ST[:, h, :], ST[:, h, :], gC[:, h, ch:ch + 1], pst[0:64, h * 64:(h + 1) * 64], ALU.mult, ALU.add)
                k.cp("pool", STb.v, ST.v)
                yield
            yo_t = yout.next()
            yc = tmp2.next()
            k.cp("pool", A_yb.v, A_y.v)
            for hq in range(2):
                pm = C.bank()
                for hh in range(2):
                    k.mm(pm[0:64, hh * RG:(hh + 1) * RG], ones64, A_yb[:, hq * 2 + hh, :])
                k.stt(yc[:, hq * 2:hq * 2 + 2, :], pm[0:64, 0:2 * RG].re("p (h t) -> p h t", h=2), -1.0 / 64,
                      A_y[:, hq * 2:hq * 2 + 2, :], ALU.mult, ALU.add)
            sq = tmpb.next()
            k.tt("pool", sq.v, yc.v, yc.v, ALU.mult)
            yield
            rs = tmp2.next()
            for hq in range(2):
                pvv = C.bank()
                for hh in range(2):
                    k.mm(pvv[0:64, hh * RG:(hh + 1) * RG], ones64, sq[:, hq * 2 + hh, :])
                k.act(rs[:, hq * 2:hq * 2 + 2, :], pvv[0:64, 0:2 * RG].re("p (h t) -> p h t", h=2), AF.Ln, scale=1.0 / 64,
                      bias=eps_gn[0:64, 0:1])
            k.act(rs.v, rs.v, AF.Exp, scale=-0.5)
            k.tt("dve", yc.v, yc.v, rs.v, ALU.mult)
            yield
            for h in range(4):
                k.ts("dve", yc[:, h, :], yc[:, h, :], gng[0:64, h:h + 1], ALU.mult, gnb[0:64, h:h + 1], ALU.add)
            k.tt("pool", yc.v, yc.v, A_bon.v, ALU.add)
            k.tt("pool", yo_t.v, yc.v, A_g.v, ALU.mult)
            for h in range(4):
                k.dma(y_d[(h % 2) * 64:(h % 2) * 64 + 64, h // 2, tsl], yo_t[:, h, :], q="pool")
            yield

        for step in range(NRG + 1):
            gens = []
            if step < NRG:
                gens.append(gen_AB(step, SETS[step % 2]))
            if step >= 1:
                gens.append(gen_CD(step - 1, SETS[(step - 1) % 2]))
            interleave(gens)


SSD_OFF = 768 + 1024


def ssd_phase(k, C, S, xt_d, win_d, prm, y_d):
    TG = min(512, S)
    NTG = S // TG
    TPG = TG // 128
    with k.phase():
        Ui = k.sb([128, 128], F32, "sdUi")
        k.aselect(Ui.v, C.ones_f[:, 0:128], [[1, 128]], ALU.is_ge, 0.0, 0, -1)
        WL = WLoader(k, 8 * 512, nstage=2, nbuf=2, name="sdw")
        w_zx = WL.load(win_d[:, 0:512], 8, 512)
        w_bc = WL.load(win_d[:, 512:1024], 8, 512)
        st = k.sb([128, 8, 4], F32, "sdst")
        k.dma(st.v, win_d[:, 1024:1028].re("(c p) n -> p c n", p=128))
        w_dt = k.sb([128, 8, 4], BF16, "sdwdt")
        k.cp("pool", w_dt.v, st.v)
        cw = k.sb([128, 6, 4], F32, "sdcw")
        for n in range(6):
            for j in range(4):
                k.dma(cw[:, n, j:j + 1], prm["conv_w"][j, n * 128:(n + 1) * 128].re("(p o) -> p o", o=1))
        cb = load_cols(k, prm["conv_b"], 6, "sdcb")
        dtb = load_bcast(k, prm["dt_bias"], 4, "sddtb")
        alog = load_bcast(k, prm["a_log"], 4, "sdalog")
        dsk = load_bcast(k, prm["d"], 4, "sddsk")
        ng = load_bcast(k, prm["norm_g"], 256, "sdng")
        aneg = k.sb([128, 4], F32, "sdaneg")
        k.act(aneg.v, alog.v, AF.Exp)
        k.ts("dve", aneg.v, aneg.v, -1.0, ALU.mult)
        eps5g = C.eps5
        Sst = k.sb([128, 4, 64], F32, "sdS")
        k.memset("pool", Sst.v, 0.0)
        Sbf = k.sb([128, 4, 64], BF16, "sdSb")
        k.memset("pool", Sbf.v, 0.0)
        xraw = k.sb([128, 6, TG + 3], F32, "sdxr")
        k.memset("pool", xraw[:, :, 0:3], 0.0)
        xc = k.sb([128, 6, TG], F32, "sdxc")
        bcb = k.sb([128, 4, TG], BF16, "sdbcb")
        r_xt = Ring(k, 2, [128, 8, TG], BF16, "sdxt")
        r_acc = Ring(k, 2, [128, TG], F32, "sdacc")
        r_s = Ring(k, 4, [128, 48], F32, "sds")
        r_m = Ring(k, 8, [128, 128], F32, "sdm")
        r_mb = Ring(k, 20, [128, 128], BF16, "sdmb")
        r_tok = Ring(k, 3, [128, 512], F32, "sdtok")
        r_tb = Ring(k, 3, [128, 768], BF16, "sdtb")
        r_y = Ring(k, 3, [128, 256], F32, "sdy")
        r_z = Ring(k, 3, [128, 256], F32, "sdz")
        r_gt = Ring(k, 3, [128, 256], F32, "sdgt")
        r_yb = Ring(k, 3, [128, 256], BF16, "sdyb")
        r_yo = Ring(k, 2, [128, 2, TG], BF16, "sdyo")
        for tg in range(NTG):
            tsl = slice(tg * TG, (tg + 1) * TG)
            XT = r_xt.next()
            k.dma(XT.v, xt_d[:, :, tsl])
            for n in range(6):
                pb = C.bank()
                wsl = w_zx[:, :, 256 + n * 128:256 + (n + 1) * 128] if n < 2 else w_bc[:, :, (n - 2) * 128:(n - 1) * 128]
                for kc in range(8):
                    k.mm(pb[:, 0:TG], wsl[:, kc, :], XT[:, kc, :], start=(kc == 0), stop=(kc == 7))
                k.cp("act", xraw[:, n, 3:TG + 3], pb[:, 0:TG])
                acc = r_acc.next()
                k.ts("dve", acc.v, xraw[:, n, 3:TG + 3], cw[:, n, 3:4], ALU.mult, cb[:, n:n + 1], ALU.add)
                for j in (2, 1, 0):
                    k.stt(acc.v, xraw[:, n, j:TG + j], cw[:, n, j:j + 1], acc.v, ALU.mult, ALU.add)
                k.act(xc[:, n, :], acc.v, AF.Silu)
                if n >= 2:
                    k.cp("pool", bcb[:, n - 2, :], xc[:, n, :])
            yo = r_yo.next()

            def tile(jj, slot, XT=XT, yo=yo):
                lsl = slice(jj * 128, (jj + 1) * 128)
                s = r_s.next()
                py = C.ps[4 + 2 * slot]
                pst = C.ps[5 + 2 * slot]
                pd = C.bank()
                for kc in range(8):
                    k.mm(pd[:, 0:4], XT[:, kc, lsl], w_dt[:, kc, :], start=(kc == 0), stop=(kc == 7))
                k.tt("dve", s[:, 0:4], pd[:, 0:4], dtb.v, ALU.add)
                k.act(s[:, 0:4], s[:, 0:4], AF.Exp)
                k.act(s[:, 4:8], s[:, 0:4], AF.Ln, bias=C.one[:, 0:1])
                k.tt("dve", s[:, 8:12], s[:, 4:8], aneg.v, ALU.mult)
                pc = C.bank()
                k.mm(pc[:, 0:4], Ui.v, s[:, 8:12])
                k.mm(pc[:, 8:12], C.ones_f[:, 0:128], s[:, 8:12])
                k.cp("dve", s[:, 12:16], pc[:, 0:4])
                k.cp("dve", s[:, 16:20], pc[:, 8:12])
                yield
                k.tt("dve", s[:, 20:24], s[:, 16:20], s[:, 12:16], ALU.subtract)
                k.act(s[:, 20:24], s[:, 20:24], AF.Exp)
                k.act(s[:, 24:28], s[:, 16:20], AF.Exp)
                pz = C.bank()
                for kc in range(8):
                    k.mm(pz[:, 0:256], XT[:, kc, lsl], w_zx[:, kc, 0:256], start=(kc == 0), stop=(kc == 7))
                zs = r_z.next()
                k.act(zs.v, pz[:, 0:256], AF.Silu)
                yield
                pt = C.bank()
                for n in range(4):
                    k.tr(pt[:, n * 128:(n + 1) * 128], xc[:, n, lsl], C.id_f.v)
                tok = r_tok.next()
                k.cp("act", tok.v, pt.v)
                yield
                tb = r_tb.next()
                for h in range(4):
                    k.ts("dve", tb[:, h * 64:(h + 1) * 64], tok[:, h * 64:(h + 1) * 64], s[:, 4 + h:5 + h], ALU.mult)
                    k.stt(tb[:, 512 + h * 64:512 + (h + 1) * 64], tok[:, h * 64:(h + 1) * 64], s[:, 4 + h:5 + h],
                          s[:, 20 + h:21 + h].bro([128, 64]), ALU.mult, ALU.mult)
                k.cp("pool", tb[:, 256:512], tok[:, 256:512])
                pg = C.bank()
                for g in range(2):
                    k.mm(pg[:, g * 128:(g + 1) * 128], bcb[:, g, lsl], bcb[:, 2 + g, lsl])
                gts = r_gt.next()
                k.cp("act", gts.v, pg[:, 0:256])
                yield
                csts = []
                for h in range(4):
                    g = h // 2
                    abc = r_m.next()
                    k.ts("dve", abc.v, C.ones_f[:, 0:128], s[:, 8 + h:9 + h], ALU.mult)
                    pr = C.bank()
                    k.mm(pr[:, 0:128], abc.v, Ui.v)
                    dm = r_m.next()
                    k.ts("dve", dm.v, pr[:, 0:128], s[:, 12 + h:13 + h], ALU.subtract, 0.0, ALU.min)
                    k.act(dm.v, dm.v, AF.Exp)
                    k.tt("dve", dm.v, dm.v, gts[:, g * 128:(g + 1) * 128], ALU.mult)
                    mh = r_mb.next()
                    k.tt("pool", mh.v, dm.v, Ui.v, ALU.mult)
                    er = r_m.next()
                    k.act(er.v, pr[:, 0:128], AF.Exp)
                    cst = r_mb.next()
                    k.tt("pool", cst.v, xc[:, 4 + g, lsl], er.v, ALU.mult)
                    csts.append(cst)
                    k.I("pe", "matmul", out=py[:, h * 64:(h + 1) * 64], lhsT=mh.v, rhs=tb[:, h * 64:(h + 1) * 64],
                        start=(h == 0), stop=False, skip_group_check=True)
                    k.mm(pst[:, h * 64:(h + 1) * 64], tb[:, 256 + g * 128:256 + (g + 1) * 128],
                         tb[:, 512 + h * 64:512 + (h + 1) * 64])
                    yield
                for h in range(4):
                    k.I("pe", "matmul", out=py[:, h * 64:(h + 1) * 64], lhsT=csts[h].v, rhs=Sbf[:, h, :],
                        start=False, stop=True, skip_group_check=True)
                for h in range(4):
                    k.stt(Sst[:, h, :], Sst[:, h, :], s[:, 24 + h:25 + h], pst[:, h * 64:(h + 1) * 64], ALU.mult, ALU.add)
                k.cp("pool", Sbf.v, Sst.v)
                y = r_y.next()
                for h in range(4):
                    k.stt(y[:, h * 64:(h + 1) * 64], tok[:, h * 64:(h + 1) * 64], dsk[:, h:h + 1], py[:, h * 64:(h + 1) * 64],
                          ALU.mult, ALU.add)
                yield
                k.tt("dve", y.v, y.v, zs.v, ALU.mult)
                for g in range(2):
                    k.act(zs[:, g * 128:(g + 1) * 128], y[:, g * 128:(g + 1) * 128], AF.Square, accum_out=s[:, 28 + g:29 + g])
                k.act(s[:, 30:32], s[:, 28:30], AF.Ln, scale=1.0 / 128, bias=eps5g[:, 0:1])
                k.act(s[:, 32:34], s[:, 30:32], AF.Exp, scale=-0.5)
                yb = r_yb.next()
                for g in range(2):
                    k.stt(yb[:, g * 128:(g + 1) * 128], y[:, g * 128:(g + 1) * 128], s[:, 32 + g:33 + g],
                          ng[:, g * 128:(g + 1) * 128], ALU.mult, ALU.mult)
                yield
                pv = C.bank().v.bc(BF16)
                for c in range(2):
                    k.tr(pv[:, c * 128:(c + 1) * 128], yb[:, c * 128:(c + 1) * 128], C.id_b.v)
                k.cp("act", yo[:, :, lsl], pv[:, 0:256].re("p (c t) -> p c t", c=2))
                yield

            def stream(par):
                if par == 1:
                    yield
                for jj in range(par, TPG, 2):
                    yield from tile(jj, par)

            C.pool = [0, 1, 2, 3]
            interleave([stream(0), stream(1)])
            C.pool = list(range(8))
            k.cp("pool", xraw[:, :, 0:3], xraw[:, :, TG:TG + 3])
            k.dma(y_d[:, :, tsl], yo.v, q="pool")


IN_SPECS = [
    ("mix_w_in", [2, 1024, 3236]), ("rw_mu", [2, 1024]), ("rw_w0", [2, 256]), ("rw_w_up", [2, 64, 256]),
    ("rw_a0", [2, 256]), ("rw_a_up", [2, 64, 256]), ("rw_g_up", [2, 128, 256]), ("rw_k_k", [2, 256]),
    ("rw_k_a", [2, 256]), ("rw_r_k", [2, 4, 64]), ("rw_gn_g", [2, 256]), ("rw_gn_b", [2, 256]),
    ("ssd_conv_w", [2, 4, 768]), ("ssd_conv_b", [2, 768]), ("ssd_dt_bias", [2, 4]), ("ssd_a_log", [2, 4]),
    ("ssd_d", [2, 4]), ("ssd_norm_g", [2, 256]), ("mla_q_norm_g", [2, 256]), ("mla_w_uq", [2, 256, 384]),
    ("mla_kv_norm_g", [2, 128]), ("mla_w_ukv", [2, 128, 512]), ("mix_w_br_out", [2, 4, 256, 1024]),
    ("mix_w_gate", [2, 4, 1024, 1024]), ("mix_w_out", [2, 1024, 1024]), ("ln1_g", [2, 1024]), ("ln1_b", [2, 1024]),
    ("xa_w_q", [2, 1024, 1024]), ("xa_w_kv", [2, 1024, 2048]), ("xa_w_o", [2, 1024, 1024]), ("ln2_g", [2, 1024]),
    ("ln2_b", [2, 1024]), ("ffn_w13", [1, 1024, 5632]), ("ffn_w2", [1, 2816, 1024]), ("moe_router", [1, 1024, 8]),
    ("moe_w13", [1, 8, 1024, 7168]), ("moe_w2", [1, 8, 3584, 1024]), ("ln3_g", [2, 1024]), ("ln3_b", [2, 1024]),
]


def build_program(S, NB, L=2):
    nc = bass.Bass("TRN2", target_bir_lowering=False)
    k = K(nc)
    x = k.dram("x", [NB, S, 1024], F32, kind="ExternalInput")
    mem = k.dram("mem", [NB, 256, 1024], F32, kind="ExternalInput")
    pos = k.dram("positions", [NB, S], I32, kind="ExternalInput")
    W = {n: k.dram(n, shp, F32, kind="ExternalInput") for n, shp in IN_SPECS}
    out = k.dram("out", [NB, S, 1024], F32, kind="ExternalOutput")
    xt = k.dram("s_xt", [128, 8, S], BF16)
    Xa = k.dram("s_xa", [S, 1024], F32)
    Xb = k.dram("s_xb", [S, 1024], F32)
    Xc = k.dram("s_xc", [S, 1024], F32)
    U = k.dram("s_u", [S, 1024], F32)
    ybr = [k.dram("s_y%d" % i, [128, 2, S], BF16) for i in range(4)]
    mT = k.dram("s_mt", [128, 8, S], BF16)
    rope = k.dram("s_rope", [2, 128, S], F32)
    C = Consts(k)
    sb_consts(k, C, min(512, S))
    for b in range(NB):
        rope_phase(k, C, S, pos[b], rope)
        xt0_phase(k, C, S, x[b], xt)
        xcur = x[b]
        for l in range(L):
            win = W["mix_w_in"][l]
            sb_phase(k, C, S, xt, win[:, 0:768], ybr[0])
            rprm = dict(mu=W["rw_mu"][l], w0=W["rw_w0"][l], w_up=W["rw_w_up"][l], a0=W["rw_a0"][l], a_up=W["rw_a_up"][l],
                        g_up=W["rw_g_up"][l], k_k=W["rw_k_k"][l], k_a=W["rw_k_a"][l],
                        r_k=W["rw_r_k"][l].re("h n -> (h n)"), gn_g=W["rw_gn_g"][l], gn_b=W["rw_gn_b"][l])
            rwkv_phase(k, C, S, xt, win[:, RW_OFF:RW_OFF + 1024], rprm, ybr[1])
            sprm = dict(conv_w=W["ssd_conv_w"][l], conv_b=W["ssd_conv_b"][l], dt_bias=W["ssd_dt_bias"][l],
                        a_log=W["ssd_a_log"][l], d=W["ssd_d"][l], norm_g=W["ssd_norm_g"][l])
            ssd_phase(k, C, S, xt, win[:, SSD_OFF:SSD_OFF + 1028], sprm, ybr[2])
            mla_phase(k, C, S, xt, win[:, MLA_OFF:MLA_OFF + 416], W["mla_q_norm_g"][l], W["mla_w_uq"][l],
                      W["mla_kv_norm_g"][l], W["mla_w_ukv"][l], rope, ybr[3])
            gate_phase(k, C, S, xt, ybr, W["mix_w_gate"][l], W["mix_w_br_out"][l], mT)
            outproj_phase(k, C, S, mT, 8, W["mix_w_out"][l], U)
            ln_phase(k, C, S, U, xcur, W["ln1_g"][l], W["ln1_b"][l], Xa, xt)
            xattn_phase(k, C, S, xt, mem[b], W["xa_w_q"][l], W["xa_w_kv"][l], W["xa_w_o"][l], U)
            ln_phase(k, C, S, U, Xa.v, W["ln2_g"][l], W["ln2_b"][l], Xb, xt)
            if l % 2 == 0:
                ffn_phase(k, C, S, xt, Xb.v, U, [(W["ffn_w13"][l // 2], W["ffn_w2"][l // 2], 2816)])
            else:
                ex = [(W["moe_w13"][l // 2, e], W["moe_w2"][l // 2, e], 3584) for e in range(8)]
                ffn_phase(k, C, S, xt, Xb.v, U, ex, router_d=W["moe_router"][l // 2])
            last = l == L - 1
            ln_phase(k, C, S, U, Xb.v, W["ln3_g"][l], W["ln3_b"][l], out[b] if last else Xc, xt)
            xcur = Xc.v
    k.barrier()
    return nc, k


def kernel(**inputs):
    B, S = inputs["x"].shape[0], inputs["x"].shape[1]
    NB = B // NCORES
    nc, _ = build_program(S, NB)
    shared = {n: np.ascontiguousarray(inputs[n], dtype=np.float32) for n, _ in IN_SPECS}
    in_maps = []
    for c in range(NCORES):
        sl = slice(c * NB, (c + 1) * NB)
        m = dict(shared)
        m["x"] = np.ascontiguousarray(inputs["x"][sl], dtype=np.float32)
        m["mem"] = np.ascontiguousarray(inputs["mem"][sl], dtype=np.float32)
        m["positions"] = np.ascontiguousarray(inputs["positions"][sl], dtype=np.int32)
        in_maps.append(m)
    res = run_bass_kernel_spmd(nc, in_maps, core_ids=list(range(NCORES)))
    return np.concatenate([np.asarray(r["out"]) for r in res.results], axis=0).astype(np.float32)
```

```python
import math
from contextlib import ExitStack
import numpy as np
import concourse.bass as bass
import concourse.mybir as mybir
from concourse.bass_utils import run_bass_kernel_spmd

F32 = mybir.dt.float32
BF16 = mybir.dt.bfloat16
I32 = mybir.dt.int32
AF = mybir.ActivationFunctionType
ALU = mybir.AluOpType
AX = mybir.AxisListType

D = 1024
NCORES = 8
WRITE_KW = ("out", "accum_out")


class T:
    def __init__(self, ap, name):
        self.ap = ap
        self.name = name
        self.w = None
        self.r = {}

    def __getitem__(self, idx):
        return V(self, self.ap[idx])

    @property
    def v(self):
        return V(self, self.ap)


class V:
    def __init__(self, t, ap):
        self.t = t
        self.ap = ap

    def __getitem__(self, idx):
        return V(self.t, self.ap[idx])

    def bc(self, dt):
        return V(self.t, self.ap.bitcast(dt))

    def re(self, pat, **kw):
        return V(self.t, self.ap.rearrange(pat, **kw))

    def bro(self, shape):
        return V(self.t, self.ap.to_broadcast(shape))

    def pbro(self, n):
        return V(self.t, self.ap.partition_broadcast(n))


class K:
    COMPUTE = ("pe", "act", "dve", "pool")

    def __init__(self, nc):
        self.nc = nc
        self.es = ExitStack()
        self.engs = {"pe": nc.tensor, "act": nc.scalar, "dve": nc.vector, "pool": nc.gpsimd, "sp": nc.sync}
        self.sems = {}
        self.cnt = {}
        for e in self.COMPUTE:
            self.sems[e] = self.es.enter_context(nc.semaphore("s_" + e))
            self.cnt[e] = 0
        self.dq = {}
        for q in ("sp", "pool", "act"):
            lst = []
            for i in range(6):
                key = "d_%s%d" % (q, i)
                self.sems[key] = self.es.enter_context(nc.semaphore(key))
                self.cnt[key] = 0
                lst.append(key)
            self.dq[q] = [lst, 0]
        self.seen = {e: {} for e in self.engs}
        self.n_ins = 0
        self.phase_es = None
        self.uid = 0

    def sb(self, shape, dt, name=None):
        self.uid += 1
        name = "%s_%d" % (name or "t", self.uid)
        h = (self.phase_es or self.es).enter_context(self.nc.sbuf_tensor(name, list(shape), dt))
        return T(h[:], name)

    def sbg(self, shape, dt, name=None):
        self.uid += 1
        name = "%s_%d" % (name or "g", self.uid)
        h = self.es.enter_context(self.nc.sbuf_tensor(name, list(shape), dt))
        return T(h[:], name)

    def psum(self, name):
        h = self.es.enter_context(self.nc.psum_tensor(name, [128, 512], F32))
        return T(h[:], name)

    def dram(self, name, shape, dt, kind="Internal"):
        h = self.nc.dram_tensor(name, list(shape), dt, kind=kind)
        return T(h.ap(), name)

    def phase(self):
        k = self

        class _P:
            def __enter__(s):
                k.barrier()
                k.phase_es = ExitStack()
                return s

            def __exit__(s, *a):
                k.barrier()
                k.phase_es.close()
                k.phase_es = None
                return False

        return _P()

    def _wait(self, eng, key, val):
        if self.seen[eng].get(key, 0) >= val:
            return
        self.engs[eng].wait_ge(self.sems[key], val)
        self.seen[eng][key] = val
        self.n_ins += 1

    def _sync(self, eng, reads, writes):
        deps = {}

        def add(d, war=False):
            if d is None:
                return
            key, val = d
            if key == eng and eng == "pe":
                return
            if deps.get(key, 0) < val:
                deps[key] = val

        for t in reads:
            add(t.w)
        for t in writes:
            add(t.w)
            for key, val in t.r.items():
                add((key, val), war=True)
        for key, val in deps.items():
            self._wait(eng, key, val)

    def _done(self, me, reads, writes):
        key, val = me
        for t in reads:
            if t.r.get(key, 0) < val:
                t.r[key] = val
        for t in writes:
            t.w = me
            t.r = {}

    def barrier(self):
        for eng in self.engs:
            for key in self.sems:
                if key == eng:
                    continue
                self._wait(eng, key, self.cnt[key])

    def _split(self, kw):
        reads, writes, args = [], [], {}
        for name, v in kw.items():
            if isinstance(v, V):
                (writes if name in WRITE_KW else reads).append(v.t)
                args[name] = v.ap
            else:
                args[name] = v
        return reads, writes, args

    def I(self, eng, meth, **kw):
        reads, writes, args = self._split(kw)
        self._sync(eng, reads, writes)
        ins = getattr(self.engs[eng], meth)(**args)
        self.cnt[eng] += 1
        ins.then_inc(self.sems[eng], 1)
        self.n_ins += 1
        self._done((eng, self.cnt[eng]), reads, writes)

    def dma(self, out, in_, q="sp", **kw):
        lst, i = self.dq[q]
        key = lst[i % len(lst)]
        self.dq[q][1] = i + 1
        self._wait(q, key, self.cnt[key])
        self._sync(q, [in_.t], [out.t])
        ins = self.engs[q].dma_start(out=out.ap, in_=in_.ap, **kw)
        self.cnt[key] += 16
        ins.then_inc(self.sems[key], 16)
        self.n_ins += 1
        self._done((key, self.cnt[key]), [in_.t], [out.t])

    def mm(self, out, lhsT, rhs, start=True, stop=True):
        self.I("pe", "matmul", out=out, lhsT=lhsT, rhs=rhs, start=start, stop=stop)

    def tr(self, out, in_, ident):
        self.I("pe", "transpose", out=out, in_=in_, identity=ident)

    def act(self, out, in_, func, bias=0.0, scale=1.0, **kw):
        self.I("act", "activation", out=out, in_=in_, func=func, bias=bias, scale=scale, **kw)

    def tt(self, eng, out, in0, in1, op):
        self.I(eng, "tensor_tensor", out=out, in0=in0, in1=in1, op=op)

    def ts(self, eng, out, in0, s1, op0, s2=None, op1=ALU.bypass, **kw):
        self.I(eng, "tensor_scalar", out=out, in0=in0, scalar1=s1, scalar2=s2, op0=op0, op1=op1, **kw)

    def stt(self, out, in0, scalar, in1, op0, op1):
        self.I("dve", "scalar_tensor_tensor", out=out, in0=in0, scalar=scalar, in1=in1, op0=op0, op1=op1)

    def cp(self, eng, out, in_):
        if eng == "act":
            self.I("act", "copy", out=out, in_=in_)
        else:
            self.I(eng, "tensor_copy", out=out, in_=in_)

    def memset(self, eng, out, val):
        reads, writes = [], [out.t]
        self._sync(eng, reads, writes)
        ins = self.engs[eng].memset(out.ap, val)
        self.cnt[eng] += 1
        ins.then_inc(self.sems[eng], 1)
        self._done((eng, self.cnt[eng]), reads, writes)

    def aselect(self, out, in_, pattern, cmp, fill, base, cm):
        self.I("pool", "affine_select", out=out, in_=in_, pattern=pattern, compare_op=cmp, fill=fill,
               base=base, channel_multiplier=cm)


class Consts:
    def __init__(self, k):
        self.k = k
        ones = k.sbg([128, 512], F32, "onesf")
        k.memset("pool", ones.v, 1.0)
        self.ones_f = ones
        idf = k.sbg([128, 128], F32, "idf")
        k.aselect(idf.v, ones[:, 0:128], [[1, 128]], ALU.is_equal, 0.0, 0, -1)
        self.id_f = idf
        idb = k.sbg([128, 128], BF16, "idb")
        k.cp("pool", idb.v, idf.v)
        self.id_b = idb
        onesb = k.sbg([128, 128], BF16, "onesb")
        k.cp("pool", onesb.v, ones[:, 0:128])
        self.ones_b = onesb
        self.eps5 = k.sbg([128, 1], F32, "eps5")
        k.memset("pool", self.eps5.v, 1e-5)
        self.eps6 = k.sbg([128, 1], F32, "eps6")
        k.memset("pool", self.eps6.v, 1e-6)
        self.ps = [k.psum("ps%d" % i) for i in range(8)]
        self.psi = 0
        self.pool = list(range(8))

    def bank(self):
        b = self.ps[self.pool[self.psi % len(self.pool)]]
        self.psi += 1
        return b

    def mask(self, shape_f, base, cm, cmp=ALU.is_ge, dt=F32, name="mask", local=False):
        k = self.k
        m = (k.sb if local else k.sbg)([128, shape_f], F32, name)
        k.aselect(m.v, self.ones_f[:, 0:shape_f], [[1, shape_f]], cmp, 0.0, base, cm)
        if dt == F32:
            return m
        mb = k.sbg([128, shape_f], dt, name + "b")
        k.cp("pool", mb.v, m.v)
        return mb


class WLoader:
    def __init__(self, k, max_elems, nstage=2, nbuf=3, ceng="pool", name="w"):
        self.k = k
        self.st = [k.sb([128, max_elems], F32, name + "st") for _ in range(nstage)]
        self.bf = [k.sb([128, max_elems], BF16, name + "bf") for _ in range(nbuf)]
        self.i = 0
        self.j = 0
        self.ceng = ceng

    def load(self, wv, kc, n, p=128):
        k = self.k
        st = self.st[self.i % len(self.st)]
        self.i += 1
        bf = self.bf[self.j % len(self.bf)]
        self.j += 1
        sv = st[0:p, 0:kc * n].re("p (c n) -> p c n", c=kc)
        k.dma(sv, wv.re("(c p) n -> p c n", p=p))
        bv = bf[0:p, 0:kc * n].re("p (c n) -> p c n", c=kc)
        k.cp(self.ceng, bv, sv)
        return bv


def load_bcast(k, dv, n, name="bc"):
    t = k.sb([128, n], F32, name)
    k.dma(t.v, dv.pbro(128))
    return t


DN_ALPHA = (2.0 * 2) ** 0.25


def interleave(gens):
    gens = list(gens)
    while gens:
        for g in list(gens):
            try:
                next(g)
            except StopIteration:
                gens.remove(g)


class Ring:
    def __init__(self, k, n, shape, dt, name="r"):
        self.ts = [k.sb(shape, dt, name) for _ in range(n)]
        self.i = 0

    def next(self):
        t = self.ts[self.i % len(self.ts)]
        self.i += 1
        return t


def ln_phase(k, C, S, u_d, xres_d, g_d, b_d, xout_d, xt_d, eps=1e-5):
    NT = S // 128
    with k.phase():
        g_bc = load_bcast(k, g_d, 1024, "lng")
        b_bc = load_bcast(k, b_d, 1024, "lnb")
        r_x = Ring(k, 6, [128, 1024], F32, "lnx")
        r_u = Ring(k, 6, [128, 1024], F32, "lnu")
        r_o = Ring(k, 5, [128, 1024], F32, "lno")
        r_b = Ring(k, 5, [128, 1024], BF16, "lnbf")
        r_s = Ring(k, 8, [128, 16], F32, "lns")
        GT = min(4, NT)
        xtgs = [k.sb([128, 8, GT * 128], BF16, "lnxt") for _ in range(2)]
        done = {}

        def tile(j):
            rows = slice(j * 128, (j + 1) * 128)
            xr = r_x.next()
            k.dma(xr.v, xres_d[rows, :])
            u = r_u.next()
            k.dma(u.v, u_d[rows, :])
            yield
            k.stt(u.v, xr.v, DN_ALPHA, u.v, ALU.mult, ALU.add)
            st = r_s.next()
            for h in range(2):
                k.I("dve", "bn_stats", out=st[:, h * 6:(h + 1) * 6], in_=u[:, h * 512:(h + 1) * 512])
            k.I("dve", "bn_aggr", out=st[:, 12:14], in_=st[:, 0:12])
            k.act(st[:, 14:15], st[:, 13:14], AF.Ln, bias=C.eps5[:, 0:1])
            k.act(st[:, 15:16], st[:, 14:15], AF.Exp, scale=-0.5)
            yield
            k.ts("dve", u.v, u.v, st[:, 12:13], ALU.subtract, st[:, 15:16], ALU.mult)
            xo = r_o.next()
            k.tt("pool", xo.v, u.v, g_bc.v, ALU.mult)
            k.tt("dve", xo.v, xo.v, b_bc.v, ALU.add)
            k.dma(xout_d[rows, :], xo.v, q="act")
            xb = r_b.next()
            k.cp("act", xb.v, xo.v)
            yield
            g = j // GT
            xtg = xtgs[g % 2]
            pv = C.bank().v.bc(BF16)
            for c in range(8):
                k.tr(pv[:, c * 128:(c + 1) * 128], xb[:, c * 128:(c + 1) * 128], C.id_b.v)
            jj = j % GT
            k.cp("dve", xtg[:, :, jj * 128:(jj + 1) * 128], pv.re("p (c t) -> p c t", c=8))
            done[g] = done.get(g, 0) + 1
            if done[g] == GT:
                k.dma(xt_d[:, :, g * GT * 128:(g + 1) * GT * 128], xtg.v, q="act")
            yield

        NS = min(4, NT)

        def stream(par):
            for j in range(par, NT, NS):
                yield from tile(j)

        interleave([stream(i) for i in range(NS)])


def xt0_phase(k, C, S, x_d, xt_d):
    NT = S // 128
    with k.phase():
        r_x = Ring(k, 2, [128, 1024], F32, "x0")
        r_b = Ring(k, 2, [128, 1024], BF16, "x0b")
        GT = min(4, NT)
        r_g = Ring(k, 2, [128, 8, GT * 128], BF16, "x0t")
        for j in range(NT):
            rows = slice(j * 128, (j + 1) * 128)
            xr = r_x.next()
            k.dma(xr.v, x_d[rows, :])
            xb = r_b.next()
            k.cp("act", xb.v, xr.v)
            if j % GT == 0:
                xtg = r_g.next()
            pst = C.bank()
            pv = pst.v.bc(BF16)
            for c in range(8):
                k.tr(pv[:, c * 128:(c + 1) * 128], xb[:, c * 128:(c + 1) * 128], C.id_b.v)
            jj = j % GT
            k.cp("dve", xtg[:, :, jj * 128:(jj + 1) * 128], pv.re("p (c t) -> p c t", c=8))
            if jj == GT - 1:
                t0 = (j - jj) * 128
                k.dma(xt_d[:, :, t0:t0 + GT * 128], xtg.v, q="pool")


def ffn_phase(k, C, S, xt_d, x_d, u_d, experts, router_d=None):
    NT = S // 128
    TG = min(512, S)
    NTG = S // TG
    TPG = TG // 128
    with k.phase():
        XT = k.sb([128, 8, S], BF16, "ffxt")
        k.dma(XT.v, xt_d.v)
        acc = k.sb([128, NT, 1024], F32, "ffacc")
        G = None
        if router_d is not None:
            G = k.sb([128, NT, 8], F32, "ffG")
            rt = k.sb([128, 8, 8], F32, "ffrt")
            k.dma(rt.v, router_d.re("(c p) e -> p c e", p=128))
            r_x = Ring(k, 1, [128, 1024], F32, "ffx")
            r_t = Ring(k, 1, [128, 1024], F32, "ffxT")
            r_s = Ring(k, 2, [128, 48], F32, "ffs")
            for j in range(NT):
                xr = r_x.next()
                k.dma(xr.v, x_d[j * 128:(j + 1) * 128, :])
                xT = r_t.next()
                for hh in range(2):
                    pb = C.bank()
                    for c in range(4):
                        cc = hh * 4 + c
                        k.tr(pb[:, c * 128:(c + 1) * 128], xr[:, cc * 128:(cc + 1) * 128], C.id_f.v)
                    k.cp("act", xT[:, hh * 512:(hh + 1) * 512], pb.v)
                pl = C.bank()
                for c in range(8):
                    k.mm(pl[:, 0:8], xT[:, c * 128:(c + 1) * 128], rt[:, c, :], start=(c == 0), stop=(c == 7))
                s = r_s.next()
                lg = s[:, 0:8]
                k.cp("dve", lg, pl[:, 0:8])
                k.I("dve", "reduce_max", out=s[:, 8:9], in_=lg, axis=AX.X)
                k.ts("dve", s[:, 16:24], lg, s[:, 8:9], ALU.is_equal)
                k.stt(s[:, 24:32], s[:, 16:24], -1e30, lg, ALU.mult, ALU.add)
                k.I("dve", "reduce_max", out=s[:, 9:10], in_=s[:, 24:32], axis=AX.X)
                k.ts("dve", s[:, 16:24], lg, s[:, 9:10], ALU.is_ge)
                k.ts("dve", s[:, 10:11], s[:, 8:9], -1.0, ALU.mult)
                k.act(s[:, 32:40], lg, AF.Exp, bias=s[:, 10:11])
                k.tt("dve", s[:, 32:40], s[:, 32:40], s[:, 16:24], ALU.mult)
                k.I("dve", "reduce_sum", out=s[:, 11:12], in_=s[:, 32:40], axis=AX.X)
                k.I("dve", "reciprocal", out=s[:, 12:13], in_=s[:, 11:12])
                k.ts("dve", G[:, j, :], s[:, 32:40], s[:, 12:13], ALU.mult)
        WL = WLoader(k, 8 * 512, nstage=2, nbuf=4, name="ffw")
        r_h = Ring(k, 2, [128, 4, TG], BF16, "ffh")
        r_sg = Ring(k, 2, [128, TG], F32, "ffsg")
        first = True
        for e, (w13, w2, H) in enumerate(experts):
            h0 = 0
            while h0 < H:
                hw = min(512, H - h0)
                nc_ = hw // 128
                w1 = WL.load(w13[:, h0:h0 + hw], 8, hw)
                w3 = WL.load(w13[:, H + h0:H + h0 + hw], 8, hw)
                w2s = WL.load(w2[h0:h0 + hw, :], nc_, 1024)
                for tg in range(NTG):
                    tsl = slice(tg * TG, (tg + 1) * TG)
                    hT = r_h.next()
                    for c in range(nc_):
                        gp = C.bank()
                        for kc in range(8):
                            k.mm(gp[:, 0:TG], w1[:, kc, c * 128:(c + 1) * 128], XT[:, kc, tsl], start=(kc == 0), stop=(kc == 7))
                        up = C.bank()
                        for kc in range(8):
                            k.mm(up[:, 0:TG], w3[:, kc, c * 128:(c + 1) * 128], XT[:, kc, tsl], start=(kc == 0), stop=(kc == 7))
                        sg = r_sg.next()
                        k.act(sg.v, gp[:, 0:TG], AF.Silu)
                        k.tt("dve", hT[:, c, :], up[:, 0:TG], sg.v, ALU.mult)
                    for j in range(TPG):
                        tix = tg * TPG + j
                        for hh in range(2):
                            yp = C.bank()
                            for c in range(nc_):
                                k.mm(yp.v, hT[:, c, j * 128:(j + 1) * 128], w2s[:, c, hh * 512:(hh + 1) * 512],
                                     start=(c == 0), stop=(c == nc_ - 1))
                            av = acc[:, tix, hh * 512:(hh + 1) * 512]
                            if G is None:
                                if first:
                                    k.cp("act", av, yp.v)
                                else:
                                    k.tt("dve", av, yp.v, av, ALU.add)
                            else:
                                gs = G[:, tix, e:e + 1]
                                if first:
                                    k.ts("dve", av, yp.v, gs, ALU.mult)
                                else:
                                    k.stt(av, yp.v, gs, av, ALU.mult, ALU.add)
                first = False
                h0 += hw
        for j in range(NT):
            k.dma(u_d[j * 128:(j + 1) * 128, :], acc[:, j, :], q="pool")


def outproj_phase(k, C, S, ht_d, KC, w_d, u_d, p=128):
    NT = S // 128
    with k.phase():
        HT = k.sb([128, KC, S], BF16, "opH")
        k.dma(HT[0:p], ht_d.v)
        WL = WLoader(k, KC * 512, nstage=2, nbuf=2, name="opw")
        wh = [WL.load(w_d[:, hh * 512:(hh + 1) * 512], KC, 512, p=p) for hh in range(2)]
        r_u = Ring(k, 3, [128, 1024], F32, "opu")
        for j in range(NT):
            u = r_u.next()
            for hh in range(2):
                yp = C.bank()
                for c in range(KC):
                    k.mm(yp.v, HT[0:p, c, j * 128:(j + 1) * 128], wh[hh][:, c, :], start=(c == 0), stop=(c == KC - 1))
                k.cp("act" if hh == 0 else "dve", u[:, hh * 512:(hh + 1) * 512], yp.v)
            k.dma(u_d[j * 128:(j + 1) * 128, :], u.v, q="pool")


def to_fm_bf16(k, C, src_d, n_tok, dstT, r_x, r_b):
    for j in range(n_tok // 128):
        xr = r_x.next()
        k.dma(xr.v, src_d[j * 128:(j + 1) * 128, :])
        xb = r_b.next()
        k.cp("act", xb.v, xr.v)
        pv = C.bank().v.bc(BF16)
        for c in range(8):
            k.tr(pv[:, c * 128:(c + 1) * 128], xb[:, c * 128:(c + 1) * 128], C.id_b.v)
        k.cp("dve", dstT[:, :, j * 128:(j + 1) * 128], pv.re("p (c t) -> p c t", c=8))


def xattn_phase(k, C, S, xt_d, mem_d, wq_d, wkv_d, wo_d, u_d):
    TG = min(512, S)
    NTG = S // TG
    TPG = TG // 128
    M = 256
    with k.phase():
        r_xt = Ring(k, 2, [128, 8, TG], BF16, "xaxt")
        memT = k.sb([128, 8, M], BF16, "xamT")
        r_x = Ring(k, 2, [128, 1024], F32, "xamx")
        r_b = Ring(k, 2, [128, 1024], BF16, "xamb")
        to_fm_bf16(k, C, mem_d, M, memT.v, r_x, r_b)
        WL = WLoader(k, 8 * 512, nstage=2, nbuf=6, name="xaw")
        KT = k.sb([128, 8, M], BF16, "xaKT")
        Vm = k.sb([128, 2, 1024], BF16, "xaV")
        for sl in range(4):
            w = WL.load(wkv_d[:, sl * 512:(sl + 1) * 512], 8, 512)
            if sl < 2:
                for c in range(4):
                    pb = C.bank()
                    for kc in range(8):
                        k.mm(pb[:, 0:M], w[:, kc, c * 128:(c + 1) * 128], memT[:, kc, :], start=(kc == 0), stop=(kc == 7))
                    k.cp("act", KT[:, sl * 4 + c, :], pb[:, 0:M])
            else:
                for mt in range(2):
                    pb = C.bank()
                    for kc in range(8):
                        k.mm(pb.v, memT[:, kc, mt * 128:(mt + 1) * 128], w[:, kc, :], start=(kc == 0), stop=(kc == 7))
                    k.cp("act", Vm[:, mt, (sl - 2) * 512:(sl - 1) * 512], pb.v)
        wq = [WL.load(wq_d[:, hh * 512:(hh + 1) * 512], 8, 512) for hh in range(2)]
        wo = [WL.load(wo_d[:, hh * 512:(hh + 1) * 512], 8, 512) for hh in range(2)]
        r_q = Ring(k, 2, [128, 8, TG], BF16, "xaq")
        r_o = Ring(k, 2, [128, 8, TG], BF16, "xao")
        r_p = Ring(k, 4, [128, TG], BF16, "xap")
        r_d = Ring(k, 2, [128, TG], F32, "xad")
        r_u = Ring(k, 2, [128, 1024], F32, "xau")
        for tg in range(NTG):
            tsl = slice(tg * TG, (tg + 1) * TG)
            XT = r_xt.next()
            k.dma(XT.v, xt_d[:, :, tsl])
            qT = r_q.next()
            for n in range(8):
                pb = C.bank()
                for kc in range(8):
                    k.mm(pb[:, 0:TG], wq[n // 4][:, kc, (n % 4) * 128:(n % 4 + 1) * 128], XT[:, kc, :],
                         start=(kc == 0), stop=(kc == 7))
                k.cp("act" if n % 2 == 0 else "dve", qT[:, n, :], pb[:, 0:TG])
            oT = r_o.next()
            for h in range(4):
                P = []
                for mt in range(2):
                    zb = C.bank()
                    for c in range(2):
                        k.mm(zb[:, 0:TG], KT[:, 2 * h + c, mt * 128:(mt + 1) * 128], qT[:, 2 * h + c, :],
                             start=(c == 0), stop=(c == 1))
                    p = r_p.next()
                    k.act(p.v, zb[:, 0:TG], AF.Exp, scale=1.0 / 16.0)
                    P.append(p)
                db = C.bank()
                for mt in range(2):
                    k.mm(db[:, 0:TG], C.ones_b.v, P[mt].v, start=(mt == 0), stop=(mt == 1))
                rd = r_d.next()
                k.I("dve", "reciprocal", out=rd.v, in_=db[:, 0:TG])
                for c in range(2):
                    ob = C.bank()
                    for mt in range(2):
                        k.mm(ob[:, 0:TG], Vm[:, mt, (2 * h + c) * 128:(2 * h + c + 1) * 128], P[mt].v,
                             start=(mt == 0), stop=(mt == 1))
                    k.tt("dve", oT[:, 2 * h + c, :], ob[:, 0:TG], rd.v, ALU.mult)
            for j in range(TPG):
                u = r_u.next()
                for hh in range(2):
                    yp = C.bank()
                    for c in range(8):
                        k.mm(yp.v, oT[:, c, j * 128:(j + 1) * 128], wo[hh][:, c, :], start=(c == 0), stop=(c == 7))
                    k.cp("act" if hh == 0 else "dve", u[:, hh * 512:(hh + 1) * 512], yp.v)
                t0 = tg * TG + j * 128
                k.dma(u_d[t0:t0 + 128, :], u.v, q="pool")


def gate_phase(k, C, S, xt_d, ybr_d, wgate_d, wbr_d, mergedT_d):
    TG = min(512, S)
    NTG = S // TG
    with k.phase():
        XT = k.sb([128, 8, S], BF16, "gxt")
        k.dma(XT.v, xt_d.v)
        Y = []
        for i in range(4):
            y = k.sb([128, 2, S], BF16, "gy")
            k.dma(y.v, ybr_d[i].v)
            Y.append(y)
        WL = WLoader(k, 8 * 512, nstage=2, nbuf=4, name="gw")
        macc = k.sb([128, 4, S], F32, "gacc")
        r_s = Ring(k, 3, [128, TG], F32, "gsg")
        r_m = Ring(k, 2, [128, 4, TG], BF16, "gm")
        for sl in range(2):
            for i in range(4):
                wg = WL.load(wgate_d[i, :, sl * 512:(sl + 1) * 512], 8, 512)
                wb = WL.load(wbr_d[i, :, sl * 512:(sl + 1) * 512], 2, 512)
                for tg in range(NTG):
                    tsl = slice(tg * TG, (tg + 1) * TG)
                    for c in range(4):
                        gp = C.bank()
                        for kc in range(8):
                            k.mm(gp[:, 0:TG], wg[:, kc, c * 128:(c + 1) * 128], XT[:, kc, tsl], start=(kc == 0), stop=(kc == 7))
                        pp = C.bank()
                        for kc in range(2):
                            k.mm(pp[:, 0:TG], wb[:, kc, c * 128:(c + 1) * 128], Y[i][:, kc, tsl], start=(kc == 0), stop=(kc == 1))
                        sg = r_s.next()
                        k.act(sg.v, gp[:, 0:TG], AF.Sigmoid)
                        if i == 0:
                            k.tt("dve", macc[:, c, tsl], pp[:, 0:TG], sg.v, ALU.mult)
                        else:
                            k.tt("dve", sg.v, pp[:, 0:TG], sg.v, ALU.mult)
                            k.tt("pool", macc[:, c, tsl], macc[:, c, tsl], sg.v, ALU.add)
            for tg in range(NTG):
                tsl = slice(tg * TG, (tg + 1) * TG)
                mb = r_m.next()
                k.cp("act", mb.v, macc[:, :, tsl])
                k.dma(mergedT_d[:, sl * 4:(sl + 1) * 4, tsl], mb.v, q="pool")


def sb_consts(k, C, TG):
    if hasattr(C, "sbm"):
        return
    m = k.sbg([128, 128], F32, "triu")
    k.aselect(m.v, C.ones_f[:, 0:128], [[-1, 128]], ALU.is_gt, 0.0, 0, 1)
    C.tri_gt = k.sbg([128, 128], BF16, "triub")
    k.cp("pool", C.tri_gt.v, m.v)
    m2 = k.sbg([128, 128], F32, "tril")
    k.aselect(m2.v, C.ones_f[:, 0:128], [[1, 128]], ALU.is_ge, 0.0, 0, -1)
    C.tri_le = k.sbg([128, 128], BF16, "trilb")
    k.cp("pool", C.tri_le.v, m2.v)
    C.one = k.sbg([128, 1], F32, "one")
    k.memset("pool", C.one.v, 1.0)


def sb_phase(k, C, S, xt_d, win_d, y_d):
    TG = min(512, S)
    NTG = S // TG
    NT = S // 128
    sb_consts(k, C, TG)
    with k.phase():
        C.sbm = [C.mask(TG, -r * 128, -1, ALU.is_gt, F32, "sbm%d" % r, local=True) for r in range(TG // 128)]
        XT = k.sb([128, 8, S], BF16, "sbxt")
        k.dma(XT.v, xt_d.v)
        WL = WLoader(k, 8 * 512, nstage=2, nbuf=2, name="sbw")
        wqk = WL.load(win_d[:, 0:512], 8, 512)
        wv = WL.load(win_d[:, 512:768], 8, 256)
        qk = k.sb([128, 4, S], BF16, "sbqk")
        Vt = k.sb([128, NT, 256], BF16, "sbv")
        for tg in range(NTG):
            tsl = slice(tg * TG, (tg + 1) * TG)
            for n in range(4):
                pb = C.bank()
                for kc in range(8):
                    k.mm(pb[:, 0:TG], wqk[:, kc, n * 128:(n + 1) * 128], XT[:, kc, tsl], start=(kc == 0), stop=(kc == 7))
                k.cp("act", qk[:, n, tsl], pb[:, 0:TG])
        for j in range(NT):
            pb = C.bank()
            for kc in range(8):
                k.mm(pb[:, 0:256], XT[:, kc, j * 128:(j + 1) * 128], wv[:, kc, :], start=(kc == 0), stop=(kc == 7))
            k.cp("act", Vt[:, j, :], pb[:, 0:256])
        YT = k.sb([128, 2, S], BF16, "sby")
        r_e = Ring(k, 6, [128, TG], F32, "sbe")
        r_sp = Ring(k, 2, [128, TG], F32, "sbsp")
        r_sm = Ring(k, 8, [128, TG], BF16, "sbsm")
        r_u = Ring(k, 6, [128, TG], F32, "sbu")
        r_w = Ring(k, 8, [128, TG], BF16, "sbw_")
        r_wf = Ring(k, 2, [128, TG], F32, "sbwf")
        C.pool = [0, 1]

        def unit(h, qg, slot):
            hp, bp = h // 2, (h % 2) * 64
            q0 = qg * TG
            qsl = slice(q0, q0 + TG)
            tailb = C.ps[4 + slot]
            yb = C.ps[2 + slot // 2]
            last = (q0 + TG) // 128 - 1
            for kt in range(last, -1, -1):
                first = kt == last
                zb = C.bank()
                k.mm(zb[:, 0:TG], qk[bp:bp + 64, 2 + hp, kt * 128:(kt + 1) * 128], qk[bp:bp + 64, hp, qsl])
                e = r_e.next()
                k.act(e.v, zb[:, 0:TG], AF.Exp, scale=0.125)
                diag = kt * 128 >= q0
                sm = r_sm.next()
                if diag:
                    r = (kt * 128 - q0) // 128
                    sp = r_sp.next()
                    k.act(sp.v, e.v, AF.Ln, bias=C.one[:, 0:1])
                    k.tt("dve", sm.v, sp.v, C.sbm[r].v, ALU.mult)
                else:
                    k.act(sm.v, e.v, AF.Ln, bias=C.one[:, 0:1])
                k.I("pe", "matmul", out=tailb[:, 0:TG], lhsT=C.tri_gt.v, rhs=sm.v, start=first, stop=True,
                    skip_group_check=True)
                u = r_u.next()
                k.stt(u.v, zb[:, 0:TG], 0.125, sm.v, ALU.mult, ALU.subtract)
                yield
                k.tt("dve", u.v, u.v, tailb[:, 0:TG], ALU.subtract)
                w = r_w.next()
                if diag:
                    wf = r_wf.next()
                    k.act(wf.v, u.v, AF.Exp)
                    k.tt("pool", w.v, wf.v, C.sbm[r].v, ALU.mult)
                else:
                    k.act(w.v, u.v, AF.Exp)
                if kt > 0:
                    k.I("pe", "matmul", out=tailb[:, 0:TG], lhsT=C.tri_le.v, rhs=sm.v, start=False, stop=True,
                        skip_group_check=True)
                k.I("pe", "matmul", out=yb[bp:bp + 64, 0:TG], lhsT=Vt[:, kt, h * 64:(h + 1) * 64], rhs=w.v,
                    start=first, stop=(kt == 0), skip_group_check=True)
                yield
            k.cp("act", YT[bp:bp + 64, hp, qsl], yb[bp:bp + 64, 0:TG])

        units = [(h, qg) for qg in range(NTG) for h in range(4)]

        def stream(slot):
            for (h, qg) in units[slot::4]:
                yield from unit(h, qg, slot)

        interleave([stream(i) for i in range(4)])
        C.pool = list(range(8))
        k.dma(y_d.v, YT.v, q="pool")


def rope_phase(k, C, S, pos_d, rope_d):
    with k.phase():
        pi_ = k.sb([128, S], I32, "rpi")
        k.dma(pi_.v, pos_d.pbro(128))
        pf = k.sb([128, S], F32, "rpf")
        k.cp("dve", pf.v, pi_.v)
        pidx = k.sb([128, 4], I32, "rpx")
        k.I("pool", "iota", out=pidx[:, 0:1], pattern=[[0, 1]], base=0, channel_multiplier=1)
        k.ts("dve", pidx[:, 1:2], pidx[:, 0:1], 15, ALU.bitwise_and)
        k.ts("dve", pidx[:, 2:3], pidx[:, 0:1], 16, ALU.bitwise_and)
        cf = k.sb([128, 8], F32, "rcf")
        k.cp("dve", cf[:, 0:1], pidx[:, 1:2])
        k.cp("dve", cf[:, 1:2], pidx[:, 2:3])
        k.act(cf[:, 2:3], cf[:, 0:1], AF.Exp, scale=-math.log(10000.0) / 16.0)
        k.ts("dve", cf[:, 3:4], cf[:, 1:2], 0.125, ALU.mult, -1.0, ALU.add)
        ang = k.sb([128, S], F32, "rang")
        k.ts("dve", ang.v, pf.v, cf[:, 2:3], ALU.mult)
        C1, C2 = 6.28125, 2 * math.pi - 6.28125
        MAGIC = 12582912.0
        kf = k.sb([128, S], F32, "rkf")
        r = k.sb([128, S], F32, "rr")
        out = k.sb([128, S], F32, "rout")
        for which in range(2):
            off = 0.25 if which == 0 else 0.0
            k.ts("dve", kf.v, ang.v, 1.0 / (2 * math.pi), ALU.mult, off, ALU.add)
            k.ts("dve", kf.v, kf.v, MAGIC, ALU.add)
            k.ts("dve", kf.v, kf.v, MAGIC, ALU.subtract)
            k.stt(r.v, kf.v, -C1, ang.v, ALU.mult, ALU.add)
            k.stt(r.v, kf.v, -C2, r.v, ALU.mult, ALU.add)
            if which == 0:
                k.ts("dve", r.v, r.v, math.pi / 2, ALU.add)
            k.ts("dve", r.v, r.v, 3.1415925, ALU.min, -3.1415925, ALU.max)
            k.act(out.v, r.v, AF.Sin)
            if which == 1:
                k.ts("dve", out.v, out.v, cf[:, 3:4], ALU.mult)
            k.dma(rope_d[which], out.v, q="pool")


MLA_OFF = 768 + 1024 + 1028


def mla_phase(k, C, S, xt_d, win_d, qg_d, wuq_d, kvg_d, wukv_d, rope_d, y_d):
    TG = min(512, S)
    NTG = S // TG
    NT = S // 128
    TPG = TG // 128
    sb_consts(k, C, TG)
    SC = 96.0 ** -0.5
    with k.phase():
        C.cam = [C.mask(TG, -r * 128, -1, ALU.is_ge, F32, "cam%d" % r, local=True) for r in range(TG // 128)]
        WL = WLoader(k, 8 * 512, nstage=2, nbuf=1, name="mlw")
        wm = WL.load(win_d[:, 0:416], 8, 416)
        wkr2 = k.sb([128, 8, 64], BF16, "mlkr")
        k.cp("pool", wkr2[:, :, 0:32], wm[:, :, 384:416])
        k.cp("pool", wkr2[:, :, 32:48], wm[:, :, 400:416])
        k.cp("pool", wkr2[:, :, 48:64], wm[:, :, 384:400])
        gq = k.sb([128, 4], F32, "mlg")
        for c in range(2):
            k.dma(gq[:, c:c + 1], qg_d[c * 128:(c + 1) * 128].re("(p o) -> p o", o=1))
        k.dma(gq[:, 2:3], kvg_d.re("(p o) -> p o", o=1))
        st = k.sb([128, 2 * 384], F32, "mlst")
        k.dma(st.v.re("p (c n) -> p c n", c=2), wuq_d.re("(c p) n -> p c n", p=128))
        wuq = k.sb([128, 2, 384], BF16, "mluq")
        for c in range(2):
            k.ts("pool", wuq[:, c, :], st[:, c * 384:(c + 1) * 384], gq[:, c:c + 1], ALU.mult)
        wuqs = k.sb([128, 2, 128], BF16, "mluqs")
        for h in range(4):
            k.cp("pool", wuqs[:, :, h * 32:h * 32 + 16], wuq[:, :, h * 96 + 80:h * 96 + 96])
            k.cp("pool", wuqs[:, :, h * 32 + 16:h * 32 + 32], wuq[:, :, h * 96 + 64:h * 96 + 80])
        st2 = k.sb([128, 512], F32, "mlst2")
        k.dma(st2.v, wukv_d)
        wukv = k.sb([128, 512], BF16, "mlukv")
        k.ts("pool", wukv.v, st2.v, gq[:, 2:3], ALU.mult)
        wv = k.sb([128, 256], BF16, "mlwv")
        for h in range(4):
            k.cp("pool", wv[:, h * 64:(h + 1) * 64], wukv[:, h * 128 + 64:h * 128 + 128])
        cs = k.sb([128, 2, S], F32, "mlcs")
        for w_ in range(2):
            k.dma(cs[:, w_, :], rope_d[w_])
        cT = k.sb([128, 3, S], BF16, "mlcT")
        qT = k.sb([128, 4, S], BF16, "mlqT")
        kT = k.sb([128, 4, S], BF16, "mlkT")
        Vt = k.sb([128, NT, 256], BF16, "mlV")
        r_xt = Ring(k, 2, [128, 8, TG], BF16, "mlxt")
        r_c = Ring(k, 2, [128, 384], BF16, "mlc")
        r_j = Ring(k, 2, [128, 256], F32, "mlj")
        r_s = Ring(k, 2, [128, 8], F32, "mls")
        r_t = Ring(k, 4, [128, TG], F32, "mlt")
        for tg in range(NTG):
            tsl = slice(tg * TG, (tg + 1) * TG)
            XT = r_xt.next()
            k.dma(XT.v, xt_d[:, :, tsl])
            for jj in range(TPG):
                j = tg * TPG + jj
                pb = C.bank()
                for kc in range(8):
                    k.mm(pb[:, 0:384], XT[:, kc, jj * 128:(jj + 1) * 128], wm[:, kc, 0:384], start=(kc == 0), stop=(kc == 7))
                s = r_s.next()
                jk = r_j.next()
                k.act(jk[:, 0:256], pb[:, 0:256], AF.Square, accum_out=s[:, 0:1])
                k.act(jk[:, 0:128], pb[:, 256:384], AF.Square, accum_out=s[:, 1:2])
                k.act(s[:, 2:3], s[:, 0:1], AF.Ln, scale=1.0 / 256, bias=C.eps6[:, 0:1])
                k.act(s[:, 3:4], s[:, 1:2], AF.Ln, scale=1.0 / 128, bias=C.eps6[:, 0:1])
                k.act(s[:, 4:6], s[:, 2:4], AF.Exp, scale=-0.5)
                cb = r_c.next()
                k.ts("dve", cb[:, 0:256], pb[:, 0:256], s[:, 4:5], ALU.mult)
                k.ts("dve", cb[:, 256:384], pb[:, 256:384], s[:, 5:6], ALU.mult)
                pv = C.bank().v.bc(BF16)
                for c in range(3):
                    k.tr(pv[:, c * 128:(c + 1) * 128], cb[:, c * 128:(c + 1) * 128], C.id_b.v)
                k.cp("act", cT[:, :, j * 128:(j + 1) * 128], pv[:, 0:384].re("p (c t) -> p c t", c=3))
                pvb = C.bank()
                k.mm(pvb[:, 0:256], cT[:, 2, j * 128:(j + 1) * 128], wv.v)
                k.cp("act", Vt[:, j, :], pvb[:, 0:256])
            p1 = C.bank()
            p2 = C.bank()
            for kc in range(8):
                k.mm(p1[64:96, 0:TG], wkr2[:, kc, 0:32], XT[:, kc, :], start=(kc == 0), stop=(kc == 7))
            for kc in range(8):
                k.mm(p2[64:96, 0:TG], wkr2[:, kc, 32:64], XT[:, kc, :], start=(kc == 0), stop=(kc == 7))
            t1 = r_t.next()
            t2 = r_t.next()
            k.tt("dve", t1[64:96, :], p1[64:96, 0:TG], cs[64:96, 0, tsl], ALU.mult)
            k.tt("dve", t2[64:96, :], p2[64:96, 0:TG], cs[64:96, 1, tsl], ALU.mult)
            k.tt("dve", kT[64:96, 0, tsl], t1[64:96, :], t2[64:96, :], ALU.add)
            for h in range(1, 4):
                k.cp("pool", kT[64:96, h, tsl], kT[64:96, 0, tsl])
            for h in range(4):
                pk = C.bank()
                k.mm(pk[0:64, 0:TG], wukv[:, h * 128:h * 128 + 64], cT[:, 2, tsl])
                k.cp("act", kT[0:64, h, tsl], pk[0:64, 0:TG])
                pq = C.bank()
                for c in range(2):
                    k.mm(pq[0:96, 0:TG], wuq[:, c, h * 96:(h + 1) * 96], cT[:, c, tsl], start=(c == 0), stop=(c == 1))
                pq2 = C.bank()
                for c in range(2):
                    k.mm(pq2[64:96, 0:TG], wuqs[:, c, h * 32:(h + 1) * 32], cT[:, c, tsl], start=(c == 0), stop=(c == 1))
                k.cp("act", qT[0:64, h, tsl], pq[0:64, 0:TG])
                t1 = r_t.next()
                t2 = r_t.next()
                k.tt("dve", t1[64:96, :], pq[64:96, 0:TG], cs[64:96, 0, tsl], ALU.mult)
                k.tt("dve", t2[64:96, :], pq2[64:96, 0:TG], cs[64:96, 1, tsl], ALU.mult)
                k.tt("dve", qT[64:96, h, tsl], t1[64:96, :], t2[64:96, :], ALU.add)
        YT = k.sb([128, 2, S], BF16, "mly")
        r_p = Ring(k, 4, [128, TG], BF16, "mlp")
        r_pf = Ring(k, 2, [128, TG], F32, "mlpf")
        r_d = Ring(k, 2, [128, TG], F32, "mld")
        C.pool = [0, 1, 2, 3]

        def unit(h, qg, slot):
            hp, bp = h // 2, (h % 2) * 64
            q0 = qg * TG
            qsl = slice(q0, q0 + TG)
            db = C.ps[4 + slot]
            yb = C.ps[6 + slot]
            last = (q0 + TG) // 128 - 1
            for kt in range(last + 1):
                zb = C.bank()
                k.mm(zb[:, 0:TG], kT[0:96, h, kt * 128:(kt + 1) * 128], qT[0:96, h, qsl])
                p = r_p.next()
                if kt * 128 >= q0:
                    r = (kt * 128 - q0) // 128
                    pf = r_pf.next()
                    k.act(pf.v, zb[:, 0:TG], AF.Exp, scale=SC)
                    k.tt("pool", p.v, pf.v, C.cam[r].v, ALU.mult)
                else:
                    k.act(p.v, zb[:, 0:TG], AF.Exp, scale=SC)
                yield
                k.I("pe", "matmul", out=yb[bp:bp + 64, 0:TG], lhsT=Vt[:, kt, h * 64:(h + 1) * 64], rhs=p.v,
                    start=(kt == 0), stop=(kt == last), skip_group_check=True)
                k.I("pe", "matmul", out=db[bp:bp + 64, 0:TG], lhsT=C.ones_b[:, 0:64], rhs=p.v,
                    start=(kt == 0), stop=(kt == last), skip_group_check=True)
            rd = r_d.next()
            k.I("dve", "reciprocal", out=rd[bp:bp + 64, :], in_=db[bp:bp + 64, 0:TG])
            k.tt("dve", YT[bp:bp + 64, hp, qsl], yb[bp:bp + 64, 0:TG], rd[bp:bp + 64, :], ALU.mult)
            yield

        units = [(h, qg) for qg in range(NTG) for h in range(4)]

        def stream(slot):
            for (h, qg) in units[slot::2]:
                yield from unit(h, qg, slot)

        interleave([stream(0), stream(1)])
        C.pool = list(range(8))
        k.dma(y_d.v, YT.v, q="pool")


def load_cols(k, dv, nchunk, name="pc", p=128):
    t = k.sb([128, nchunk], F32, name)
    for c in range(nchunk):
        k.dma(t[0:p, c:c + 1], dv[c * p:(c + 1) * p].re("(p o) -> p o", o=1))
    return t


RW_OFF = 768
EXPM05 = math.exp(-0.5)


def rwkv_phase(k, C, S, xt_d, win_d, prm, y_d, stop=None):
    RG = min(128, S)
    NRG = S // RG
    CH = 64
    NCH = RG // CH
    NQ = 14
    with k.phase():
        o64 = C.ones_f[0:64, 0:256].re("p (h s) -> p h s", h=4)
        ones64 = C.ones_b[0:64, 0:64]
        idb64 = C.id_b[0:64, 0:64]
        mLs = k.sb([64, 4, 64], F32, "rwLs")
        k.aselect(mLs.v, o64, [[0, 4], [-1, 64]], ALU.is_gt, 0.0, 0, 1)
        mUs = k.sb([64, 4, 64], F32, "rwUs")
        k.aselect(mUs.v, o64, [[0, 4], [1, 64]], ALU.is_gt, 0.0, 0, -1)
        mUi = k.sb([64, 4, 64], F32, "rwUi")
        k.aselect(mUi.v, o64, [[0, 4], [1, 64]], ALU.is_ge, 0.0, 0, -1)
        mI = k.sb([64, 4, 64], F32, "rwI")
        k.aselect(mI.v, o64, [[0, 4], [1, 64]], ALU.is_equal, 0.0, 0, -1)
        rmi = k.sb([64, 4 * RG], I32, "rwrmi")
        k.I("pool", "iota", out=rmi.v, pattern=[[1, 4 * RG]], base=0, channel_multiplier=0)
        k.ts("dve", rmi.v, rmi.v, CH - 1, ALU.bitwise_and)
        rmask = k.sb([64, 4 * RG], F32, "rwrm")
        k.cp("dve", rmask.v, rmi.v)
        k.ts("dve", rmask.v, rmask.v, 1.0, ALU.min)
        mu = load_cols(k, prm["mu"][0:896], NQ, "rwmu", p=64)
        mug = load_cols(k, prm["mu"][896:1024], 1, "rwmug")
        w0 = load_cols(k, prm["w0"], 4, "rww0", p=64)
        a0 = load_cols(k, prm["a0"], 4, "rwa0", p=64)
        kk_ = load_cols(k, prm["k_k"], 4, "rwkk", p=64)
        ka = load_cols(k, prm["k_a"], 4, "rwka", p=64)
        rk = load_cols(k, prm["r_k"], 4, "rwrk", p=64)
        gng = load_cols(k, prm["gn_g"], 4, "rwgg", p=64)
        gnb = load_cols(k, prm["gn_b"], 4, "rwgb", p=64)
        omka = k.sb([128, 4], F32, "rwomka")
        k.ts("dve", omka[0:64, :], ka[0:64, :], -1.0, ALU.mult, 1.0, ALU.add)
        eps_gn = k.sb([128, 1], F32, "rwepsgn")
        k.memset("pool", eps_gn.v, 64e-5)
        WL = WLoader(k, 8 * 512, nstage=1, nbuf=2, name="rww")
        wrw = [WL.load(win_d[:, hh * 512:(hh + 1) * 512], 8, 512) for hh in range(2)]
        lst = k.sb([128, 768], F32, "rwlst")
        k.dma(lst[0:64, 0:256], prm["w_up"])
        k.dma(lst[0:64, 256:512], prm["a_up"])
        k.dma(lst[:, 512:768], prm["g_up"])
        lup = k.sb([128, 768], BF16, "rwlup")
        k.cp("pool", lup[0:64, 0:512], lst[0:64, 0:512])
        k.cp("pool", lup[:, 512:768], lst[:, 512:768])
        ST = k.sb([64, 4, 64], F32, "rwST")
        k.memset("pool", ST.v, 0.0)
        carry = k.sb([64, NQ, 1], F32, "rwcar")
        k.memset("pool", carry.v, 0.0)
        carryg = k.sb([128, 1], F32, "rwcarg")
        k.memset("pool", carryg.v, 0.0)
        SG = min(512, S)
        GPS = SG // RG
        r_xt = Ring(k, 1, [128, 8, SG], BF16, "rwxt")
        pT = k.sb([64, NQ, SG + 1], F32, "rwpT")
        pG = k.sb([128, SG + 1], F32, "rwpG")
        ps = k.sb([64, NQ, RG], F32, "rwps")
        psg = k.sb([128, RG], F32, "rwpsg")
        tmp = Ring(k, 4, [64, 4, RG], F32, "rwtmp")
        tmp2 = Ring(k, 3, [64, 4, RG], F32, "rwtmp2")
        tmpg = k.sb([128, RG], F32, "rwtmpg")
        r_bf = Ring(k, 2, [128, RG], BF16, "rwbf")

        def arr(name):
            return k.sb([64, 4, RG], F32, name)

        A_a, A_kap, A_kp, A_lw, A_cl = arr("rwa"), arr("rwkap"), arr("rwkp"), arr("rwlw"), arr("rwcl")
        def arr16(name):
            return k.sb([64, 4, RG], BF16, name)

        A_kt, A_bt, A_kbar, A_bbar, A_y = arr16("rwkt"), arr16("rwbt"), arr16("rwkbar"), arr16("rwbbar"), arr("rwy")
        A_yb, Vb = arr16("rwyb"), arr16("rwvb")
        tmpb = Ring(k, 3, [64, 4, RG], BF16, "rwtmpb")
        STb = k.sb([64, 4, 64], BF16, "rwSTb")
        k.memset("pool", STb.v, 0.0)
        yout = Ring(k, 2, [64, 4, RG], BF16, "rwyo")
        sg = k.sb([128, RG], BF16, "rwsg")

        def cset(n):
            return [dict(G=[k.sb([64, 4, 64], BF16, "rwG%d" % i) for i in range(5)],
                         Pb=k.sb([64, 4, 64], BF16, "rwPb"),
                         N=[k.sb([64, 4, 64], BF16, "rwN%d" % i) for i in range(2)],
                         M=[k.sb([64, 4, 64], BF16, "rwM%d" % i) for i in range(2)],
                         P=[k.sb([64, 4, 64], BF16, "rwP%d" % i) for i in range(2)],
                         tok=[k.sb([64, 4, 64], BF16, "rwtok%d" % i) for i in range(3)]) for _ in range(n)]

        def mkset():
            return dict(A_rt=arr16("rwrt"), A_kapt=arr16("rwkapt"), A_g=arr("rwg"), A_bon=arr("rwbon"),
                        gC=k.sb([64, 4, NCH], F32, "rwgC"), CS=cset(NCH), curP=[None] * NCH)

        SETS = [mkset(), mkset()]
        Wt = k.sb([64, 4, 64], BF16, "rwW")
        Ut = k.sb([64, 4, 64], BF16, "rwU")

        def v4(bank):
            return bank[0:64, 0:256].re("p (h s) -> p h s", h=4)

        def fm(Aarr, h, ch):
            return Aarr[:, h, ch * CH:(ch + 1) * CH]

        def gen_AB(rg, B):
            A_rt, A_kapt, A_g, A_bon, gC, CS = B["A_rt"], B["A_kapt"], B["A_g"], B["A_bon"], B["gC"], B["CS"]
            tsl = slice(rg * RG, (rg + 1) * RG)
            o = (rg % GPS) * RG
            if rg % GPS == 0:
                XT = r_xt.next()
                k.dma(XT.v, xt_d[:, :, rg * RG:rg * RG + SG])
                k.cp("pool", pT[:, :, 0:1], carry.v)
                k.cp("pool", pG[:, 0:1], carryg.v)
                for q in range(NQ):
                    c0 = q * 64
                    pb = C.bank()
                    for kc in range(8):
                        k.mm(pb[0:64, 0:SG], wrw[c0 // 512][:, kc, c0 % 512:c0 % 512 + 64], XT[:, kc, :],
                             start=(kc == 0), stop=(kc == 7))
                    k.cp("act", pT[:, q, 1:SG + 1], pb[0:64, 0:SG])
                    if q % 2 == 1:
                        yield
                pb = C.bank()
                for kc in range(8):
                    k.mm(pb[:, 0:SG], wrw[1][:, kc, 384:512], XT[:, kc, :], start=(kc == 0), stop=(kc == 7))
                k.cp("act", pG[:, 1:SG + 1], pb[:, 0:SG])
                k.cp("pool", carry.v, pT[:, :, SG:SG + 1])
                k.cp("pool", carryg.v, pG[:, SG:SG + 1])
                yield
            for q0 in range(0, NQ, 4):
                nq = min(4, NQ - q0)
                t = tmp.next()
                k.tt("pool", t[:, 0:nq, :], pT[:, q0:q0 + nq, o:o + RG], pT[:, q0:q0 + nq, o + 1:o + RG + 1], ALU.subtract)
                for q in range(q0, q0 + nq):
                    k.stt(ps[:, q, :], t[:, q - q0, :], mu[0:64, q:q + 1], pT[:, q, o + 1:o + RG + 1], ALU.mult, ALU.add)
                yield
            k.tt("pool", tmpg.v, pG[:, o:o + RG], pG[:, o + 1:o + RG + 1], ALU.subtract)
            k.stt(psg.v, tmpg.v, mug[:, 0:1], pG[:, o + 1:o + RG + 1], ALU.mult, ALU.add)
            R_, K_, V_ = ps[:, 0:4, :], ps[:, 4:8, :], ps[:, 8:12, :]
            tw = r_bf.next()
            k.act(tw[0:64, :], ps[:, 12, :], AF.Tanh)
            alo = r_bf.next()
            k.cp("pool", alo[0:64, :], ps[:, 13, :])
            k.act(sg.v, psg.v, AF.Sigmoid)
            yield
            for hq in range(2):
                pw = C.bank()
                pa = C.bank()
                pg = C.bank()
                for hh in range(2):
                    h = hq * 2 + hh
                    k.mm(pw[0:64, hh * RG:(hh + 1) * RG], lup[0:64, h * 64:(h + 1) * 64], tw[0:64, :])
                    k.mm(pa[0:64, hh * RG:(hh + 1) * RG], lup[0:64, 256 + h * 64:256 + (h + 1) * 64], alo[0:64, :])
                    k.mm(pg[0:64, hh * RG:(hh + 1) * RG], lup[:, 512 + h * 64:512 + (h + 1) * 64], sg.v)
                for hh in range(2):
                    h = hq * 2 + hh
                    k.act(A_lw[:, h, :], pw[0:64, hh * RG:(hh + 1) * RG], AF.Sigmoid, bias=w0[0:64, h:h + 1])
                    k.act(A_a[:, h, :], pa[0:64, hh * RG:(hh + 1) * RG], AF.Sigmoid, bias=a0[0:64, h:h + 1])
                    k.cp("act", A_g[:, h, :], pg[0:64, hh * RG:(hh + 1) * RG])
                yield
            k.ts("pool", A_lw.v, A_lw.v, -EXPM05, ALU.mult)
            kkv = tmp.next()
            for h in range(4):
                k.ts("dve", kkv[:, h, :], K_[:, h, :], kk_[0:64, h:h + 1], ALU.mult)
            sq = tmpb.next()
            k.tt("pool", sq.v, kkv.v, kkv.v, ALU.mult)
            k.cp("pool", Vb.v, V_)
            yield
            rs = tmp.next()
            for hq in range(2):
                pss = C.bank()
                for hh in range(2):
                    k.mm(pss[0:64, hh * RG:(hh + 1) * RG], ones64, sq[:, hq * 2 + hh, :])
                k.ts("dve", rs[:, hq * 2:hq * 2 + 2, :], pss[0:64, 0:2 * RG].re("p (h t) -> p h t", h=2), 1e-24, ALU.max)
            k.act(rs.v, rs.v, AF.Ln)
            k.act(rs.v, rs.v, AF.Exp, scale=-0.5)
            k.tt("dve", A_kap.v, kkv.v, rs.v, ALU.mult)
            yield
            k.I("dve", "tensor_tensor_scan", out=A_cl.v.re("p h t -> p (h t)"), data0=rmask.v,
                data1=A_lw.v.re("p h t -> p (h t)"), initial=0.0, op0=ALU.mult, op1=ALU.add)
            t = tmp.next()
            for h in range(4):
                k.ts("dve", t[:, h, :], A_a[:, h, :], ka[0:64, h:h + 1], ALU.mult, omka[0:64, h:h + 1], ALU.add)
            k.tt("pool", A_kp.v, K_, t.v, ALU.mult)
            yield
            t2 = tmpb.next()
            for h in range(4):
                k.stt(t2[:, h, :], R_[:, h, :], rk[0:64, h:h + 1], A_kp[:, h, :], ALU.mult, ALU.mult)
            for hq in range(2):
                pbn = C.bank()
                for hh in range(2):
                    k.mm(pbn[0:64, hh * RG:(hh + 1) * RG], ones64, t2[:, hq * 2 + hh, :])
                k.tt("dve", A_bon[:, hq * 2:hq * 2 + 2, :], pbn[0:64, 0:2 * RG].re("p (h t) -> p h t", h=2),
                     V_[:, hq * 2:hq * 2 + 2, :], ALU.mult)
            yield
            ep = tmp.next()
            k.act(ep.v, A_cl.v, AF.Exp)
            k.tt("pool", A_rt.v, R_, ep.v, ALU.mult)
            en = tmp.next()
            k.act(en.v, A_cl.v, AF.Exp, scale=-1.0)
            k.tt("dve", A_kt.v, A_kp.v, en.v, ALU.mult)
            b_ = tmp.next()
            k.tt("pool", b_.v, A_kap.v, A_a.v, ALU.mult)
            k.tt("dve", A_bt.v, b_.v, en.v, ALU.mult)
            yield
            t3 = tmp.next()
            k.tt("pool", t3.v, A_cl.v, A_lw.v, ALU.subtract)
            k.act(t3.v, t3.v, AF.Exp)
            k.tt("dve", A_kapt.v, A_kap.v, t3.v, ALU.mult)
            clv = A_cl.v.re("p h (c t) -> p (h c) t", t=CH)
            t4 = tmp.next()
            t4v = t4.v.re("p h (c t) -> p (h c) t", t=CH)
            k.tt("pool", t4v, clv[:, :, CH - 1:CH].bro([64, 4 * NCH, CH]), clv, ALU.subtract)
            k.act(t4.v, t4.v, AF.Exp)
            k.tt("dve", A_kbar.v, A_kp.v, t4.v, ALU.mult)
            k.tt("pool", A_bbar.v, b_.v, t4.v, ALU.mult)
            k.act(gC.v.re("p h c -> p (h c)"), clv[:, :, CH - 1], AF.Exp)
            yield
            for ch in range(NCH):
                cs = CS[ch]
                specs = [(A_kapt, A_bt, mLs, 1.0), (A_bt, A_kapt, mUs, 1.0), (A_kt, A_kapt, mUs, 1.0),
                         (A_kt, A_rt, mUi, 1.0), (A_bt, A_rt, mUi, -1.0)]
                for gi, (La, Ra, msk, sgn) in enumerate(specs):
                    pb = C.bank()
                    for h in range(4):
                        k.mm(pb[0:64, h * 64:(h + 1) * 64], fm(La, h, ch), fm(Ra, h, ch))
                    if sgn == 1.0:
                        k.tt("dve", cs["G"][gi].v, v4(pb), msk.v, ALU.mult)
                    else:
                        k.stt(cs["G"][gi].v, v4(pb), sgn, msk.v, ALU.mult, ALU.mult)
                    yield
                k.tt("pool", cs["P"][0].v, mI.v, cs["G"][1].v, ALU.subtract)
                for ti, sgn in enumerate([1.0, 1.0, -1.0]):
                    pbb = C.bank().v.bc(BF16)
                    for h in range(4):
                        src = fm(Vb if ti == 0 else (A_kbar if ti == 1 else A_bbar), h, ch)
                        k.tr(pbb[0:64, h * 64:(h + 1) * 64], src, idb64)
                    pv_ = pbb[0:64, 0:256].re("p (h s) -> p h s", h=4)
                    if sgn == 1.0:
                        k.cp("act", cs["tok"][ti].v, pv_)
                    else:
                        k.act(cs["tok"][ti].v, pv_, AF.Copy, scale=-1.0)
                    yield
            curN = [CS[ch]["G"][0] for ch in range(NCH)]
            curM = [CS[ch]["G"][1] for ch in range(NCH)]
            curP = [CS[ch]["P"][0] for ch in range(NCH)]
            for st in range(5):
                lastst = st == 4
                for ch in range(NCH):
                    cs = CS[ch]
                    Nn, Mn, Pn = cs["N"][st % 2], cs["M"][st % 2], cs["P"][(st + 1) % 2]
                    pb = C.bank()
                    for h in range(4):
                        k.mm(pb[0:64, h * 64:(h + 1) * 64], curM[ch][:, h, :], curN[ch][:, h, :])
                    k.cp("act", Nn.v, v4(pb))
                    if not lastst:
                        pb2 = C.bank()
                        for h in range(4):
                            k.mm(pb2[0:64, h * 64:(h + 1) * 64], curN[ch][:, h, :], curM[ch][:, h, :])
                        k.cp("act", Mn.v, v4(pb2))
                    pb3 = C.bank()
                    for h in range(4):
                        k.mm(pb3[0:64, h * 64:(h + 1) * 64], Nn[:, h, :], curP[ch][:, h, :])
                    if lastst:
                        k.tt("dve", cs["Pb"].v, v4(pb3), curP[ch].v, ALU.add)
                        Pn = cs["Pb"]
                    else:
                        k.tt("dve", Pn.v, v4(pb3), curP[ch].v, ALU.add)
                    curN[ch], curM[ch], curP[ch] = Nn, Mn, Pn
                    yield
            B["curP"] = curP

        def gen_CD(rg, B):
            A_rt, A_kapt, A_g, A_bon, gC, CS = B["A_rt"], B["A_kapt"], B["A_g"], B["A_bon"], B["gC"], B["CS"]
            curP = B["curP"]
            tsl = slice(rg * RG, (rg + 1) * RG)
            for ch in range(NCH):
                cs = CS[ch]
                TT = curP[ch]
                Vt_, KB, BBn = cs["tok"]
                AkkT, ArkT, ArbTn = cs["G"][2], cs["G"][3], cs["G"][4]
                pw = C.bank()
                for h in range(4):
                    k.I("pe", "matmul", out=pw[0:64, h * 64:(h + 1) * 64], lhsT=fm(A_kapt, h, ch), rhs=STb[:, h, :],
                        start=True, stop=False, skip_group_check=True)
                    k.I("pe", "matmul", out=pw[0:64, h * 64:(h + 1) * 64], lhsT=AkkT[:, h, :], rhs=Vt_[:, h, :],
                        start=False, stop=True, skip_group_check=True)
                k.cp("act", Wt.v, v4(pw))
                yield
                pu = C.bank()
                for h in range(4):
                    k.mm(pu[0:64, h * 64:(h + 1) * 64], TT[:, h, :], Wt[:, h, :])
                k.cp("act", Ut.v, v4(pu))
                yield
                py = C.bank()
                pst = C.bank()
                for h in range(4):
                    yo = py[0:64, h * 64:(h + 1) * 64]
                    k.I("pe", "matmul", out=yo, lhsT=STb[:, h, :], rhs=fm(A_rt, h, ch), start=True, stop=False,
                        skip_group_check=True)
                    k.I("pe", "matmul", out=yo, lhsT=Vt_[:, h, :], rhs=ArkT[:, h, :], start=False, stop=False,
                        skip_group_check=True)
                    k.I("pe", "matmul", out=yo, lhsT=Ut[:, h, :], rhs=ArbTn[:, h, :], start=False, stop=True,
                        skip_group_check=True)
                    so = pst[0:64, h * 64:(h + 1) * 64]
                    k.I("pe", "matmul", out=so, lhsT=KB[:, h, :], rhs=Vt_[:, h, :], start=True, stop=False,
                        skip_group_check=True)
                    k.I("pe", "matmul", out=so, lhsT=BBn[:, h, :], rhs=Ut[:, h, :], start=False, stop=True,
                        skip_group_check=True)
                k.cp("act", A_y[:, :, ch * CH:(ch + 1) * CH], v4(py))
                for h in range(4):
                    k.stt(ST[:, h, :], ST[:, h, :], gC[:, h, ch:ch + 1], pst[0:64, h * 64:(h + 1) * 64], ALU.mult, ALU.add)
                k.cp("pool", STb.v, ST.v)
                yield
            yo_t = yout.next()
            yc = tmp2.next()
            k.cp("pool", A_yb.v, A_y.v)
            for hq in range(2):
                pm = C.bank()
                for hh in range(2):
                    k.mm(pm[0:64, hh * RG:(hh + 1) * RG], ones64, A_yb[:, hq * 2 + hh, :])
                k.stt(yc[:, hq * 2:hq * 2 + 2, :], pm[0:64, 0:2 * RG].re("p (h t) -> p h t", h=2), -1.0 / 64,
                      A_y[:, hq * 2:hq * 2 + 2, :], ALU.mult, ALU.add)
            sq = tmpb.next()
            k.tt("pool", sq.v, yc.v, yc.v, ALU.mult)
            yield
            rs = tmp2.next()
            for hq in range(2):
                pvv = C.bank()
                for hh in range(2):
                    k.mm(pvv[0:64, hh * RG:(hh + 1) * RG], ones64, sq[:, hq * 2 + hh, :])
                k.act(rs[:, hq * 2:hq * 2 + 2, :], pvv[0:64, 0:2 * RG].re("p (h t) -> p h t", h=2), AF.Ln, scale=1.0 / 64,
                      bias=eps_gn[0:64, 0:1])
            k.act(rs.v, rs.v, AF.Exp, scale=-0.5)
            k.tt("dve", yc.v, yc.v, rs.v, ALU.mult)
            yield
            for h in range(4):
                k.ts("dve", yc[:, h, :], yc[:, h, :], gng[0:64, h:h + 1], ALU.mult, gnb[0:64, h:h + 1], ALU.add)
            k.tt("pool", yc.v, yc.v, A_bon.v, ALU.add)
            k.tt("pool", yo_t.v, yc.v, A_g.v, ALU.mult)
            for h in range(4):
                k.dma(y_d[(h % 2) * 64:(h % 2) * 64 + 64, h // 2, tsl], yo_t[:, h, :], q="pool")
            yield

        for step in range(NRG + 1):
            gens = []
            if step < NRG:
                gens.append(gen_AB(step, SETS[step % 2]))
            if step >= 1:
                gens.append(gen_CD(step - 1, SETS[(step - 1) % 2]))
            interleave(gens)


SSD_OFF = 768 + 1024


def ssd_phase(k, C, S, xt_d, win_d, prm, y_d):
    TG = min(512, S)
    NTG = S // TG
    TPG = TG // 128
    with k.phase():
        Ui = k.sb([128, 128], F32, "sdUi")
        k.aselect(Ui.v, C.ones_f[:, 0:128], [[1, 128]], ALU.is_ge, 0.0, 0, -1)
        WL = WLoader(k, 8 * 512, nstage=2, nbuf=2, name="sdw")
        w_zx = WL.load(win_d[:, 0:512], 8, 512)
        w_bc = WL.load(win_d[:, 512:1024], 8, 512)
        st = k.sb([128, 8, 4], F32, "sdst")
        k.dma(st.v, win_d[:, 1024:1028].re("(c p) n -> p c n", p=128))
        w_dt = k.sb([128, 8, 4], BF16, "sdwdt")
        k.cp("pool", w_dt.v, st.v)
        cw = k.sb([128, 6, 4], F32, "sdcw")
        for n in range(6):
            for j in range(4):
                k.dma(cw[:, n, j:j + 1], prm["conv_w"][j, n * 128:(n + 1) * 128].re("(p o) -> p o", o=1))
        cb = load_cols(k, prm["conv_b"], 6, "sdcb")
        dtb = load_bcast(k, prm["dt_bias"], 4, "sddtb")
        alog = load_bcast(k, prm["a_log"], 4, "sdalog")
        dsk = load_bcast(k, prm["d"], 4, "sddsk")
        ng = load_bcast(k, prm["norm_g"], 256, "sdng")
        aneg = k.sb([128, 4], F32, "sdaneg")
        k.act(aneg.v, alog.v, AF.Exp)
        k.ts("dve", aneg.v, aneg.v, -1.0, ALU.mult)
        eps5g = C.eps5
        Sst = k.sb([128, 4, 64], F32, "sdS")
        k.memset("pool", Sst.v, 0.0)
        Sbf = k.sb([128, 4, 64], BF16, "sdSb")
        k.memset("pool", Sbf.v, 0.0)
        xraw = k.sb([128, 6, TG + 3], F32, "sdxr")
        k.memset("pool", xraw[:, :, 0:3], 0.0)
        xc = k.sb([128, 6, TG], F32, "sdxc")
        bcb = k.sb([128, 4, TG], BF16, "sdbcb")
        r_xt = Ring(k, 2, [128, 8, TG], BF16, "sdxt")
        r_acc = Ring(k, 2, [128, TG], F32, "sdacc")
        r_s = Ring(k, 4, [128, 48], F32, "sds")
        r_m = Ring(k, 8, [128, 128], F32, "sdm")
        r_mb = Ring(k, 20, [128, 128], BF16, "sdmb")
        r_tok = Ring(k, 3, [128, 512], F32, "sdtok")
        r_tb = Ring(k, 3, [128, 768], BF16, "sdtb")
        r_y = Ring(k, 3, [128, 256], F32, "sdy")
        r_z = Ring(k, 3, [128, 256], F32, "sdz")
        r_gt = Ring(k, 3, [128, 256], F32, "sdgt")
        r_yb = Ring(k, 3, [128, 256], BF16, "sdyb")
        r_yo = Ring(k, 2, [128, 2, TG], BF16, "sdyo")
        for tg in range(NTG):
            tsl = slice(tg * TG, (tg + 1) * TG)
            XT = r_xt.next()
            k.dma(XT.v, xt_d[:, :, tsl])
            for n in range(6):
                pb = C.bank()
                wsl = w_zx[:, :, 256 + n * 128:256 + (n + 1) * 128] if n < 2 else w_bc[:, :, (n - 2) * 128:(n - 1) * 128]
                for kc in range(8):
                    k.mm(pb[:, 0:TG], wsl[:, kc, :], XT[:, kc, :], start=(kc == 0), stop=(kc == 7))
                k.cp("act", xraw[:, n, 3:TG + 3], pb[:, 0:TG])
                acc = r_acc.next()
                k.ts("dve", acc.v, xraw[:, n, 3:TG + 3], cw[:, n, 3:4], ALU.mult, cb[:, n:n + 1], ALU.add)
                for j in (2, 1, 0):
                    k.stt(acc.v, xraw[:, n, j:TG + j], cw[:, n, j:j + 1], acc.v, ALU.mult, ALU.add)
                k.act(xc[:, n, :], acc.v, AF.Silu)
                if n >= 2:
                    k.cp("pool", bcb[:, n - 2, :], xc[:, n, :])
            yo = r_yo.next()

            def tile(jj, slot, XT=XT, yo=yo):
                lsl = slice(jj * 128, (jj + 1) * 128)
                s = r_s.next()
                py = C.ps[4 + 2 * slot]
                pst = C.ps[5 + 2 * slot]
                pd = C.bank()
                for kc in range(8):
                    k.mm(pd[:, 0:4], XT[:, kc, lsl], w_dt[:, kc, :], start=(kc == 0), stop=(kc == 7))
                k.tt("dve", s[:, 0:4], pd[:, 0:4], dtb.v, ALU.add)
                k.act(s[:, 0:4], s[:, 0:4], AF.Exp)
                k.act(s[:, 4:8], s[:, 0:4], AF.Ln, bias=C.one[:, 0:1])
                k.tt("dve", s[:, 8:12], s[:, 4:8], aneg.v, ALU.mult)
                pc = C.bank()
                k.mm(pc[:, 0:4], Ui.v, s[:, 8:12])
                k.mm(pc[:, 8:12], C.ones_f[:, 0:128], s[:, 8:12])
                k.cp("dve", s[:, 12:16], pc[:, 0:4])
                k.cp("dve", s[:, 16:20], pc[:, 8:12])
                yield
                k.tt("dve", s[:, 20:24], s[:, 16:20], s[:, 12:16], ALU.subtract)
                k.act(s[:, 20:24], s[:, 20:24], AF.Exp)
                k.act(s[:, 24:28], s[:, 16:20], AF.Exp)
                pz = C.bank()
                for kc in range(8):
                    k.mm(pz[:, 0:256], XT[:, kc, lsl], w_zx[:, kc, 0:256], start=(kc == 0), stop=(kc == 7))
                zs = r_z.next()
                k.act(zs.v, pz[:, 0:256], AF.Silu)
                yield
                pt = C.bank()
                for n in range(4):
                    k.tr(pt[:, n * 128:(n + 1) * 128], xc[:, n, lsl], C.id_f.v)
                tok = r_tok.next()
                k.cp("act", tok.v, pt.v)
                yield
                tb = r_tb.next()
                for h in range(4):
                    k.ts("dve", tb[:, h * 64:(h + 1) * 64], tok[:, h * 64:(h + 1) * 64], s[:, 4 + h:5 + h], ALU.mult)
                    k.stt(tb[:, 512 + h * 64:512 + (h + 1) * 64], tok[:, h * 64:(h + 1) * 64], s[:, 4 + h:5 + h],
                          s[:, 20 + h:21 + h].bro([128, 64]), ALU.mult, ALU.mult)
                k.cp("pool", tb[:, 256:512], tok[:, 256:512])
                pg = C.bank()
                for g in range(2):
                    k.mm(pg[:, g * 128:(g + 1) * 128], bcb[:, g, lsl], bcb[:, 2 + g, lsl])
                gts = r_gt.next()
                k.cp("act", gts.v, pg[:, 0:256])
                yield
                csts = []
                for h in range(4):
                    g = h // 2
                    abc = r_m.next()
                    k.ts("dve", abc.v, C.ones_f[:, 0:128], s[:, 8 + h:9 + h], ALU.mult)
                    pr = C.bank()
                    k.mm(pr[:, 0:128], abc.v, Ui.v)
                    dm = r_m.next()
                    k.ts("dve", dm.v, pr[:, 0:128], s[:, 12 + h:13 + h], ALU.subtract, 0.0, ALU.min)
                    k.act(dm.v, dm.v, AF.Exp)
                    k.tt("dve", dm.v, dm.v, gts[:, g * 128:(g + 1) * 128], ALU.mult)
                    mh = r_mb.next()
                    k.tt("pool", mh.v, dm.v, Ui.v, ALU.mult)
                    er = r_m.next()
                    k.act(er.v, pr[:, 0:128], AF.Exp)
                    cst = r_mb.next()
                    k.tt("pool", cst.v, xc[:, 4 + g, lsl], er.v, ALU.mult)
                    csts.append(cst)
                    k.I("pe", "matmul", out=py[:, h * 64:(h + 1) * 64], lhsT=mh.v, rhs=tb[:, h * 64:(h + 1) * 64],
                        start=(h == 0), stop=False, skip_group_check=True)
                    k.mm(pst[:, h * 64:(h + 1) * 64], tb[:, 256 + g * 128:256 + (g + 1) * 128],
                         tb[:, 512 + h * 64:512 + (h + 1) * 64])
                    yield
                for h in range(4):
                    k.I("pe", "matmul", out=py[:, h * 64:(h + 1) * 64], lhsT=csts[h].v, rhs=Sbf[:, h, :],
                        start=False, stop=True, skip_group_check=True)
                for h in range(4):
                    k.stt(Sst[:, h, :], Sst[:, h, :], s[:, 24 + h:25 + h], pst[:, h * 64:(h + 1) * 64], ALU.mult, ALU.add)
                k.cp("pool", Sbf.v, Sst.v)
                y = r_y.next()
                for h in range(4):
                    k.stt(y[:, h * 64:(h + 1) * 64], tok[:, h * 64:(h + 1) * 64], dsk[:, h:h + 1], py[:, h * 64:(h + 1) * 64],
                          ALU.mult, ALU.add)
                yield
                k.tt("dve", y.v, y.v, zs.v, ALU.mult)
                for g in range(2):
                    k.act(zs[:, g * 128:(g + 1) * 128], y[:, g * 128:(g + 1) * 128], AF.Square, accum_out=s[:, 28 + g:29 + g])
                k.act(s[:, 30:32], s[:, 28:30], AF.Ln, scale=1.0 / 128, bias=eps5g[:, 0:1])
                k.act(s[:, 32:34], s[:, 30:32], AF.Exp, scale=-0.5)
                yb = r_yb.next()
                for g in range(2):
                    k.stt(yb[:, g * 128:(g + 1) * 128], y[:, g * 128:(g + 1) * 128], s[:, 32 + g:33 + g],
                          ng[:, g * 128:(g + 1) * 128], ALU.mult, ALU.mult)
                yield
                pv = C.bank().v.bc(BF16)
                for c in range(2):
                    k.tr(pv[:, c * 128:(c + 1) * 128], yb[:, c * 128:(c + 1) * 128], C.id_b.v)
                k.cp("act", yo[:, :, lsl], pv[:, 0:256].re("p (c t) -> p c t", c=2))
                yield

            def stream(par):
                if par == 1:
                    yield
                for jj in range(par, TPG, 2):
                    yield from tile(jj, par)

            C.pool = [0, 1, 2, 3]
            interleave([stream(0), stream(1)])
            C.pool = list(range(8))
            k.cp("pool", xraw[:, :, 0:3], xraw[:, :, TG:TG + 3])
            k.dma(y_d[:, :, tsl], yo.v, q="pool")


IN_SPECS = [
    ("mix_w_in", [2, 1024, 3236]), ("rw_mu", [2, 1024]), ("rw_w0", [2, 256]), ("rw_w_up", [2, 64, 256]),
    ("rw_a0", [2, 256]), ("rw_a_up", [2, 64, 256]), ("rw_g_up", [2, 128, 256]), ("rw_k_k", [2, 256]),
    ("rw_k_a", [2, 256]), ("rw_r_k", [2, 4, 64]), ("rw_gn_g", [2, 256]), ("rw_gn_b", [2, 256]),
    ("ssd_conv_w", [2, 4, 768]), ("ssd_conv_b", [2, 768]), ("ssd_dt_bias", [2, 4]), ("ssd_a_log", [2, 4]),
    ("ssd_d", [2, 4]), ("ssd_norm_g", [2, 256]), ("mla_q_norm_g", [2, 256]), ("mla_w_uq", [2, 256, 384]),
    ("mla_kv_norm_g", [2, 128]), ("mla_w_ukv", [2, 128, 512]), ("mix_w_br_out", [2, 4, 256, 1024]),
    ("mix_w_gate", [2, 4, 1024, 1024]), ("mix_w_out", [2, 1024, 1024]), ("ln1_g", [2, 1024]), ("ln1_b", [2, 1024]),
    ("xa_w_q", [2, 1024, 1024]), ("xa_w_kv", [2, 1024, 2048]), ("xa_w_o", [2, 1024, 1024]), ("ln2_g", [2, 1024]),
    ("ln2_b", [2, 1024]), ("ffn_w13", [1, 1024, 5632]), ("ffn_w2", [1, 2816, 1024]), ("moe_router", [1, 1024, 8]),
    ("moe_w13", [1, 8, 1024, 7168]), ("moe_w2", [1, 8, 3584, 1024]), ("ln3_g", [2, 1024]), ("ln3_b", [2, 1024]),
]


def build_program(S, NB, L=2):
    nc = bass.Bass("TRN2", target_bir_lowering=False)
    k = K(nc)
    x = k.dram("x", [NB, S, 1024], F32, kind="ExternalInput")
    mem = k.dram("mem", [NB, 256, 1024], F32, kind="ExternalInput")
    pos = k.dram("positions", [NB, S], I32, kind="ExternalInput")
    W = {n: k.dram(n, shp, F32, kind="ExternalInput") for n, shp in IN_SPECS}
    out = k.dram("out", [NB, S, 1024], F32, kind="ExternalOutput")
    xt = k.dram("s_xt", [128, 8, S], BF16)
    Xa = k.dram("s_xa", [S, 1024], F32)
    Xb = k.dram("s_xb", [S, 1024], F32)
    Xc = k.dram("s_xc", [S, 1024], F32)
    U = k.dram("s_u", [S, 1024], F32)
    ybr = [k.dram("s_y%d" % i, [128, 2, S], BF16) for i in range(4)]
    mT = k.dram("s_mt", [128, 8, S], BF16)
    rope = k.dram("s_rope", [2, 128, S], F32)
    C = Consts(k)
    sb_consts(k, C, min(512, S))
    for b in range(NB):
        rope_phase(k, C, S, pos[b], rope)
        xt0_phase(k, C, S, x[b], xt)
        xcur = x[b]
        for l in range(L):
            win = W["mix_w_in"][l]
            sb_phase(k, C, S, xt, win[:, 0:768], ybr[0])
            rprm = dict(mu=W["rw_mu"][l], w0=W["rw_w0"][l], w_up=W["rw_w_up"][l], a0=W["rw_a0"][l], a_up=W["rw_a_up"][l],
                        g_up=W["rw_g_up"][l], k_k=W["rw_k_k"][l], k_a=W["rw_k_a"][l],
                        r_k=W["rw_r_k"][l].re("h n -> (h n)"), gn_g=W["rw_gn_g"][l], gn_b=W["rw_gn_b"][l])
            rwkv_phase(k, C, S, xt, win[:, RW_OFF:RW_OFF + 1024], rprm, ybr[1])
            sprm = dict(conv_w=W["ssd_conv_w"][l], conv_b=W["ssd_conv_b"][l], dt_bias=W["ssd_dt_bias"][l],
                        a_log=W["ssd_a_log"][l], d=W["ssd_d"][l], norm_g=W["ssd_norm_g"][l])
            ssd_phase(k, C, S, xt, win[:, SSD_OFF:SSD_OFF + 1028], sprm, ybr[2])
            mla_phase(k, C, S, xt, win[:, MLA_OFF:MLA_OFF + 416], W["mla_q_norm_g"][l], W["mla_w_uq"][l],
                      W["mla_kv_norm_g"][l], W["mla_w_ukv"][l], rope, ybr[3])
            gate_phase(k, C, S, xt, ybr, W["mix_w_gate"][l], W["mix_w_br_out"][l], mT)
            outproj_phase(k, C, S, mT, 8, W["mix_w_out"][l], U)
            ln_phase(k, C, S, U, xcur, W["ln1_g"][l], W["ln1_b"][l], Xa, xt)
            xattn_phase(k, C, S, xt, mem[b], W["xa_w_q"][l], W["xa_w_kv"][l], W["xa_w_o"][l], U)
            ln_phase(k, C, S, U, Xa.v, W["ln2_g"][l], W["ln2_b"][l], Xb, xt)
            if l % 2 == 0:
                ffn_phase(k, C, S, xt, Xb.v, U, [(W["ffn_w13"][l // 2], W["ffn_w2"][l // 2], 2816)])
            else:
                ex = [(W["moe_w13"][l // 2, e], W["moe_w2"][l // 2, e], 3584) for e in range(8)]
                ffn_phase(k, C, S, xt, Xb.v, U, ex, router_d=W["moe_router"][l // 2])
            last = l == L - 1
            ln_phase(k, C, S, U, Xb.v, W["ln3_g"][l], W["ln3_b"][l], out[b] if last else Xc, xt)
            xcur = Xc.v
    k.barrier()
    return nc, k


def kernel(**inputs):
    B, S = inputs["x"].shape[0], inputs["x"].shape[1]
    NB = B // NCORES
    nc, _ = build_program(S, NB)
    shared = {n: np.ascontiguousarray(inputs[n], dtype=np.float32) for n, _ in IN_SPECS}
    in_maps = []
    for c in range(NCORES):
        sl = slice(c * NB, (c + 1) * NB)
        m = dict(shared)
        m["x"] = np.ascontiguousarray(inputs["x"][sl], dtype=np.float32)
        m["mem"] = np.ascontiguousarray(inputs["mem"][sl], dtype=np.float32)
        m["positions"] = np.ascontiguousarray(inputs["positions"][sl], dtype=np.int32)
        in_maps.append(m)
    res = run_bass_kernel_spmd(nc, in_maps, core_ids=list(range(NCORES)))
    return np.concatenate([np.asarray(r["out"]) for r in res.results], axis=0).astype(np.float32)
```

```python
import math
from contextlib import ExitStack
import numpy as np
import concourse.bass as bass
import concourse.mybir as mybir
from concourse.bass_utils import run_bass_kernel_spmd

F32 = mybir.dt.float32
BF16 = mybir.dt.bfloat16
I32 = mybir.dt.int32
AF = mybir.ActivationFunctionType
ALU = mybir.AluOpType
AX = mybir.AxisListType

D = 1024
NCORES = 8
WRITE_KW = ("out", "accum_out")


class T:
    def __init__(self, ap, name):
        self.ap = ap
        self.name = name
        self.w = None
        self.r = {}

    def __getitem__(self, idx):
        return V(self, self.ap[idx])

    @property
    def v(self):
        return V(self, self.ap)


class V:
    def __init__(self, t, ap):
        self.t = t
        self.ap = ap

    def __getitem__(self, idx):
        return V(self.t, self.ap[idx])

    def bc(self, dt):
        return V(self.t, self.ap.bitcast(dt))

    def re(self, pat, **kw):
        return V(self.t, self.ap.rearrange(pat, **kw))

    def bro(self, shape):
        return V(self.t, self.ap.to_broadcast(shape))

    def pbro(self, n):
        return V(self.t, self.ap.partition_broadcast(n))


class K:
    COMPUTE = ("pe", "act", "dve", "pool")

    def __init__(self, nc):
        self.nc = nc
        self.es = ExitStack()
        self.engs = {"pe": nc.tensor, "act": nc.scalar, "dve": nc.vector, "pool": nc.gpsimd, "sp": nc.sync}
        self.sems = {}
        self.cnt = {}
        for e in self.COMPUTE:
            self.sems[e] = self.es.enter_context(nc.semaphore("s_" + e))
            self.cnt[e] = 0
        self.dq = {}
        for q in ("sp", "pool", "act"):
            lst = []
            for i in range(6):
                key = "d_%s%d" % (q, i)
                self.sems[key] = self.es.enter_context(nc.semaphore(key))
                self.cnt[key] = 0
                lst.append(key)
            self.dq[q] = [lst, 0]
        self.seen = {e: {} for e in self.engs}
        self.n_ins = 0
        self.phase_es = None
        self.uid = 0

    def sb(self, shape, dt, name=None):
        self.uid += 1
        name = "%s_%d" % (name or "t", self.uid)
        h = (self.phase_es or self.es).enter_context(self.nc.sbuf_tensor(name, list(shape), dt))
        return T(h[:], name)

    def sbg(self, shape, dt, name=None):
        self.uid += 1
        name = "%s_%d" % (name or "g", self.uid)
        h = self.es.enter_context(self.nc.sbuf_tensor(name, list(shape), dt))
        return T(h[:], name)

    def psum(self, name):
        h = self.es.enter_context(self.nc.psum_tensor(name, [128, 512], F32))
        return T(h[:], name)

    def dram(self, name, shape, dt, kind="Internal"):
        h = self.nc.dram_tensor(name, list(shape), dt, kind=kind)
        return T(h.ap(), name)

    def phase(self):
        k = self

        class _P:
            def __enter__(s):
                k.barrier()
                k.phase_es = ExitStack()
                return s

            def __exit__(s, *a):
                k.barrier()
                k.phase_es.close()
                k.phase_es = None
                return False

        return _P()

    def _wait(self, eng, key, val):
        if self.seen[eng].get(key, 0) >= val:
            return
        self.engs[eng].wait_ge(self.sems[key], val)
        self.seen[eng][key] = val
        self.n_ins += 1

    def _sync(self, eng, reads, writes):
        deps = {}

        def add(d, war=False):
            if d is None:
                return
            key, val = d
            if key == eng and eng == "pe":
                return
            if deps.get(key, 0) < val:
                deps[key] = val

        for t in reads:
            add(t.w)
        for t in writes:
            add(t.w)
            for key, val in t.r.items():
                add((key, val), war=True)
        for key, val in deps.items():
            self._wait(eng, key, val)

    def _done(self, me, reads, writes):
        key, val = me
        for t in reads:
            if t.r.get(key, 0) < val:
                t.r[key] = val
        for t in writes:
            t.w = me
            t.r = {}

    def barrier(self):
        for eng in self.engs:
            for key in self.sems:
                if key == eng:
                    continue
                self._wait(eng, key, self.cnt[key])

    def _split(self, kw):
        reads, writes, args = [], [], {}
        for name, v in kw.items():
            if isinstance(v, V):
                (writes if name in WRITE_KW else reads).append(v.t)
                args[name] = v.ap
            else:
                args[name] = v
        return reads, writes, args

    def I(self, eng, meth, **kw):
        reads, writes, args = self._split(kw)
        self._sync(eng, reads, writes)
        ins = getattr(self.engs[eng], meth)(**args)
        self.cnt[eng] += 1
        ins.then_inc(self.sems[eng], 1)
        self.n_ins += 1
        self._done((eng, self.cnt[eng]), reads, writes)

    def dma(self, out, in_, q="sp", **kw):
        lst, i = self.dq[q]
        key = lst[i % len(lst)]
        self.dq[q][1] = i + 1
        self._wait(q, key, self.cnt[key])
        self._sync(q, [in_.t], [out.t])
        ins = self.engs[q].dma_start(out=out.ap, in_=in_.ap, **kw)
        self.cnt[key] += 16
        ins.then_inc(self.sems[key], 16)
        self.n_ins += 1
        self._done((key, self.cnt[key]), [in_.t], [out.t])

    def mm(self, out, lhsT, rhs, start=True, stop=True):
        self.I("pe", "matmul", out=out, lhsT=lhsT, rhs=rhs, start=start, stop=stop)

    def tr(self, out, in_, ident):
        self.I("pe", "transpose", out=out, in_=in_, identity=ident)

    def act(self, out, in_, func, bias=0.0, scale=1.0, **kw):
        self.I("act", "activation", out=out, in_=in_, func=func, bias=bias, scale=scale, **kw)

    def tt(self, eng, out, in0, in1, op):
        self.I(eng, "tensor_tensor", out=out, in0=in0, in1=in1, op=op)

    def ts(self, eng, out, in0, s1, op0, s2=None, op1=ALU.bypass, **kw):
        self.I(eng, "tensor_scalar", out=out, in0=in0, scalar1=s1, scalar2=s2, op0=op0, op1=op1, **kw)

    def stt(self, out, in0, scalar, in1, op0, op1):
        self.I("dve", "scalar_tensor_tensor", out=out, in0=in0, scalar=scalar, in1=in1, op0=op0, op1=op1)

    def cp(self, eng, out, in_):
        if eng == "act":
            self.I("act", "copy", out=out, in_=in_)
        else:
            self.I(eng, "tensor_copy", out=out, in_=in_)

    def memset(self, eng, out, val):
        reads, writes = [], [out.t]
        self._sync(eng, reads, writes)
        ins = self.engs[eng].memset(out.ap, val)
        self.cnt[eng] += 1
        ins.then_inc(self.sems[eng], 1)
        self._done((eng, self.cnt[eng]), reads, writes)

    def aselect(self, out, in_, pattern, cmp, fill, base, cm):
        self.I("pool", "affine_select", out=out, in_=in_, pattern=pattern, compare_op=cmp, fill=fill,
               base=base, channel_multiplier=cm)


class Consts:
    def __init__(self, k):
        self.k = k
        ones = k.sbg([128, 512], F32, "onesf")
        k.memset("pool", ones.v, 1.0)
        self.ones_f = ones
        idf = k.sbg([128, 128], F32, "idf")
        k.aselect(idf.v, ones[:, 0:128], [[1, 128]], ALU.is_equal, 0.0, 0, -1)
        self.id_f = idf
        idb = k.sbg([128, 128], BF16, "idb")
        k.cp("pool", idb.v, idf.v)
        self.id_b = idb
        onesb = k.sbg([128, 128], BF16, "onesb")
        k.cp("pool", onesb.v, ones[:, 0:128])
        self.ones_b = onesb
        self.eps5 = k.sbg([128, 1], F32, "eps5")
        k.memset("pool", self.eps5.v, 1e-5)
        self.eps6 = k.sbg([128, 1], F32, "eps6")
        k.memset("pool", self.eps6.v, 1e-6)
        self.ps = [k.psum("ps%d" % i) for i in range(8)]
        self.psi = 0
        self.pool = list(range(8))

    def bank(self):
        b = self.ps[self.pool[self.psi % len(self.pool)]]
        self.psi += 1
        return b

    def mask(self, shape_f, base, cm, cmp=ALU.is_ge, dt=F32, name="mask", local=False):
        k = self.k
        m = (k.sb if local else k.sbg)([128, shape_f], F32, name)
        k.aselect(m.v, self.ones_f[:, 0:shape_f], [[1, shape_f]], cmp, 0.0, base, cm)
        if dt == F32:
            return m
        mb = k.sbg([128, shape_f], dt, name + "b")
        k.cp("pool", mb.v, m.v)
        return mb


class WLoader:
    def __init__(self, k, max_elems, nstage=2, nbuf=3, ceng="pool", name="w"):
        self.k = k
        self.st = [k.sb([128, max_elems], F32, name + "st") for _ in range(nstage)]
        self.bf = [k.sb([128, max_elems], BF16, name + "bf") for _ in range(nbuf)]
        self.i = 0
        self.j = 0
        self.ceng = ceng

    def load(self, wv, kc, n, p=128):
        k = self.k
        st = self.st[self.i % len(self.st)]
        self.i += 1
        bf = self.bf[self.j % len(self.bf)]
        self.j += 1
        sv = st[0:p, 0:kc * n].re("p (c n) -> p c n", c=kc)
        k.dma(sv, wv.re("(c p) n -> p c n", p=p))
        bv = bf[0:p, 0:kc * n].re("p (c n) -> p c n", c=kc)
        k.cp(self.ceng, bv, sv)
        return bv


def load_bcast(k, dv, n, name="bc"):
    t = k.sb([128, n], F32, name)
    k.dma(t.v, dv.pbro(128))
    return t


DN_ALPHA = (2.0 * 2) ** 0.25


def interleave(gens):
    gens = list(gens)
    while gens:
        for g in list(gens):
            try:
                next(g)
            except StopIteration:
                gens.remove(g)


class Ring:
    def __init__(self, k, n, shape, dt, name="r"):
        self.ts = [k.sb(shape, dt, name) for _ in range(n)]
        self.i = 0

    def next(self):
        t = self.ts[self.i % len(self.ts)]
        self.i += 1
        return t


def ln_phase(k, C, S, u_d, xres_d, g_d, b_d, xout_d, xt_d, eps=1e-5):
    NT = S // 128
    with k.phase():
        g_bc = load_bcast(k, g_d, 1024, "lng")
        b_bc = load_bcast(k, b_d, 1024, "lnb")
        r_x = Ring(k, 6, [128, 1024], F32, "lnx")
        r_u = Ring(k, 6, [128, 1024], F32, "lnu")
        r_o = Ring(k, 5, [128, 1024], F32, "lno")
        r_b = Ring(k, 5, [128, 1024], BF16, "lnbf")
        r_s = Ring(k, 8, [128, 16], F32, "lns")
        GT = min(4, NT)
        xtgs = [k.sb([128, 8, GT * 128], BF16, "lnxt") for _ in range(2)]
        done = {}

        def tile(j):
            rows = slice(j * 128, (j + 1) * 128)
            xr = r_x.next()
            k.dma(xr.v, xres_d[rows, :])
            u = r_u.next()
            k.dma(u.v, u_d[rows, :])
            yield
            k.stt(u.v, xr.v, DN_ALPHA, u.v, ALU.mult, ALU.add)
            st = r_s.next()
            for h in range(2):
                k.I("dve", "bn_stats", out=st[:, h * 6:(h + 1) * 6], in_=u[:, h * 512:(h + 1) * 512])
            k.I("dve", "bn_aggr", out=st[:, 12:14], in_=st[:, 0:12])
            k.act(st[:, 14:15], st[:, 13:14], AF.Ln, bias=C.eps5[:, 0:1])
            k.act(st[:, 15:16], st[:, 14:15], AF.Exp, scale=-0.5)
            yield
            k.ts("dve", u.v, u.v, st[:, 12:13], ALU.subtract, st[:, 15:16], ALU.mult)
            xo = r_o.next()
            k.tt("pool", xo.v, u.v, g_bc.v, ALU.mult)
            k.tt("dve", xo.v, xo.v, b_bc.v, ALU.add)
            k.dma(xout_d[rows, :], xo.v, q="act")
            xb = r_b.next()
            k.cp("act", xb.v, xo.v)
            yield
            g = j // GT
            xtg = xtgs[g % 2]
            pv = C.bank().v.bc(BF16)
            for c in range(8):
                k.tr(pv[:, c * 128:(c + 1) * 128], xb[:, c * 128:(c + 1) * 128], C.id_b.v)
            jj = j % GT
            k.cp("dve", xtg[:, :, jj * 128:(jj + 1) * 128], pv.re("p (c t) -> p c t", c=8))
            done[g] = done.get(g, 0) + 1
            if done[g] == GT:
                k.dma(xt_d[:, :, g * GT * 128:(g + 1) * GT * 128], xtg.v, q="act")
            yield

        NS = min(4, NT)

        def stream(par):
            for j in range(par, NT, NS):
                yield from tile(j)

        interleave([stream(i) for i in range(NS)])


def xt0_phase(k, C, S, x_d, xt_d):
    NT = S // 128
    with k.phase():
        r_x = Ring(k, 2, [128, 1024], F32, "x0")
        r_b = Ring(k, 2, [128, 1024], BF16, "x0b")
        GT = min(4, NT)
        r_g = Ring(k, 2, [128, 8, GT * 128], BF16, "x0t")
        for j in range(NT):
            rows = slice(j * 128, (j + 1) * 128)
            xr = r_x.next()
            k.dma(xr.v, x_d[rows, :])
            xb = r_b.next()
            k.cp("act", xb.v, xr.v)
            if j % GT == 0:
                xtg = r_g.next()
            pst = C.bank()
            pv = pst.v.bc(BF16)
            for c in range(8):
                k.tr(pv[:, c * 128:(c + 1) * 128], xb[:, c * 128:(c + 1) * 128], C.id_b.v)
            jj = j % GT
            k.cp("dve", xtg[:, :, jj * 128:(jj + 1) * 128], pv.re("p (c t) -> p c t", c=8))
            if jj == GT - 1:
                t0 = (j - jj) * 128
                k.dma(xt_d[:, :, t0:t0 + GT * 128], xtg.v, q="pool")


def ffn_phase(k, C, S, xt_d, x_d, u_d, experts, router_d=None):
    NT = S // 128
    TG = min(512, S)
    NTG = S // TG
    TPG = TG // 128
    with k.phase():
        XT = k.sb([128, 8, S], BF16, "ffxt")
        k.dma(XT.v, xt_d.v)
        acc = k.sb([128, NT, 1024], F32, "ffacc")
        G = None
        if router_d is not None:
            G = k.sb([128, NT, 8], F32, "ffG")
            rt = k.sb([128, 8, 8], F32, "ffrt")
            k.dma(rt.v, router_d.re("(c p) e -> p c e", p=128))
            r_x = Ring(k, 1, [128, 1024], F32, "ffx")
            r_t = Ring(k, 1, [128, 1024], F32, "ffxT")
            r_s = Ring(k, 2, [128, 48], F32, "ffs")
            for j in range(NT):
                xr = r_x.next()
                k.dma(xr.v, x_d[j * 128:(j + 1) * 128, :])
                xT = r_t.next()
                for hh in range(2):
                    pb = C.bank()
                    for c in range(4):
                        cc = hh * 4 + c
                        k.tr(pb[:, c * 128:(c + 1) * 128], xr[:, cc * 128:(cc + 1) * 128], C.id_f.v)
                    k.cp("act", xT[:, hh * 512:(hh + 1) * 512], pb.v)
                pl = C.bank()
                for c in range(8):
                    k.mm(pl[:, 0:8], xT[:, c * 128:(c + 1) * 128], rt[:, c, :], start=(c == 0), stop=(c == 7))
                s = r_s.next()
                lg = s[:, 0:8]
                k.cp("dve", lg, pl[:, 0:8])
                k.I("dve", "reduce_max", out=s[:, 8:9], in_=lg, axis=AX.X)
                k.ts("dve", s[:, 16:24], lg, s[:, 8:9], ALU.is_equal)
                k.stt(s[:, 24:32], s[:, 16:24], -1e30, lg, ALU.mult, ALU.add)
                k.I("dve", "reduce_max", out=s[:, 9:10], in_=s[:, 24:32], axis=AX.X)
                k.ts("dve", s[:, 16:24], lg, s[:, 9:10], ALU.is_ge)
                k.ts("dve", s[:, 10:11], s[:, 8:9], -1.0, ALU.mult)
                k.act(s[:, 32:40], lg, AF.Exp, bias=s[:, 10:11])
                k.tt("dve", s[:, 32:40], s[:, 32:40], s[:, 16:24], ALU.mult)
                k.I("dve", "reduce_sum", out=s[:, 11:12], in_=s[:, 32:40], axis=AX.X)
                k.I("dve", "reciprocal", out=s[:, 12:13], in_=s[:, 11:12])
                k.ts("dve", G[:, j, :], s[:, 32:40], s[:, 12:13], ALU.mult)
        WL = WLoader(k, 8 * 512, nstage=2, nbuf=4, name="ffw")
        r_h = Ring(k, 2, [128, 4, TG], BF16, "ffh")
        r_sg = Ring(k, 2, [128, TG], F32, "ffsg")
        first = True
        for e, (w13, w2, H) in enumerate(experts):
            h0 = 0
            while h0 < H:
                hw = min(512, H - h0)
                nc_ = hw // 128
                w1 = WL.load(w13[:, h0:h0 + hw], 8, hw)
                w3 = WL.load(w13[:, H + h0:H + h0 + hw], 8, hw)
                w2s = WL.load(w2[h0:h0 + hw, :], nc_, 1024)
                for tg in range(NTG):
                    tsl = slice(tg * TG, (tg + 1) * TG)
                    hT = r_h.next()
                    for c in range(nc_):
                        gp = C.bank()
                        for kc in range(8):
                            k.mm(gp[:, 0:TG], w1[:, kc, c * 128:(c + 1) * 128], XT[:, kc, tsl], start=(kc == 0), stop=(kc == 7))
                        up = C.bank()
                        for kc in range(8):
                            k.mm(up[:, 0:TG], w3[:, kc, c * 128:(c + 1) * 128], XT[:, kc, tsl], start=(kc == 0), stop=(kc == 7))
                        sg = r_sg.next()
                        k.act(sg.v, gp[:, 0:TG], AF.Silu)
                        k.tt("dve", hT[:, c, :], up[:, 0:TG], sg.v, ALU.mult)
                    for j in range(TPG):
                        tix = tg * TPG + j
                        for hh in range(2):
                            yp = C.bank()
                            for c in range(nc_):
                                k.mm(yp.v, hT[:, c, j * 128:(j + 1) * 128], w2s[:, c, hh * 512:(hh + 1) * 512],
                                     start=(c == 0), stop=(c == nc_ - 1))
                            av = acc[:, tix, hh * 512:(hh + 1) * 512]
                            if G is None:
                                if first:
                                    k.cp("act", av, yp.v)
                                else:
                                    k.tt("dve", av, yp.v, av, ALU.add)
                            else:
                                gs = G[:, tix, e:e + 1]
                                if first:
                                    k.ts("dve", av, yp.v, gs, ALU.mult)
                                else:
                                    k.stt(av, yp.v, gs, av, ALU.mult, ALU.add)
                first = False
                h0 += hw
        for j in range(NT):
            k.dma(u_d[j * 128:(j + 1) * 128, :], acc[:, j, :], q="pool")


def outproj_phase(k, C, S, ht_d, KC, w_d, u_d, p=128):
    NT = S // 128
    with k.phase():
        HT = k.sb([128, KC, S], BF16, "opH")
        k.dma(HT[0:p], ht_d.v)
        WL = WLoader(k, KC * 512, nstage=2, nbuf=2, name="opw")
        wh = [WL.load(w_d[:, hh * 512:(hh + 1) * 512], KC, 512, p=p) for hh in range(2)]
        r_u = Ring(k, 3, [128, 1024], F32, "opu")
        for j in range(NT):
            u = r_u.next()
            for hh in range(2):
                yp = C.bank()
                for c in range(KC):
                    k.mm(yp.v, HT[0:p, c, j * 128:(j + 1) * 128], wh[hh][:, c, :], start=(c == 0), stop=(c == KC - 1))
                k.cp("act" if hh == 0 else "dve", u[:, hh * 512:(hh + 1) * 512], yp.v)
            k.dma(u_d[j * 128:(j + 1) * 128, :], u.v, q="pool")


def to_fm_bf16(k, C, src_d, n_tok, dstT, r_x, r_b):
    for j in range(n_tok // 128):
        xr = r_x.next()
        k.dma(xr.v, src_d[j * 128:(j + 1) * 128, :])
        xb = r_b.next()
        k.cp("act", xb.v, xr.v)
        pv = C.bank().v.bc(BF16)
        for c in range(8):
            k.tr(pv[:, c * 128:(c + 1) * 128], xb[:, c * 128:(c + 1) * 128], C.id_b.v)
        k.cp("dve", dstT[:, :, j * 128:(j + 1) * 128], pv.re("p (c t) -> p c t", c=8))


def xattn_phase(k, C, S, xt_d, mem_d, wq_d, wkv_d, wo_d, u_d):
    TG = min(512, S)
    NTG = S // TG
    TPG = TG // 128
    M = 256
    with k.phase():
        r_xt = Ring(k, 2, [128, 8, TG], BF16, "xaxt")
        memT = k.sb([128, 8, M], BF16, "xamT")
        r_x = Ring(k, 2, [128, 1024], F32, "xamx")
        r_b = Ring(k, 2, [128, 1024], BF16, "xamb")
        to_fm_bf16(k, C, mem_d, M, memT.v, r_x, r_b)
        WL = WLoader(k, 8 * 512, nstage=2, nbuf=6, name="xaw")
        KT = k.sb([128, 8, M], BF16, "xaKT")
        Vm = k.sb([128, 2, 1024], BF16, "xaV")
        for sl in range(4):
            w = WL.load(wkv_d[:, sl * 512:(sl + 1) * 512], 8, 512)
            if sl < 2:
                for c in range(4):
                    pb = C.bank()
                    for kc in range(8):
                        k.mm(pb[:, 0:M], w[:, kc, c * 128:(c + 1) * 128], memT[:, kc, :], start=(kc == 0), stop=(kc == 7))
                    k.cp("act", KT[:, sl * 4 + c, :], pb[:, 0:M])
            else:
                for mt in range(2):
                    pb = C.bank()
                    for kc in range(8):
                        k.mm(pb.v, memT[:, kc, mt * 128:(mt + 1) * 128], w[:, kc, :], start=(kc == 0), stop=(kc == 7))
                    k.cp("act", Vm[:, mt, (sl - 2) * 512:(sl - 1) * 512], pb.v)
        wq = [WL.load(wq_d[:, hh * 512:(hh + 1) * 512], 8, 512) for hh in range(2)]
        wo = [WL.load(wo_d[:, hh * 512:(hh + 1) * 512], 8, 512) for hh in range(2)]
        r_q = Ring(k, 2, [128, 8, TG], BF16, "xaq")
        r_o = Ring(k, 2, [128, 8, TG], BF16, "xao")
        r_p = Ring(k, 4, [128, TG], BF16, "xap")
        r_d = Ring(k, 2, [128, TG], F32, "xad")
        r_u = Ring(k, 2, [128, 1024], F32, "xau")
        for tg in range(NTG):
            tsl = slice(tg * TG, (tg + 1) * TG)
            XT = r_xt.next()
            k.dma(XT.v, xt_d[:, :, tsl])
            qT = r_q.next()
            for n in range(8):
                pb = C.bank()
                for kc in range(8):
                    k.mm(pb[:, 0:TG], wq[n // 4][:, kc, (n % 4) * 128:(n % 4 + 1) * 128], XT[:, kc, :],
                         start=(kc == 0), stop=(kc == 7))
                k.cp("act" if n % 2 == 0 else "dve", qT[:, n, :], pb[:, 0:TG])
            oT = r_o.next()
            for h in range(4):
                P = []
                for mt in range(2):
                    zb = C.bank()
                    for c in range(2):
                        k.mm(zb[:, 0:TG], KT[:, 2 * h + c, mt * 128:(mt + 1) * 128], qT[:, 2 * h + c, :],
                             start=(c == 0), stop=(c == 1))
                    p = r_p.next()
                    k.act(p.v, zb[:, 0:TG], AF.Exp, scale=1.0 / 16.0)
                    P.append(p)
                db = C.bank()
                for mt in range(2):
                    k.mm(db[:, 0:TG], C.ones_b.v, P[mt].v, start=(mt == 0), stop=(mt == 1))
                rd = r_d.next()
                k.I("dve", "reciprocal", out=rd.v, in_=db[:, 0:TG])
                for c in range(2):
                    ob = C.bank()
                    for mt in range(2):
                        k.mm(ob[:, 0:TG], Vm[:, mt, (2 * h + c) * 128:(2 * h + c + 1) * 128], P[mt].v,
                             start=(mt == 0), stop=(mt == 1))
                    k.tt("dve", oT[:, 2 * h + c, :], ob[:, 0:TG], rd.v, ALU.mult)
            for j in range(TPG):
                u = r_u.next()
                for hh in range(2):
                    yp = C.bank()
                    for c in range(8):
                        k.mm(yp.v, oT[:, c, j * 128:(j + 1) * 128], wo[hh][:, c, :], start=(c == 0), stop=(c == 7))
                    k.cp("act" if hh == 0 else "dve", u[:, hh * 512:(hh + 1) * 512], yp.v)
                t0 = tg * TG + j * 128
                k.dma(u_d[t0:t0 + 128, :], u.v, q="pool")


def gate_phase(k, C, S, xt_d, ybr_d, wgate_d, wbr_d, mergedT_d):
    TG = min(512, S)
    NTG = S // TG
    with k.phase():
        XT = k.sb([128, 8, S], BF16, "gxt")
        k.dma(XT.v, xt_d.v)
        Y = []
        for i in range(4):
            y = k.sb([128, 2, S], BF16, "gy")
            k.dma(y.v, ybr_d[i].v)
            Y.append(y)
        WL = WLoader(k, 8 * 512, nstage=2, nbuf=4, name="gw")
        macc = k.sb([128, 4, S], F32, "gacc")
        r_s = Ring(k, 3, [128, TG], F32, "gsg")
        r_m = Ring(k, 2, [128, 4, TG], BF16, "gm")
        for sl in range(2):
            for i in range(4):
                wg = WL.load(wgate_d[i, :, sl * 512:(sl + 1) * 512], 8, 512)
                wb = WL.load(wbr_d[i, :, sl * 512:(sl + 1) * 512], 2, 512)
                for tg in range(NTG):
                    tsl = slice(tg * TG, (tg + 1) * TG)
                    for c in range(4):
                        gp = C.bank()
                        for kc in range(8):
                            k.mm(gp[:, 0:TG], wg[:, kc, c * 128:(c + 1) * 128], XT[:, kc, tsl], start=(kc == 0), stop=(kc == 7))
                        pp = C.bank()
                        for kc in range(2):
                            k.mm(pp[:, 0:TG], wb[:, kc, c * 128:(c + 1) * 128], Y[i][:, kc, tsl], start=(kc == 0), stop=(kc == 1))
                        sg = r_s.next()
                        k.act(sg.v, gp[:, 0:TG], AF.Sigmoid)
                        if i == 0:
                            k.tt("dve", macc[:, c, tsl], pp[:, 0:TG], sg.v, ALU.mult)
                        else:
                            k.tt("dve", sg.v, pp[:, 0:TG], sg.v, ALU.mult)
                            k.tt("pool", macc[:, c, tsl], macc[:, c, tsl], sg.v, ALU.add)
            for tg in range(NTG):
                tsl = slice(tg * TG, (tg + 1) * TG)
                mb = r_m.next()
                k.cp("act", mb.v, macc[:, :, tsl])
                k.dma(mergedT_d[:, sl * 4:(sl + 1) * 4, tsl], mb.v, q="pool")


def sb_consts(k, C, TG):
    if hasattr(C, "sbm"):
        return
    m = k.sbg([128, 128], F32, "triu")
    k.aselect(m.v, C.ones_f[:, 0:128], [[-1, 128]], ALU.is_gt, 0.0, 0, 1)
    C.tri_gt = k.sbg([128, 128], BF16, "triub")
    k.cp("pool", C.tri_gt.v, m.v)
    m2 = k.sbg([128, 128], F32, "tril")
    k.aselect(m2.v, C.ones_f[:, 0:128], [[1, 128]], ALU.is_ge, 0.0, 0, -1)
    C.tri_le = k.sbg([128, 128], BF16, "trilb")
    k.cp("pool", C.tri_le.v, m2.v)
    C.one = k.sbg([128, 1], F32, "one")
    k.memset("pool", C.one.v, 1.0)


def sb_phase(k, C, S, xt_d, win_d, y_d):
    TG = min(512, S)
    NTG = S // TG
    NT = S // 128
    sb_consts(k, C, TG)
    with k.phase():
        C.sbm = [C.mask(TG, -r * 128, -1, ALU.is_gt, F32, "sbm%d" % r, local=True) for r in range(TG // 128)]
        XT = k.sb([128, 8, S], BF16, "sbxt")
        k.dma(XT.v, xt_d.v)
        WL = WLoader(k, 8 * 512, nstage=2, nbuf=2, name="sbw")
        wqk = WL.load(win_d[:, 0:512], 8, 512)
        wv = WL.load(win_d[:, 512:768], 8, 256)
        qk = k.sb([128, 4, S], BF16, "sbqk")
        Vt = k.sb([128, NT, 256], BF16, "sbv")
        for tg in range(NTG):
            tsl = slice(tg * TG, (tg + 1) * TG)
            for n in range(4):
                pb = C.bank()
                for kc in range(8):
                    k.mm(pb[:, 0:TG], wqk[:, kc, n * 128:(n + 1) * 128], XT[:, kc, tsl], start=(kc == 0), stop=(kc == 7))
                k.cp("act", qk[:, n, tsl], pb[:, 0:TG])
        for j in range(NT):
            pb = C.bank()
            for kc in range(8):
                k.mm(pb[:, 0:256], XT[:, kc, j * 128:(j + 1) * 128], wv[:, kc, :], start=(kc == 0), stop=(kc == 7))
            k.cp("act", Vt[:, j, :], pb[:, 0:256])
        YT = k.sb([128, 2, S], BF16, "sby")
        r_e = Ring(k, 6, [128, TG], F32, "sbe")
        r_sp = Ring(k, 2, [128, TG], F32, "sbsp")
        r_sm = Ring(k, 8, [128, TG], BF16, "sbsm")
        r_u = Ring(k, 6, [128, TG], F32, "sbu")
        r_w = Ring(k, 8, [128, TG], BF16, "sbw_")
        r_wf = Ring(k, 2, [128, TG], F32, "sbwf")
        C.pool = [0, 1]

        def unit(h, qg, slot):
            hp, bp = h // 2, (h % 2) * 64
            q0 = qg * TG
            qsl = slice(q0, q0 + TG)
            tailb = C.ps[4 + slot]
            yb = C.ps[2 + slot // 2]
            last = (q0 + TG) // 128 - 1
            for kt in range(last, -1, -1):
                first = kt == last
                zb = C.bank()
                k.mm(zb[:, 0:TG], qk[bp:bp + 64, 2 + hp, kt * 128:(kt + 1) * 128], qk[bp:bp + 64, hp, qsl])
                e = r_e.next()
                k.act(e.v, zb[:, 0:TG], AF.Exp, scale=0.125)
                diag = kt * 128 >= q0
                sm = r_sm.next()
                if diag:
                    r = (kt * 128 - q0) // 128
                    sp = r_sp.next()
                    k.act(sp.v, e.v, AF.Ln, bias=C.one[:, 0:1])
                    k.tt("dve", sm.v, sp.v, C.sbm[r].v, ALU.mult)
                else:
                    k.act(sm.v, e.v, AF.Ln, bias=C.one[:, 0:1])
                k.I("pe", "matmul", out=tailb[:, 0:TG], lhsT=C.tri_gt.v, rhs=sm.v, start=first, stop=True,
                    skip_group_check=True)
                u = r_u.next()
                k.stt(u.v, zb[:, 0:TG], 0.125, sm.v, ALU.mult, ALU.subtract)
                yield
                k.tt("dve", u.v, u.v, tailb[:, 0:TG], ALU.subtract)
                w = r_w.next()
                if diag:
                    wf = r_wf.next()
                    k.act(wf.v, u.v, AF.Exp)
                    k.tt("pool", w.v, wf.v, C.sbm[r].v, ALU.mult)
                else:
                    k.act(w.v, u.v, AF.Exp)
                if kt > 0:
                    k.I("pe", "matmul", out=tailb[:, 0:TG], lhsT=C.tri_le.v, rhs=sm.v, start=False, stop=True,
                        skip_group_check=True)
                k.I("pe", "matmul", out=yb[bp:bp + 64, 0:TG], lhsT=Vt[:, kt, h * 64:(h + 1) * 64], rhs=w.v,
                    start=first, stop=(kt == 0), skip_group_check=True)
                yield
            k.cp("act", YT[bp:bp + 64, hp, qsl], yb[bp:bp + 64, 0:TG])

        units = [(h, qg) for qg in range(NTG) for h in range(4)]

        def stream(slot):
            for (h, qg) in units[slot::4]:
                yield from unit(h, qg, slot)

        interleave([stream(i) for i in range(4)])
        C.pool = list(range(8))
        k.dma(y_d.v, YT.v, q="pool")


def rope_phase(k, C, S, pos_d, rope_d):
    with k.phase():
        pi_ = k.sb([128, S], I32, "rpi")
        k.dma(pi_.v, pos_d.pbro(128))
        pf = k.sb([128, S], F32, "rpf")
        k.cp("dve", pf.v, pi_.v)
        pidx = k.sb([128, 4], I32, "rpx")
        k.I("pool", "iota", out=pidx[:, 0:1], pattern=[[0, 1]], base=0, channel_multiplier=1)
        k.ts("dve", pidx[:, 1:2], pidx[:, 0:1], 15, ALU.bitwise_and)
        k.ts("dve", pidx[:, 2:3], pidx[:, 0:1], 16, ALU.bitwise_and)
        cf = k.sb([128, 8], F32, "rcf")
        k.cp("dve", cf[:, 0:1], pidx[:, 1:2])
        k.cp("dve", cf[:, 1:2], pidx[:, 2:3])
        k.act(cf[:, 2:3], cf[:, 0:1], AF.Exp, scale=-math.log(10000.0) / 16.0)
        k.ts("dve", cf[:, 3:4], cf[:, 1:2], 0.125, ALU.mult, -1.0, ALU.add)
        ang = k.sb([128, S], F32, "rang")
        k.ts("dve", ang.v, pf.v, cf[:, 2:3], ALU.mult)
        C1, C2 = 6.28125, 2 * math.pi - 6.28125
        MAGIC = 12582912.0
        kf = k.sb([128, S], F32, "rkf")
        r = k.sb([128, S], F32, "rr")
        out = k.sb([128, S], F32, "rout")
        for which in range(2):
            off = 0.25 if which == 0 else 0.0
            k.ts("dve", kf.v, ang.v, 1.0 / (2 * math.pi), ALU.mult, off, ALU.add)
            k.ts("dve", kf.v, kf.v, MAGIC, ALU.add)
            k.ts("dve", kf.v, kf.v, MAGIC, ALU.subtract)
            k.stt(r.v, kf.v, -C1, ang.v, ALU.mult, ALU.add)
            k.stt(r.v, kf.v, -C2, r.v, ALU.mult, ALU.add)
            if which == 0:
                k.ts("dve", r.v, r.v, math.pi / 2, ALU.add)
            k.ts("dve", r.v, r.v, 3.1415925, ALU.min, -3.1415925, ALU.max)
            k.act(out.v, r.v, AF.Sin)
            if which == 1:
                k.ts("dve", out.v, out.v, cf[:, 3:4], ALU.mult)
            k.dma(rope_d[which], out.v, q="pool")


MLA_OFF = 768 + 1024 + 1028


def mla_phase(k, C, S, xt_d, win_d, qg_d, wuq_d, kvg_d, wukv_d, rope_d, y_d):
    TG = min(512, S)
    NTG = S // TG
    NT = S // 128
    TPG = TG // 128
    sb_consts(k, C, TG)
    SC = 96.0 ** -0.5
    with k.phase():
        C.cam = [C.mask(TG, -r * 128, -1, ALU.is_ge, F32, "cam%d" % r, local=True) for r in range(TG // 128)]
        WL = WLoader(k, 8 * 512, nstage=2, nbuf=1, name="mlw")
        wm = WL.load(win_d[:, 0:416], 8, 416)
        wkr2 = k.sb([128, 8, 64], BF16, "mlkr")
        k.cp("pool", wkr2[:, :, 0:32], wm[:, :, 384:416])
        k.cp("pool", wkr2[:, :, 32:48], wm[:, :, 400:416])
        k.cp("pool", wkr2[:, :, 48:64], wm[:, :, 384:400])
        gq = k.sb([128, 4], F32, "mlg")
        for c in range(2):
            k.dma(gq[:, c:c + 1], qg_d[c * 128:(c + 1) * 128].re("(p o) -> p o", o=1))
        k.dma(gq[:, 2:3], kvg_d.re("(p o) -> p o", o=1))
        st = k.sb([128, 2 * 384], F32, "mlst")
        k.dma(st.v.re("p (c n) -> p c n", c=2), wuq_d.re("(c p) n -> p c n", p=128))
        wuq = k.sb([128, 2, 384], BF16, "mluq")
        for c in range(2):
            k.ts("pool", wuq[:, c, :], st[:, c * 384:(c + 1) * 384], gq[:, c:c + 1], ALU.mult)
        wuqs = k.sb([128, 2, 128], BF16, "mluqs")
        for h in range(4):
            k.cp("pool", wuqs[:, :, h * 32:h * 32 + 16], wuq[:, :, h * 96 + 80:h * 96 + 96])
            k.cp("pool", wuqs[:, :, h * 32 + 16:h * 32 + 32], wuq[:, :, h * 96 + 64:h * 96 + 80])
        st2 = k.sb([128, 512], F32, "mlst2")
        k.dma(st2.v, wukv_d)
        wukv = k.sb([128, 512], BF16, "mlukv")
        k.ts("pool", wukv.v, st2.v, gq[:, 2:3], ALU.mult)
        wv = k.sb([128, 256], BF16, "mlwv")
        for h in range(4):
            k.cp("pool", wv[:, h * 64:(h + 1) * 64], wukv[:, h * 128 + 64:h * 128 + 128])
        cs = k.sb([128, 2, S], F32, "mlcs")
        for w_ in range(2):
            k.dma(cs[:, w_, :], rope_d[w_])
        cT = k.sb([128, 3, S], BF16, "mlcT")
        qT = k.sb([128, 4, S], BF16, "mlqT")
        kT = k.sb([128, 4, S], BF16, "mlkT")
        Vt = k.sb([128, NT, 256], BF16, "mlV")
        r_xt = Ring(k, 2, [128, 8, TG], BF16, "mlxt")
        r_c = Ring(k, 2, [128, 384], BF16, "mlc")
        r_j = Ring(k, 2, [128, 256], F32, "mlj")
        r_s = Ring(k, 2, [128, 8], F32, "mls")
        r_t = Ring(k, 4, [128, TG], F32, "mlt")
        for tg in range(NTG):
            tsl = slice(tg * TG, (tg + 1) * TG)
            XT = r_xt.next()
            k.dma(XT.v, xt_d[:, :, tsl])
            for jj in range(TPG):
                j = tg * TPG + jj
                pb = C.bank()
                for kc in range(8):
                    k.mm(pb[:, 0:384], XT[:, kc, jj * 128:(jj + 1) * 128], wm[:, kc, 0:384], start=(kc == 0), stop=(kc == 7))
                s = r_s.next()
                jk = r_j.next()
                k.act(jk[:, 0:256], pb[:, 0:256], AF.Square, accum_out=s[:, 0:1])
                k.act(jk[:, 0:128], pb[:, 256:384], AF.Square, accum_out=s[:, 1:2])
                k.act(s[:, 2:3], s[:, 0:1], AF.Ln, scale=1.0 / 256, bias=C.eps6[:, 0:1])
                k.act(s[:, 3:4], s[:, 1:2], AF.Ln, scale=1.0 / 128, bias=C.eps6[:, 0:1])
                k.act(s[:, 4:6], s[:, 2:4], AF.Exp, scale=-0.5)
                cb = r_c.next()
                k.ts("dve", cb[:, 0:256], pb[:, 0:256], s[:, 4:5], ALU.mult)
                k.ts("dve", cb[:, 256:384], pb[:, 256:384], s[:, 5:6], ALU.mult)
                pv = C.bank().v.bc(BF16)
                for c in range(3):
                    k.tr(pv[:, c * 128:(c + 1) * 128], cb[:, c * 128:(c + 1) * 128], C.id_b.v)
                k.cp("act", cT[:, :, j * 128:(j + 1) * 128], pv[:, 0:384].re("p (c t) -> p c t", c=3))
                pvb = C.bank()
                k.mm(pvb[:, 0:256], cT[:, 2, j * 128:(j + 1) * 128], wv.v)
                k.cp("act", Vt[:, j, :], pvb[:, 0:256])
            p1 = C.bank()
            p2 = C.bank()
            for kc in range(8):
                k.mm(p1[64:96, 0:TG], wkr2[:, kc, 0:32], XT[:, kc, :], start=(kc == 0), stop=(kc == 7))
            for kc in range(8):
                k.mm(p2[64:96, 0:TG], wkr2[:, kc, 32:64], XT[:, kc, :], start=(kc == 0), stop=(kc == 7))
            t1 = r_t.next()
            t2 = r_t.next()
            k.tt("dve", t1[64:96, :], p1[64:96, 0:TG], cs[64:96, 0, tsl], ALU.mult)
            k.tt("dve", t2[64:96, :], p2[64:96, 0:TG], cs[64:96, 1, tsl], ALU.mult)
            k.tt("dve", kT[64:96, 0, tsl], t1[64:96, :], t2[64:96, :], ALU.add)
            for h in range(1, 4):
                k.cp("pool", kT[64:96, h, tsl], kT[64:96, 0, tsl])
            for h in range(4):
                pk = C.bank()
                k.mm(pk[0:64, 0:TG], wukv[:, h * 128:h * 128 + 64], cT[:, 2, tsl])
                k.cp("act", kT[0:64, h, tsl], pk[0:64, 0:TG])
                pq = C.bank()
                for c in range(2):
                    k.mm(pq[0:96, 0:TG], wuq[:, c, h * 96:(h + 1) * 96], cT[:, c, tsl], start=(c == 0), stop=(c == 1))
                pq2 = C.bank()
                for c in range(2):
                    k.mm(pq2[64:96, 0:TG], wuqs[:, c, h * 32:(h + 1) * 32], cT[:, c, tsl], start=(c == 0), stop=(c == 1))
                k.cp("act", qT[0:64, h, tsl], pq[0:64, 0:TG])
                t1 = r_t.next()
                t2 = r_t.next()
                k.tt("dve", t1[64:96, :], pq[64:96, 0:TG], cs[64:96, 0, tsl], ALU.mult)
                k.tt("dve", t2[64:96, :], pq2[64:96, 0:TG], cs[64:96, 1, tsl], ALU.mult)
                k.tt("dve", qT[64:96, h, tsl], t1[64:96, :], t2[64:96, :], ALU.add)
        YT = k.sb([128, 2, S], BF16, "mly")
        r_p = Ring(k, 8, [128, TG], BF16, "mlp")
        r_pf = Ring(k, 4, [128, TG], F32, "mlpf")
        r_d = Ring(k, 4, [128, TG], F32, "mld")
        C.pool = [0, 1, 2, 3]

        def unit(h, qg, slot):
            hp, bp = h // 2, (h % 2) * 64
            q0 = qg * TG
            qsl = slice(q0, q0 + TG)
            db = C.ps[6 + slot // 2]
            yb = C.ps[4 + slot // 2]
            last = (q0 + TG) // 128 - 1
            for kt in range(last + 1):
                zb = C.bank()
                k.mm(zb[:, 0:TG], kT[0:96, h, kt * 128:(kt + 1) * 128], qT[0:96, h, qsl])
                p = r_p.next()
                if kt * 128 >= q0:
                    r = (kt * 128 - q0) // 128
                    pf = r_pf.next()
                    k.act(pf.v, zb[:, 0:TG], AF.Exp, scale=SC)
                    k.tt("pool", p.v, pf.v, C.cam[r].v, ALU.mult)
                else:
                    k.act(p.v, zb[:, 0:TG], AF.Exp, scale=SC)
                yield
                k.I("pe", "matmul", out=yb[bp:bp + 64, 0:TG], lhsT=Vt[:, kt, h * 64:(h + 1) * 64], rhs=p.v,
                    start=(kt == 0), stop=(kt == last), skip_group_check=True)
                k.I("pe", "matmul", out=db[bp:bp + 64, 0:TG], lhsT=C.ones_b[:, 0:64], rhs=p.v,
                    start=(kt == 0), stop=(kt == last), skip_group_check=True)
            rd = r_d.next()
            k.I("dve", "reciprocal", out=rd[bp:bp + 64, :], in_=db[bp:bp + 64, 0:TG])
            k.tt("dve", YT[bp:bp + 64, hp, qsl], yb[bp:bp + 64, 0:TG], rd[bp:bp + 64, :], ALU.mult)
            yield

        units = [(h, qg) for qg in range(NTG) for h in range(4)]

        def stream(slot):
            for (h, qg) in units[slot::4]:
                yield from unit(h, qg, slot)

        interleave([stream(i) for i in range(4)])
        C.pool = list(range(8))
        k.dma(y_d.v, YT.v, q="pool")


def load_cols(k, dv, nchunk, name="pc", p=128):
    t = k.sb([128, nchunk], F32, name)
    for c in range(nchunk):
        k.dma(t[0:p, c:c + 1], dv[c * p:(c + 1) * p].re("(p o) -> p o", o=1))
    return t


RW_OFF = 768
EXPM05 = math.exp(-0.5)


def rwkv_phase(k, C, S, xt_d, win_d, prm, y_d, stop=None):
    RG = min(128, S)
    NRG = S // RG
    CH = 64
    NCH = RG // CH
    NQ = 14
    with k.phase():
        o64 = C.ones_f[0:64, 0:256].re("p (h s) -> p h s", h=4)
        ones64 = C.ones_b[0:64, 0:64]
        idb64 = C.id_b[0:64, 0:64]
        mLs = k.sb([64, 4, 64], F32, "rwLs")
        k.aselect(mLs.v, o64, [[0, 4], [-1, 64]], ALU.is_gt, 0.0, 0, 1)
        mUs = k.sb([64, 4, 64], F32, "rwUs")
        k.aselect(mUs.v, o64, [[0, 4], [1, 64]], ALU.is_gt, 0.0, 0, -1)
        mUi = k.sb([64, 4, 64], F32, "rwUi")
        k.aselect(mUi.v, o64, [[0, 4], [1, 64]], ALU.is_ge, 0.0, 0, -1)
        mI = k.sb([64, 4, 64], F32, "rwI")
        k.aselect(mI.v, o64, [[0, 4], [1, 64]], ALU.is_equal, 0.0, 0, -1)
        rmi = k.sb([64, 4 * RG], I32, "rwrmi")
        k.I("pool", "iota", out=rmi.v, pattern=[[1, 4 * RG]], base=0, channel_multiplier=0)
        k.ts("dve", rmi.v, rmi.v, CH - 1, ALU.bitwise_and)
        rmask = k.sb([64, 4 * RG], F32, "rwrm")
        k.cp("dve", rmask.v, rmi.v)
        k.ts("dve", rmask.v, rmask.v, 1.0, ALU.min)
        mu = load_cols(k, prm["mu"][0:896], NQ, "rwmu", p=64)
        mug = load_cols(k, prm["mu"][896:1024], 1, "rwmug")
        w0 = load_cols(k, prm["w0"], 4, "rww0", p=64)
        a0 = load_cols(k, prm["a0"], 4, "rwa0", p=64)
        kk_ = load_cols(k, prm["k_k"], 4, "rwkk", p=64)
        ka = load_cols(k, prm["k_a"], 4, "rwka", p=64)
        rk = load_cols(k, prm["r_k"], 4, "rwrk", p=64)
        gng = load_cols(k, prm["gn_g"], 4, "rwgg", p=64)
        gnb = load_cols(k, prm["gn_b"], 4, "rwgb", p=64)
        omka = k.sb([128, 4], F32, "rwomka")
        k.ts("dve", omka[0:64, :], ka[0:64, :], -1.0, ALU.mult, 1.0, ALU.add)
        eps_gn = k.sb([128, 1], F32, "rwepsgn")
        k.memset("pool", eps_gn.v, 64e-5)
        WL = WLoader(k, 8 * 512, nstage=1, nbuf=2, name="rww")
        wrw = [WL.load(win_d[:, hh * 512:(hh + 1) * 512], 8, 512) for hh in range(2)]
        lst = k.sb([128, 768], F32, "rwlst")
        k.dma(lst[0:64, 0:256], prm["w_up"])
        k.dma(lst[0:64, 256:512], prm["a_up"])
        k.dma(lst[:, 512:768], prm["g_up"])
        lup = k.sb([128, 768], BF16, "rwlup")
        k.cp("pool", lup[0:64, 0:512], lst[0:64, 0:512])
        k.cp("pool", lup[:, 512:768], lst[:, 512:768])
        ST = k.sb([64, 4, 64], F32, "rwST")
        k.memset("pool", ST.v, 0.0)
        carry = k.sb([64, NQ, 1], F32, "rwcar")
        k.memset("pool", carry.v, 0.0)
        carryg = k.sb([128, 1], F32, "rwcarg")
        k.memset("pool", carryg.v, 0.0)
        SG = min(512, S)
        GPS = SG // RG
        r_xt = Ring(k, 1, [128, 8, SG], BF16, "rwxt")
        pT = k.sb([64, NQ, SG + 1], F32, "rwpT")
        pG = k.sb([128, SG + 1], F32, "rwpG")
        ps = k.sb([64, NQ, RG], F32, "rwps")
        psg = k.sb([128, RG], F32, "rwpsg")
        tmp = Ring(k, 4, [64, 4, RG], F32, "rwtmp")
        tmp2 = Ring(k, 3, [64, 4, RG], F32, "rwtmp2")
        tmpg = k.sb([128, RG], F32, "rwtmpg")
        r_bf = Ring(k, 2, [128, RG], BF16, "rwbf")

        def arr(name):
            return k.sb([64, 4, RG], F32, name)

        A_a, A_kap, A_kp, A_lw, A_cl = arr("rwa"), arr("rwkap"), arr("rwkp"), arr("rwlw"), arr("rwcl")
        def arr16(name):
            return k.sb([64, 4, RG], BF16, name)

        A_kt, A_bt, A_kbar, A_bbar, A_y = arr16("rwkt"), arr16("rwbt"), arr16("rwkbar"), arr16("rwbbar"), arr("rwy")
        A_yb, Vb = arr16("rwyb"), arr16("rwvb")
        tmpb = Ring(k, 3, [64, 4, RG], BF16, "rwtmpb")
        STb = k.sb([64, 4, 64], BF16, "rwSTb")
        k.memset("pool", STb.v, 0.0)
        yout = Ring(k, 2, [64, 4, RG], BF16, "rwyo")
        sg = k.sb([128, RG], BF16, "rwsg")

        def cset(n):
            return [dict(G=[k.sb([64, 4, 64], BF16, "rwG%d" % i) for i in range(5)],
                         Pb=k.sb([64, 4, 64], BF16, "rwPb"),
                         N=[k.sb([64, 4, 64], BF16, "rwN%d" % i) for i in range(2)],
                         M=[k.sb([64, 4, 64], BF16, "rwM%d" % i) for i in range(2)],
                         P=[k.sb([64, 4, 64], BF16, "rwP%d" % i) for i in range(2)],
                         tok=[k.sb([64, 4, 64], BF16, "rwtok%d" % i) for i in range(3)]) for _ in range(n)]

        def mkset():
            return dict(A_rt=arr16("rwrt"), A_kapt=arr16("rwkapt"), A_g=arr("rwg"), A_bon=arr("rwbon"),
                        gC=k.sb([64, 4, NCH], F32, "rwgC"), CS=cset(NCH), curP=[None] * NCH)

        SETS = [mkset(), mkset()]
        Wt = k.sb([64, 4, 64], BF16, "rwW")
        Ut = k.sb([64, 4, 64], BF16, "rwU")

        def v4(bank):
            return bank[0:64, 0:256].re("p (h s) -> p h s", h=4)

        def fm(Aarr, h, ch):
            return Aarr[:, h, ch * CH:(ch + 1) * CH]

        def gen_AB(rg, B):
            A_rt, A_kapt, A_g, A_bon, gC, CS = B["A_rt"], B["A_kapt"], B["A_g"], B["A_bon"], B["gC"], B["CS"]
            tsl = slice(rg * RG, (rg + 1) * RG)
            o = (rg % GPS) * RG
            if rg % GPS == 0:
                XT = r_xt.next()
                k.dma(XT.v, xt_d[:, :, rg * RG:rg * RG + SG])
                k.cp("pool", pT[:, :, 0:1], carry.v)
                k.cp("pool", pG[:, 0:1], carryg.v)
                for q in range(NQ):
                    c0 = q * 64
                    pb = C.bank()
                    for kc in range(8):
                        k.mm(pb[0:64, 0:SG], wrw[c0 // 512][:, kc, c0 % 512:c0 % 512 + 64], XT[:, kc, :],
                             start=(kc == 0), stop=(kc == 7))
                    k.cp("act", pT[:, q, 1:SG + 1], pb[0:64, 0:SG])
                    if q % 2 == 1:
                        yield
                pb = C.bank()
                for kc in range(8):
                    k.mm(pb[:, 0:SG], wrw[1][:, kc, 384:512], XT[:, kc, :], start=(kc == 0), stop=(kc == 7))
                k.cp("act", pG[:, 1:SG + 1], pb[:, 0:SG])
                k.cp("pool", carry.v, pT[:, :, SG:SG + 1])
                k.cp("pool", carryg.v, pG[:, SG:SG + 1])
                yield
            for q0 in range(0, NQ, 4):
                nq = min(4, NQ - q0)
                t = tmp.next()
                k.tt("pool", t[:, 0:nq, :], pT[:, q0:q0 + nq, o:o + RG], pT[:, q0:q0 + nq, o + 1:o + RG + 1], ALU.subtract)
                for q in range(q0, q0 + nq):
                    k.stt(ps[:, q, :], t[:, q - q0, :], mu[0:64, q:q + 1], pT[:, q, o + 1:o + RG + 1], ALU.mult, ALU.add)
                yield
            k.tt("pool", tmpg.v, pG[:, o:o + RG], pG[:, o + 1:o + RG + 1], ALU.subtract)
            k.stt(psg.v, tmpg.v, mug[:, 0:1], pG[:, o + 1:o + RG + 1], ALU.mult, ALU.add)
            R_, K_, V_ = ps[:, 0:4, :], ps[:, 4:8, :], ps[:, 8:12, :]
            tw = r_bf.next()
            k.act(tw[0:64, :], ps[:, 12, :], AF.Tanh)
            alo = r_bf.next()
            k.cp("pool", alo[0:64, :], ps[:, 13, :])
            k.act(sg.v, psg.v, AF.Sigmoid)
            yield
            for hq in range(2):
                pw = C.bank()
                pa = C.bank()
                pg = C.bank()
                for hh in range(2):
                    h = hq * 2 + hh
                    k.mm(pw[0:64, hh * RG:(hh + 1) * RG], lup[0:64, h * 64:(h + 1) * 64], tw[0:64, :])
                    k.mm(pa[0:64, hh * RG:(hh + 1) * RG], lup[0:64, 256 + h * 64:256 + (h + 1) * 64], alo[0:64, :])
                    k.mm(pg[0:64, hh * RG:(hh + 1) * RG], lup[:, 512 + h * 64:512 + (h + 1) * 64], sg.v)
                for hh in range(2):
                    h = hq * 2 + hh
                    k.act(A_lw[:, h, :], pw[0:64, hh * RG:(hh + 1) * RG], AF.Sigmoid, bias=w0[0:64, h:h + 1])
                    k.act(A_a[:, h, :], pa[0:64, hh * RG:(hh + 1) * RG], AF.Sigmoid, bias=a0[0:64, h:h + 1])
                    k.cp("act", A_g[:, h, :], pg[0:64, hh * RG:(hh + 1) * RG])
                yield
            k.ts("pool", A_lw.v, A_lw.v, -EXPM05, ALU.mult)
            kkv = tmp.next()
            for h in range(4):
                k.ts("dve", kkv[:, h, :], K_[:, h, :], kk_[0:64, h:h + 1], ALU.mult)
            sq = tmpb.next()
            k.tt("pool", sq.v, kkv.v, kkv.v, ALU.mult)
            k.cp("pool", Vb.v, V_)
            yield
            rs = tmp.next()
            for hq in range(2):
                pss = C.bank()
                for hh in range(2):
                    k.mm(pss[0:64, hh * RG:(hh + 1) * RG], ones64, sq[:, hq * 2 + hh, :])
                k.ts("dve", rs[:, hq * 2:hq * 2 + 2, :], pss[0:64, 0:2 * RG].re("p (h t) -> p h t", h=2), 1e-24, ALU.max)
            k.act(rs.v, rs.v, AF.Ln)
            k.act(rs.v, rs.v, AF.Exp, scale=-0.5)
            k.tt("dve", A_kap.v, kkv.v, rs.v, ALU.mult)
            yield
            k.I("dve", "tensor_tensor_scan", out=A_cl.v.re("p h t -> p (h t)"), data0=rmask.v,
                data1=A_lw.v.re("p h t -> p (h t)"), initial=0.0, op0=ALU.mult, op1=ALU.add)
            t = tmp.next()
            for h in range(4):
                k.ts("dve", t[:, h, :], A_a[:, h, :], ka[0:64, h:h + 1], ALU.mult, omka[0:64, h:h + 1], ALU.add)
            k.tt("pool", A_kp.v, K_, t.v, ALU.mult)
            yield
            t2 = tmpb.next()
            for h in range(4):
                k.stt(t2[:, h, :], R_[:, h, :], rk[0:64, h:h + 1], A_kp[:, h, :], ALU.mult, ALU.mult)
            for hq in range(2):
                pbn = C.bank()
                for hh in range(2):
                    k.mm(pbn[0:64, hh * RG:(hh + 1) * RG], ones64, t2[:, hq * 2 + hh, :])
                k.tt("dve", A_bon[:, hq * 2:hq * 2 + 2, :], pbn[0:64, 0:2 * RG].re("p (h t) -> p h t", h=2),
                     V_[:, hq * 2:hq * 2 + 2, :], ALU.mult)
            yield
            ep = tmp.next()
            k.act(ep.v, A_cl.v, AF.Exp)
            k.tt("pool", A_rt.v, R_, ep.v, ALU.mult)
            en = tmp.next()
            k.act(en.v, A_cl.v, AF.Exp, scale=-1.0)
            k.tt("dve", A_kt.v, A_kp.v, en.v, ALU.mult)
            b_ = tmp.next()
            k.tt("pool", b_.v, A_kap.v, A_a.v, ALU.mult)
            k.tt("dve", A_bt.v, b_.v, en.v, ALU.mult)
            yield
            t3 = tmp.next()
            k.tt("pool", t3.v, A_cl.v, A_lw.v, ALU.subtract)
            k.act(t3.v, t3.v, AF.Exp)
            k.tt("dve", A_kapt.v, A_kap.v, t3.v, ALU.mult)
            clv = A_cl.v.re("p h (c t) -> p (h c) t", t=CH)
            t4 = tmp.next()
            t4v = t4.v.re("p h (c t) -> p (h c) t", t=CH)
            k.tt("pool", t4v, clv[:, :, CH - 1:CH].bro([64, 4 * NCH, CH]), clv, ALU.subtract)
            k.act(t4.v, t4.v, AF.Exp)
            k.tt("dve", A_kbar.v, A_kp.v, t4.v, ALU.mult)
            k.tt("pool", A_bbar.v, b_.v, t4.v, ALU.mult)
            k.act(gC.v.re("p h c -> p (h c)"), clv[:, :, CH - 1], AF.Exp)
            yield
            for ch in range(NCH):
                cs = CS[ch]
                specs = [(A_kapt, A_bt, mLs, 1.0), (A_bt, A_kapt, mUs, 1.0), (A_kt, A_kapt, mUs, 1.0),
                         (A_kt, A_rt, mUi, 1.0), (A_bt, A_rt, mUi, -1.0)]
                for gi, (La, Ra, msk, sgn) in enumerate(specs):
                    pb = C.bank()
                    for h in range(4):
                        k.mm(pb[0:64, h * 64:(h + 1) * 64], fm(La, h, ch), fm(Ra, h, ch))
                    if sgn == 1.0:
                        k.tt("dve", cs["G"][gi].v, v4(pb), msk.v, ALU.mult)
                    else:
                        k.stt(cs["G"][gi].v, v4(pb), sgn, msk.v, ALU.mult, ALU.mult)
                    yield
                k.tt("pool", cs["P"][0].v, mI.v, cs["G"][1].v, ALU.subtract)
                for ti, sgn in enumerate([1.0, 1.0, -1.0]):
                    pbb = C.bank().v.bc(BF16)
                    for h in range(4):
                        src = fm(Vb if ti == 0 else (A_kbar if ti == 1 else A_bbar), h, ch)
                        k.tr(pbb[0:64, h * 64:(h + 1) * 64], src, idb64)
                    pv_ = pbb[0:64, 0:256].re("p (h s) -> p h s", h=4)
                    if sgn == 1.0:
                        k.cp("act", cs["tok"][ti].v, pv_)
                    else:
                        k.act(cs["tok"][ti].v, pv_, AF.Copy, scale=-1.0)
                    yield
            curN = [CS[ch]["G"][0] for ch in range(NCH)]
            curM = [CS[ch]["G"][1] for ch in range(NCH)]
            curP = [CS[ch]["P"][0] for ch in range(NCH)]
            for st in range(5):
                lastst = st == 4
                for ch in range(NCH):
                    cs = CS[ch]
                    Nn, Mn, Pn = cs["N"][st % 2], cs["M"][st % 2], cs["P"][(st + 1) % 2]
                    pb = C.bank()
                    for h in range(4):
                        k.mm(pb[0:64, h * 64:(h + 1) * 64], curM[ch][:, h, :], curN[ch][:, h, :])
                    k.cp("act", Nn.v, v4(pb))
                    if not lastst:
                        pb2 = C.bank()
                        for h in range(4):
                            k.mm(pb2[0:64, h * 64:(h + 1) * 64], curN[ch][:, h, :], curM[ch][:, h, :])
                        k.cp("act", Mn.v, v4(pb2))
                    pb3 = C.bank()
                    for h in range(4):
                        k.mm(pb3[0:64, h * 64:(h + 1) * 64], Nn[:, h, :], curP[ch][:, h, :])
                    if lastst:
                        k.tt("dve", cs["Pb"].v, v4(pb3), curP[ch].v, ALU.add)
                        Pn = cs["Pb"]
                    else:
                        k.tt("dve", Pn.v, v4(pb3), curP[ch].v, ALU.add)
                    curN[ch], curM[ch], curP[ch] = Nn, Mn, Pn
                    yield
            B["curP"] = curP

        def gen_CD(rg, B):
            A_rt, A_kapt, A_g, A_bon, gC, CS = B["A_rt"], B["A_kapt"], B["A_g"], B["A_bon"], B["gC"], B["CS"]
            curP = B["curP"]
            tsl = slice(rg * RG, (rg + 1) * RG)
            for ch in range(NCH):
                cs = CS[ch]
                TT = curP[ch]
                Vt_, KB, BBn = cs["tok"]
                AkkT, ArkT, ArbTn = cs["G"][2], cs["G"][3], cs["G"][4]
                pw = C.bank()
                for h in range(4):
                    k.I("pe", "matmul", out=pw[0:64, h * 64:(h + 1) * 64], lhsT=fm(A_kapt, h, ch), rhs=STb[:, h, :],
                        start=True, stop=False, skip_group_check=True)
                    k.I("pe", "matmul", out=pw[0:64, h * 64:(h + 1) * 64], lhsT=AkkT[:, h, :], rhs=Vt_[:, h, :],
                        start=False, stop=True, skip_group_check=True)
                k.cp("act", Wt.v, v4(pw))
                yield
                pu = C.bank()
                for h in range(4):
                    k.mm(pu[0:64, h * 64:(h + 1) * 64], TT[:, h, :], Wt[:, h, :])
                k.cp("act", Ut.v, v4(pu))
                yield
                py = C.bank()
                pst = C.bank()
                for h in range(4):
                    yo = py[0:64, h * 64:(h + 1) * 64]
                    k.I("pe", "matmul", out=yo, lhsT=STb[:, h, :], rhs=fm(A_rt, h, ch), start=True, stop=False,
                        skip_group_check=True)
                    k.I("pe", "matmul", out=yo, lhsT=Vt_[:, h, :], rhs=ArkT[:, h, :], start=False, stop=False,
                        skip_group_check=True)
                    k.I("pe", "matmul", out=yo, lhsT=Ut[:, h, :], rhs=ArbTn[:, h, :], start=False, stop=True,
                        skip_group_check=True)
                    so = pst[0:64, h * 64:(h + 1) * 64]
                    k.I("pe", "matmul", out=so, lhsT=KB[:, h, :], rhs=Vt_[:, h, :], start=True, stop=False,
                        skip_group_check=True)
                    k.I("pe", "matmul", out=so, lhsT=BBn[:, h, :], rhs=Ut[:, h, :], start=False, stop=True,
                        skip_group_check=True)
                k.cp("act", A_y[:, :, ch * CH:(ch + 1) * CH], v4(py))
                for h in range(4):
                    k.stt(ST[:, h, :], ST[:, h, :], gC[:, h, ch:ch + 1], pst[0:64, h * 64:(h + 1) * 64], ALU.mult, ALU.add)
                k.cp("pool", STb.v, ST.v)
                yield
            yo_t = yout.next()
            yc = tmp2.next()
            k.cp("pool", A_yb.v, A_y.v)
            for hq in range(2):
                pm = C.bank()
                for hh in range(2):
                    k.mm(pm[0:64, hh * RG:(hh + 1) * RG], ones64, A_yb[:, hq * 2 + hh, :])
                k.stt(yc[:, hq * 2:hq * 2 + 2, :], pm[0:64, 0:2 * RG].re("p (h t) -> p h t", h=2), -1.0 / 64,
                      A_y[:, hq * 2:hq * 2 + 2, :], ALU.mult, ALU.add)
            sq = tmpb.next()
            k.tt("pool", sq.v, yc.v, yc.v, ALU.mult)
            yield
            rs = tmp2.next()
            for hq in range(2):
                pvv = C.bank()
                for hh in range(2):
                    k.mm(pvv[0:64, hh * RG:(hh + 1) * RG], ones64, sq[:, hq * 2 + hh, :])
                k.act(rs[:, hq * 2:hq * 2 + 2, :], pvv[0:64, 0:2 * RG].re("p (h t) -> p h t", h=2), AF.Ln, scale=1.0 / 64,
                      bias=eps_gn[0:64, 0:1])
            k.act(rs.v, rs.v, AF.Exp, scale=-0.5)
            k.tt("dve", yc.v, yc.v, rs.v, ALU.mult)
            yield
            for h in range(4):
                k.ts("dve", yc[:, h, :], yc[:, h, :], gng[0:64, h:h + 1], ALU.mult, gnb[0:64, h:h + 1], ALU.add)
            k.tt("pool", yc.v, yc.v, A_bon.v, ALU.add)
            k.tt("pool", yo_t.v, yc.v, A_g.v, ALU.mult)
            for h in range(4):
                k.dma(y_d[(h % 2) * 64:(h % 2) * 64 + 64, h // 2, tsl], yo_t[:, h, :], q="pool")
            yield

        for step in range(NRG + 1):
            gens = []
            if step < NRG:
                gens.append(gen_AB(step, SETS[step % 2]))
            if step >= 1:
                gens.append(gen_CD(step - 1, SETS[(step - 1) % 2]))
            interleave(gens)


SSD_OFF = 768 + 1024


def ssd_phase(k, C, S, xt_d, win_d, prm, y_d):
    TG = min(512, S)
    NTG = S // TG
    TPG = TG // 128
    with k.phase():
        Ui = k.sb([128, 128], F32, "sdUi")
        k.aselect(Ui.v, C.ones_f[:, 0:128], [[1, 128]], ALU.is_ge, 0.0, 0, -1)
        WL = WLoader(k, 8 * 512, nstage=2, nbuf=2, name="sdw")
        w_zx = WL.load(win_d[:, 0:512], 8, 512)
        w_bc = WL.load(win_d[:, 512:1024], 8, 512)
        st = k.sb([128, 8, 4], F32, "sdst")
        k.dma(st.v, win_d[:, 1024:1028].re("(c p) n -> p c n", p=128))
        w_dt = k.sb([128, 8, 4], BF16, "sdwdt")
        k.cp("pool", w_dt.v, st.v)
        cw = k.sb([128, 6, 4], F32, "sdcw")
        for n in range(6):
            for j in range(4):
                k.dma(cw[:, n, j:j + 1], prm["conv_w"][j, n * 128:(n + 1) * 128].re("(p o) -> p o", o=1))
        cb = load_cols(k, prm["conv_b"], 6, "sdcb")
        dtb = load_bcast(k, prm["dt_bias"], 4, "sddtb")
        alog = load_bcast(k, prm["a_log"], 4, "sdalog")
        dsk = load_bcast(k, prm["d"], 4, "sddsk")
        ng = load_bcast(k, prm["norm_g"], 256, "sdng")
        aneg = k.sb([128, 4], F32, "sdaneg")
        k.act(aneg.v, alog.v, AF.Exp)
        k.ts("dve", aneg.v, aneg.v, -1.0, ALU.mult)
        eps5g = C.eps5
        Sst = k.sb([128, 4, 64], F32, "sdS")
        k.memset("pool", Sst.v, 0.0)
        Sbf = k.sb([128, 4, 64], BF16, "sdSb")
        k.memset("pool", Sbf.v, 0.0)
        xraw = k.sb([128, 6, TG + 3], F32, "sdxr")
        k.memset("pool", xraw[:, :, 0:3], 0.0)
        xc = k.sb([128, 6, TG], F32, "sdxc")
        bcb = k.sb([128, 4, TG], BF16, "sdbcb")
        r_xt = Ring(k, 2, [128, 8, TG], BF16, "sdxt")
        r_acc = Ring(k, 2, [128, TG], F32, "sdacc")
        r_s = Ring(k, 4, [128, 48], F32, "sds")
        r_m = Ring(k, 8, [128, 128], F32, "sdm")
        r_mb = Ring(k, 20, [128, 128], BF16, "sdmb")
        r_tok = Ring(k, 3, [128, 512], F32, "sdtok")
        r_tb = Ring(k, 3, [128, 768], BF16, "sdtb")
        r_y = Ring(k, 3, [128, 256], F32, "sdy")
        r_z = Ring(k, 3, [128, 256], F32, "sdz")
        r_gt = Ring(k, 3, [128, 256], F32, "sdgt")
        r_yb = Ring(k, 3, [128, 256], BF16, "sdyb")
        r_yo = Ring(k, 2, [128, 2, TG], BF16, "sdyo")
        for tg in range(NTG):
            tsl = slice(tg * TG, (tg + 1) * TG)
            XT = r_xt.next()
            k.dma(XT.v, xt_d[:, :, tsl])
            for n in range(6):
                pb = C.bank()
                wsl = w_zx[:, :, 256 + n * 128:256 + (n + 1) * 128] if n < 2 else w_bc[:, :, (n - 2) * 128:(n - 1) * 128]
                for kc in range(8):
                    k.mm(pb[:, 0:TG], wsl[:, kc, :], XT[:, kc, :], start=(kc == 0), stop=(kc == 7))
                k.cp("act", xraw[:, n, 3:TG + 3], pb[:, 0:TG])
                acc = r_acc.next()
                k.ts("dve", acc.v, xraw[:, n, 3:TG + 3], cw[:, n, 3:4], ALU.mult, cb[:, n:n + 1], ALU.add)
                for j in (2, 1, 0):
                    k.stt(acc.v, xraw[:, n, j:TG + j], cw[:, n, j:j + 1], acc.v, ALU.mult, ALU.add)
                k.act(xc[:, n, :], acc.v, AF.Silu)
                if n >= 2:
                    k.cp("pool", bcb[:, n - 2, :], xc[:, n, :])
            yo = r_yo.next()

            def tile(jj, slot, XT=XT, yo=yo):
                lsl = slice(jj * 128, (jj + 1) * 128)
                s = r_s.next()
                py = C.ps[4 + 2 * slot]
                pst = C.ps[5 + 2 * slot]
                pd = C.bank()
                for kc in range(8):
                    k.mm(pd[:, 0:4], XT[:, kc, lsl], w_dt[:, kc, :], start=(kc == 0), stop=(kc == 7))
                k.tt("dve", s[:, 0:4], pd[:, 0:4], dtb.v, ALU.add)
                k.act(s[:, 0:4], s[:, 0:4], AF.Exp)
                k.act(s[:, 4:8], s[:, 0:4], AF.Ln, bias=C.one[:, 0:1])
                k.tt("dve", s[:, 8:12], s[:, 4:8], aneg.v, ALU.mult)
                pc = C.bank()
                k.mm(pc[:, 0:4], Ui.v, s[:, 8:12])
                k.mm(pc[:, 8:12], C.ones_f[:, 0:128], s[:, 8:12])
                k.cp("dve", s[:, 12:16], pc[:, 0:4])
                k.cp("dve", s[:, 16:20], pc[:, 8:12])
                yield
                k.tt("dve", s[:, 20:24], s[:, 16:20], s[:, 12:16], ALU.subtract)
                k.act(s[:, 20:24], s[:, 20:24], AF.Exp)
                k.act(s[:, 24:28], s[:, 16:20], AF.Exp)
                pz = C.bank()
                for kc in range(8):
                    k.mm(pz[:, 0:256], XT[:, kc, lsl], w_zx[:, kc, 0:256], start=(kc == 0), stop=(kc == 7))
                zs = r_z.next()
                k.act(zs.v, pz[:, 0:256], AF.Silu)
                yield
                pt = C.bank()
                for n in range(4):
                    k.tr(pt[:, n * 128:(n + 1) * 128], xc[:, n, lsl], C.id_f.v)
                tok = r_tok.next()
                k.cp("act", tok.v, pt.v)
                yield
                tb = r_tb.next()
                for h in range(4):
                    k.ts("dve", tb[:, h * 64:(h + 1) * 64], tok[:, h * 64:(h + 1) * 64], s[:, 4 + h:5 + h], ALU.mult)
                    k.stt(tb[:, 512 + h * 64:512 + (h + 1) * 64], tok[:, h * 64:(h + 1) * 64], s[:, 4 + h:5 + h],
                          s[:, 20 + h:21 + h].bro([128, 64]), ALU.mult, ALU.mult)
                k.cp("pool", tb[:, 256:512], tok[:, 256:512])
                pg = C.bank()
                for g in range(2):
                    k.mm(pg[:, g * 128:(g + 1) * 128], bcb[:, g, lsl], bcb[:, 2 + g, lsl])
                gts = r_gt.next()
                k.cp("act", gts.v, pg[:, 0:256])
                yield
                csts = []
                for h in range(4):
                    g = h // 2
                    abc = r_m.next()
                    k.ts("dve", abc.v, C.ones_f[:, 0:128], s[:, 8 + h:9 + h], ALU.mult)
                    pr = C.bank()
                    k.mm(pr[:, 0:128], abc.v, Ui.v)
                    dm = r_m.next()
                    k.ts("dve", dm.v, pr[:, 0:128], s[:, 12 + h:13 + h], ALU.subtract, 0.0, ALU.min)
                    k.act(dm.v, dm.v, AF.Exp)
                    k.tt("dve", dm.v, dm.v, gts[:, g * 128:(g + 1) * 128], ALU.mult)
                    mh = r_mb.next()
                    k.tt("pool", mh.v, dm.v, Ui.v, ALU.mult)
                    er = r_m.next()
                    k.act(er.v, pr[:, 0:128], AF.Exp)
                    cst = r_mb.next()
                    k.tt("pool", cst.v, xc[:, 4 + g, lsl], er.v, ALU.mult)
                    csts.append(cst)
                    k.I("pe", "matmul", out=py[:, h * 64:(h + 1) * 64], lhsT=mh.v, rhs=tb[:, h * 64:(h + 1) * 64],
                        start=(h == 0), stop=False, skip_group_check=True)
                    k.mm(pst[:, h * 64:(h + 1) * 64], tb[:, 256 + g * 128:256 + (g + 1) * 128],
                         tb[:, 512 + h * 64:512 + (h + 1) * 64])
                    yield
                for h in range(4):
                    k.I("pe", "matmul", out=py[:, h * 64:(h + 1) * 64], lhsT=csts[h].v, rhs=Sbf[:, h, :],
                        start=False, stop=True, skip_group_check=True)
                for h in range(4):
                    k.stt(Sst[:, h, :], Sst[:, h, :], s[:, 24 + h:25 + h], pst[:, h * 64:(h + 1) * 64], ALU.mult, ALU.add)
                k.cp("pool", Sbf.v, Sst.v)
                y = r_y.next()
                for h in range(4):
                    k.stt(y[:, h * 64:(h + 1) * 64], tok[:, h * 64:(h + 1) * 64], dsk[:, h:h + 1], py[:, h * 64:(h + 1) * 64],
                          ALU.mult, ALU.add)
                yield
                k.tt("dve", y.v, y.v, zs.v, ALU.mult)
                for g in range(2):
                    k.act(zs[:, g * 128:(g + 1) * 128], y[:, g * 128:(g + 1) * 128], AF.Square, accum_out=s[:, 28 + g:29 + g])
                k.act(s[:, 30:32], s[:, 28:30], AF.Ln, scale=1.0 / 128, bias=eps5g[:, 0:1])
                k.act(s[:, 32:34], s[:, 30:32], AF.Exp, scale=-0.5)
                yb = r_yb.next()
                for g in range(2):
                    k.stt(yb[:, g * 128:(g + 1) * 128], y[:, g * 128:(g + 1) * 128], s[:, 32 + g:33 + g],
                          ng[:, g * 128:(g + 1) * 128], ALU.mult, ALU.mult)
                yield
                pv = C.bank().v.bc(BF16)
                for c in range(2):
                    k.tr(pv[:, c * 128:(c + 1) * 128], yb[:, c * 128:(c + 1) * 128], C.id_b.v)
                k.cp("act", yo[:, :, lsl], pv[:, 0:256].re("p (c t) -> p c t", c=2))
                yield

            def stream(par):
                if par == 1:
                    yield
                for jj in range(par, TPG, 2):
                    yield from tile(jj, par)

            C.pool = [0, 1, 2, 3]
            interleave([stream(0), stream(1)])
            C.pool = list(range(8))
            k.cp("pool", xraw[:, :, 0:3], xraw[:, :, TG:TG + 3])
            k.dma(y_d[:, :, tsl], yo.v, q="pool")


IN_SPECS = [
    ("mix_w_in", [2, 1024, 3236]), ("rw_mu", [2, 1024]), ("rw_w0", [2, 256]), ("rw_w_up", [2, 64, 256]),
    ("rw_a0", [2, 256]), ("rw_a_up", [2, 64, 256]), ("rw_g_up", [2, 128, 256]), ("rw_k_k", [2, 256]),
    ("rw_k_a", [2, 256]), ("rw_r_k", [2, 4, 64]), ("rw_gn_g", [2, 256]), ("rw_gn_b", [2, 256]),
    ("ssd_conv_w", [2, 4, 768]), ("ssd_conv_b", [2, 768]), ("ssd_dt_bias", [2, 4]), ("ssd_a_log", [2, 4]),
    ("ssd_d", [2, 4]), ("ssd_norm_g", [2, 256]), ("mla_q_norm_g", [2, 256]), ("mla_w_uq", [2, 256, 384]),
    ("mla_kv_norm_g", [2, 128]), ("mla_w_ukv", [2, 128, 512]), ("mix_w_br_out", [2, 4, 256, 1024]),
    ("mix_w_gate", [2, 4, 1024, 1024]), ("mix_w_out", [2, 1024, 1024]), ("ln1_g", [2, 1024]), ("ln1_b", [2, 1024]),
    ("xa_w_q", [2, 1024, 1024]), ("xa_w_kv", [2, 1024, 2048]), ("xa_w_o", [2, 1024, 1024]), ("ln2_g", [2, 1024]),
    ("ln2_b", [2, 1024]), ("ffn_w13", [1, 1024, 5632]), ("ffn_w2", [1, 2816, 1024]), ("moe_router", [1, 1024, 8]),
    ("moe_w13", [1, 8, 1024, 7168]), ("moe_w2", [1, 8, 3584, 1024]), ("ln3_g", [2, 1024]), ("ln3_b", [2, 1024]),
]


def build_program(S, NB, L=2):
    nc = bass.Bass("TRN2", target_bir_lowering=False)
    k = K(nc)
    x = k.dram("x", [NB, S, 1024], F32, kind="ExternalInput")
    mem = k.dram("mem", [NB, 256, 1024], F32, kind="ExternalInput")
    pos = k.dram("positions", [NB, S], I32, kind="ExternalInput")
    W = {n: k.dram(n, shp, F32, kind="ExternalInput") for n, shp in IN_SPECS}
    out = k.dram("out", [NB, S, 1024], F32, kind="ExternalOutput")
    xt = k.dram("s_xt", [128, 8, S], BF16)
    Xa = k.dram("s_xa", [S, 1024], F32)
    Xb = k.dram("s_xb", [S, 1024], F32)
    Xc = k.dram("s_xc", [S, 1024], F32)
    U = k.dram("s_u", [S, 1024], F32)
    ybr = [k.dram("s_y%d" % i, [128, 2, S], BF16) for i in range(4)]
    mT = k.dram("s_mt", [128, 8, S], BF16)
    rope = k.dram("s_rope", [2, 128, S], F32)
    C = Consts(k)
    sb_consts(k, C, min(512, S))
    for b in range(NB):
        rope_phase(k, C, S, pos[b], rope)
        xt0_phase(k, C, S, x[b], xt)
        xcur = x[b]
        for l in range(L):
            win = W["mix_w_in"][l]
            sb_phase(k, C, S, xt, win[:, 0:768], ybr[0])
            rprm = dict(mu=W["rw_mu"][l], w0=W["rw_w0"][l], w_up=W["rw_w_up"][l], a0=W["rw_a0"][l], a_up=W["rw_a_up"][l],
                        g_up=W["rw_g_up"][l], k_k=W["rw_k_k"][l], k_a=W["rw_k_a"][l],
                        r_k=W["rw_r_k"][l].re("h n -> (h n)"), gn_g=W["rw_gn_g"][l], gn_b=W["rw_gn_b"][l])
            rwkv_phase(k, C, S, xt, win[:, RW_OFF:RW_OFF + 1024], rprm, ybr[1])
            sprm = dict(conv_w=W["ssd_conv_w"][l], conv_b=W["ssd_conv_b"][l], dt_bias=W["ssd_dt_bias"][l],
                        a_log=W["ssd_a_log"][l], d=W["ssd_d"][l], norm_g=W["ssd_norm_g"][l])
            ssd_phase(k, C, S, xt, win[:, SSD_OFF:SSD_OFF + 1028], sprm, ybr[2])
            mla_phase(k, C, S, xt, win[:, MLA_OFF:MLA_OFF + 416], W["mla_q_norm_g"][l], W["mla_w_uq"][l],
                      W["mla_kv_norm_g"][l], W["mla_w_ukv"][l], rope, ybr[3])
            gate_phase(k, C, S, xt, ybr, W["mix_w_gate"][l], W["mix_w_br_out"][l], mT)
            outproj_phase(k, C, S, mT, 8, W["mix_w_out"][l], U)
            ln_phase(k, C, S, U, xcur, W["ln1_g"][l], W["ln1_b"][l], Xa, xt)
            xattn_phase(k, C, S, xt, mem[b], W["xa_w_q"][l], W["xa_w_kv"][l], W["xa_w_o"][l], U)
            ln_phase(k, C, S, U, Xa.v, W["ln2_g"][l], W["ln2_b"][l], Xb, xt)
            if l % 2 == 0:
                ffn_phase(k, C, S, xt, Xb.v, U, [(W["ffn_w13"][l // 2], W["ffn_w2"][l // 2], 2816)])
            else:
                ex = [(W["moe_w13"][l // 2, e], W["moe_w2"][l // 2, e], 3584) for e in range(8)]
                ffn_phase(k, C, S, xt, Xb.v, U, ex, router_d=W["moe_router"][l // 2])
            last = l == L - 1
            ln_phase(k, C, S, U, Xb.v, W["ln3_g"][l], W["ln3_b"][l], out[b] if last else Xc, xt)
            xcur = Xc.v
    k.barrier()
    return nc, k


def kernel(**inputs):
    B, S = inputs["x"].shape[0], inputs["x"].shape[1]
    NB = B // NCORES
    nc, _ = build_program(S, NB)
    shared = {n: np.ascontiguousarray(inputs[n], dtype=np.float32) for n, _ in IN_SPECS}
    in_maps = []
    for c in range(NCORES):
        sl = slice(c * NB, (c + 1) * NB)
        m = dict(shared)
        m["x"] = np.ascontiguousarray(inputs["x"][sl], dtype=np.float32)
        m["mem"] = np.ascontiguousarray(inputs["mem"][sl], dtype=np.float32)
        m["positions"] = np.ascontiguousarray(inputs["positions"][sl], dtype=np.int32)
        in_maps.append(m)
    res = run_bass_kernel_spmd(nc, in_maps, core_ids=list(range(NCORES)))
    return np.concatenate([np.asarray(r["out"]) for r in res.results], axis=0).astype(np.float32)
```

```python
import math
from contextlib import ExitStack
import numpy as np
import concourse.bass as bass
import concourse.mybir as mybir
from concourse.bass_utils import run_bass_kernel_spmd

F32 = mybir.dt.float32
BF16 = mybir.dt.bfloat16
I32 = mybir.dt.int32
AF = mybir.ActivationFunctionType
ALU = mybir.AluOpType
AX = mybir.AxisListType

D = 1024
NCORES = 8
WRITE_KW = ("out", "accum_out")


class T:
    def __init__(self, ap, name):
        self.ap = ap
        self.name = name
        self.w = None
        self.r = {}

    def __getitem__(self, idx):
        return V(self, self.ap[idx])

    @property
    def v(self):
        return V(self, self.ap)


class V:
    def __init__(self, t, ap):
        self.t = t
        self.ap = ap

    def __getitem__(self, idx):
        return V(self.t, self.ap[idx])

    def bc(self, dt):
        return V(self.t, self.ap.bitcast(dt))

    def re(self, pat, **kw):
        return V(self.t, self.ap.rearrange(pat, **kw))

    def bro(self, shape):
        return V(self.t, self.ap.to_broadcast(shape))

    def pbro(self, n):
        return V(self.t, self.ap.partition_broadcast(n))


class K:
    COMPUTE = ("pe", "act", "dve", "pool")

    def __init__(self, nc):
        self.nc = nc
        self.es = ExitStack()
        self.engs = {"pe": nc.tensor, "act": nc.scalar, "dve": nc.vector, "pool": nc.gpsimd, "sp": nc.sync}
        self.sems = {}
        self.cnt = {}
        for e in self.COMPUTE:
            self.sems[e] = self.es.enter_context(nc.semaphore("s_" + e))
            self.cnt[e] = 0
        self.dq = {}
        for q in ("sp", "pool", "act"):
            lst = []
            for i in range(6):
                key = "d_%s%d" % (q, i)
                self.sems[key] = self.es.enter_context(nc.semaphore(key))
                self.cnt[key] = 0
                lst.append(key)
            self.dq[q] = [lst, 0]
        self.seen = {e: {} for e in self.engs}
        self.n_ins = 0
        self.phase_es = None
        self.uid = 0

    def sb(self, shape, dt, name=None):
        self.uid += 1
        name = "%s_%d" % (name or "t", self.uid)
        h = (self.phase_es or self.es).enter_context(self.nc.sbuf_tensor(name, list(shape), dt))
        return T(h[:], name)

    def sbg(self, shape, dt, name=None):
        self.uid += 1
        name = "%s_%d" % (name or "g", self.uid)
        h = self.es.enter_context(self.nc.sbuf_tensor(name, list(shape), dt))
        return T(h[:], name)

    def psum(self, name):
        h = self.es.enter_context(self.nc.psum_tensor(name, [128, 512], F32))
        return T(h[:], name)

    def dram(self, name, shape, dt, kind="Internal"):
        h = self.nc.dram_tensor(name, list(shape), dt, kind=kind)
        return T(h.ap(), name)

    def phase(self):
        k = self

        class _P:
            def __enter__(s):
                k.barrier()
                k.phase_es = ExitStack()
                return s

            def __exit__(s, *a):
                k.barrier()
                k.phase_es.close()
                k.phase_es = None
                return False

        return _P()

    def _wait(self, eng, key, val):
        if self.seen[eng].get(key, 0) >= val:
            return
        self.engs[eng].wait_ge(self.sems[key], val)
        self.seen[eng][key] = val
        self.n_ins += 1

    def _sync(self, eng, reads, writes):
        deps = {}

        def add(d, war=False):
            if d is None:
                return
            key, val = d
            if key == eng and eng == "pe":
                return
            if deps.get(key, 0) < val:
                deps[key] = val

        for t in reads:
            add(t.w)
        for t in writes:
            add(t.w)
            for key, val in t.r.items():
                add((key, val), war=True)
        for key, val in deps.items():
            self._wait(eng, key, val)

    def _done(self, me, reads, writes):
        key, val = me
        for t in reads:
            if t.r.get(key, 0) < val:
                t.r[key] = val
        for t in writes:
            t.w = me
            t.r = {}

    def barrier(self):
        for eng in self.engs:
            for key in self.sems:
                if key == eng:
                    continue
                self._wait(eng, key, self.cnt[key])

    def _split(self, kw):
        reads, writes, args = [], [], {}
        for name, v in kw.items():
            if isinstance(v, V):
                (writes if name in WRITE_KW else reads).append(v.t)
                args[name] = v.ap
            else:
                args[name] = v
        return reads, writes, args

    def I(self, eng, meth, **kw):
        reads, writes, args = self._split(kw)
        self._sync(eng, reads, writes)
        ins = getattr(self.engs[eng], meth)(**args)
        self.cnt[eng] += 1
        ins.then_inc(self.sems[eng], 1)
        self.n_ins += 1
        self._done((eng, self.cnt[eng]), reads, writes)

    def dma(self, out, in_, q="sp", **kw):
        lst, i = self.dq[q]
        key = lst[i % len(lst)]
        self.dq[q][1] = i + 1
        self._wait(q, key, self.cnt[key])
        self._sync(q, [in_.t], [out.t])
        ins = self.engs[q].dma_start(out=out.ap, in_=in_.ap, **kw)
        self.cnt[key] += 16
        ins.then_inc(self.sems[key], 16)
        self.n_ins += 1
        self._done((key, self.cnt[key]), [in_.t], [out.t])

    def mm(self, out, lhsT, rhs, start=True, stop=True):
        self.I("pe", "matmul", out=out, lhsT=lhsT, rhs=rhs, start=start, stop=stop)

    def tr(self, out, in_, ident):
        self.I("pe", "transpose", out=out, in_=in_, identity=ident)

    def act(self, out, in_, func, bias=0.0, scale=1.0, **kw):
        self.I("act", "activation", out=out, in_=in_, func=func, bias=bias, scale=scale, **kw)

    def tt(self, eng, out, in0, in1, op):
        self.I(eng, "tensor_tensor", out=out, in0=in0, in1=in1, op=op)

    def ts(self, eng, out, in0, s1, op0, s2=None, op1=ALU.bypass, **kw):
        self.I(eng, "tensor_scalar", out=out, in0=in0, scalar1=s1, scalar2=s2, op0=op0, op1=op1, **kw)

    def stt(self, out, in0, scalar, in1, op0, op1):
        self.I("dve", "scalar_tensor_tensor", out=out, in0=in0, scalar=scalar, in1=in1, op0=op0, op1=op1)

    def cp(self, eng, out, in_):
        if eng == "act":
            self.I("act", "copy", out=out, in_=in_)
        else:
            self.I(eng, "tensor_copy", out=out, in_=in_)

    def memset(self, eng, out, val):
        reads, writes = [], [out.t]
        self._sync(eng, reads, writes)
        ins = self.engs[eng].memset(out.ap, val)
        self.cnt[eng] += 1
        ins.then_inc(self.sems[eng], 1)
        self._done((eng, self.cnt[eng]), reads, writes)

    def aselect(self, out, in_, pattern, cmp, fill, base, cm):
        self.I("pool", "affine_select", out=out, in_=in_, pattern=pattern, compare_op=cmp, fill=fill,
               base=base, channel_multiplier=cm)


class Consts:
    def __init__(self, k):
        self.k = k
        ones = k.sbg([128, 512], F32, "onesf")
        k.memset("pool", ones.v, 1.0)
        self.ones_f = ones
        idf = k.sbg([128, 128], F32, "idf")
        k.aselect(idf.v, ones[:, 0:128], [[1, 128]], ALU.is_equal, 0.0, 0, -1)
        self.id_f = idf
        idb = k.sbg([128, 128], BF16, "idb")
        k.cp("pool", idb.v, idf.v)
        self.id_b = idb
        onesb = k.sbg([128, 128], BF16, "onesb")
        k.cp("pool", onesb.v, ones[:, 0:128])
        self.ones_b = onesb
        self.eps5 = k.sbg([128, 1], F32, "eps5")
        k.memset("pool", self.eps5.v, 1e-5)
        self.eps6 = k.sbg([128, 1], F32, "eps6")
        k.memset("pool", self.eps6.v, 1e-6)
        self.ps = [k.psum("ps%d" % i) for i in range(8)]
        self.psi = 0
        self.pool = list(range(8))

    def bank(self):
        b = self.ps[self.pool[self.psi % len(self.pool)]]
        self.psi += 1
        return b

    def mask(self, shape_f, base, cm, cmp=ALU.is_ge, dt=F32, name="mask", local=False):
        k = self.k
        m = (k.sb if local else k.sbg)([128, shape_f], F32, name)
        k.aselect(m.v, self.ones_f[:, 0:shape_f], [[1, shape_f]], cmp, 0.0, base, cm)
        if dt == F32:
            return m
        mb = k.sbg([128, shape_f], dt, name + "b")
        k.cp("pool", mb.v, m.v)
        return mb


class WLoader:
    def __init__(self, k, max_elems, nstage=2, nbuf=3, ceng="pool", name="w"):
        self.k = k
        self.st = [k.sb([128, max_elems], F32, name + "st") for _ in range(nstage)]
        self.bf = [k.sb([128, max_elems], BF16, name + "bf") for _ in range(nbuf)]
        self.i = 0
        self.j = 0
        self.ceng = ceng

    def load(self, wv, kc, n, p=128):
        k = self.k
        st = self.st[self.i % len(self.st)]
        self.i += 1
        bf = self.bf[self.j % len(self.bf)]
        self.j += 1
        sv = st[0:p, 0:kc * n].re("p (c n) -> p c n", c=kc)
        k.dma(sv, wv.re("(c p) n -> p c n", p=p))
        bv = bf[0:p, 0:kc * n].re("p (c n) -> p c n", c=kc)
        k.cp(self.ceng, bv, sv)
        return bv


def load_bcast(k, dv, n, name="bc"):
    t = k.sb([128, n], F32, name)
    k.dma(t.v, dv.pbro(128))
    return t


DN_ALPHA = (2.0 * 2) ** 0.25


def interleave(gens):
    gens = list(gens)
    while gens:
        for g in list(gens):
            try:
                next(g)
            except StopIteration:
                gens.remove(g)


class Ring:
    def __init__(self, k, n, shape, dt, name="r"):
        self.ts = [k.sb(shape, dt, name) for _ in range(n)]
        self.i = 0

    def next(self):
        t = self.ts[self.i % len(self.ts)]
        self.i += 1
        return t


def ln_phase(k, C, S, u_d, xres_d, g_d, b_d, xout_d, xt_d, eps=1e-5):
    NT = S // 128
    with k.phase():
        g_bc = load_bcast(k, g_d, 1024, "lng")
        b_bc = load_bcast(k, b_d, 1024, "lnb")
        r_x = Ring(k, 6, [128, 1024], F32, "lnx")
        r_u = Ring(k, 6, [128, 1024], F32, "lnu")
        r_o = Ring(k, 5, [128, 1024], F32, "lno")
        r_b = Ring(k, 5, [128, 1024], BF16, "lnbf")
        r_s = Ring(k, 8, [128, 16], F32, "lns")
        GT = min(4, NT)
        xtgs = [k.sb([128, 8, GT * 128], BF16, "lnxt") for _ in range(2)]
        done = {}

        def tile(j):
            rows = slice(j * 128, (j + 1) * 128)
            xr = r_x.next()
            k.dma(xr.v, xres_d[rows, :])
            u = r_u.next()
            k.dma(u.v, u_d[rows, :])
            yield
            k.stt(u.v, xr.v, DN_ALPHA, u.v, ALU.mult, ALU.add)
            st = r_s.next()
            for h in range(2):
                k.I("dve", "bn_stats", out=st[:, h * 6:(h + 1) * 6], in_=u[:, h * 512:(h + 1) * 512])
            k.I("dve", "bn_aggr", out=st[:, 12:14], in_=st[:, 0:12])
            k.act(st[:, 14:15], st[:, 13:14], AF.Ln, bias=C.eps5[:, 0:1])
            k.act(st[:, 15:16], st[:, 14:15], AF.Exp, scale=-0.5)
            yield
            k.ts("dve", u.v, u.v, st[:, 12:13], ALU.subtract, st[:, 15:16], ALU.mult)
            xo = r_o.next()
            k.tt("pool", xo.v, u.v, g_bc.v, ALU.mult)
            k.tt("dve", xo.v, xo.v, b_bc.v, ALU.add)
            k.dma(xout_d[rows, :], xo.v, q="act")
            xb = r_b.next()
            k.cp("act", xb.v, xo.v)
            yield
            g = j // GT
            xtg = xtgs[g % 2]
            pv = C.bank().v.bc(BF16)
            for c in range(8):
                k.tr(pv[:, c * 128:(c + 1) * 128], xb[:, c * 128:(c + 1) * 128], C.id_b.v)
            jj = j % GT
            k.cp("dve", xtg[:, :, jj * 128:(jj + 1) * 128], pv.re("p (c t) -> p c t", c=8))
            done[g] = done.get(g, 0) + 1
            if done[g] == GT:
                k.dma(xt_d[:, :, g * GT * 128:(g + 1) * GT * 128], xtg.v, q="act")
            yield

        NS = min(4, NT)

        def stream(par):
            for j in range(par, NT, NS):
                yield from tile(j)

        interleave([stream(i) for i in range(NS)])


def xt0_phase(k, C, S, x_d, xt_d):
    NT = S // 128
    with k.phase():
        r_x = Ring(k, 2, [128, 1024], F32, "x0")
        r_b = Ring(k, 2, [128, 1024], BF16, "x0b")
        GT = min(4, NT)
        r_g = Ring(k, 2, [128, 8, GT * 128], BF16, "x0t")
        for j in range(NT):
            rows = slice(j * 128, (j + 1) * 128)
            xr = r_x.next()
            k.dma(xr.v, x_d[rows, :])
            xb = r_b.next()
            k.cp("act", xb.v, xr.v)
            if j % GT == 0:
                xtg = r_g.next()
            pst = C.bank()
            pv = pst.v.bc(BF16)
            for c in range(8):
                k.tr(pv[:, c * 128:(c + 1) * 128], xb[:, c * 128:(c + 1) * 128], C.id_b.v)
            jj = j % GT
            k.cp("dve", xtg[:, :, jj * 128:(jj + 1) * 128], pv.re("p (c t) -> p c t", c=8))
            if jj == GT - 1:
                t0 = (j - jj) * 128
                k.dma(xt_d[:, :, t0:t0 + GT * 128], xtg.v, q="pool")


def ffn_phase(k, C, S, xt_d, x_d, u_d, experts, router_d=None):
    NT = S // 128
    TG = min(512, S)
    NTG = S // TG
    TPG = TG // 128
    with k.phase():
        XT = k.sb([128, 8, S], BF16, "ffxt")
        k.dma(XT.v, xt_d.v)
        acc = k.sb([128, NT, 1024], F32, "ffacc")
        G = None
        if router_d is not None:
            G = k.sb([128, NT, 8], F32, "ffG")
            rt = k.sb([128, 8, 8], F32, "ffrt")
            k.dma(rt.v, router_d.re("(c p) e -> p c e", p=128))
            r_x = Ring(k, 1, [128, 1024], F32, "ffx")
            r_t = Ring(k, 1, [128, 1024], F32, "ffxT")
            r_s = Ring(k, 2, [128, 48], F32, "ffs")
            for j in range(NT):
                xr = r_x.next()
                k.dma(xr.v, x_d[j * 128:(j + 1) * 128, :])
                xT = r_t.next()
                for hh in range(2):
                    pb = C.bank()
                    for c in range(4):
                        cc = hh * 4 + c
                        k.tr(pb[:, c * 128:(c + 1) * 128], xr[:, cc * 128:(cc + 1) * 128], C.id_f.v)
                    k.cp("act", xT[:, hh * 512:(hh + 1) * 512], pb.v)
                pl = C.bank()
                for c in range(8):
                    k.mm(pl[:, 0:8], xT[:, c * 128:(c + 1) * 128], rt[:, c, :], start=(c == 0), stop=(c == 7))
                s = r_s.next()
                lg = s[:, 0:8]
                k.cp("dve", lg, pl[:, 0:8])
                k.I("dve", "reduce_max", out=s[:, 8:9], in_=lg, axis=AX.X)
                k.ts("dve", s[:, 16:24], lg, s[:, 8:9], ALU.is_equal)
                k.stt(s[:, 24:32], s[:, 16:24], -1e30, lg, ALU.mult, ALU.add)
                k.I("dve", "reduce_max", out=s[:, 9:10], in_=s[:, 24:32], axis=AX.X)
                k.ts("dve", s[:, 16:24], lg, s[:, 9:10], ALU.is_ge)
                k.ts("dve", s[:, 10:11], s[:, 8:9], -1.0, ALU.mult)
                k.act(s[:, 32:40], lg, AF.Exp, bias=s[:, 10:11])
                k.tt("dve", s[:, 32:40], s[:, 32:40], s[:, 16:24], ALU.mult)
                k.I("dve", "reduce_sum", out=s[:, 11:12], in_=s[:, 32:40], axis=AX.X)
                k.I("dve", "reciprocal", out=s[:, 12:13], in_=s[:, 11:12])
                k.ts("dve", G[:, j, :], s[:, 32:40], s[:, 12:13], ALU.mult)
        WL = WLoader(k, 8 * 512, nstage=2, nbuf=4, name="ffw")
        r_h = Ring(k, 2, [128, 4, TG], BF16, "ffh")
        r_sg = Ring(k, 2, [128, TG], F32, "ffsg")
        first = True
        for e, (w13, w2, H) in enumerate(experts):
            h0 = 0
            while h0 < H:
                hw = min(512, H - h0)
                nc_ = hw // 128
                w1 = WL.load(w13[:, h0:h0 + hw], 8, hw)
                w3 = WL.load(w13[:, H + h0:H + h0 + hw], 8, hw)
                w2s = WL.load(w2[h0:h0 + hw, :], nc_, 1024)
                for tg in range(NTG):
                    tsl = slice(tg * TG, (tg + 1) * TG)
                    hT = r_h.next()
                    for c in range(nc_):
                        gp = C.bank()
                        for kc in range(8):
                            k.mm(gp[:, 0:TG], w1[:, kc, c * 128:(c + 1) * 128], XT[:, kc, tsl], start=(kc == 0), stop=(kc == 7))
                        up = C.bank()
                        for kc in range(8):
                            k.mm(up[:, 0:TG], w3[:, kc, c * 128:(c + 1) * 128], XT[:, kc, tsl], start=(kc == 0), stop=(kc == 7))
                        sg = r_sg.next()
                        k.act(sg.v, gp[:, 0:TG], AF.Silu)
                        k.tt("dve", hT[:, c, :], up[:, 0:TG], sg.v, ALU.mult)
                    for j in range(TPG):
                        tix = tg * TPG + j
                        for hh in range(2):
                            yp = C.bank()
                            for c in range(nc_):
                                k.mm(yp.v, hT[:, c, j * 128:(j + 1) * 128], w2s[:, c, hh * 512:(hh + 1) * 512],
                                     start=(c == 0), stop=(c == nc_ - 1))
                            av = acc[:, tix, hh * 512:(hh + 1) * 512]
                            if G is None:
                                if first:
                                    k.cp("act", av, yp.v)
                                else:
                                    k.tt("dve", av, yp.v, av, ALU.add)
                            else:
                                gs = G[:, tix, e:e + 1]
                                if first:
                                    k.ts("dve", av, yp.v, gs, ALU.mult)
                                else:
                                    k.stt(av, yp.v, gs, av, ALU.mult, ALU.add)
                first = False
                h0 += hw
        for j in range(NT):
            k.dma(u_d[j * 128:(j + 1) * 128, :], acc[:, j, :], q="pool")


def outproj_phase(k, C, S, ht_d, KC, w_d, u_d, p=128):
    NT = S // 128
    with k.phase():
        HT = k.sb([128, KC, S], BF16, "opH")
        k.dma(HT[0:p], ht_d.v)
        WL = WLoader(k, KC * 512, nstage=2, nbuf=2, name="opw")
        wh = [WL.load(w_d[:, hh * 512:(hh + 1) * 512], KC, 512, p=p) for hh in range(2)]
        r_u = Ring(k, 3, [128, 1024], F32, "opu")
        for j in range(NT):
            u = r_u.next()
            for hh in range(2):
                yp = C.bank()
                for c in range(KC):
                    k.mm(yp.v, HT[0:p, c, j * 128:(j + 1) * 128], wh[hh][:, c, :], start=(c == 0), stop=(c == KC - 1))
                k.cp("act" if hh == 0 else "dve", u[:, hh * 512:(hh + 1) * 512], yp.v)
            k.dma(u_d[j * 128:(j + 1) * 128, :], u.v, q="pool")


def to_fm_bf16(k, C, src_d, n_tok, dstT, r_x, r_b):
    for j in range(n_tok // 128):
        xr = r_x.next()
        k.dma(xr.v, src_d[j * 128:(j + 1) * 128, :])
        xb = r_b.next()
        k.cp("act", xb.v, xr.v)
        pv = C.bank().v.bc(BF16)
        for c in range(8):
            k.tr(pv[:, c * 128:(c + 1) * 128], xb[:, c * 128:(c + 1) * 128], C.id_b.v)
        k.cp("dve", dstT[:, :, j * 128:(j + 1) * 128], pv.re("p (c t) -> p c t", c=8))


def xattn_phase(k, C, S, xt_d, mem_d, wq_d, wkv_d, wo_d, u_d):
    TG = min(512, S)
    NTG = S // TG
    TPG = TG // 128
    M = 256
    with k.phase():
        r_xt = Ring(k, 2, [128, 8, TG], BF16, "xaxt")
        memT = k.sb([128, 8, M], BF16, "xamT")
        r_x = Ring(k, 2, [128, 1024], F32, "xamx")
        r_b = Ring(k, 2, [128, 1024], BF16, "xamb")
        to_fm_bf16(k, C, mem_d, M, memT.v, r_x, r_b)
        WL = WLoader(k, 8 * 512, nstage=2, nbuf=6, name="xaw")
        KT = k.sb([128, 8, M], BF16, "xaKT")
        Vm = k.sb([128, 2, 1024], BF16, "xaV")
        for sl in range(4):
            w = WL.load(wkv_d[:, sl * 512:(sl + 1) * 512], 8, 512)
            if sl < 2:
                for c in range(4):
                    pb = C.bank()
                    for kc in range(8):
                        k.mm(pb[:, 0:M], w[:, kc, c * 128:(c + 1) * 128], memT[:, kc, :], start=(kc == 0), stop=(kc == 7))
                    k.cp("act", KT[:, sl * 4 + c, :], pb[:, 0:M])
            else:
                for mt in range(2):
                    pb = C.bank()
                    for kc in range(8):
                        k.mm(pb.v, memT[:, kc, mt * 128:(mt + 1) * 128], w[:, kc, :], start=(kc == 0), stop=(kc == 7))
                    k.cp("act", Vm[:, mt, (sl - 2) * 512:(sl - 1) * 512], pb.v)
        wq = [WL.load(wq_d[:, hh * 512:(hh + 1) * 512], 8, 512) for hh in range(2)]
        wo = [WL.load(wo_d[:, hh * 512:(hh + 1) * 512], 8, 512) for hh in range(2)]
        r_q = Ring(k, 2, [128, 8, TG], BF16, "xaq")
        r_o = Ring(k, 2, [128, 8, TG], BF16, "xao")
        r_p = Ring(k, 4, [128, TG], BF16, "xap")
        r_d = Ring(k, 2, [128, TG], F32, "xad")
        r_u = Ring(k, 2, [128, 1024], F32, "xau")
        for tg in range(NTG):
            tsl = slice(tg * TG, (tg + 1) * TG)
            XT = r_xt.next()
            k.dma(XT.v, xt_d[:, :, tsl])
            qT = r_q.next()
            for n in range(8):
                pb = C.bank()
                for kc in range(8):
                    k.mm(pb[:, 0:TG], wq[n // 4][:, kc, (n % 4) * 128:(n % 4 + 1) * 128], XT[:, kc, :],
                         start=(kc == 0), stop=(kc == 7))
                k.cp("act" if n % 2 == 0 else "dve", qT[:, n, :], pb[:, 0:TG])
            oT = r_o.next()
            for h in range(4):
                P = []
                for mt in range(2):
                    zb = C.bank()
                    for c in range(2):
                        k.mm(zb[:, 0:TG], KT[:, 2 * h + c, mt * 128:(mt + 1) * 128], qT[:, 2 * h + c, :],
                             start=(c == 0), stop=(c == 1))
                    p = r_p.next()
                    k.act(p.v, zb[:, 0:TG], AF.Exp, scale=1.0 / 16.0)
                    P.append(p)
                db = C.bank()
                for mt in range(2):
                    k.mm(db[:, 0:TG], C.ones_b.v, P[mt].v, start=(mt == 0), stop=(mt == 1))
                rd = r_d.next()
                k.I("dve", "reciprocal", out=rd.v, in_=db[:, 0:TG])
                for c in range(2):
                    ob = C.bank()
                    for mt in range(2):
                        k.mm(ob[:, 0:TG], Vm[:, mt, (2 * h + c) * 128:(2 * h + c + 1) * 128], P[mt].v,
                             start=(mt == 0), stop=(mt == 1))
                    k.tt("dve", oT[:, 2 * h + c, :], ob[:, 0:TG], rd.v, ALU.mult)
            for j in range(TPG):
                u = r_u.next()
                for hh in range(2):
                    yp = C.bank()
                    for c in range(8):
                        k.mm(yp.v, oT[:, c, j * 128:(j + 1) * 128], wo[hh][:, c, :], start=(c == 0), stop=(c == 7))
                    k.cp("act" if hh == 0 else "dve", u[:, hh * 512:(hh + 1) * 512], yp.v)
                t0 = tg * TG + j * 128
                k.dma(u_d[t0:t0 + 128, :], u.v, q="pool")


def gate_phase(k, C, S, xt_d, ybr_d, wgate_d, wbr_d, mergedT_d):
    TG = min(512, S)
    NTG = S // TG
    with k.phase():
        XT = k.sb([128, 8, S], BF16, "gxt")
        k.dma(XT.v, xt_d.v)
        Y = []
        for i in range(4):
            y = k.sb([128, 2, S], BF16, "gy")
            k.dma(y.v, ybr_d[i].v)
            Y.append(y)
        WL = WLoader(k, 8 * 512, nstage=2, nbuf=4, name="gw")
        macc = k.sb([128, 4, S], F32, "gacc")
        r_s = Ring(k, 3, [128, TG], F32, "gsg")
        r_m = Ring(k, 2, [128, 4, TG], BF16, "gm")
        for sl in range(2):
            for i in range(4):
                wg = WL.load(wgate_d[i, :, sl * 512:(sl + 1) * 512], 8, 512)
                wb = WL.load(wbr_d[i, :, sl * 512:(sl + 1) * 512], 2, 512)
                for tg in range(NTG):
                    tsl = slice(tg * TG, (tg + 1) * TG)
                    for c in range(4):
                        gp = C.bank()
                        for kc in range(8):
                            k.mm(gp[:, 0:TG], wg[:, kc, c * 128:(c + 1) * 128], XT[:, kc, tsl], start=(kc == 0), stop=(kc == 7))
                        pp = C.bank()
                        for kc in range(2):
                            k.mm(pp[:, 0:TG], wb[:, kc, c * 128:(c + 1) * 128], Y[i][:, kc, tsl], start=(kc == 0), stop=(kc == 1))
                        sg = r_s.next()
                        k.act(sg.v, gp[:, 0:TG], AF.Sigmoid)
                        if i == 0:
                            k.tt("dve", macc[:, c, tsl], pp[:, 0:TG], sg.v, ALU.mult)
                        else:
                            k.tt("dve", sg.v, pp[:, 0:TG], sg.v, ALU.mult)
                            k.tt("pool", macc[:, c, tsl], macc[:, c, tsl], sg.v, ALU.add)
            for tg in range(NTG):
                tsl = slice(tg * TG, (tg + 1) * TG)
                mb = r_m.next()
                k.cp("act", mb.v, macc[:, :, tsl])
                k.dma(mergedT_d[:, sl * 4:(sl + 1) * 4, tsl], mb.v, q="pool")


def sb_consts(k, C, TG):
    if hasattr(C, "sbm"):
        return
    m = k.sbg([128, 128], F32, "triu")
    k.aselect(m.v, C.ones_f[:, 0:128], [[-1, 128]], ALU.is_gt, 0.0, 0, 1)
    C.tri_gt = k.sbg([128, 128], BF16, "triub")
    k.cp("pool", C.tri_gt.v, m.v)
    m2 = k.sbg([128, 128], F32, "tril")
    k.aselect(m2.v, C.ones_f[:, 0:128], [[1, 128]], ALU.is_ge, 0.0, 0, -1)
    C.tri_le = k.sbg([128, 128], BF16, "trilb")
    k.cp("pool", C.tri_le.v, m2.v)
    C.one = k.sbg([128, 1], F32, "one")
    k.memset("pool", C.one.v, 1.0)


def sb_phase(k, C, S, xt_d, win_d, y_d):
    TG = min(512, S)
    NTG = S // TG
    NT = S // 128
    sb_consts(k, C, TG)
    with k.phase():
        C.sbm = [C.mask(TG, -r * 128, -1, ALU.is_gt, F32, "sbm%d" % r, local=True) for r in range(TG // 128)]
        XT = k.sb([128, 8, S], BF16, "sbxt")
        k.dma(XT.v, xt_d.v)
        WL = WLoader(k, 8 * 512, nstage=2, nbuf=2, name="sbw")
        wqk = WL.load(win_d[:, 0:512], 8, 512)
        wv = WL.load(win_d[:, 512:768], 8, 256)
        qk = k.sb([128, 4, S], BF16, "sbqk")
        Vt = k.sb([128, NT, 256], BF16, "sbv")
        for tg in range(NTG):
            tsl = slice(tg * TG, (tg + 1) * TG)
            for n in range(4):
                pb = C.bank()
                for kc in range(8):
                    k.mm(pb[:, 0:TG], wqk[:, kc, n * 128:(n + 1) * 128], XT[:, kc, tsl], start=(kc == 0), stop=(kc == 7))
                k.cp("act", qk[:, n, tsl], pb[:, 0:TG])
        for j in range(NT):
            pb = C.bank()
            for kc in range(8):
                k.mm(pb[:, 0:256], XT[:, kc, j * 128:(j + 1) * 128], wv[:, kc, :], start=(kc == 0), stop=(kc == 7))
            k.cp("act", Vt[:, j, :], pb[:, 0:256])
        YT = k.sb([128, 2, S], BF16, "sby")
        r_e = Ring(k, 6, [128, TG], F32, "sbe")
        r_sp = Ring(k, 2, [128, TG], F32, "sbsp")
        r_sm = Ring(k, 8, [128, TG], BF16, "sbsm")
        r_u = Ring(k, 6, [128, TG], F32, "sbu")
        r_w = Ring(k, 8, [128, TG], BF16, "sbw_")
        r_wf = Ring(k, 2, [128, TG], F32, "sbwf")
        C.pool = [0, 1]

        def unit(h, qg, slot):
            hp, bp = h // 2, (h % 2) * 64
            q0 = qg * TG
            qsl = slice(q0, q0 + TG)
            tailb = C.ps[4 + slot]
            yb = C.ps[2 + slot // 2]
            last = (q0 + TG) // 128 - 1
            for kt in range(last, -1, -1):
                first = kt == last
                zb = C.bank()
                k.mm(zb[:, 0:TG], qk[bp:bp + 64, 2 + hp, kt * 128:(kt + 1) * 128], qk[bp:bp + 64, hp, qsl])
                e = r_e.next()
                k.act(e.v, zb[:, 0:TG], AF.Exp, scale=0.125)
                diag = kt * 128 >= q0
                sm = r_sm.next()
                if diag:
                    r = (kt * 128 - q0) // 128
                    sp = r_sp.next()
                    k.act(sp.v, e.v, AF.Ln, bias=C.one[:, 0:1])
                    k.tt("dve", sm.v, sp.v, C.sbm[r].v, ALU.mult)
                else:
                    k.act(sm.v, e.v, AF.Ln, bias=C.one[:, 0:1])
                u = r_u.next()
                k.stt(u.v, zb[:, 0:TG], 0.125, sm.v, ALU.mult, ALU.subtract)
                yield
                k.I("pe", "matmul", out=tailb[:, 0:TG], lhsT=C.tri_gt.v, rhs=sm.v, start=first, stop=True,
                    skip_group_check=True)
                k.tt("dve", u.v, u.v, tailb[:, 0:TG], ALU.subtract)
                w = r_w.next()
                if diag:
                    wf = r_wf.next()
                    k.act(wf.v, u.v, AF.Exp)
                    k.tt("pool", w.v, wf.v, C.sbm[r].v, ALU.mult)
                else:
                    k.act(w.v, u.v, AF.Exp)
                yield
                if kt > 0:
                    k.I("pe", "matmul", out=tailb[:, 0:TG], lhsT=C.tri_le.v, rhs=sm.v, start=False, stop=True,
                        skip_group_check=True)
                k.I("pe", "matmul", out=yb[bp:bp + 64, 0:TG], lhsT=Vt[:, kt, h * 64:(h + 1) * 64], rhs=w.v,
                    start=first, stop=(kt == 0), skip_group_check=True)
                yield
            k.cp("act", YT[bp:bp + 64, hp, qsl], yb[bp:bp + 64, 0:TG])

        units = [(h, qg) for qg in range(NTG) for h in range(4)]

        def stream(slot):
            for (h, qg) in units[slot::4]:
                yield from unit(h, qg, slot)

        interleave([stream(i) for i in range(4)])
        C.pool = list(range(8))
        k.dma(y_d.v, YT.v, q="pool")


def rope_phase(k, C, S, pos_d, rope_d):
    with k.phase():
        pi_ = k.sb([128, S], I32, "rpi")
        k.dma(pi_.v, pos_d.pbro(128))
        pf = k.sb([128, S], F32, "rpf")
        k.cp("dve", pf.v, pi_.v)
        pidx = k.sb([128, 4], I32, "rpx")
        k.I("pool", "iota", out=pidx[:, 0:1], pattern=[[0, 1]], base=0, channel_multiplier=1)
        k.ts("dve", pidx[:, 1:2], pidx[:, 0:1], 15, ALU.bitwise_and)
        k.ts("dve", pidx[:, 2:3], pidx[:, 0:1], 16, ALU.bitwise_and)
        cf = k.sb([128, 8], F32, "rcf")
        k.cp("dve", cf[:, 0:1], pidx[:, 1:2])
        k.cp("dve", cf[:, 1:2], pidx[:, 2:3])
        k.act(cf[:, 2:3], cf[:, 0:1], AF.Exp, scale=-math.log(10000.0) / 16.0)
        k.ts("dve", cf[:, 3:4], cf[:, 1:2], 0.125, ALU.mult, -1.0, ALU.add)
        ang = k.sb([128, S], F32, "rang")
        k.ts("dve", ang.v, pf.v, cf[:, 2:3], ALU.mult)
        C1, C2 = 6.28125, 2 * math.pi - 6.28125
        MAGIC = 12582912.0
        kf = k.sb([128, S], F32, "rkf")
        r = k.sb([128, S], F32, "rr")
        out = k.sb([128, S], F32, "rout")
        for which in range(2):
            off = 0.25 if which == 0 else 0.0
            k.ts("dve", kf.v, ang.v, 1.0 / (2 * math.pi), ALU.mult, off, ALU.add)
            k.ts("dve", kf.v, kf.v, MAGIC, ALU.add)
            k.ts("dve", kf.v, kf.v, MAGIC, ALU.subtract)
            k.stt(r.v, kf.v, -C1, ang.v, ALU.mult, ALU.add)
            k.stt(r.v, kf.v, -C2, r.v, ALU.mult, ALU.add)
            if which == 0:
                k.ts("dve", r.v, r.v, math.pi / 2, ALU.add)
            k.ts("dve", r.v, r.v, 3.1415925, ALU.min, -3.1415925, ALU.max)
            k.act(out.v, r.v, AF.Sin)
            if which == 1:
                k.ts("dve", out.v, out.v, cf[:, 3:4], ALU.mult)
            k.dma(rope_d[which], out.v, q="pool")


MLA_OFF = 768 + 1024 + 1028


def mla_phase(k, C, S, xt_d, win_d, qg_d, wuq_d, kvg_d, wukv_d, rope_d, y_d):
    TG = min(512, S)
    NTG = S // TG
    NT = S // 128
    TPG = TG // 128
    sb_consts(k, C, TG)
    SC = 96.0 ** -0.5
    with k.phase():
        C.cam = [C.mask(TG, -r * 128, -1, ALU.is_ge, F32, "cam%d" % r, local=True) for r in range(TG // 128)]
        WL = WLoader(k, 8 * 512, nstage=2, nbuf=1, name="mlw")
        wm = WL.load(win_d[:, 0:416], 8, 416)
        wkr2 = k.sb([128, 8, 64], BF16, "mlkr")
        k.cp("pool", wkr2[:, :, 0:32], wm[:, :, 384:416])
        k.cp("pool", wkr2[:, :, 32:48], wm[:, :, 400:416])
        k.cp("pool", wkr2[:, :, 48:64], wm[:, :, 384:400])
        gq = k.sb([128, 4], F32, "mlg")
        for c in range(2):
            k.dma(gq[:, c:c + 1], qg_d[c * 128:(c + 1) * 128].re("(p o) -> p o", o=1))
        k.dma(gq[:, 2:3], kvg_d.re("(p o) -> p o", o=1))
        st = k.sb([128, 2 * 384], F32, "mlst")
        k.dma(st.v.re("p (c n) -> p c n", c=2), wuq_d.re("(c p) n -> p c n", p=128))
        wuq = k.sb([128, 2, 384], BF16, "mluq")
        for c in range(2):
            k.ts("pool", wuq[:, c, :], st[:, c * 384:(c + 1) * 384], gq[:, c:c + 1], ALU.mult)
        wuqs = k.sb([128, 2, 128], BF16, "mluqs")
        for h in range(4):
            k.cp("pool", wuqs[:, :, h * 32:h * 32 + 16], wuq[:, :, h * 96 + 80:h * 96 + 96])
            k.cp("pool", wuqs[:, :, h * 32 + 16:h * 32 + 32], wuq[:, :, h * 96 + 64:h * 96 + 80])
        st2 = k.sb([128, 512], F32, "mlst2")
        k.dma(st2.v, wukv_d)
        wukv = k.sb([128, 512], BF16, "mlukv")
        k.ts("pool", wukv.v, st2.v, gq[:, 2:3], ALU.mult)
        wv = k.sb([128, 256], BF16, "mlwv")
        for h in range(4):
            k.cp("pool", wv[:, h * 64:(h + 1) * 64], wukv[:, h * 128 + 64:h * 128 + 128])
        cs = k.sb([128, 2, S], F32, "mlcs")
        for w_ in range(2):
            k.dma(cs[:, w_, :], rope_d[w_])
        cT = k.sb([128, 3, S], BF16, "mlcT")
        qT = k.sb([128, 4, S], BF16, "mlqT")
        kT = k.sb([128, 4, S], BF16, "mlkT")
        Vt = k.sb([128, NT, 256], BF16, "mlV")
        r_xt = Ring(k, 2, [128, 8, TG], BF16, "mlxt")
        r_c = Ring(k, 2, [128, 384], BF16, "mlc")
        r_j = Ring(k, 2, [128, 256], F32, "mlj")
        r_s = Ring(k, 2, [128, 8], F32, "mls")
        r_t = Ring(k, 4, [128, TG], F32, "mlt")
        for tg in range(NTG):
            tsl = slice(tg * TG, (tg + 1) * TG)
            XT = r_xt.next()
            k.dma(XT.v, xt_d[:, :, tsl])
            for jj in range(TPG):
                j = tg * TPG + jj
                pb = C.bank()
                for kc in range(8):
                    k.mm(pb[:, 0:384], XT[:, kc, jj * 128:(jj + 1) * 128], wm[:, kc, 0:384], start=(kc == 0), stop=(kc == 7))
                s = r_s.next()
                jk = r_j.next()
                k.act(jk[:, 0:256], pb[:, 0:256], AF.Square, accum_out=s[:, 0:1])
                k.act(jk[:, 0:128], pb[:, 256:384], AF.Square, accum_out=s[:, 1:2])
                k.act(s[:, 2:3], s[:, 0:1], AF.Ln, scale=1.0 / 256, bias=C.eps6[:, 0:1])
                k.act(s[:, 3:4], s[:, 1:2], AF.Ln, scale=1.0 / 128, bias=C.eps6[:, 0:1])
                k.act(s[:, 4:6], s[:, 2:4], AF.Exp, scale=-0.5)
                cb = r_c.next()
                k.ts("dve", cb[:, 0:256], pb[:, 0:256], s[:, 4:5], ALU.mult)
                k.ts("dve", cb[:, 256:384], pb[:, 256:384], s[:, 5:6], ALU.mult)
                pv = C.bank().v.bc(BF16)
                for c in range(3):
                    k.tr(pv[:, c * 128:(c + 1) * 128], cb[:, c * 128:(c + 1) * 128], C.id_b.v)
                k.cp("act", cT[:, :, j * 128:(j + 1) * 128], pv[:, 0:384].re("p (c t) -> p c t", c=3))
                pvb = C.bank()
                k.mm(pvb[:, 0:256], cT[:, 2, j * 128:(j + 1) * 128], wv.v)
                k.cp("act", Vt[:, j, :], pvb[:, 0:256])
            p1 = C.bank()
            p2 = C.bank()
            for kc in range(8):
                k.mm(p1[64:96, 0:TG], wkr2[:, kc, 0:32], XT[:, kc, :], start=(kc == 0), stop=(kc == 7))
            for kc in range(8):
                k.mm(p2[64:96, 0:TG], wkr2[:, kc, 32:64], XT[:, kc, :], start=(kc == 0), stop=(kc == 7))
            t1 = r_t.next()
            t2 = r_t.next()
            k.tt("dve", t1[64:96, :], p1[64:96, 0:TG], cs[64:96, 0, tsl], ALU.mult)
            k.tt("dve", t2[64:96, :], p2[64:96, 0:TG], cs[64:96, 1, tsl], ALU.mult)
            k.tt("dve", kT[64:96, 0, tsl], t1[64:96, :], t2[64:96, :], ALU.add)
            for h in range(1, 4):
                k.cp("pool", kT[64:96, h, tsl], kT[64:96, 0, tsl])
            for h in range(4):
                pk = C.bank()
                k.mm(pk[0:64, 0:TG], wukv[:, h * 128:h * 128 + 64], cT[:, 2, tsl])
                k.cp("act", kT[0:64, h, tsl], pk[0:64, 0:TG])
                pq = C.bank()
                for c in range(2):
                    k.mm(pq[0:96, 0:TG], wuq[:, c, h * 96:(h + 1) * 96], cT[:, c, tsl], start=(c == 0), stop=(c == 1))
                pq2 = C.bank()
                for c in range(2):
                    k.mm(pq2[64:96, 0:TG], wuqs[:, c, h * 32:(h + 1) * 32], cT[:, c, tsl], start=(c == 0), stop=(c == 1))
                k.cp("act", qT[0:64, h, tsl], pq[0:64, 0:TG])
                t1 = r_t.next()
                t2 = r_t.next()
                k.tt("dve", t1[64:96, :], pq[64:96, 0:TG], cs[64:96, 0, tsl], ALU.mult)
                k.tt("dve", t2[64:96, :], pq2[64:96, 0:TG], cs[64:96, 1, tsl], ALU.mult)
                k.tt("dve", qT[64:96, h, tsl], t1[64:96, :], t2[64:96, :], ALU.add)
        YT = k.sb([128, 2, S], BF16, "mly")
        r_p = Ring(k, 8, [128, TG], BF16, "mlp")
        r_pf = Ring(k, 4, [128, TG], F32, "mlpf")
        r_d = Ring(k, 4, [128, TG], F32, "mld")
        C.pool = [0, 1, 2, 3]

        def unit(h, qg, slot):
            hp, bp = h // 2, (h % 2) * 64
            q0 = qg * TG
            qsl = slice(q0, q0 + TG)
            db = C.ps[6 + slot // 2]
            yb = C.ps[4 + slot // 2]
            last = (q0 + TG) // 128 - 1
            for kt in range(last + 1):
                zb = C.bank()
                k.mm(zb[:, 0:TG], kT[0:96, h, kt * 128:(kt + 1) * 128], qT[0:96, h, qsl])
                p = r_p.next()
                if kt * 128 >= q0:
                    r = (kt * 128 - q0) // 128
                    pf = r_pf.next()
                    k.act(pf.v, zb[:, 0:TG], AF.Exp, scale=SC)
                    k.tt("pool", p.v, pf.v, C.cam[r].v, ALU.mult)
                else:
                    k.act(p.v, zb[:, 0:TG], AF.Exp, scale=SC)
                yield
                k.I("pe", "matmul", out=yb[bp:bp + 64, 0:TG], lhsT=Vt[:, kt, h * 64:(h + 1) * 64], rhs=p.v,
                    start=(kt == 0), stop=(kt == last), skip_group_check=True)
                k.I("pe", "matmul", out=db[bp:bp + 64, 0:TG], lhsT=C.ones_b[:, 0:64], rhs=p.v,
                    start=(kt == 0), stop=(kt == last), skip_group_check=True)
            rd = r_d.next()
            k.I("dve", "reciprocal", out=rd[bp:bp + 64, :], in_=db[bp:bp + 64, 0:TG])
            k.tt("dve", YT[bp:bp + 64, hp, qsl], yb[bp:bp + 64, 0:TG], rd[bp:bp + 64, :], ALU.mult)
            yield

        units = [(h, qg) for qg in range(NTG) for h in range(4)]

        def stream(slot):
            for (h, qg) in units[slot::4]:
                yield from unit(h, qg, slot)

        interleave([stream(i) for i in range(4)])
        C.pool = list(range(8))
        k.dma(y_d.v, YT.v, q="pool")


def load_cols(k, dv, nchunk, name="pc", p=128):
    t = k.sb([128, nchunk], F32, name)
    for c in range(nchunk):
        k.dma(t[0:p, c:c + 1], dv[c * p:(c + 1) * p].re("(p o) -> p o", o=1))
    return t


RW_OFF = 768
EXPM05 = math.exp(-0.5)


def rwkv_phase(k, C, S, xt_d, win_d, prm, y_d, stop=None):
    RG = min(128, S)
    NRG = S // RG
    CH = 64
    NCH = RG // CH
    NQ = 14
    with k.phase():
        o64 = C.ones_f[0:64, 0:256].re("p (h s) -> p h s", h=4)
        ones64 = C.ones_b[0:64, 0:64]
        idb64 = C.id_b[0:64, 0:64]
        mLs = k.sb([64, 4, 64], F32, "rwLs")
        k.aselect(mLs.v, o64, [[0, 4], [-1, 64]], ALU.is_gt, 0.0, 0, 1)
        mUs = k.sb([64, 4, 64], F32, "rwUs")
        k.aselect(mUs.v, o64, [[0, 4], [1, 64]], ALU.is_gt, 0.0, 0, -1)
        mUi = k.sb([64, 4, 64], F32, "rwUi")
        k.aselect(mUi.v, o64, [[0, 4], [1, 64]], ALU.is_ge, 0.0, 0, -1)
        mI = k.sb([64, 4, 64], F32, "rwI")
        k.aselect(mI.v, o64, [[0, 4], [1, 64]], ALU.is_equal, 0.0, 0, -1)
        rmi = k.sb([64, 4 * RG], I32, "rwrmi")
        k.I("pool", "iota", out=rmi.v, pattern=[[1, 4 * RG]], base=0, channel_multiplier=0)
        k.ts("dve", rmi.v, rmi.v, CH - 1, ALU.bitwise_and)
        rmask = k.sb([64, 4 * RG], F32, "rwrm")
        k.cp("dve", rmask.v, rmi.v)
        k.ts("dve", rmask.v, rmask.v, 1.0, ALU.min)
        mu = load_cols(k, prm["mu"][0:896], NQ, "rwmu", p=64)
        mug = load_cols(k, prm["mu"][896:1024], 1, "rwmug")
        w0 = load_cols(k, prm["w0"], 4, "rww0", p=64)
        a0 = load_cols(k, prm["a0"], 4, "rwa0", p=64)
        kk_ = load_cols(k, prm["k_k"], 4, "rwkk", p=64)
        ka = load_cols(k, prm["k_a"], 4, "rwka", p=64)
        rk = load_cols(k, prm["r_k"], 4, "rwrk", p=64)
        gng = load_cols(k, prm["gn_g"], 4, "rwgg", p=64)
        gnb = load_cols(k, prm["gn_b"], 4, "rwgb", p=64)
        omka = k.sb([128, 4], F32, "rwomka")
        k.ts("dve", omka[0:64, :], ka[0:64, :], -1.0, ALU.mult, 1.0, ALU.add)
        eps_gn = k.sb([128, 1], F32, "rwepsgn")
        k.memset("pool", eps_gn.v, 64e-5)
        WL = WLoader(k, 8 * 512, nstage=1, nbuf=2, name="rww")
        wrw = [WL.load(win_d[:, hh * 512:(hh + 1) * 512], 8, 512) for hh in range(2)]
        lst = k.sb([128, 768], F32, "rwlst")
        k.dma(lst[0:64, 0:256], prm["w_up"])
        k.dma(lst[0:64, 256:512], prm["a_up"])
        k.dma(lst[:, 512:768], prm["g_up"])
        lup = k.sb([128, 768], BF16, "rwlup")
        k.cp("pool", lup[0:64, 0:512], lst[0:64, 0:512])
        k.cp("pool", lup[:, 512:768], lst[:, 512:768])
        ST = k.sb([64, 4, 64], F32, "rwST")
        k.memset("pool", ST.v, 0.0)
        carry = k.sb([64, NQ, 1], F32, "rwcar")
        k.memset("pool", carry.v, 0.0)
        carryg = k.sb([128, 1], F32, "rwcarg")
        k.memset("pool", carryg.v, 0.0)
        SG = min(512, S)
        GPS = SG // RG
        r_xt = Ring(k, 1, [128, 8, SG], BF16, "rwxt")
        pT = k.sb([64, NQ, SG + 1], F32, "rwpT")
        pG = k.sb([128, SG + 1], F32, "rwpG")
        ps = k.sb([64, NQ, RG], F32, "rwps")
        psg = k.sb([128, RG], F32, "rwpsg")
        tmp = Ring(k, 4, [64, 4, RG], F32, "rwtmp")
        tmp2 = Ring(k, 3, [64, 4, RG], F32, "rwtmp2")
        tmpg = k.sb([128, RG], F32, "rwtmpg")
        r_bf = Ring(k, 2, [128, RG], BF16, "rwbf")

        def arr(name):
            return k.sb([64, 4, RG], F32, name)

        A_a, A_kap, A_kp, A_lw, A_cl = arr("rwa"), arr("rwkap"), arr("rwkp"), arr("rwlw"), arr("rwcl")
        def arr16(name):
            return k.sb([64, 4, RG], BF16, name)

        A_kt, A_bt, A_kbar, A_bbar, A_y = arr16("rwkt"), arr16("rwbt"), arr16("rwkbar"), arr16("rwbbar"), arr("rwy")
        A_yb, Vb = arr16("rwyb"), arr16("rwvb")
        tmpb = Ring(k, 3, [64, 4, RG], BF16, "rwtmpb")
        STb = k.sb([64, 4, 64], BF16, "rwSTb")
        k.memset("pool", STb.v, 0.0)
        yout = Ring(k, 2, [64, 4, RG], BF16, "rwyo")
        sg = k.sb([128, RG], BF16, "rwsg")

        def cset(n):
            return [dict(G=[k.sb([64, 4, 64], BF16, "rwG%d" % i) for i in range(5)],
                         Pb=k.sb([64, 4, 64], BF16, "rwPb"),
                         N=[k.sb([64, 4, 64], BF16, "rwN%d" % i) for i in range(2)],
                         M=[k.sb([64, 4, 64], BF16, "rwM%d" % i) for i in range(2)],
                         P=[k.sb([64, 4, 64], BF16, "rwP%d" % i) for i in range(2)],
                         tok=[k.sb([64, 4, 64], BF16, "rwtok%d" % i) for i in range(3)]) for _ in range(n)]

        def mkset():
            return dict(A_rt=arr16("rwrt"), A_kapt=arr16("rwkapt"), A_g=arr("rwg"), A_bon=arr("rwbon"),
                        gC=k.sb([64, 4, NCH], F32, "rwgC"), CS=cset(NCH), curP=[None] * NCH)

        SETS = [mkset(), mkset()]
        Wt = k.sb([64, 4, 64], BF16, "rwW")
        Ut = k.sb([64, 4, 64], BF16, "rwU")

        def v4(bank):
            return bank[0:64, 0:256].re("p (h s) -> p h s", h=4)

        def fm(Aarr, h, ch):
            return Aarr[:, h, ch * CH:(ch + 1) * CH]

        def gen_AB(rg, B):
            A_rt, A_kapt, A_g, A_bon, gC, CS = B["A_rt"], B["A_kapt"], B["A_g"], B["A_bon"], B["gC"], B["CS"]
            tsl = slice(rg * RG, (rg + 1) * RG)
            o = (rg % GPS) * RG
            if rg % GPS == 0:
                XT = r_xt.next()
                k.dma(XT.v, xt_d[:, :, rg * RG:rg * RG + SG])
                k.cp("pool", pT[:, :, 0:1], carry.v)
                k.cp("pool", pG[:, 0:1], carryg.v)
                for q in range(NQ):
                    c0 = q * 64
                    pb = C.bank()
                    for kc in range(8):
                        k.mm(pb[0:64, 0:SG], wrw[c0 // 512][:, kc, c0 % 512:c0 % 512 + 64], XT[:, kc, :],
                             start=(kc == 0), stop=(kc == 7))
                    k.cp("act", pT[:, q, 1:SG + 1], pb[0:64, 0:SG])
                    if q % 2 == 1:
                        yield
                pb = C.bank()
                for kc in range(8):
                    k.mm(pb[:, 0:SG], wrw[1][:, kc, 384:512], XT[:, kc, :], start=(kc == 0), stop=(kc == 7))
                k.cp("act", pG[:, 1:SG + 1], pb[:, 0:SG])
                k.cp("pool", carry.v, pT[:, :, SG:SG + 1])
                k.cp("pool", carryg.v, pG[:, SG:SG + 1])
                yield
            for q0 in range(0, NQ, 4):
                nq = min(4, NQ - q0)
                t = tmp.next()
                k.tt("pool", t[:, 0:nq, :], pT[:, q0:q0 + nq, o:o + RG], pT[:, q0:q0 + nq, o + 1:o + RG + 1], ALU.subtract)
                for q in range(q0, q0 + nq):
                    k.stt(ps[:, q, :], t[:, q - q0, :], mu[0:64, q:q + 1], pT[:, q, o + 1:o + RG + 1], ALU.mult, ALU.add)
                yield
            k.tt("pool", tmpg.v, pG[:, o:o + RG], pG[:, o + 1:o + RG + 1], ALU.subtract)
            k.stt(psg.v, tmpg.v, mug[:, 0:1], pG[:, o + 1:o + RG + 1], ALU.mult, ALU.add)
            R_, K_, V_ = ps[:, 0:4, :], ps[:, 4:8, :], ps[:, 8:12, :]
            tw = r_bf.next()
            k.act(tw[0:64, :], ps[:, 12, :], AF.Tanh)
            alo = r_bf.next()
            k.cp("pool", alo[0:64, :], ps[:, 13, :])
            k.act(sg.v, psg.v, AF.Sigmoid)
            yield
            for hq in range(2):
                pw = C.bank()
                pa = C.bank()
                pg = C.bank()
                for hh in range(2):
                    h = hq * 2 + hh
                    k.mm(pw[0:64, hh * RG:(hh + 1) * RG], lup[0:64, h * 64:(h + 1) * 64], tw[0:64, :])
                    k.mm(pa[0:64, hh * RG:(hh + 1) * RG], lup[0:64, 256 + h * 64:256 + (h + 1) * 64], alo[0:64, :])
                    k.mm(pg[0:64, hh * RG:(hh + 1) * RG], lup[:, 512 + h * 64:512 + (h + 1) * 64], sg.v)
                for hh in range(2):
                    h = hq * 2 + hh
                    k.act(A_lw[:, h, :], pw[0:64, hh * RG:(hh + 1) * RG], AF.Sigmoid, bias=w0[0:64, h:h + 1])
                    k.act(A_a[:, h, :], pa[0:64, hh * RG:(hh + 1) * RG], AF.Sigmoid, bias=a0[0:64, h:h + 1])
                    k.cp("act", A_g[:, h, :], pg[0:64, hh * RG:(hh + 1) * RG])
                yield
            k.ts("pool", A_lw.v, A_lw.v, -EXPM05, ALU.mult)
            kkv = tmp.next()
            for h in range(4):
                k.ts("dve", kkv[:, h, :], K_[:, h, :], kk_[0:64, h:h + 1], ALU.mult)
            sq = tmpb.next()
            k.tt("pool", sq.v, kkv.v, kkv.v, ALU.mult)
            k.cp("pool", Vb.v, V_)
            yield
            rs = tmp.next()
            for hq in range(2):
                pss = C.bank()
                for hh in range(2):
                    k.mm(pss[0:64, hh * RG:(hh + 1) * RG], ones64, sq[:, hq * 2 + hh, :])
                k.ts("dve", rs[:, hq * 2:hq * 2 + 2, :], pss[0:64, 0:2 * RG].re("p (h t) -> p h t", h=2), 1e-24, ALU.max)
            k.act(rs.v, rs.v, AF.Ln)
            k.act(rs.v, rs.v, AF.Exp, scale=-0.5)
            k.tt("dve", A_kap.v, kkv.v, rs.v, ALU.mult)
            yield
            k.I("dve", "tensor_tensor_scan", out=A_cl.v.re("p h t -> p (h t)"), data0=rmask.v,
                data1=A_lw.v.re("p h t -> p (h t)"), initial=0.0, op0=ALU.mult, op1=ALU.add)
            t = tmp.next()
            for h in range(4):
                k.ts("dve", t[:, h, :], A_a[:, h, :], ka[0:64, h:h + 1], ALU.mult, omka[0:64, h:h + 1], ALU.add)
            k.tt("pool", A_kp.v, K_, t.v, ALU.mult)
            yield
            t2 = tmpb.next()
            for h in range(4):
                k.stt(t2[:, h, :], R_[:, h, :], rk[0:64, h:h + 1], A_kp[:, h, :], ALU.mult, ALU.mult)
            for hq in range(2):
                pbn = C.bank()
                for hh in range(2):
                    k.mm(pbn[0:64, hh * RG:(hh + 1) * RG], ones64, t2[:, hq * 2 + hh, :])
                k.tt("dve", A_bon[:, hq * 2:hq * 2 + 2, :], pbn[0:64, 0:2 * RG].re("p (h t) -> p h t", h=2),
                     V_[:, hq * 2:hq * 2 + 2, :], ALU.mult)
            yield
            ep = tmp.next()
            k.act(ep.v, A_cl.v, AF.Exp)
            k.tt("pool", A_rt.v, R_, ep.v, ALU.mult)
            en = tmp.next()
            k.act(en.v, A_cl.v, AF.Exp, scale=-1.0)
            k.tt("dve", A_kt.v, A_kp.v, en.v, ALU.mult)
            b_ = tmp.next()
            k.tt("pool", b_.v, A_kap.v, A_a.v, ALU.mult)
            k.tt("dve", A_bt.v, b_.v, en.v, ALU.mult)
            yield
            t3 = tmp.next()
            k.tt("pool", t3.v, A_cl.v, A_lw.v, ALU.subtract)
            k.act(t3.v, t3.v, AF.Exp)
            k.tt("dve", A_kapt.v, A_kap.v, t3.v, ALU.mult)
            clv = A_cl.v.re("p h (c t) -> p (h c) t", t=CH)
            t4 = tmp.next()
            t4v = t4.v.re("p h (c t) -> p (h c) t", t=CH)
            k.tt("pool", t4v, clv[:, :, CH - 1:CH].bro([64, 4 * NCH, CH]), clv, ALU.subtract)
            k.act(t4.v, t4.v, AF.Exp)
            k.tt("dve", A_kbar.v, A_kp.v, t4.v, ALU.mult)
            k.tt("pool", A_bbar.v, b_.v, t4.v, ALU.mult)
            k.act(gC.v.re("p h c -> p (h c)"), clv[:, :, CH - 1], AF.Exp)
            yield
            for ch in range(NCH):
                cs = CS[ch]
                specs = [(A_kapt, A_bt, mLs, 1.0), (A_bt, A_kapt, mUs, 1.0), (A_kt, A_kapt, mUs, 1.0),
                         (A_kt, A_rt, mUi, 1.0), (A_bt, A_rt, mUi, -1.0)]
                for gi, (La, Ra, msk, sgn) in enumerate(specs):
                    pb = C.bank()
                    for h in range(4):
                        k.mm(pb[0:64, h * 64:(h + 1) * 64], fm(La, h, ch), fm(Ra, h, ch))
                    if sgn == 1.0:
                        k.tt("dve", cs["G"][gi].v, v4(pb), msk.v, ALU.mult)
                    else:
                        k.stt(cs["G"][gi].v, v4(pb), sgn, msk.v, ALU.mult, ALU.mult)
                    yield
                k.tt("pool", cs["P"][0].v, mI.v, cs["G"][1].v, ALU.subtract)
                for ti, sgn in enumerate([1.0, 1.0, -1.0]):
                    pbb = C.bank().v.bc(BF16)
                    for h in range(4):
                        src = fm(Vb if ti == 0 else (A_kbar if ti == 1 else A_bbar), h, ch)
                        k.tr(pbb[0:64, h * 64:(h + 1) * 64], src, idb64)
                    pv_ = pbb[0:64, 0:256].re("p (h s) -> p h s", h=4)
                    if sgn == 1.0:
                        k.cp("act", cs["tok"][ti].v, pv_)
                    else:
                        k.act(cs["tok"][ti].v, pv_, AF.Copy, scale=-1.0)
                    yield
            curN = [CS[ch]["G"][0] for ch in range(NCH)]
            curM = [CS[ch]["G"][1] for ch in range(NCH)]
            curP = [CS[ch]["P"][0] for ch in range(NCH)]
            for st in range(5):
                lastst = st == 4
                for ch in range(NCH):
                    cs = CS[ch]
                    Nn, Mn, Pn = cs["N"][st % 2], cs["M"][st % 2], cs["P"][(st + 1) % 2]
                    pb = C.bank()
                    for h in range(4):
                        k.mm(pb[0:64, h * 64:(h + 1) * 64], curM[ch][:, h, :], curN[ch][:, h, :])
                    k.cp("act", Nn.v, v4(pb))
                    if not lastst:
                        pb2 = C.bank()
                        for h in range(4):
                            k.mm(pb2[0:64, h * 64:(h + 1) * 64], curN[ch][:, h, :], curM[ch][:, h, :])
                        k.cp("act", Mn.v, v4(pb2))
                    pb3 = C.bank()
                    for h in range(4):
                        k.mm(pb3[0:64, h * 64:(h + 1) * 64], Nn[:, h, :], curP[ch][:, h, :])
                    if lastst:
                        k.tt("dve", cs["Pb"].v, v4(pb3), curP[ch].v, ALU.add)
                        Pn = cs["Pb"]
                    else:
                        k.tt("dve", Pn.v, v4(pb3), curP[ch].v, ALU.add)
                    curN[ch], curM[ch], curP[ch] = Nn, Mn, Pn
                    yield
            B["curP"] = curP

        def gen_CD(rg, B):
            A_rt, A_kapt, A_g, A_bon, gC, CS = B["A_rt"], B["A_kapt"], B["A_g"], B["A_bon"], B["gC"], B["CS"]
            curP = B["curP"]
            tsl = slice(rg * RG, (rg + 1) * RG)
            for ch in range(NCH):
                cs = CS[ch]
                TT = curP[ch]
                Vt_, KB, BBn = cs["tok"]
                AkkT, ArkT, ArbTn = cs["G"][2], cs["G"][3], cs["G"][4]
                pw = C.bank()
                for h in range(4):
                    k.I("pe", "matmul", out=pw[0:64, h * 64:(h + 1) * 64], lhsT=fm(A_kapt, h, ch), rhs=STb[:, h, :],
                        start=True, stop=False, skip_group_check=True)
                    k.I("pe", "matmul", out=pw[0:64, h * 64:(h + 1) * 64], lhsT=AkkT[:, h, :], rhs=Vt_[:, h, :],
                        start=False, stop=True, skip_group_check=True)
                k.cp("act", Wt.v, v4(pw))
                yield
                pu = C.bank()
                for h in range(4):
                    k.mm(pu[0:64, h * 64:(h + 1) * 64], TT[:, h, :], Wt[:, h, :])
                k.cp("act", Ut.v, v4(pu))
                yield
                py = C.bank()
                pst = C.bank()
                for h in range(4):
                    yo = py[0:64, h * 64:(h + 1) * 64]
                    k.I("pe", "matmul", out=yo, lhsT=STb[:, h, :], rhs=fm(A_rt, h, ch), start=True, stop=False,
                        skip_group_check=True)
                    k.I("pe", "matmul", out=yo, lhsT=Vt_[:, h, :], rhs=ArkT[:, h, :], start=False, stop=False,
                        skip_group_check=True)
                    k.I("pe", "matmul", out=yo, lhsT=Ut[:, h, :], rhs=ArbTn[:, h, :], start=False, stop=True,
                        skip_group_check=True)
                    so = pst[0:64, h * 64:(h + 1) * 64]
                    k.I("pe", "matmul", out=so, lhsT=KB[:, h, :], rhs=Vt_[:, h, :], start=True, stop=False,
                        skip_group_check=True)
                    k.I("pe", "matmul", out=so, lhsT=BBn[:, h, :], rhs=Ut[:, h, :], start=False, stop=True,
                        skip_group_check=True)
                k.cp("act", A_y[:, :, ch * CH:(ch + 1) * CH], v4(py))
                for h in range(4):
                    k.stt(ST[:, h, :], ST[:, h, :], gC[:, h, ch:ch + 1], pst[0:64, h * 64:(h + 1) * 64], ALU.mult, ALU.add)
                k.cp("pool", STb.v, ST.v)
                yield
            yo_t = yout.next()
            yc = tmp2.next()
            k.cp("pool", A_yb.v, A_y.v)
            for hq in range(2):
                pm = C.bank()
                for hh in range(2):
                    k.mm(pm[0:64, hh * RG:(hh + 1) * RG], ones64, A_yb[:, hq * 2 + hh, :])
                k.stt(yc[:, hq * 2:hq * 2 + 2, :], pm[0:64, 0:2 * RG].re("p (h t) -> p h t", h=2), -1.0 / 64,
                      A_y[:, hq * 2:hq * 2 + 2, :], ALU.mult, ALU.add)
            sq = tmpb.next()
            k.tt("pool", sq.v, yc.v, yc.v, ALU.mult)
            yield
            rs = tmp2.next()
            for hq in range(2):
                pvv = C.bank()
                for hh in range(2):
                    k.mm(pvv[0:64, hh * RG:(hh + 1) * RG], ones64, sq[:, hq * 2 + hh, :])
                k.act(rs[:, hq * 2:hq * 2 + 2, :], pvv[0:64, 0:2 * RG].re("p (h t) -> p h t", h=2), AF.Ln, scale=1.0 / 64,
                      bias=eps_gn[0:64, 0:1])
            k.act(rs.v, rs.v, AF.Exp, scale=-0.5)
            k.tt("dve", yc.v, yc.v, rs.v, ALU.mult)
            yield
            for h in range(4):
                k.ts("dve", yc[:, h, :], yc[:, h, :], gng[0:64, h:h + 1], ALU.mult, gnb[0:64, h:h + 1], ALU.add)
            k.tt("pool", yc.v, yc.v, A_bon.v, ALU.add)
            k.tt("pool", yo_t.v, yc.v, A_g.v, ALU.mult)
            for h in range(4):
                k.dma(y_d[(h % 2) * 64:(h % 2) * 64 + 64, h // 2, tsl], yo_t[:, h, :], q="pool")
            yield

        for step in range(NRG + 1):
            gens = []
            if step < NRG:
                gens.append(gen_AB(step, SETS[step % 2]))
            if step >= 1:
                gens.append(gen_CD(step - 1, SETS[(step - 1) % 2]))
            interleave(gens)


SSD_OFF = 768 + 1024


def ssd_phase(k, C, S, xt_d, win_d, prm, y_d):
    TG = min(512, S)
    NTG = S // TG
    TPG = TG // 128
    with k.phase():
        Ui = k.sb([128, 128], F32, "sdUi")
        k.aselect(Ui.v, C.ones_f[:, 0:128], [[1, 128]], ALU.is_ge, 0.0, 0, -1)
        WL = WLoader(k, 8 * 512, nstage=2, nbuf=2, name="sdw")
        w_zx = WL.load(win_d[:, 0:512], 8, 512)
        w_bc = WL.load(win_d[:, 512:1024], 8, 512)
        st = k.sb([128, 8, 4], F32, "sdst")
        k.dma(st.v, win_d[:, 1024:1028].re("(c p) n -> p c n", p=128))
        w_dt = k.sb([128, 8, 4], BF16, "sdwdt")
        k.cp("pool", w_dt.v, st.v)
        cw = k.sb([128, 6, 4], F32, "sdcw")
        for n in range(6):
            for j in range(4):
                k.dma(cw[:, n, j:j + 1], prm["conv_w"][j, n * 128:(n + 1) * 128].re("(p o) -> p o", o=1))
        cb = load_cols(k, prm["conv_b"], 6, "sdcb")
        dtb = load_bcast(k, prm["dt_bias"], 4, "sddtb")
        alog = load_bcast(k, prm["a_log"], 4, "sdalog")
        dsk = load_bcast(k, prm["d"], 4, "sddsk")
        ng = load_bcast(k, prm["norm_g"], 256, "sdng")
        aneg = k.sb([128, 4], F32, "sdaneg")
        k.act(aneg.v, alog.v, AF.Exp)
        k.ts("dve", aneg.v, aneg.v, -1.0, ALU.mult)
        eps5g = C.eps5
        Sst = k.sb([128, 4, 64], F32, "sdS")
        k.memset("pool", Sst.v, 0.0)
        Sbf = k.sb([128, 4, 64], BF16, "sdSb")
        k.memset("pool", Sbf.v, 0.0)
        xraw = k.sb([128, 6, TG + 3], F32, "sdxr")
        k.memset("pool", xraw[:, :, 0:3], 0.0)
        xc = k.sb([128, 6, TG], F32, "sdxc")
        bcb = k.sb([128, 4, TG], BF16, "sdbcb")
        r_xt = Ring(k, 2, [128, 8, TG], BF16, "sdxt")
        r_acc = Ring(k, 2, [128, TG], F32, "sdacc")
        r_s = Ring(k, 4, [128, 48], F32, "sds")
        r_m = Ring(k, 8, [128, 128], F32, "sdm")
        r_mb = Ring(k, 20, [128, 128], BF16, "sdmb")
        r_tok = Ring(k, 3, [128, 512], F32, "sdtok")
        r_tb = Ring(k, 3, [128, 768], BF16, "sdtb")
        r_y = Ring(k, 3, [128, 256], F32, "sdy")
        r_z = Ring(k, 3, [128, 256], F32, "sdz")
        r_gt = Ring(k, 3, [128, 256], F32, "sdgt")
        r_yb = Ring(k, 3, [128, 256], BF16, "sdyb")
        r_yo = Ring(k, 2, [128, 2, TG], BF16, "sdyo")
        for tg in range(NTG):
            tsl = slice(tg * TG, (tg + 1) * TG)
            XT = r_xt.next()
            k.dma(XT.v, xt_d[:, :, tsl])
            for n in range(6):
                pb = C.bank()
                wsl = w_zx[:, :, 256 + n * 128:256 + (n + 1) * 128] if n < 2 else w_bc[:, :, (n - 2) * 128:(n - 1) * 128]
                for kc in range(8):
                    k.mm(pb[:, 0:TG], wsl[:, kc, :], XT[:, kc, :], start=(kc == 0), stop=(kc == 7))
                k.cp("act", xraw[:, n, 3:TG + 3], pb[:, 0:TG])
                acc = r_acc.next()
                k.ts("dve", acc.v, xraw[:, n, 3:TG + 3], cw[:, n, 3:4], ALU.mult, cb[:, n:n + 1], ALU.add)
                for j in (2, 1, 0):
                    k.stt(acc.v, xraw[:, n, j:TG + j], cw[:, n, j:j + 1], acc.v, ALU.mult, ALU.add)
                k.act(xc[:, n, :], acc.v, AF.Silu)
                if n >= 2:
                    k.cp("pool", bcb[:, n - 2, :], xc[:, n, :])
            yo = r_yo.next()

            def tile(jj, slot, XT=XT, yo=yo):
                lsl = slice(jj * 128, (jj + 1) * 128)
                s = r_s.next()
                py = C.ps[4 + 2 * slot]
                pst = C.ps[5 + 2 * slot]
                pd = C.bank()
                for kc in range(8):
                    k.mm(pd[:, 0:4], XT[:, kc, lsl], w_dt[:, kc, :], start=(kc == 0), stop=(kc == 7))
                k.tt("dve", s[:, 0:4], pd[:, 0:4], dtb.v, ALU.add)
                k.act(s[:, 0:4], s[:, 0:4], AF.Exp)
                k.act(s[:, 4:8], s[:, 0:4], AF.Ln, bias=C.one[:, 0:1])
                k.tt("dve", s[:, 8:12], s[:, 4:8], aneg.v, ALU.mult)
                pc = C.bank()
                k.mm(pc[:, 0:4], Ui.v, s[:, 8:12])
                k.mm(pc[:, 8:12], C.ones_f[:, 0:128], s[:, 8:12])
                k.cp("dve", s[:, 12:16], pc[:, 0:4])
                k.cp("dve", s[:, 16:20], pc[:, 8:12])
                yield
                k.tt("dve", s[:, 20:24], s[:, 16:20], s[:, 12:16], ALU.subtract)
                k.act(s[:, 20:24], s[:, 20:24], AF.Exp)
                k.act(s[:, 24:28], s[:, 16:20], AF.Exp)
                pz = C.bank()
                for kc in range(8):
                    k.mm(pz[:, 0:256], XT[:, kc, lsl], w_zx[:, kc, 0:256], start=(kc == 0), stop=(kc == 7))
                zs = r_z.next()
                k.act(zs.v, pz[:, 0:256], AF.Silu)
                yield
                pt = C.bank()
                for n in range(4):
                    k.tr(pt[:, n * 128:(n + 1) * 128], xc[:, n, lsl], C.id_f.v)
                tok = r_tok.next()
                k.cp("act", tok.v, pt.v)
                yield
                tb = r_tb.next()
                for h in range(4):
                    k.ts("dve", tb[:, h * 64:(h + 1) * 64], tok[:, h * 64:(h + 1) * 64], s[:, 4 + h:5 + h], ALU.mult)
                    k.stt(tb[:, 512 + h * 64:512 + (h + 1) * 64], tok[:, h * 64:(h + 1) * 64], s[:, 4 + h:5 + h],
                          s[:, 20 + h:21 + h].bro([128, 64]), ALU.mult, ALU.mult)
                k.cp("pool", tb[:, 256:512], tok[:, 256:512])
                pg = C.bank()
                for g in range(2):
                    k.mm(pg[:, g * 128:(g + 1) * 128], bcb[:, g, lsl], bcb[:, 2 + g, lsl])
                gts = r_gt.next()
                k.cp("act", gts.v, pg[:, 0:256])
                yield
                csts = []
                for h in range(4):
                    g = h // 2
                    abc = r_m.next()
                    k.ts("dve", abc.v, C.ones_f[:, 0:128], s[:, 8 + h:9 + h], ALU.mult)
                    pr = C.bank()
                    k.mm(pr[:, 0:128], abc.v, Ui.v)
                    dm = r_m.next()
                    k.ts("dve", dm.v, pr[:, 0:128], s[:, 12 + h:13 + h], ALU.subtract, 0.0, ALU.min)
                    k.act(dm.v, dm.v, AF.Exp)
                    k.tt("dve", dm.v, dm.v, gts[:, g * 128:(g + 1) * 128], ALU.mult)
                    mh = r_mb.next()
                    k.tt("pool", mh.v, dm.v, Ui.v, ALU.mult)
                    er = r_m.next()
                    k.act(er.v, pr[:, 0:128], AF.Exp)
                    cst = r_mb.next()
                    k.tt("pool", cst.v, xc[:, 4 + g, lsl], er.v, ALU.mult)
                    csts.append(cst)
                    k.I("pe", "matmul", out=py[:, h * 64:(h + 1) * 64], lhsT=mh.v, rhs=tb[:, h * 64:(h + 1) * 64],
                        start=(h == 0), stop=False, skip_group_check=True)
                    k.mm(pst[:, h * 64:(h + 1) * 64], tb[:, 256 + g * 128:256 + (g + 1) * 128],
                         tb[:, 512 + h * 64:512 + (h + 1) * 64])
                    yield
                for h in range(4):
                    k.I("pe", "matmul", out=py[:, h * 64:(h + 1) * 64], lhsT=csts[h].v, rhs=Sbf[:, h, :],
                        start=False, stop=True, skip_group_check=True)
                for h in range(4):
                    k.stt(Sst[:, h, :], Sst[:, h, :], s[:, 24 + h:25 + h], pst[:, h * 64:(h + 1) * 64], ALU.mult, ALU.add)
                k.cp("pool", Sbf.v, Sst.v)
                y = r_y.next()
                for h in range(4):
                    k.stt(y[:, h * 64:(h + 1) * 64], tok[:, h * 64:(h + 1) * 64], dsk[:, h:h + 1], py[:, h * 64:(h + 1) * 64],
                          ALU.mult, ALU.add)
                yield
                k.tt("dve", y.v, y.v, zs.v, ALU.mult)
                for g in range(2):
                    k.act(zs[:, g * 128:(g + 1) * 128], y[:, g * 128:(g + 1) * 128], AF.Square, accum_out=s[:, 28 + g:29 + g])
                k.act(s[:, 30:32], s[:, 28:30], AF.Ln, scale=1.0 / 128, bias=eps5g[:, 0:1])
                k.act(s[:, 32:34], s[:, 30:32], AF.Exp, scale=-0.5)
                yb = r_yb.next()
                for g in range(2):
                    k.stt(yb[:, g * 128:(g + 1) * 128], y[:, g * 128:(g + 1) * 128], s[:, 32 + g:33 + g],
                          ng[:, g * 128:(g + 1) * 128], ALU.mult, ALU.mult)
                yield
                pv = C.bank().v.bc(BF16)
                for c in range(2):
                    k.tr(pv[:, c * 128:(c + 1) * 128], yb[:, c * 128:(c + 1) * 128], C.id_b.v)
                k.cp("act", yo[:, :, lsl], pv[:, 0:256].re("p (c t) -> p c t", c=2))
                yield

            def stream(par):
                if par == 1:
                    yield
                for jj in range(par, TPG, 2):
                    yield from tile(jj, par)

            C.pool = [0, 1, 2, 3]
            interleave([stream(0), stream(1)])
            C.pool = list(range(8))
            k.cp("pool", xraw[:, :, 0:3], xraw[:, :, TG:TG + 3])
            k.dma(y_d[:, :, tsl], yo.v, q="pool")


IN_SPECS = [
    ("mix_w_in", [2, 1024, 3236]), ("rw_mu", [2, 1024]), ("rw_w0", [2, 256]), ("rw_w_up", [2, 64, 256]),
    ("rw_a0", [2, 256]), ("rw_a_up", [2, 64, 256]), ("rw_g_up", [2, 128, 256]), ("rw_k_k", [2, 256]),
    ("rw_k_a", [2, 256]), ("rw_r_k", [2, 4, 64]), ("rw_gn_g", [2, 256]), ("rw_gn_b", [2, 256]),
    ("ssd_conv_w", [2, 4, 768]), ("ssd_conv_b", [2, 768]), ("ssd_dt_bias", [2, 4]), ("ssd_a_log", [2, 4]),
    ("ssd_d", [2, 4]), ("ssd_norm_g", [2, 256]), ("mla_q_norm_g", [2, 256]), ("mla_w_uq", [2, 256, 384]),
    ("mla_kv_norm_g", [2, 128]), ("mla_w_ukv", [2, 128, 512]), ("mix_w_br_out", [2, 4, 256, 1024]),
    ("mix_w_gate", [2, 4, 1024, 1024]), ("mix_w_out", [2, 1024, 1024]), ("ln1_g", [2, 1024]), ("ln1_b", [2, 1024]),
    ("xa_w_q", [2, 1024, 1024]), ("xa_w_kv", [2, 1024, 2048]), ("xa_w_o", [2, 1024, 1024]), ("ln2_g", [2, 1024]),
    ("ln2_b", [2, 1024]), ("ffn_w13", [1, 1024, 5632]), ("ffn_w2", [1, 2816, 1024]), ("moe_router", [1, 1024, 8]),
    ("moe_w13", [1, 8, 1024, 7168]), ("moe_w2", [1, 8, 3584, 1024]), ("ln3_g", [2, 1024]), ("ln3_b", [2, 1024]),
]


def build_program(S, NB, L=2):
    nc = bass.Bass("TRN2", target_bir_lowering=False)
    k = K(nc)
    x = k.dram("x", [NB, S, 1024], F32, kind="ExternalInput")
    mem = k.dram("mem", [NB, 256, 1024], F32, kind="ExternalInput")
    pos = k.dram("positions", [NB, S], I32, kind="ExternalInput")
    W = {n: k.dram(n, shp, F32, kind="ExternalInput") for n, shp in IN_SPECS}
    out = k.dram("out", [NB, S, 1024], F32, kind="ExternalOutput")
    xt = k.dram("s_xt", [128, 8, S], BF16)
    Xa = k.dram("s_xa", [S, 1024], F32)
    Xb = k.dram("s_xb", [S, 1024], F32)
    Xc = k.dram("s_xc", [S, 1024], F32)
    U = k.dram("s_u", [S, 1024], F32)
    ybr = [k.dram("s_y%d" % i, [128, 2, S], BF16) for i in range(4)]
    mT = k.dram("s_mt", [128, 8, S], BF16)
    rope = k.dram("s_rope", [2, 128, S], F32)
    C = Consts(k)
    sb_consts(k, C, min(512, S))
    for b in range(NB):
        rope_phase(k, C, S, pos[b], rope)
        xt0_phase(k, C, S, x[b], xt)
        xcur = x[b]
        for l in range(L):
            win = W["mix_w_in"][l]
            sb_phase(k, C, S, xt, win[:, 0:768], ybr[0])
            rprm = dict(mu=W["rw_mu"][l], w0=W["rw_w0"][l], w_up=W["rw_w_up"][l], a0=W["rw_a0"][l], a_up=W["rw_a_up"][l],
                        g_up=W["rw_g_up"][l], k_k=W["rw_k_k"][l], k_a=W["rw_k_a"][l],
                        r_k=W["rw_r_k"][l].re("h n -> (h n)"), gn_g=W["rw_gn_g"][l], gn_b=W["rw_gn_b"][l])
            rwkv_phase(k, C, S, xt, win[:, RW_OFF:RW_OFF + 1024], rprm, ybr[1])
            sprm = dict(conv_w=W["ssd_conv_w"][l], conv_b=W["ssd_conv_b"][l], dt_bias=W["ssd_dt_bias"][l],
                        a_log=W["ssd_a_log"][l], d=W["ssd_d"][l], norm_g=W["ssd_norm_g"][l])
            ssd_phase(k, C, S, xt, win[:, SSD_OFF:SSD_OFF + 1028], sprm, ybr[2])
            mla_phase(k, C, S, xt, win[:, MLA_OFF:MLA_OFF + 416], W["mla_q_norm_g"][l], W["mla_w_uq"][l],
                      W["mla_kv_norm_g"][l], W["mla_w_ukv"][l], rope, ybr[3])
            gate_phase(k, C, S, xt, ybr, W["mix_w_gate"][l], W["mix_w_br_out"][l], mT)
            outproj_phase(k, C, S, mT, 8, W["mix_w_out"][l], U)
            ln_phase(k, C, S, U, xcur, W["ln1_g"][l], W["ln1_b"][l], Xa, xt)
            xattn_phase(k, C, S, xt, mem[b], W["xa_w_q"][l], W["xa_w_kv"][l], W["xa_w_o"][l], U)
            ln_phase(k, C, S, U, Xa.v, W["ln2_g"][l], W["ln2_b"][l], Xb, xt)
            if l % 2 == 0:
                ffn_phase(k, C, S, xt, Xb.v, U, [(W["ffn_w13"][l // 2], W["ffn_w2"][l // 2], 2816)])
            else:
                ex = [(W["moe_w13"][l // 2, e], W["moe_w2"][l // 2, e], 3584) for e in range(8)]
                ffn_phase(k, C, S, xt, Xb.v, U, ex, router_d=W["moe_router"][l // 2])
            last = l == L - 1
            ln_phase(k, C, S, U, Xb.v, W["ln3_g"][l], W["ln3_b"][l], out[b] if last else Xc, xt)
            xcur = Xc.v
    k.barrier()
    return nc, k


def kernel(**inputs):
    B, S = inputs["x"].shape[0], inputs["x"].shape[1]
    NB = B // NCORES
    nc, _ = build_program(S, NB)
    shared = {n: np.ascontiguousarray(inputs[n], dtype=np.float32) for n, _ in IN_SPECS}
    in_maps = []
    for c in range(NCORES):
        sl = slice(c * NB, (c + 1) * NB)
        m = dict(shared)
        m["x"] = np.ascontiguousarray(inputs["x"][sl], dtype=np.float32)
        m["mem"] = np.ascontiguousarray(inputs["mem"][sl], dtype=np.float32)
        m["positions"] = np.ascontiguousarray(inputs["positions"][sl], dtype=np.int32)
        in_maps.append(m)
    res = run_bass_kernel_spmd(nc, in_maps, core_ids=list(range(NCORES)))
    return np.concatenate([np.asarray(r["out"]) for r in res.results], axis=0).astype(np.float32)
```
